# Optimizing a Trainium2 kernel written in Bass

```python
import math
import jax
import jax.numpy as jnp
from jax import lax
import numpy as np

D_MODEL = 1024
BATCH = 4
SEQ = 8192
DEPTH = 4

GRID_W = 64
CTX_LEN = 256
RMS_EPS = 1e-6
N_MOD = 6

SSD_HEADS = 8
SSD_HEAD_DIM = 64
SSD_INNER = SSD_HEADS * SSD_HEAD_DIM
SSD_GROUPS = 2
HEADS_PER_GROUP = SSD_HEADS // SSD_GROUPS
SSD_STATE = 128
SSD_CONV = 3
SSD_CHUNK = 128
SSD_CONV_DIM = SSD_INNER + 2 * SSD_GROUPS * SSD_STATE

FOURIER_GROUPS = 4
FOURIER_GROUP_DIM = 128
FOURIER_WIDTH = FOURIER_GROUPS * FOURIER_GROUP_DIM

EVEN_IN = SSD_INNER + SSD_CONV_DIM + 2 * SSD_HEADS + FOURIER_WIDTH
EVEN_MIX = SSD_INNER + FOURIER_WIDTH

NA_HEADS = 16
NA_HEAD_DIM = D_MODEL // NA_HEADS
NA_WIN_ROWS = 8
NA_WIN_COLS = 16

N_EXPERTS = 16
EXPERT_FF = 2048
EC_CAPACITY_FACTOR = 2

kernel_name = 'hybrid_ssd_fnet_natten_ecmoe_dit'


def rms_norm(x, g):
    x32 = x.astype(jnp.float32)
    y = x32 * lax.rsqrt(jnp.mean(x32 * x32, axis=-1, keepdims=True) + RMS_EPS)
    return (y * g.astype(jnp.float32)).astype(x.dtype)


def centred_dwconv(u, w, b):
    k_w = w.shape[0]
    pad = k_w // 2
    n = u.shape[1]
    up = jnp.pad(u, ((0, 0), (pad, pad), (0, 0)))
    out = b
    for k in range(k_w):
        out = out + up[:, k:k + n] * w[k]
    return out


def ssd_chunked(x, dt, a, bm, cm, h0):
    bsz, n = x.shape[:2]
    q = SSD_CHUNK
    nc = n // q
    xc = x.reshape(bsz, nc, q, SSD_HEADS, SSD_HEAD_DIM)
    dtc = dt.reshape(bsz, nc, q, SSD_HEADS)
    bc = bm.reshape(bsz, nc, q, SSD_GROUPS, SSD_STATE)
    cc = cm.reshape(bsz, nc, q, SSD_GROUPS, SSD_STATE)
    cum = jnp.cumsum(dtc * a, axis=2)
    cum_h = jnp.moveaxis(cum, -1, 2)
    seg = cum_h[..., :, None] - cum_h[..., None, :]
    ordered = jnp.tril(jnp.ones((q, q), dtype=bool))
    decay = jnp.exp(jnp.where(ordered, seg, -jnp.inf))
    cb = jnp.repeat(jnp.einsum('bcign,bcjgn->bcgij', cc, bc), HEADS_PER_GROUP, axis=2)
    xdt = xc * dtc[..., None]
    y_diag = jnp.einsum('bchij,bcjhp->bcihp', cb * decay, xdt)
    to_end = jnp.exp(cum[:, :, -1:, :] - cum)
    bh = jnp.repeat(bc, HEADS_PER_GROUP, axis=3)
    states = jnp.einsum('bcjhn,bcjhp->bchpn', bh * to_end[..., None], xdt)
    chunk_decay = jnp.exp(cum[:, :, -1, :])

    def step(h, inp):
        dec, st = inp
        return dec[:, :, None, None] * h + st, h

    h_final, h_start = lax.scan(step, h0, (jnp.moveaxis(chunk_decay, 1, 0), jnp.moveaxis(states, 1, 0)))
    h_start = jnp.moveaxis(h_start, 0, 1)
    ch = jnp.repeat(cc, HEADS_PER_GROUP, axis=3)
    y_off = jnp.einsum('bcihn,bchpn->bcihp', ch, h_start) * jnp.exp(cum)[..., None]
    return (y_diag + y_off).reshape(bsz, n, SSD_HEADS, SSD_HEAD_DIM), h_final


def ssd_bidirectional(xbc, dt_raw, a_log, dt_bias, d_skip, h0_fwd, h0_bwd):
    bsz, n = xbc.shape[:2]
    xbc = xbc.astype(jnp.float32)
    gn = SSD_GROUPS * SSD_STATE
    x = xbc[..., :SSD_INNER].reshape(bsz, n, SSD_HEADS, SSD_HEAD_DIM)
    bm = xbc[..., SSD_INNER:SSD_INNER + gn].reshape(bsz, n, SSD_GROUPS, SSD_STATE)
    cm = xbc[..., SSD_INNER + gn:].reshape(bsz, n, SSD_GROUPS, SSD_STATE)
    dt = jax.nn.softplus(dt_raw.astype(jnp.float32) + dt_bias.astype(jnp.float32))
    a = -jnp.exp(a_log.astype(jnp.float32))
    y_f, h_f = ssd_chunked(x, dt[:, :, 0], a[0], bm, cm, h0_fwd)
    flip = lambda t: jnp.flip(t, axis=1)
    y_b, h_b = ssd_chunked(flip(x), flip(dt[:, :, 1]), a[1], flip(bm), flip(cm), h0_bwd)
    y = y_f + flip(y_b) + d_skip.astype(jnp.float32)[:, None] * x
    return y, h_f, h_b


def fourier_mix(u):
    bsz, n = u.shape[:2]
    ug = u.astype(jnp.float32).reshape(bsz, n, FOURIER_GROUPS, FOURIER_GROUP_DIM)
    f = jnp.fft.fft2(ug, axes=(1, 3), norm='ortho').real
    return f.reshape(bsz, n, FOURIER_WIDTH)


def even_mixer(hl, hc, w_in, conv_w, conv_b, a_log, dt_bias, d_skip, g_ssd, w_out, need_ctx):
    o_dt = SSD_INNER + SSD_CONV_DIM

    def project(h):
        pr = h @ w_in
        z = pr[..., :SSD_INNER]
        xbc = jax.nn.silu(centred_dwconv(pr[..., SSD_INNER:o_dt], conv_w, conv_b))
        dt_raw = pr[..., o_dt:o_dt + 2 * SSD_HEADS].reshape(h.shape[0], h.shape[1], 2, SSD_HEADS)
        u = pr[..., o_dt + 2 * SSD_HEADS:]
        return z, xbc, dt_raw, u

    def finish(z, y, u):
        bsz, n = y.shape[:2]
        yg = y.reshape(bsz, n, SSD_INNER) * jax.nn.silu(z.astype(jnp.float32))
        yg = yg.reshape(bsz, n, SSD_GROUPS, SSD_INNER // SSD_GROUPS)
        yg = yg * lax.rsqrt(jnp.mean(yg * yg, axis=-1, keepdims=True) + RMS_EPS)
        yg = yg.reshape(bsz, n, SSD_INNER) * g_ssd.astype(jnp.float32)
        mix = jnp.concatenate([yg, fourier_mix(u)], axis=-1).astype(z.dtype)
        return mix @ w_out

    bsz = hl.shape[0]
    zeros = jnp.zeros((bsz, SSD_HEADS, SSD_HEAD_DIM, SSD_STATE), jnp.float32)
    zc, xbcc, dtc, uc = project(hc)
    yc, hc_f, hc_b = ssd_bidirectional(xbcc, dtc, a_log, dt_bias, d_skip, zeros, zeros)
    zl, xbcl, dtl, ul = project(hl)
    yl, _, _ = ssd_bidirectional(xbcl, dtl, a_log, dt_bias, d_skip, hc_f, hc_b)
    out_l = finish(zl, yl, ul)
    out_c = finish(zc, yc, uc) if need_ctx else None
    return out_l, out_c


def odd_mixer(hl, hc, w_qkv, rpb, w_o, need_ctx):
    bsz, n, _ = hl.shape
    lc = hc.shape[1]
    rows = n // GRID_W
    kr = min(NA_WIN_ROWS, rows)
    kc = NA_WIN_COLS
    scale = NA_HEAD_DIM ** -0.5
    qkv_l = (hl @ w_qkv).reshape(bsz, rows, GRID_W, 3, NA_HEADS, NA_HEAD_DIM)
    ql = qkv_l[:, :, :, 0] * scale
    kl = qkv_l[:, :, :, 1]
    vl = qkv_l[:, :, :, 2]
    qkv_c = (hc @ w_qkv).reshape(bsz, lc, 3, NA_HEADS, NA_HEAD_DIM)
    kctx = qkv_c[:, :, 1]
    vctx = qkv_c[:, :, 2]
    cols = jnp.arange(GRID_W)
    col_start = jnp.clip(cols - kc // 2, 0, GRID_W - kc)
    col_idx = col_start[:, None] + jnp.arange(kc)
    col_off = col_idx - cols[:, None] + NA_WIN_COLS - 1

    def row_block(r):
        rs = jnp.clip(r - kr // 2, 0, rows - kr)
        q = lax.dynamic_index_in_dim(ql, r, axis=1, keepdims=False)
        k_rows = lax.dynamic_slice_in_dim(kl, rs, kr, axis=1)
        v_rows = lax.dynamic_slice_in_dim(vl, rs, kr, axis=1)
        k_nb = k_rows[:, :, col_idx]
        v_nb = v_rows[:, :, col_idx]
        row_off = rs + jnp.arange(kr) - r + NA_WIN_ROWS - 1
        bias = rpb[:, row_off[:, None, None], col_off[None, :, :]]
        s_lat = jnp.einsum('bqhd,biqjhd->bhqij', q, k_nb).astype(jnp.float32)
        s_lat = s_lat + jnp.transpose(bias, (0, 2, 1, 3)).astype(jnp.float32)[None]
        s_lat = s_lat.reshape(bsz, NA_HEADS, GRID_W, kr * kc)
        s_ctx = jnp.einsum('bqhd,bkhd->bhqk', q, kctx).astype(jnp.float32)
        p = jax.nn.softmax(jnp.concatenate([s_lat, s_ctx], axis=-1), axis=-1).astype(vl.dtype)
        p_lat = p[..., :kr * kc].reshape(bsz, NA_HEADS, GRID_W, kr, kc)
        p_ctx = p[..., kr * kc:]
        return (jnp.einsum('bhqij,biqjhd->bqhd', p_lat, v_nb)
                + jnp.einsum('bhqk,bkhd->bqhd', p_ctx, vctx))

    o_rows = lax.map(row_block, jnp.arange(rows))
    out_l = jnp.moveaxis(o_rows, 0, 1).reshape(bsz, n, D_MODEL) @ w_o
    out_c = None
    if need_ctx:
        qc = qkv_c[:, :, 0] * scale
        s = jnp.einsum('bqhd,bkhd->bhqk', qc, kctx).astype(jnp.float32)
        p = jax.nn.softmax(s, axis=-1).astype(vctx.dtype)
        out_c = jnp.einsum('bhqk,bkhd->bqhd', p, vctx).reshape(bsz, lc, D_MODEL) @ w_o
    return out_l, out_c


def ec_moe(h, w_router, w1, w3, w2):
    bsz, n, d = h.shape
    cap = EC_CAPACITY_FACTOR * n // N_EXPERTS
    aff = jax.nn.softmax((h @ w_router).astype(jnp.float32), axis=-1)
    gate, idx = lax.top_k(jnp.swapaxes(aff, 1, 2), cap)
    xg = jax.vmap(lambda hb, ib: hb[ib])(h, idx)
    a = jnp.einsum('becd,edf->becf', xg, w1)
    b = jnp.einsum('becd,edf->becf', xg, w3)
    y = jnp.einsum('becf,efd->becd', jax.nn.silu(a) * b, w2) * gate[..., None].astype(h.dtype)
    return jax.vmap(lambda yb, ib: jnp.zeros((n, d), yb.dtype).at[ib].add(yb))(y, idx)


def setup_inputs(seed: int = 0) -> dict:
    key = jax.random.key(seed)
    ks = jax.random.split(key, 24)
    f32 = jnp.float32
    ne = (DEPTH + 1) // 2
    no = DEPTH // 2
    nrm = lambda k, shape, s: jax.random.normal(k, shape, f32) * s
    dt0 = jnp.exp(jax.random.uniform(ks[12], (ne, 2, SSD_HEADS), f32, math.log(1e-3), math.log(1e-1)))
    return {
        'x': nrm(ks[0], (BATCH, SEQ, D_MODEL), 1.0),
        'c': nrm(ks[1], (BATCH, D_MODEL), 1.0),
        'ctx': nrm(ks[2], (BATCH, CTX_LEN, D_MODEL), 1.0),
        'c_ctx': nrm(ks[3], (D_MODEL,), 1.0),
        'w_mod': nrm(ks[4], (DEPTH, D_MODEL, N_MOD * D_MODEL), 0.5 * D_MODEL ** -0.5),
        'b_mod': nrm(ks[5], (DEPTH, N_MOD * D_MODEL), 0.02),
        'g_mix': 1.0 + nrm(ks[6], (DEPTH, D_MODEL), 0.02),
        'g_ffn': 1.0 + nrm(ks[7], (DEPTH, D_MODEL), 0.02),
        'w_in_e': nrm(ks[8], (ne, D_MODEL, EVEN_IN), D_MODEL ** -0.5),
        'conv_w': nrm(ks[9], (ne, SSD_CONV, SSD_CONV_DIM), SSD_CONV ** -0.5),
        'conv_b': nrm(ks[10], (ne, SSD_CONV_DIM), 0.02),
        'a_log': jnp.log(jax.random.uniform(ks[11], (ne, 2, SSD_HEADS), f32, 1.0, 16.0)),
        'dt_bias': dt0 + jnp.log(-jnp.expm1(-dt0)),
        'd_skip': 1.0 + nrm(ks[13], (ne, SSD_HEADS), 0.1),
        'g_ssd': 1.0 + nrm(ks[14], (ne, SSD_INNER), 0.02),
        'w_out_e': nrm(ks[15], (ne, EVEN_MIX, D_MODEL), EVEN_MIX ** -0.5),
        'w_qkv': nrm(ks[16], (no, D_MODEL, 3 * D_MODEL), D_MODEL ** -0.5),
        'rpb': nrm(ks[17], (no, NA_HEADS, 2 * NA_WIN_ROWS - 1, 2 * NA_WIN_COLS - 1), 0.1),
        'w_o': nrm(ks[18], (no, D_MODEL, D_MODEL), D_MODEL ** -0.5),
        'w_router': nrm(ks[19], (DEPTH, D_MODEL, N_EXPERTS), D_MODEL ** -0.5),
        'w_e1': nrm(ks[20], (DEPTH, N_EXPERTS, D_MODEL, EXPERT_FF), D_MODEL ** -0.5),
        'w_e3': nrm(ks[21], (DEPTH, N_EXPERTS, D_MODEL, EXPERT_FF), D_MODEL ** -0.5),
        'w_e2': nrm(ks[22], (DEPTH, N_EXPERTS, EXPERT_FF, D_MODEL), EXPERT_FF ** -0.5),
        'g_final': 1.0 + nrm(ks[23], (D_MODEL,), 0.02),
    }


def reference(x, c, ctx, c_ctx, w_mod, b_mod, g_mix, g_ffn, w_in_e, conv_w, conv_b, a_log, dt_bias,
              d_skip, g_ssd, w_out_e, w_qkv, rpb, w_o, w_router, w_e1, w_e3, w_e2, g_final):
    xl = x
    xc = ctx
    silu_c = jax.nn.silu(c)
    silu_cc = jax.nn.silu(c_ctx)
    for i in range(DEPTH):
        need_ctx = i < DEPTH - 1
        j = i // 2
        mod_l = (silu_c @ w_mod[i] + b_mod[i])[:, None, :]
        mod_c = (silu_cc @ w_mod[i] + b_mod[i])[None, None, :]
        sh1_l, sc1_l, ga1_l, sh2_l, sc2_l, ga2_l = jnp.split(mod_l, N_MOD, axis=-1)
        sh1_c, sc1_c, ga1_c, sh2_c, sc2_c, ga2_c = jnp.split(mod_c, N_MOD, axis=-1)
        hl = rms_norm(xl, g_mix[i]) * (1.0 + sc1_l) + sh1_l
        hc = rms_norm(xc, g_mix[i]) * (1.0 + sc1_c) + sh1_c
        if i % 2 == 0:
            out_l, out_c = even_mixer(hl, hc, w_in_e[j], conv_w[j], conv_b[j], a_log[j], dt_bias[j],
                                      d_skip[j], g_ssd[j], w_out_e[j], need_ctx)
        else:
            out_l, out_c = odd_mixer(hl, hc, w_qkv[j], rpb[j], w_o[j], need_ctx)
        xl = xl + ga1_l * out_l
        hl2 = rms_norm(xl, g_ffn[i]) * (1.0 + sc2_l) + sh2_l
        xl = xl + ga2_l * ec_moe(hl2, w_router[i], w_e1[i], w_e3[i], w_e2[i])
        if need_ctx:
            xc = xc + ga1_c * out_c
            hc2 = rms_norm(xc, g_ffn[i]) * (1.0 + sc2_c) + sh2_c
            xc = xc + ga2_c * ec_moe(hc2, w_router[i], w_e1[i], w_e3[i], w_e2[i])
    return rms_norm(xl, g_final)
```

```python
import contextlib
import numpy as np
import ml_dtypes
import concourse.bass as bass
import concourse.mybir as mybir
from concourse.bass_utils import run_bass_kernel_spmd

F32 = mybir.dt.float32
BF16 = mybir.dt.bfloat16
I32 = mybir.dt.int32
AF = mybir.ActivationFunctionType
ALU = mybir.AluOpType
AX = mybir.AxisListType
IOA = bass.IndirectOffsetOnAxis if hasattr(bass, "IndirectOffsetOnAxis") else None

D = 1024
L = 8192
LC = 256
NT = L + LC
NTILE = NT // 128
DEPTH = 4
NE = 16
FF = 2048
CAPB = 192
NSLOT = 8 * CAPB
NCALL = NSLOT // 128
CAPC = 32
XR = NT + (NCALL + 1) * 128
EPS = 1e-6


class Trk:
    __slots__ = ("w", "r", "x")

    def __init__(self, x=False):
        self.w = None
        self.r = {}
        self.x = x


class Buf:
    def __init__(self, t, x=False):
        self.t = t
        self.k = Trk(x)

    def __getitem__(self, key):
        return self.t[key]


class KB:
    ND = 40

    def __init__(self, nc):
        self.nc = nc
        self.es = contextlib.ExitStack()
        self.eng = {"pe": nc.tensor, "act": nc.scalar, "dve": nc.vector, "pool": nc.gpsimd, "sp": nc.sync}
        self.csem = {}
        self.ccnt = {}
        for e in ("pe", "act", "dve", "pool"):
            self.csem[e] = self.es.enter_context(nc.semaphore("cs_" + e))
            self.ccnt[e] = 0
        self.dsem = [self.es.enter_context(nc.semaphore("ds%d" % i)) for i in range(self.ND)]
        self.dcnt = [0] * self.ND
        self.dnext = 0
        self.seen = {e: {} for e in self.eng}
        self.uid = 0

    def sb(self, es, shape, dtype, name=None):
        self.uid += 1
        return Buf(es.enter_context(self.nc.sbuf_tensor("%s_%d" % (name or "sb", self.uid), list(shape), dtype)))

    def ps(self, es, shape, dtype, name=None):
        self.uid += 1
        return Buf(es.enter_context(self.nc.psum_tensor("%s_%d" % (name or "ps", self.uid), list(shape), dtype)), x=True)

    def dram(self, name, shape, dtype):
        return Buf(self.nc.dram_tensor(name, list(shape), dtype, kind="Internal").ap())

    def _sem(self, ch):
        return self.csem[ch] if isinstance(ch, str) else self.dsem[ch]

    def _deps(self, eng, reads, writes, is_dma):
        deps = {}

        def add(ev, raw):
            if ev is None:
                return
            ch, v = ev
            if (not is_dma) and ch == eng:
                if eng == "pe" or not raw:
                    return
            if deps.get(ch, 0) < v:
                deps[ch] = v

        for t in reads:
            add(t.w, True)
            if t.x:
                for ch, v in t.r.items():
                    if ch != eng:
                        add((ch, v), False)
        for t in writes:
            add(t.w, False)
            for ch, v in t.r.items():
                add((ch, v), False)
        return deps

    def _wait(self, eng, deps):
        s = self.seen[eng]
        for ch, v in deps.items():
            if s.get(ch, 0) < v:
                self.eng[eng].wait_ge(self._sem(ch), v)
                s[ch] = v

    @staticmethod
    def _trks(lst):
        return [b.k if isinstance(b, Buf) else b for b in lst]

    def op(self, eng, fn, reads=(), writes=()):
        reads = self._trks(reads)
        writes = self._trks(writes)
        self._wait(eng, self._deps(eng, reads, writes, False))
        ins = fn(self.eng[eng])
        self.ccnt[eng] += 1
        v = self.ccnt[eng]
        ins.then_inc(self.csem[eng], 1)
        for t in reads:
            if t.r.get(eng, 0) < v:
                t.r[eng] = v
        for t in writes:
            t.w = (eng, v)
            t.r = {}
        return ins

    def dma(self, q, out=None, in_=None, reads=(), writes=(), fn=None, **kw):
        reads = self._trks(reads)
        writes = self._trks(writes)
        s = self.dnext
        self.dnext = (s + 1) % self.ND
        deps = self._deps(q, reads, writes, True)
        if self.dcnt[s] > 0 and deps.get(s, 0) < self.dcnt[s]:
            deps[s] = self.dcnt[s]
        self._wait(q, deps)
        if fn is None:
            ins = self.eng[q].dma_start(out=out, in_=in_, **kw)
        else:
            ins = fn(self.eng[q])
        self.dcnt[s] += 16
        v = self.dcnt[s]
        ins.then_inc(self.dsem[s], 16)
        for t in reads:
            if t.r.get(s, 0) < v:
                t.r[s] = v
        for t in writes:
            t.w = (s, v)
            t.r = {}
        return ins

    def barrier(self):
        for e in self.eng:
            deps = {}
            for c in self.csem:
                if self.ccnt[c] > 0 and c != e:
                    deps[c] = self.ccnt[c]
            for s in range(self.ND):
                if self.dcnt[s] > 0:
                    deps[s] = self.dcnt[s]
            self._wait(e, deps)
        for e in self.csem:
            if self.ccnt[e] > 0:
                s = self.seen[e]
                if s.get(e, 0) < self.ccnt[e]:
                    self.eng[e].wait_ge(self.csem[e], self.ccnt[e])
                    s[e] = self.ccnt[e]


def host_consts():
    c = {}
    c["ident_bf"] = np.eye(128, dtype=np.float32).astype(ml_dtypes.bfloat16)
    c["ident_f"] = np.eye(128, dtype=np.float32)
    p = np.arange(128)
    c["gsum"] = (p[:, None] // 8 == p[None, :] // 8).astype(np.float32)
    c["keyl"] = np.broadcast_to((1024 - np.arange(1024, dtype=np.float32))[None, :], (128, 1024)).copy()
    c["keyc"] = np.broadcast_to((256 - np.arange(256, dtype=np.float32))[None, :], (128, 256)).copy()
    c["basel"] = ((p % 8) * 1024 + 1024).astype(np.float32)[:, None].copy()
    c["basec"] = np.full((128, 1), L + 256, np.float32)
    sig = (p[:, None] % 8) * CAPB + np.arange(CAPB)[None, :]
    c["dumpl"] = (NT + sig).astype(np.float32)
    c["dumpc"] = np.broadcast_to((NT + NSLOT + np.arange(CAPC, dtype=np.float32))[None, :], (128, CAPC)).copy()
    k = np.arange(128)
    c["triU"] = (k[:, None] <= k[None, :]).astype(np.float32)
    c["triL"] = (k[:, None] >= k[None, :]).astype(np.float32)
    c["onesf"] = np.ones((128, 128), np.float32)
    c["negU"] = np.where(k[None, :] >= k[:, None], 0.0, -30000.0).astype(np.float32)
    c["negL"] = np.where(k[None, :] <= k[:, None], 0.0, -30000.0).astype(np.float32)
    ang = 2 * np.pi * np.outer(k, k) / 128.0
    bf = ml_dtypes.bfloat16
    c["CS"] = np.concatenate([np.cos(ang), np.sin(ang)], axis=1).astype(np.float32).astype(bf)
    c["F1"] = np.stack([np.cos(ang), -np.sin(ang), -np.cos(ang)], axis=1).astype(np.float32).astype(bf)
    n2 = np.arange(64)[:, None, None]
    p1 = np.arange(128)[None, :, None]
    p2 = np.arange(64)[None, None, :]
    th = 2 * np.pi * (n2 * p2 / 64.0 + n2 * p1 / 8192.0)
    c["TW3"] = np.concatenate([np.cos(th), np.sin(th)], axis=0).astype(np.float32).astype(bf)
    n = np.arange(256)
    a2 = 2 * np.pi * np.outer(n, n) / 256.0
    c["C256"] = np.cos(a2).reshape(2, 128, 256).transpose(1, 0, 2).astype(np.float32).astype(bf)
    c["nS256"] = (-np.sin(a2)).reshape(2, 128, 256).transpose(1, 0, 2).astype(np.float32).astype(bf)
    return c


CONST_SPECS = {
    "triU": ([128, 128], F32), "triL": ([128, 128], F32), "onesf": ([128, 128], F32), "negU": ([128, 128], F32),
    "negL": ([128, 128], F32), "CS": ([128, 256], BF16), "F1": ([128, 3, 128], BF16), "TW3": ([128, 128, 64], BF16),
    "C256": ([128, 2, 256], BF16), "nS256": ([128, 2, 256], BF16),
    "ident_bf": ([128, 128], BF16), "ident_f": ([128, 128], F32), "gsum": ([128, 128], F32),
    "keyl": ([128, 1024], F32), "keyc": ([128, 256], F32), "basel": ([128, 1], F32), "basec": ([128, 1], F32),
    "dumpl": ([128, CAPB], F32), "dumpc": ([128, CAPC], F32),
}


IN_SPECS = {
    "x": ([L, D], F32), "ctx": ([LC, D], F32), "c": ([D], F32), "c_ctx": ([D], F32),
    "w_mod": ([DEPTH, D, 6 * D], F32), "b_mod": ([DEPTH, 6 * D], F32), "g_mix": ([DEPTH, D], F32), "g_ffn": ([DEPTH, D], F32),
    "w_router": ([DEPTH, D, NE], F32), "w_e1": ([DEPTH, NE, D, FF], F32), "w_e3": ([DEPTH, NE, D, FF], F32),
    "w_e2": ([DEPTH, NE, FF, D], F32), "g_final": ([D], F32),
    "w_in_e": ([2, D, 2064], F32), "conv_w": ([2, 3, D], F32), "conv_b": ([2, D], F32), "a_log": ([2, 16], F32),
    "dt_bias": ([2, 16], F32), "d_skip": ([2, 8], F32), "g_ssd": ([2, 512], F32), "w_out_e": ([2, D, D], F32),
    "w_qkv": ([2, D, 3 * D], F32), "w_o": ([2, D, D], F32), "biasT": ([2, 8, 16, 4, 128, 64], F32),
}


class Prog:
    def __init__(self, phases, debug_out=None):
        self.phases = phases
        self.debug_out = debug_out
        nc = bass.Bass("TRN2", target_bir_lowering=False)
        self.nc = nc
        self.kb = KB(nc)
        kb = self.kb
        dt = nc.dram_tensor

        class LazyIn(dict):
            def __missing__(d, k):
                shp, ty = IN_SPECS[k] if k in IN_SPECS else CONST_SPECS[k]
                d[k] = dt(k, list(shp), ty, kind="ExternalInput").ap()
                return d[k]
        self.I = LazyIn()
        self.out = dt("out", [L, D], F32, kind="ExternalOutput").ap()
        self.X = kb.dram("Xres", [XR, D], F32)
        self.H2 = kb.dram("H2", [XR, D], BF16)
        self.AFF = kb.dram("AFF", [XR, NE], F32)
        self.AFFT = kb.dram("AFFT", [NE, L], F32)
        self.AFFTC = kb.dram("AFFTC", [NE, LC], F32)
        self.IDXD = kb.dram("IDXD", [128, CAPB], I32)
        self.IDXC = kb.dram("IDXC", [NE, CAPC], I32)
        self.MODD = kb.dram("MODD", [2, 6 * D], F32)
        self.outk = Trk()
        self.build()

    def build(self):
        kb = self.kb
        with contextlib.ExitStack() as ges:
            self.ges = ges
            self.ident_bf = kb.sb(ges, [128, 128], BF16, "identbf")
            self.ident_f = kb.sb(ges, [128, 128], F32, "identf")
            kb.dma("sp", out=self.ident_bf[:, :], in_=self.I["ident_bf"][:, :], writes=[self.ident_bf])
            kb.dma("sp", out=self.ident_f[:, :], in_=self.I["ident_f"][:, :], writes=[self.ident_f])
            for ph in self.phases:
                name = ph[0]
                getattr(self, "ph_" + name)(*ph[1:])
                kb.barrier()
            kb.barrier()
        kb.es.close()

    def ph_init(self):
        kb = self.kb
        with contextlib.ExitStack() as es:
            for r0 in range(0, L, 2048):
                kb.dma("sp", out=self.X[r0:r0 + 2048, :], in_=self.I["x"][r0:r0 + 2048, :], writes=[self.X])
            kb.dma("sp", out=self.X[L:NT, :], in_=self.I["ctx"][:, :], writes=[self.X])
            z = kb.sb(es, [128, D], F32, "z")
            zb = kb.sb(es, [128, D], BF16, "zb")
            kb.op("dve", lambda e: e.memset(z[:, :], 0.0), writes=[z])
            kb.op("dve", lambda e: e.memset(zb[:, :], 0.0), writes=[zb])
            for j in range(NCALL + 1):
                r0 = NT + j * 128
                kb.dma("sp", out=self.X[r0:r0 + 128, :], in_=z[:, :], reads=[z], writes=[self.X])
                kb.dma("sp", out=self.H2[r0:r0 + 128, :], in_=zb[:, :], reads=[zb], writes=[self.H2])
                kb.dma("sp", out=self.AFF[r0:r0 + 128, :], in_=z[:, 0:NE], reads=[z], writes=[self.AFF])

    def ph_mod(self, li):
        kb = self.kb
        I = self.I
        with contextlib.ExitStack() as es:
            cv = kb.sb(es, [128, 2, 8], F32, "cv")
            kb.dma("sp", out=cv[:, 0, :], in_=I["c"].rearrange("(kc p) -> p kc", p=128), writes=[cv],
                   allow_slow_non_contiguous=True)
            kb.dma("sp", out=cv[:, 1, :], in_=I["c_ctx"].rearrange("(kc p) -> p kc", p=128), writes=[cv],
                   allow_slow_non_contiguous=True)
            sv = kb.sb(es, [128, 2, 8], F32, "sv")
            kb.op("act", lambda e: e.activation(out=sv[:, :, :], in_=cv[:, :, :], func=AF.Silu), reads=[cv], writes=[sv])
            lb = kb.sb(es, [128, 2, 8, 128], F32, "lb")
            for s in range(2):
                kb.op("dve", lambda e, s=s: e.tensor_copy(out=lb[:, s, :, :], in_=sv[:, s, :].unsqueeze(2).to_broadcast([128, 8, 128])),
                      reads=[sv], writes=[lb])
            gmix = kb.sb(es, [128, D], F32, "gmix")
            gffn = kb.sb(es, [128, D], F32, "gffn")
            kb.dma("sp", out=gmix[:, :], in_=I["g_mix"][li, :].partition_broadcast(128), writes=[gmix])
            kb.dma("sp", out=gffn[:, :], in_=I["g_ffn"][li, :].partition_broadcast(128), writes=[gffn])
            wm = [kb.sb(es, [128, 8, 512], F32, "wm") for _ in range(2)]
            bm = [kb.sb(es, [128, 512], F32, "bm") for _ in range(2)]
            pp = [kb.ps(es, [128, 512], F32, "pm") for _ in range(4)]
            res = [kb.sb(es, [128, 512], F32, "res") for _ in range(4)]
            wsrc = I["w_mod"][li].rearrange("(kc p) n -> p kc n", p=128)
            for n in range(12):
                w = wm[n % 2]
                b = bm[n % 2]
                kb.dma("sp", out=w[:, :, :], in_=wsrc[:, :, n * 512:(n + 1) * 512], writes=[w])
                kb.dma("sp", out=b[:, :], in_=I["b_mod"][li, n * 512:(n + 1) * 512].partition_broadcast(128), writes=[b])
                for s in range(2):
                    p = pp[(2 * n + s) % 4]
                    r = res[(2 * n + s) % 4]
                    for kc in range(8):
                        kb.op("pe", lambda e, kc=kc, s=s, p=p, w=w: e.matmul(p[:, :], lhsT=lb[:, s, kc, :], rhs=w[:, kc, :],
                                                                          start=(kc == 0), stop=(kc == 7)),
                              reads=[lb, w], writes=[p])
                    which = n // 2
                    if which in (1, 4):
                        g = gmix if which == 1 else gffn
                        c0 = (n % 2) * 512
                        kb.op("dve", lambda e, p=p, r=r, b=b: e.tensor_tensor(out=r[:, :], in0=p[:, :], in1=b[:, :], op=ALU.add),
                              reads=[p, b], writes=[r])
                        kb.op("dve", lambda e, r=r, g=g, c0=c0: e.scalar_tensor_tensor(out=r[:, :], in0=r[:, :], scalar=1.0,
                                                                                     in1=g[:, c0:c0 + 512], op0=ALU.add, op1=ALU.mult),
                              reads=[r, g], writes=[r])
                    else:
                        kb.op("dve", lambda e, p=p, r=r, b=b: e.tensor_tensor(out=r[:, :], in0=p[:, :], in1=b[:, :], op=ALU.add),
                              reads=[p, b], writes=[r])
                    kb.dma("sp", out=self.MODD[s:s + 1, n * 512:(n + 1) * 512], in_=r[0:1, :], reads=[r], writes=[self.MODD])

    def load_vec(self, es, s, which, name="vec"):
        kb = self.kb
        t = kb.sb(es, [128, D], F32, name)
        kb.dma("sp", out=t[:, :], in_=self.MODD[s, which * D:(which + 1) * D].partition_broadcast(128),
               reads=[self.MODD], writes=[t])
        return t

    def norm_tile(self, rstd_tmp, xt, s_bc, sh_bc, out_f32, out_bf, junk, negh):
        kb = self.kb
        ss, rs = rstd_tmp
        kb.op("act", lambda e: e.activation(out=junk[:, :], in_=xt[:, :], func=AF.Square, accum_out=ss[:, 0:1]),
              reads=[xt], writes=[junk, ss])
        kb.op("dve", lambda e: e.tensor_scalar(out=ss[:, 0:1], in0=ss[:, 0:1], scalar1=1.0 / D, scalar2=EPS, op0=ALU.mult, op1=ALU.add),
              reads=[ss], writes=[ss])
        kb.op("pool", lambda e: e.tensor_tensor(out=rs[:, 0:1], in0=ss[:, 0:1], in1=negh[:, 0:1], op=ALU.pow),
              reads=[ss, negh], writes=[rs])
        kb.op("dve", lambda e: e.scalar_tensor_tensor(out=out_f32[:, :], in0=xt[:, :], scalar=rs[:, 0:1], in1=s_bc[:, :],
                                                      op0=ALU.mult, op1=ALU.mult),
              reads=[xt, rs, s_bc], writes=[out_f32])
        kb.op("pool", lambda e: e.tensor_tensor(out=out_f32[:, :], in0=out_f32[:, :], in1=sh_bc[:, :], op=ALU.add),
              reads=[out_f32, sh_bc], writes=[out_f32])
        if out_bf is not None:
            kb.op("act", lambda e: e.copy(out=out_bf[:, :], in_=out_f32[:, :]), reads=[out_f32], writes=[out_bf])

    def ph_router(self, li):
        kb = self.kb
        I = self.I
        with contextlib.ExitStack() as es:
            svec = [self.load_vec(es, s, 4, "s2") for s in range(2)]
            shvec = [self.load_vec(es, s, 3, "sh2") for s in range(2)]
            negh = kb.sb(es, [128, 1], F32, "negh")
            kb.op("dve", lambda e: e.memset(negh[:, :], -0.5), writes=[negh])
            wr = kb.sb(es, [128, 8, NE], F32, "wr")
            kb.dma("sp", out=wr[:, :, :], in_=I["w_router"][li].rearrange("(kc p) n -> p kc n", p=128), writes=[wr])
            AT = kb.sb(es, [NE, L], F32, "AT")
            ATC = kb.sb(es, [NE, LC], F32, "ATC")
            NB = 2
            xt = [kb.sb(es, [128, D], F32, "xt") for _ in range(NB)]
            hf = [kb.sb(es, [128, D], F32, "hf") for _ in range(NB)]
            hb = [kb.sb(es, [128, D], BF16, "hb") for _ in range(NB)]
            junk = kb.sb(es, [128, D], BF16, "junk")
            hT = [kb.sb(es, [128, 8, 128], F32, "hT") for _ in range(NB)]
            sm = [[kb.sb(es, [128, 1], F32, "sm") for _ in range(6)] for _ in range(NB)]
            lg = [kb.sb(es, [128, NE], F32, "lg") for _ in range(NB)]
            af = [kb.sb(es, [128, NE], F32, "af") for _ in range(NB)]
            pT = [kb.ps(es, [128, 512], F32, "pT") for _ in range(4)]
            pl = [kb.ps(es, [128, NE], F32, "pl") for _ in range(2)]
            pa = [kb.ps(es, [NE, 128], F32, "pa") for _ in range(2)]
            for ti in range(NTILE):
                b = ti % NB
                s = 0 if ti < 64 else 1
                r0 = ti * 128
                kb.dma("sp", out=xt[b][:, :], in_=self.X[r0:r0 + 128, :], reads=[self.X], writes=[xt[b]])
                self.norm_tile((sm[b][0], sm[b][1]), xt[b], svec[s], shvec[s], hf[b], hb[b], junk, negh)
                kb.dma("sp", out=self.H2[r0:r0 + 128, :], in_=hb[b][:, :], reads=[hb[b]], writes=[self.H2])
                for half in range(2):
                    p = pT[(2 * ti + half) % 4]
                    for q in range(4):
                        kc = half * 4 + q
                        kb.op("pe", lambda e, p=p, q=q, kc=kc, b=b: e.transpose(out=p[:, q * 128:(q + 1) * 128],
                                                                              in_=hf[b][:, kc * 128:(kc + 1) * 128],
                                                                              identity=self.ident_f[:, :]),
                              reads=[hf[b], self.ident_f], writes=[p])
                    kb.op("act", lambda e, p=p, half=half, b=b: e.copy(out=hT[b][:, half * 4:half * 4 + 4, :],
                                                                     in_=p[:, :].rearrange("p (q t) -> p q t", q=4)),
                          reads=[p], writes=[hT[b]])
                pp = pl[ti % 2]
                for kc in range(8):
                    kb.op("pe", lambda e, kc=kc, pp=pp, b=b: e.matmul(pp[:, :], lhsT=hT[b][:, kc, :], rhs=wr[:, kc, :],
                                                                    start=(kc == 0), stop=(kc == 7)),
                          reads=[hT[b], wr], writes=[pp])
                mx, nmx, se, rse = sm[b][2], sm[b][3], sm[b][4], sm[b][5]
                kb.op("dve", lambda e, pp=pp, mx=mx: e.reduce_max(out=mx[:, 0:1], in_=pp[:, :], axis=AX.X), reads=[pp], writes=[mx])
                kb.op("dve", lambda e, mx=mx, nmx=nmx: e.tensor_scalar(out=nmx[:, 0:1], in0=mx[:, 0:1], scalar1=-1.0, scalar2=None, op0=ALU.mult),
                      reads=[mx], writes=[nmx])
                kb.op("act", lambda e, pp=pp, b=b, nmx=nmx, se=se: e.activation(out=lg[b][:, :], in_=pp[:, :], func=AF.Exp, bias=nmx[:, 0:1],
                                                                              accum_out=se[:, 0:1]),
                      reads=[pp, nmx], writes=[lg[b], se])
                kb.op("dve", lambda e, se=se, rse=rse: e.reciprocal(out=rse[:, 0:1], in_=se[:, 0:1]), reads=[se], writes=[rse])
                kb.op("dve", lambda e, b=b, rse=rse: e.tensor_scalar(out=af[b][:, :], in0=lg[b][:, :], scalar1=rse[:, 0:1], scalar2=None, op0=ALU.mult),
                      reads=[lg[b], rse], writes=[af[b]])
                kb.dma("sp", out=self.AFF[r0:r0 + 128, :], in_=af[b][:, :], reads=[af[b]], writes=[self.AFF])
                pq = pa[ti % 2]
                kb.op("pe", lambda e, pq=pq, b=b: e.matmul(pq[:, :], lhsT=af[b][:, :], rhs=self.ident_f[:, :], start=True, stop=True),
                      reads=[af[b], self.ident_f], writes=[pq])
                if s == 0:
                    kb.op("act", lambda e, pq=pq, r0=r0: e.copy(out=AT[:, r0:r0 + 128], in_=pq[:, :]), reads=[pq], writes=[AT])
                else:
                    kb.op("act", lambda e, pq=pq, r0=r0: e.copy(out=ATC[:, r0 - L:r0 - L + 128], in_=pq[:, :]), reads=[pq], writes=[ATC])
            kb.dma("sp", out=self.AFFT[:, :], in_=AT[:, :], reads=[AT], writes=[self.AFFT])
            kb.dma("sp", out=self.AFFTC[:, :], in_=ATC[:, :], reads=[ATC], writes=[self.AFFTC])

    def select(self, es, A, P, F, K, cap, key_c, base_c, dump_c, gsum, idx_out_dram, tag):
        kb = self.kb
        lo = kb.sb(es, [P, 1], F32, "lo" + tag)
        hi = kb.sb(es, [P, 1], F32, "hi" + tag)
        mid = kb.sb(es, [P, 1], F32, "mid" + tag)
        cnt = kb.sb(es, [P, 1], F32, "cnt" + tag)
        ge = kb.sb(es, [P, 1], F32, "ge" + tag)
        d1 = kb.sb(es, [P, 1], F32, "d1" + tag)
        W = kb.sb(es, [P, F], F32, "W" + tag)
        pc = kb.ps(es, [P, 1], F32, "pc" + tag)
        kb.op("dve", lambda e: e.memset(lo[:, :], 0.0), writes=[lo])
        kb.op("dve", lambda e: e.memset(hi[:, :], 1.0), writes=[hi])
        for it in range(36):
            kb.op("dve", lambda e: e.tensor_tensor(out=mid[:, :], in0=lo[:, :], in1=hi[:, :], op=ALU.add), reads=[lo, hi], writes=[mid])
            kb.op("dve", lambda e: e.tensor_scalar(out=mid[:, :], in0=mid[:, :], scalar1=0.5, scalar2=None, op0=ALU.mult), reads=[mid], writes=[mid])
            kb.op("dve", lambda e: e.tensor_scalar(out=W[:, :], in0=A[:, :], scalar1=mid[:, 0:1], scalar2=0.0, op0=ALU.is_ge, op1=ALU.add,
                                                   accum_out=cnt[:, 0:1]), reads=[A, mid], writes=[W, cnt])
            if gsum is not None:
                kb.op("pe", lambda e: e.matmul(pc[:, :], lhsT=gsum[:, :], rhs=cnt[:, :], start=True, stop=True), reads=[gsum, cnt], writes=[pc])
                src = pc
            else:
                src = cnt
            kb.op("dve", lambda e, src=src: e.tensor_scalar(out=ge[:, :], in0=src[:, :], scalar1=float(K) - 0.5, scalar2=None, op0=ALU.is_ge),
                  reads=[src], writes=[ge])
            kb.op("dve", lambda e: e.tensor_tensor(out=d1[:, :], in0=mid[:, :], in1=lo[:, :], op=ALU.subtract), reads=[mid, lo], writes=[d1])
            kb.op("dve", lambda e: e.scalar_tensor_tensor(out=lo[:, :], in0=d1[:, :], scalar=ge[:, 0:1], in1=lo[:, :], op0=ALU.mult, op1=ALU.add),
                  reads=[d1, ge, lo], writes=[lo])
            kb.op("dve", lambda e: e.tensor_tensor(out=d1[:, :], in0=hi[:, :], in1=mid[:, :], op=ALU.subtract), reads=[hi, mid], writes=[d1])
            kb.op("dve", lambda e: e.scalar_tensor_tensor(out=hi[:, :], in0=d1[:, :], scalar=ge[:, 0:1], in1=mid[:, :], op0=ALU.mult, op1=ALU.add),
                  reads=[d1, ge, mid], writes=[hi])
        kb.op("dve", lambda e: e.scalar_tensor_tensor(out=W[:, :], in0=A[:, :], scalar=lo[:, 0:1], in1=key_c[0:P, :], op0=ALU.is_ge, op1=ALU.mult),
              reads=[A, lo, key_c], writes=[W])
        Lv = kb.sb(es, [P, cap], F32, "Lv" + tag)
        for r in range(cap // 8):
            kb.op("dve", lambda e, r=r: e.max(out=Lv[:, 8 * r:8 * r + 8], in_=W[:, :]), reads=[W], writes=[Lv])
            kb.op("dve", lambda e, r=r: e.match_replace(out=W[:, :], in_to_replace=Lv[:, 8 * r:8 * r + 8], in_values=W[:, :], imm_value=0.0),
                  reads=[W, Lv], writes=[W])
        tok = kb.sb(es, [P, cap], F32, "tok" + tag)
        vm = kb.sb(es, [P, cap], F32, "vm" + tag)
        idx = kb.sb(es, [P, cap], I32, "idx" + tag)
        kb.op("dve", lambda e: e.tensor_scalar(out=tok[:, :], in0=Lv[:, :], scalar1=-1.0, scalar2=base_c[0:P, 0:1], op0=ALU.mult, op1=ALU.add),
              reads=[Lv, base_c], writes=[tok])
        kb.op("dve", lambda e: e.tensor_tensor(out=tok[:, :], in0=tok[:, :], in1=dump_c[0:P, :], op=ALU.subtract), reads=[tok, dump_c], writes=[tok])
        kb.op("dve", lambda e: e.tensor_scalar(out=vm[:, :], in0=Lv[:, :], scalar1=0.5, scalar2=None, op0=ALU.is_ge), reads=[Lv], writes=[vm])
        kb.op("dve", lambda e: e.tensor_tensor(out=tok[:, :], in0=tok[:, :], in1=vm[:, :], op=ALU.mult), reads=[tok, vm], writes=[tok])
        kb.op("dve", lambda e: e.tensor_tensor(out=tok[:, :], in0=tok[:, :], in1=dump_c[0:P, :], op=ALU.add), reads=[tok, dump_c], writes=[tok])
        kb.op("dve", lambda e: e.tensor_copy(out=idx[:, :], in_=tok[:, :]), reads=[tok], writes=[idx])
        kb.dma("sp", out=idx_out_dram[:, :], in_=idx[:, :], reads=[idx], writes=[idx_out_dram])

    def ph_select(self, li):
        kb = self.kb
        I = self.I
        with contextlib.ExitStack() as es:
            cs = {}
            for k in ("gsum", "keyl", "keyc", "basel", "basec", "dumpl", "dumpc"):
                shp, ty = CONST_SPECS[k]
                cs[k] = kb.sb(es, shp, ty, k)
                kb.dma("sp", out=cs[k][:, :], in_=I[k][:, :], writes=[cs[k]])
            A = kb.sb(es, [128, 1024], F32, "Asel")
            kb.dma("sp", out=A[:, :], in_=self.AFFT[:, :].rearrange("e (b t) -> (e b) t", b=8), reads=[self.AFFT], writes=[A])
            self.select(es, A, 128, 1024, 1024, CAPB, cs["keyl"], cs["basel"], cs["dumpl"], cs["gsum"], self.IDXD, "l")
            Ac = kb.sb(es, [NE, LC], F32, "Aselc")
            kb.dma("sp", out=Ac[:, :], in_=self.AFFTC[:, :], reads=[self.AFFTC], writes=[Ac])
            self.select(es, Ac, NE, LC, CAPC, CAPC, cs["keyc"], cs["basec"], cs["dumpc"], None, self.IDXC, "c")

    def ph_moe(self, li):
        kb = self.kb
        I = self.I
        NS = NSLOT + CAPC
        chunks = [(0, 512), (512, 512), (1024, 512), (1536, CAPC)]
        with contextlib.ExitStack() as es:
            ga2 = [self.load_vec(es, s, 5, "ga2") for s in range(2)]
            idxt = kb.sb(es, [128, NE, NCALL], I32, "idxt")
            kb.dma("sp", out=idxt[:, :, :], in_=self.IDXD[:, :].rearrange("(e b) r -> e (b r)", b=8).rearrange("e (j p) -> p e j", p=128),
                   reads=[self.IDXD], writes=[idxt], allow_slow_non_contiguous=True)
            idxc = kb.sb(es, [CAPC, NE], I32, "idxc")
            kb.dma("sp", out=idxc[:, :], in_=self.IDXC[:, :].rearrange("e p -> p e"), reads=[self.IDXC], writes=[idxc],
                   allow_slow_non_contiguous=True)
            stage = [kb.sb(es, [128, 4096], F32, "stage") for _ in range(2)]
            w13 = [[kb.sb(es, [128, 8, 512], BF16, "w13") for _ in range(2)] for _ in range(2)]
            w2 = kb.sb(es, [128, 16, D], BF16, "w2")
            w2k = [Trk() for _ in range(4)]
            xgT = kb.sb(es, [128, 8, NS], BF16, "xgT")
            gT = kb.sb(es, [128, 16, NS], BF16, "gT")
            gTk = [Trk() for _ in range(16)]
            G = [kb.sb(es, [128, D], BF16, "G") for _ in range(2)]
            gate = [kb.sb(es, [128, NE], F32, "gate") for _ in range(3)]
            yb = [kb.sb(es, [128, D], F32, "yb") for _ in range(2)]
            sa = [kb.sb(es, [128, 512], BF16, "sa") for _ in range(2)]
            pA = [kb.ps(es, [128, 512], F32, "pA") for _ in range(2)]
            pB = [kb.ps(es, [128, 512], F32, "pB") for _ in range(2)]
            pT = [kb.ps(es, [128, 1024], BF16, "pTm") for _ in range(2)]
            pY = [kb.ps(es, [128, 512], F32, "pY") for _ in range(2)]
            stage_i = [0]
            cast_i = [0]

            def load_cast(dst_ap, dst_trk, src_ap, shape3):
                st = stage[stage_i[0] % 2]
                stage_i[0] += 1
                a, b = shape3
                sv = st[:, 0:a * b].rearrange("p (a b) -> p a b", a=a)
                kb.dma("sp", out=sv, in_=src_ap, writes=[st])
                eng = "pool" if cast_i[0] % 2 == 0 else "act"
                cast_i[0] += 1
                if eng == "pool":
                    kb.op("pool", lambda e: e.tensor_copy(out=dst_ap, in_=sv), reads=[st], writes=[dst_trk])
                else:
                    kb.op("act", lambda e: e.copy(out=dst_ap, in_=sv), reads=[st], writes=[dst_trk])

            gcount = 0
            for ex in range(NE):
                w1src = I["w_e1"][li, ex].rearrange("(kc p) f -> p kc f", p=128)
                w3src = I["w_e3"][li, ex].rearrange("(kc p) f -> p kc f", p=128)
                w2src = I["w_e2"][li, ex].rearrange("(fc p) d -> p fc d", p=128)
                calls = [(j, 128, idxt[:, ex, j:j + 1], j * 128) for j in range(NCALL)] + [(NCALL, CAPC, idxc[:, ex:ex + 1], NSLOT)]
                gates = []
                for (j, rows, iap, col0) in calls:
                    g = G[gcount % 2]
                    gt = gate[gcount % 3]
                    gcount += 1
                    kb.dma("pool", reads=[self.H2, idxt, idxc], writes=[g],
                           fn=lambda e, g=g, rows=rows, iap=iap: e.indirect_dma_start(out=g[0:rows, :], out_offset=None, in_=self.H2[:, :],
                                                                                      in_offset=bass.IndirectOffsetOnAxis(ap=iap, axis=0)))
                    for kc in range(8):
                        p = pT[(kc // 8) % 2]
                    p = pT[gcount % 2]
                    for kc in range(8):
                        kb.op("pe", lambda e, p=p, g=g, kc=kc, rows=rows: e.transpose(out=p[:, kc * 128:kc * 128 + rows],
                                                                                    in_=g[0:rows, kc * 128:(kc + 1) * 128],
                                                                                    identity=self.ident_bf[0:rows, 0:rows]),
                              reads=[g, self.ident_bf], writes=[p])
                    kb.op("dve", lambda e, p=p, rows=rows, col0=col0: e.tensor_copy(
                        out=xgT[:, :, col0:col0 + rows], in_=p[:, :].rearrange("p (k t) -> p k t", k=8)[:, :, 0:rows]),
                          reads=[p], writes=[xgT])
                for q in range(4):
                    wb = w13[q % 2]
                    load_cast(wb[0][:, :, :], wb[0].k, w1src[:, :, q * 512:(q + 1) * 512], (8, 512))
                    load_cast(wb[1][:, :, :], wb[1].k, w3src[:, :, q * 512:(q + 1) * 512], (8, 512))
                    for f4 in range(4):
                        fc = q * 4 + f4
                        for ci, (c0, cn) in enumerate(chunks):
                            k = (fc * 4 + ci) % 2
                            for kc in range(8):
                                kb.op("pe", lambda e, k=k, kc=kc, f4=f4, c0=c0, cn=cn, wb=wb: e.matmul(
                                    pA[k][:, 0:cn], lhsT=wb[0][:, kc, f4 * 128:(f4 + 1) * 128], rhs=xgT[:, kc, c0:c0 + cn],
                                    start=(kc == 0), stop=(kc == 7)), reads=[wb[0], xgT], writes=[pA[k]])
                            for kc in range(8):
                                kb.op("pe", lambda e, k=k, kc=kc, f4=f4, c0=c0, cn=cn, wb=wb: e.matmul(
                                    pB[k][:, 0:cn], lhsT=wb[1][:, kc, f4 * 128:(f4 + 1) * 128], rhs=xgT[:, kc, c0:c0 + cn],
                                    start=(kc == 0), stop=(kc == 7)), reads=[wb[1], xgT], writes=[pB[k]])
                            kb.op("act", lambda e, k=k, cn=cn: e.activation(out=sa[k][:, 0:cn], in_=pA[k][:, 0:cn], func=AF.Silu),
                                  reads=[pA[k]], writes=[sa[k]])
                            kb.op("dve", lambda e, k=k, cn=cn, c0=c0, fc=fc: e.tensor_tensor(out=gT[:, fc, c0:c0 + cn], in0=sa[k][:, 0:cn],
                                                                                          in1=pB[k][:, 0:cn], op=ALU.mult),
                                  reads=[sa[k], pB[k]], writes=[gTk[fc]])
                for g4 in range(4):
                    load_cast(w2[:, g4 * 4:(g4 + 1) * 4, :], w2k[g4], w2src[:, g4 * 4:(g4 + 1) * 4, :], (4, D))
                for (j, rows, iap, col0) in calls:
                    s = 0 if j < NCALL else 1
                    gt = gate[j % 3]
                    kb.dma("pool", reads=[self.AFF, idxt, idxc], writes=[gt],
                           fn=lambda e, gt=gt, rows=rows, iap=iap: e.indirect_dma_start(out=gt[0:rows, :], out_offset=None, in_=self.AFF[:, :],
                                                                                        in_offset=bass.IndirectOffsetOnAxis(ap=iap, axis=0)))
                    y = yb[j % 2]
                    for h in range(2):
                        p = pY[h]
                        for fc in range(16):
                            kb.op("pe", lambda e, p=p, fc=fc, col0=col0, rows=rows, h=h: e.matmul(
                                p[0:rows, :], lhsT=gT[:, fc, col0:col0 + rows], rhs=w2[:, fc, h * 512:(h + 1) * 512],
                                start=(fc == 0), stop=(fc == 15)), reads=[gTk[fc], w2k[fc // 4]], writes=[p])
                        kb.op("dve", lambda e, p=p, y=y, h=h, rows=rows, gt=gt, s=s, ex=ex: e.scalar_tensor_tensor(
                            out=y[0:rows, h * 512:(h + 1) * 512], in0=p[0:rows, :], scalar=gt[0:rows, ex:ex + 1],
                            in1=ga2[s][0:rows, h * 512:(h + 1) * 512], op0=ALU.mult, op1=ALU.mult),
                              reads=[p, gt, ga2[s]], writes=[y])
                    kb.dma("pool", reads=[y, idxt, idxc], writes=[self.X],
                           fn=lambda e, y=y, rows=rows, iap=iap: e.indirect_dma_start(out=self.X[:, :],
                                                                                      out_offset=bass.IndirectOffsetOnAxis(ap=iap, axis=0),
                                                                                      in_=y[0:rows, :], in_offset=None, compute_op=ALU.add))


    def load_w_bf16(self, es, dst, src3, ncols, stage):
        kb = self.kb
        i = 0
        for c0 in range(0, ncols, 512):
            cn = min(512, ncols - c0)
            st = stage[i % 2]
            sv = st[:, 0:8 * cn].rearrange("p (a b) -> p a b", a=8)
            kb.dma("sp", out=sv, in_=src3[:, :, c0:c0 + cn], writes=[st])
            if i % 2 == 0:
                kb.op("pool", lambda e, sv=sv, c0=c0, cn=cn: e.tensor_copy(out=dst[:, :, c0:c0 + cn], in_=sv), reads=[st], writes=[dst])
            else:
                kb.op("act", lambda e, sv=sv, c0=c0, cn=cn: e.copy(out=dst[:, :, c0:c0 + cn], in_=sv), reads=[st], writes=[dst])
            i += 1

    def transpose_tile(self, hb, hT, pT):
        kb = self.kb
        for kc in range(8):
            kb.op("pe", lambda e, kc=kc: e.transpose(out=pT[:, kc * 128:(kc + 1) * 128], in_=hb[:, kc * 128:(kc + 1) * 128],
                                                     identity=self.ident_bf[:, :]), reads=[hb, self.ident_bf], writes=[pT])
        kb.op("act", lambda e: e.copy(out=hT[:, :, :], in_=pT[:, :].rearrange("p (k t) -> p k t", k=8)), reads=[pT], writes=[hT])

    def ntiles(self, need_ctx):
        return NTILE if need_ctx else 64

    def ph_qkv(self, li):
        kb = self.kb
        I = self.I
        j = li // 2
        if not hasattr(self, "QKT"):
            self.QKT = kb.dram("QKT", [16, 128, NT], BF16)
            self.VD = kb.dram("VD", [NT + 64, 16 * 65], BF16)
            self.OD = kb.dram("OD", [NT, D], BF16)
        with contextlib.ExitStack() as es:
            svec = [self.load_vec(es, s, 1, "s1") for s in range(2)]
            shvec = [self.load_vec(es, s, 0, "sh1") for s in range(2)]
            negh = kb.sb(es, [128, 1], F32, "negh")
            kb.op("dve", lambda e: e.memset(negh[:, :], -0.5), writes=[negh])
            stage = [kb.sb(es, [128, 4096], F32, "stage") for _ in range(2)]
            W = kb.sb(es, [128, 8, 3 * D], BF16, "wqkv")
            self.load_w_bf16(es, W, I["w_qkv"][j].rearrange("(kc p) n -> p kc n", p=128), 3 * D, stage)
            NB = 2
            xt = [kb.sb(es, [128, D], F32, "xt") for _ in range(NB)]
            hf = [kb.sb(es, [128, D], F32, "hf") for _ in range(NB)]
            hb = [kb.sb(es, [128, D], BF16, "hb") for _ in range(NB)]
            junk = kb.sb(es, [128, D], BF16, "junk")
            hT = [kb.sb(es, [128, 8, 128], BF16, "hT") for _ in range(NB)]
            sm = [[kb.sb(es, [128, 1], F32, "sm") for _ in range(2)] for _ in range(NB)]
            qk = [kb.sb(es, [128, 16, 128], BF16, "qk") for _ in range(NB)]
            vt = [kb.sb(es, [128, 16, 65], BF16, "vt") for _ in range(NB)]
            for b in range(NB):
                kb.op("dve", lambda e, b=b: e.memset(vt[b][:, :, :], 1.0), writes=[vt[b]])
            pT = [kb.ps(es, [128, 1024], BF16, "pT") for _ in range(2)]
            pq = [kb.ps(es, [128, 512], F32, "pq") for _ in range(4)]
            pi = 0
            for ti in range(NTILE):
                b = ti % NB
                s = 0 if ti < 64 else 1
                r0 = ti * 128
                kb.dma("sp", out=xt[b][:, :], in_=self.X[r0:r0 + 128, :], reads=[self.X], writes=[xt[b]])
                self.norm_tile((sm[b][0], sm[b][1]), xt[b], svec[s], shvec[s], hf[b], hb[b], junk, negh)
                self.transpose_tile(hb[b], hT[b], pT[ti % 2])
                for c4 in range(4):
                    p = pq[pi % 4]
                    pi += 1
                    for cc in range(4):
                        c = c4 * 4 + cc
                        for kc in range(8):
                            kb.op("pe", lambda e, p=p, cc=cc, c=c, kc=kc, b=b: e.matmul(p[:, cc * 128:(cc + 1) * 128], lhsT=W[:, kc, c * 128:(c + 1) * 128],
                                                                                     rhs=hT[b][:, kc, :], start=(kc == 0), stop=(kc == 7)),
                                  reads=[W, hT[b]], writes=[p])
                    sc = 0.125 if c4 < 2 else 1.0
                    kb.op("act", lambda e, p=p, c4=c4, b=b, sc=sc: e.activation(out=qk[b][:, c4 * 4:c4 * 4 + 4, :], in_=p[:, :].rearrange("p (c t) -> p c t", c=4),
                                                                              func=AF.Copy, scale=sc), reads=[p], writes=[qk[b]])
                kb.dma("sp", out=self.QKT[:, :, r0:r0 + 128].rearrange("c p t -> p c t"), in_=qk[b][:, :, :], reads=[qk[b]], writes=[self.QKT])
                for h2 in range(2):
                    p = pq[pi % 4]
                    pi += 1
                    for kc in range(8):
                        kb.op("pe", lambda e, p=p, kc=kc, h2=h2, b=b: e.matmul(p[:, :], lhsT=hT[b][:, kc, :], rhs=W[:, kc, 2 * D + h2 * 512:2 * D + (h2 + 1) * 512],
                                                                            start=(kc == 0), stop=(kc == 7)), reads=[W, hT[b]], writes=[p])
                    kb.op("dve", lambda e, p=p, h2=h2, b=b: e.tensor_copy(out=vt[b][:, h2 * 8:(h2 + 1) * 8, 0:64], in_=p[:, :].rearrange("p (h d) -> p h d", h=8)),
                          reads=[p], writes=[vt[b]])
                kb.dma("sp", out=self.VD[r0:r0 + 128, :], in_=vt[b][:, :, :].rearrange("p h d -> p (h d)"), reads=[vt[b]], writes=[self.VD])

    def ph_attn(self, li, need_ctx):
        kb = self.kb
        I = self.I
        j = li // 2
        with contextlib.ExitStack() as es:
            KT = kb.sb(es, [128, NT], BF16, "KT")
            QT = kb.sb(es, [128, NT], BF16, "QT")
            V0 = kb.sb(es, [128, NTILE, 130], BF16, "V0")
            V1 = kb.sb(es, [128, NTILE, 130], BF16, "V1")
            BI = kb.sb(es, [128, 2, 4, 64], F32, "BI")
            BE = [kb.sb(es, [128, 2, 4, 64], F32, "BE") for _ in range(2)]
            PTs = [kb.sb(es, [128, 384], BF16, "PTs") for _ in range(3)]
            osb = [kb.sb(es, [64, 128], BF16, "osb") for _ in range(2)]
            rec = [kb.sb(es, [64, 1], F32, "rec") for _ in range(4)]
            pS = [kb.ps(es, [128, 384], F32, "pS") for _ in range(3)]
            pO = [kb.ps(es, [64, 2, 65], F32, "pO") for _ in range(2)]
            VDv = self.VD[0:NT, :].rearrange("(t p) c -> p t c", p=128)
            VDs = self.VD[64:NT + 64, :].rearrange("(t p) c -> p t c", p=128)
            ui = 0
            ei = 0
            nq = 128 + (4 if need_ctx else 0)
            for hp in range(8):
                kb.dma("sp", out=QT[:, :], in_=self.QKT[hp, :, :], reads=[self.QKT], writes=[QT])
                kb.dma("sp", out=KT[:, :], in_=self.QKT[8 + hp, :, :], reads=[self.QKT], writes=[KT])
                kb.dma("sp", out=V0[:, :, :], in_=VDv[:, :, hp * 130:(hp + 1) * 130], reads=[self.VD], writes=[V0])
                kb.dma("sp", out=V1[:, 0:NTILE - 1, :], in_=VDs[:, 0:NTILE - 1, hp * 130:(hp + 1) * 130], reads=[self.VD], writes=[V1])
                kb.dma("sp", out=BI[:, :, :, :], in_=I["biasT"][j, 4, 2 * hp:2 * hp + 2].rearrange("h c p q -> p h c q"), writes=[BI])
                for qi in range(nq):
                    if qi < 128:
                        r = qi
                        rs = min(max(r - 4, 0), 120)
                        var = r - rs
                        q0 = 64 * r
                        k0 = 64 * rs
                        nlat = 4
                        if var == 4:
                            bias = BI
                        else:
                            bias = BE[ei % 2]
                            ei += 1
                            kb.dma("sp", out=bias[:, :, :, :], in_=I["biasT"][j, var, 2 * hp:2 * hp + 2].rearrange("h c p q -> p h c q"), writes=[bias])
                        if rs % 2 == 0:
                            vsrc = [(V0, rs // 2 + c) for c in range(4)]
                        else:
                            vsrc = [(V1, (rs - 1) // 2 + c) for c in range(4)]
                        vsrc += [(V0, 64), (V0, 65)]
                        kcols = [k0 + 128 * c for c in range(4)] + [L, L + 128]
                    else:
                        q0 = L + 64 * (qi - 128)
                        nlat = 0
                        bias = None
                        vsrc = [(V0, 64), (V0, 65)]
                        kcols = [L, L + 128]
                    nch = len(kcols)
                    ob = osb[qi % 2]
                    po = pO[qi % 2]
                    for hl in range(2):
                        ho = hl * 64
                        ps_ = pS[ui % 3]
                        pt = PTs[ui % 3]
                        ui += 1
                        for c in range(nch):
                            kb.op("pe", lambda e, ps_=ps_, c=c, ho=ho, kc0=kcols[c], q0=q0: e.matmul(
                                ps_[:, c * 64:(c + 1) * 64], lhsT=KT[ho:ho + 64, kc0:kc0 + 128], rhs=QT[ho:ho + 64, q0:q0 + 64],
                                start=True, stop=True), reads=[KT, QT], writes=[ps_])
                        if nlat:
                            kb.op("dve", lambda e, ps_=ps_, bias=bias, hl=hl: e.tensor_tensor(
                                out=ps_[:, 0:256], in0=ps_[:, 0:256], in1=bias[:, hl, :, :].rearrange("p c q -> p (c q)"), op=ALU.add),
                                  reads=[ps_, bias], writes=[ps_])
                        kb.op("act", lambda e, ps_=ps_, pt=pt, nch=nch: e.activation(out=pt[:, 0:nch * 64], in_=ps_[:, 0:nch * 64], func=AF.Exp),
                              reads=[ps_], writes=[pt])
                        for c in range(nch):
                            vb, vtile = vsrc[c]
                            kb.op("pe", lambda e, po=po, c=c, pt=pt, vb=vb, vtile=vtile, hl=hl, nch=nch: e.matmul(
                                po[:, hl, :], lhsT=pt[:, c * 64:(c + 1) * 64], rhs=vb[:, vtile, hl * 65:(hl + 1) * 65],
                                start=(c == 0), stop=(c == nch - 1)), reads=[pt, vb], writes=[po])
                        rc = rec[ui % 4]
                        kb.op("dve", lambda e, po=po, rc=rc, hl=hl: e.reciprocal(out=rc[:, 0:1], in_=po[:, hl, 64:65]), reads=[po], writes=[rc])
                        kb.op("dve", lambda e, po=po, rc=rc, hl=hl, ob=ob: e.tensor_scalar(out=ob[:, hl * 64:(hl + 1) * 64], in0=po[:, hl, 0:64],
                                                                                          scalar1=rc[:, 0:1], scalar2=None, op0=ALU.mult),
                              reads=[po, rc], writes=[ob])
                    kb.dma("sp", out=self.OD[q0:q0 + 64, hp * 128:(hp + 1) * 128], in_=ob[:, :], reads=[ob], writes=[self.OD])

    def ph_oproj(self, li, wname, srcname, need_ctx):
        kb = self.kb
        I = self.I
        j = li // 2
        SRC = getattr(self, srcname)
        with contextlib.ExitStack() as es:
            ga = [self.load_vec(es, s, 2, "ga1") for s in range(2)]
            stage = [kb.sb(es, [128, 4096], F32, "stage") for _ in range(2)]
            W = kb.sb(es, [128, 8, D], BF16, "wo")
            self.load_w_bf16(es, W, I[wname][j].rearrange("(kc p) n -> p kc n", p=128), D, stage)
            NB = 2
            xt = [kb.sb(es, [128, D], F32, "xt") for _ in range(NB)]
            hb = [kb.sb(es, [128, D], BF16, "hb") for _ in range(NB)]
            tmp = [kb.sb(es, [128, D], F32, "tmp") for _ in range(NB)]
            hT = [kb.sb(es, [128, 8, 128], BF16, "hT") for _ in range(NB)]
            pT = [kb.ps(es, [128, 1024], BF16, "pT") for _ in range(2)]
            pq = [kb.ps(es, [128, 512], F32, "pq") for _ in range(4)]
            for ti in range(self.ntiles(need_ctx)):
                b = ti % NB
                s = 0 if ti < 64 else 1
                r0 = ti * 128
                kb.dma("sp", out=xt[b][:, :], in_=self.X[r0:r0 + 128, :], reads=[self.X], writes=[xt[b]])
                kb.dma("sp", out=hb[b][:, :], in_=SRC[r0:r0 + 128, :], reads=[SRC], writes=[hb[b]])
                self.transpose_tile(hb[b], hT[b], pT[ti % 2])
                for h2 in range(2):
                    p = pq[(2 * ti + h2) % 4]
                    for kc in range(8):
                        kb.op("pe", lambda e, p=p, kc=kc, h2=h2, b=b: e.matmul(p[:, :], lhsT=hT[b][:, kc, :], rhs=W[:, kc, h2 * 512:(h2 + 1) * 512],
                                                                            start=(kc == 0), stop=(kc == 7)), reads=[W, hT[b]], writes=[p])
                    kb.op("dve", lambda e, p=p, h2=h2, b=b, s=s: e.tensor_tensor(out=tmp[b][:, h2 * 512:(h2 + 1) * 512], in0=p[:, :],
                                                                              in1=ga[s][:, h2 * 512:(h2 + 1) * 512], op=ALU.mult),
                          reads=[p, ga[s]], writes=[tmp[b]])
                kb.op("pool", lambda e, b=b: e.tensor_tensor(out=xt[b][:, :], in0=xt[b][:, :], in1=tmp[b][:, :], op=ALU.add),
                      reads=[xt[b], tmp[b]], writes=[xt[b]])
                kb.dma("sp", out=self.X[r0:r0 + 128, :], in_=xt[b][:, :], reads=[xt[b]], writes=[self.X])


    def cload(self, es, name):
        shp, ty = CONST_SPECS[name]
        t = self.kb.sb(es, shp, ty, name)
        sl = tuple(slice(None) for _ in shp)
        self.kb.dma("sp", out=t[sl], in_=self.I[name][sl], writes=[t])
        return t

    def even_scratch(self):
        kb = self.kb
        if not hasattr(self, "PR"):
            self.PR = kb.dram("PR", [NT, 1536], BF16)
            self.DTR = kb.dram("DTR", [NT, 16], F32)
            self.AD = kb.dram("AD", [NT, 1024], BF16)
            self.XBC = kb.dram("XBC", [NT, 1024], BF16)
            self.DTA = kb.dram("DTA", [NT, 32], F32)
            self.YF = kb.dram("YF", [NT, 512], F32)
            self.MIX = kb.dram("MIX", [NT, 1024], BF16)
            self.GD = kb.dram("GD", [2, 64, 128, 512], BF16)

    def ph_inproj(self, li):
        kb = self.kb
        I = self.I
        j = li // 2
        self.even_scratch()
        with contextlib.ExitStack() as es:
            svec = [self.load_vec(es, s, 1, "s1") for s in range(2)]
            shvec = [self.load_vec(es, s, 0, "sh1") for s in range(2)]
            negh = kb.sb(es, [128, 1], F32, "negh")
            kb.op("dve", lambda e: e.memset(negh[:, :], -0.5), writes=[negh])
            stage = [kb.sb(es, [128, 4096], F32, "stage") for _ in range(2)]
            W = kb.sb(es, [128, 8, 2064], BF16, "win")
            self.load_w_bf16(es, W, I["w_in_e"][j].rearrange("(kc p) n -> p kc n", p=128), 2064, stage)
            CS = self.cload(es, "CS")
            NB = 2
            xt = [kb.sb(es, [128, D], F32, "xt") for _ in range(NB)]
            hf = [kb.sb(es, [128, D], F32, "hf") for _ in range(NB)]
            hb = [kb.sb(es, [128, D], BF16, "hb") for _ in range(NB)]
            junk = kb.sb(es, [128, D], BF16, "junk")
            hT = [kb.sb(es, [128, 8, 128], BF16, "hT") for _ in range(NB)]
            sm = [[kb.sb(es, [128, 1], F32, "sm") for _ in range(2)] for _ in range(NB)]
            prt = [kb.sb(es, [128, 1536], BF16, "prt") for _ in range(NB)]
            dtr = [kb.sb(es, [128, 16], F32, "dtr") for _ in range(NB)]
            uT = [kb.sb(es, [128, 4, 128], BF16, "uT") for _ in range(NB)]
            At = [kb.sb(es, [128, 2, 4, 128], BF16, "At") for _ in range(NB)]
            pT = [kb.ps(es, [128, 1024], BF16, "pT") for _ in range(2)]
            pq = [kb.ps(es, [128, 512], F32, "pq") for _ in range(4)]
            pa = [kb.ps(es, [128, 512], F32, "pa") for _ in range(2)]
            pi = 0
            for ti in range(NTILE):
                b = ti % NB
                s = 0 if ti < 64 else 1
                r0 = ti * 128
                kb.dma("sp", out=xt[b][:, :], in_=self.X[r0:r0 + 128, :], reads=[self.X], writes=[xt[b]])
                self.norm_tile((sm[b][0], sm[b][1]), xt[b], svec[s], shvec[s], hf[b], hb[b], junk, negh)
                self.transpose_tile(hb[b], hT[b], pT[ti % 2])
                for ci, (c0, cn) in enumerate([(0, 512), (512, 512), (1024, 512), (1536, 16)]):
                    p = pq[pi % 4]
                    pi += 1
                    for kc in range(8):
                        kb.op("pe", lambda e, p=p, kc=kc, c0=c0, cn=cn, b=b: e.matmul(p[:, 0:cn], lhsT=hT[b][:, kc, :], rhs=W[:, kc, c0:c0 + cn],
                                                                                   start=(kc == 0), stop=(kc == 7)), reads=[W, hT[b]], writes=[p])
                    if ci < 3:
                        if ci % 2 == 0:
                            kb.op("act", lambda e, p=p, c0=c0, b=b: e.copy(out=prt[b][:, c0:c0 + 512], in_=p[:, :]), reads=[p], writes=[prt[b]])
                        else:
                            kb.op("dve", lambda e, p=p, c0=c0, b=b: e.tensor_copy(out=prt[b][:, c0:c0 + 512], in_=p[:, :]), reads=[p], writes=[prt[b]])
                    else:
                        kb.op("dve", lambda e, p=p, b=b: e.tensor_copy(out=dtr[b][:, :], in_=p[:, 0:16]), reads=[p], writes=[dtr[b]])
                kb.dma("sp", out=self.PR[r0:r0 + 128, :], in_=prt[b][:, :], reads=[prt[b]], writes=[self.PR])
                kb.dma("sp", out=self.DTR[r0:r0 + 128, :], in_=dtr[b][:, :], reads=[dtr[b]], writes=[self.DTR])
                p = pq[pi % 4]
                pi += 1
                for g in range(4):
                    for kc in range(8):
                        kb.op("pe", lambda e, p=p, kc=kc, g=g, b=b: e.matmul(p[:, g * 128:(g + 1) * 128], lhsT=W[:, kc, 1552 + g * 128:1552 + (g + 1) * 128],
                                                                          rhs=hT[b][:, kc, :], start=(kc == 0), stop=(kc == 7)), reads=[W, hT[b]], writes=[p])
                kb.op("act", lambda e, p=p, b=b: e.copy(out=uT[b][:, :, :], in_=p[:, :].rearrange("p (g t) -> p g t", g=4)), reads=[p], writes=[uT[b]])
                for gh in range(2):
                    for g2 in range(2):
                        g = gh * 2 + g2
                        kb.op("pe", lambda e, gh=gh, g2=g2, g=g, b=b: e.matmul(pa[gh][:, g2 * 256:(g2 + 1) * 256], lhsT=uT[b][:, g, :], rhs=CS[:, :],
                                                                            start=True, stop=True), reads=[uT[b], CS], writes=[pa[gh]])
                    kb.op("dve", lambda e, gh=gh, b=b: e.tensor_copy(out=At[b][:, :, 2 * gh:2 * gh + 2, :],
                                                                   in_=pa[gh][:, :].rearrange("p (g cs q) -> p cs g q", g=2, cs=2)),
                          reads=[pa[gh]], writes=[At[b]])
                kb.dma("sp", out=self.AD[r0:r0 + 128, :], in_=At[b][:, :, :, :].rearrange("p cs g q -> p (cs g q)"), reads=[At[b]], writes=[self.AD])

    def ph_conv(self, li):
        kb = self.kb
        I = self.I
        j = li // 2
        with contextlib.ExitStack() as es:
            cw = [kb.sb(es, [128, D], F32, "cw") for _ in range(3)]
            for k in range(3):
                kb.dma("sp", out=cw[k][:, :], in_=I["conv_w"][j, k, :].partition_broadcast(128), writes=[cw[k]])
            cb = kb.sb(es, [128, D], F32, "cb")
            kb.dma("sp", out=cb[:, :], in_=I["conv_b"][j, :].partition_broadcast(128), writes=[cb])
            dtb = kb.sb(es, [128, 16], F32, "dtb")
            kb.dma("sp", out=dtb[:, :], in_=I["dt_bias"][j, :].partition_broadcast(128), writes=[dtb])
            abc = kb.sb(es, [128, 16], F32, "abc")
            kb.dma("sp", out=abc[:, :], in_=I["a_log"][j, :].partition_broadcast(128), writes=[abc])
            kb.op("act", lambda e: e.activation(out=abc[:, :], in_=abc[:, :], func=AF.Exp), reads=[abc], writes=[abc])
            kb.op("dve", lambda e: e.tensor_scalar(out=abc[:, :], in0=abc[:, :], scalar1=-1.0, scalar2=None, op0=ALU.mult), reads=[abc], writes=[abc])
            NB = 2
            m1 = [kb.sb(es, [128, D], BF16, "m1") for _ in range(NB)]
            c0 = [kb.sb(es, [128, D], BF16, "c0") for _ in range(NB)]
            p1 = [kb.sb(es, [128, D], BF16, "p1") for _ in range(NB)]
            acc = [kb.sb(es, [128, D], F32, "acc") for _ in range(NB)]
            t2 = [kb.sb(es, [128, D], F32, "t2") for _ in range(NB)]
            xo = [kb.sb(es, [128, D], BF16, "xo") for _ in range(NB)]
            dtr = [kb.sb(es, [128, 16], F32, "dtr") for _ in range(NB)]
            dta = [kb.sb(es, [128, 32], F32, "dta") for _ in range(NB)]
            for ti in range(NTILE):
                b = ti % NB
                r0 = ti * 128
                first = ti in (0, 64)
                last = ti in (63, 65)
                src = self.PR
                if first:
                    kb.op("dve", lambda e, b=b: e.memset(m1[b][:, :], 0.0), writes=[m1[b]])
                    kb.dma("sp", out=m1[b][1:128, :], in_=src[r0:r0 + 127, 512:1536], reads=[src], writes=[m1[b]])
                else:
                    kb.dma("sp", out=m1[b][:, :], in_=src[r0 - 1:r0 + 127, 512:1536], reads=[src], writes=[m1[b]])
                kb.dma("sp", out=c0[b][:, :], in_=src[r0:r0 + 128, 512:1536], reads=[src], writes=[c0[b]])
                if last:
                    kb.op("dve", lambda e, b=b: e.memset(p1[b][:, :], 0.0), writes=[p1[b]])
                    kb.dma("sp", out=p1[b][0:127, :], in_=src[r0 + 1:r0 + 128, 512:1536], reads=[src], writes=[p1[b]])
                else:
                    kb.dma("sp", out=p1[b][:, :], in_=src[r0 + 1:r0 + 129, 512:1536], reads=[src], writes=[p1[b]])
                kb.op("dve", lambda e, b=b: e.tensor_tensor(out=acc[b][:, :], in0=m1[b][:, :], in1=cw[0][:, :], op=ALU.mult), reads=[m1[b], cw[0]], writes=[acc[b]])
                kb.op("pool", lambda e, b=b: e.tensor_tensor(out=t2[b][:, :], in0=c0[b][:, :], in1=cw[1][:, :], op=ALU.mult), reads=[c0[b], cw[1]], writes=[t2[b]])
                kb.op("dve", lambda e, b=b: e.tensor_tensor(out=acc[b][:, :], in0=acc[b][:, :], in1=t2[b][:, :], op=ALU.add), reads=[acc[b], t2[b]], writes=[acc[b]])
                kb.op("pool", lambda e, b=b: e.tensor_tensor(out=t2[b][:, :], in0=p1[b][:, :], in1=cw[2][:, :], op=ALU.mult), reads=[p1[b], cw[2]], writes=[t2[b]])
                kb.op("dve", lambda e, b=b: e.tensor_tensor(out=acc[b][:, :], in0=acc[b][:, :], in1=t2[b][:, :], op=ALU.add), reads=[acc[b], t2[b]], writes=[acc[b]])
                kb.op("pool", lambda e, b=b: e.tensor_tensor(out=acc[b][:, :], in0=acc[b][:, :], in1=cb[:, :], op=ALU.add), reads=[acc[b], cb], writes=[acc[b]])
                kb.op("act", lambda e, b=b: e.activation(out=xo[b][:, :], in_=acc[b][:, :], func=AF.Silu), reads=[acc[b]], writes=[xo[b]])
                kb.dma("sp", out=self.XBC[r0:r0 + 128, :], in_=xo[b][:, :], reads=[xo[b]], writes=[self.XBC])
                kb.dma("sp", out=dtr[b][:, :], in_=self.DTR[r0:r0 + 128, :], reads=[self.DTR], writes=[dtr[b]])
                kb.op("dve", lambda e, b=b: e.tensor_tensor(out=dtr[b][:, :], in0=dtr[b][:, :], in1=dtb[:, :], op=ALU.add), reads=[dtr[b], dtb], writes=[dtr[b]])
                kb.op("act", lambda e, b=b: e.activation(out=dtr[b][:, :], in_=dtr[b][:, :], func=AF.Exp), reads=[dtr[b]], writes=[dtr[b]])
                kb.op("act", lambda e, b=b: e.activation(out=dta[b][:, 0:16], in_=dtr[b][:, :], func=AF.Ln, bias=1.0), reads=[dtr[b]], writes=[dta[b]])
                kb.op("dve", lambda e, b=b: e.tensor_tensor(out=dta[b][:, 16:32], in0=dta[b][:, 0:16], in1=abc[:, :], op=ALU.mult), reads=[dta[b], abc], writes=[dta[b]])
                kb.dma("sp", out=self.DTA[r0:r0 + 128, :], in_=dta[b][:, :], reads=[dta[b]], writes=[self.DTA])

    def ph_ssd(self, li, d):
        kb = self.kb
        I = self.I
        j = li // 2
        with contextlib.ExitStack() as es:
            tri = self.cload(es, "triU" if d == 0 else "triL")
            ones = self.cload(es, "onesf")
            neg = self.cload(es, "negU" if d == 0 else "negL")
            dsk = kb.sb(es, [128, 8], F32, "dsk")
            kb.dma("sp", out=dsk[:, :], in_=I["d_skip"][j, :].partition_broadcast(128), writes=[dsk])
            gss = kb.sb(es, [128, 512], F32, "gss")
            kb.dma("sp", out=gss[:, :], in_=I["g_ssd"][j, :].partition_broadcast(128), writes=[gss])
            negh = kb.sb(es, [128, 1], F32, "negh")
            kb.op("dve", lambda e: e.memset(negh[:, :], -0.5), writes=[negh])
            hs = kb.sb(es, [128, 8, 64], F32, "hs")
            hsb = kb.sb(es, [128, 8, 64], BF16, "hsb")
            kb.op("dve", lambda e: e.memset(hs[:, :, :], 0.0), writes=[hs])
            kb.op("dve", lambda e: e.memset(hsb[:, :, :], 0.0), writes=[hsb])
            NB = 2
            R = lambda shape, ty, nm, n=NB: [kb.sb(es, shape, ty, nm) for _ in range(n)]
            xbc = R([128, D], BF16, "xbc")
            dta = R([128, 32], F32, "dta")
            BCT = R([128, 4, 128], BF16, "BCT")
            cum = R([128, 16], F32, "cum")
            te = R([128, 8], F32, "te")
            cd = R([128, 8], F32, "cd")
            xdt = R([128, 8, 64], BF16, "xdt")
            xw = R([128, 8, 64], BF16, "xw")
            CBT = R([128, 2, 128], BF16, "CBT")
            dU = R([128, 8, 128], F32, "dU")
            Ebc = R([128, 8, 128], BF16, "Ebc")
            CsT = R([128, 8, 128], BF16, "CsT")
            Sg = R([128, 8, 128], F32, "Sg")
            Dm = R([128, 8, 128], BF16, "Dm")
            MT = R([128, 8, 128], BF16, "MT")
            tmp = R([128, 8, 64], F32, "tmp")
            yo = R([128, 512], F32, "yo")
            yf = R([128, 512], F32, "yf")
            zt = R([128, 512], BF16, "zt")
            sz = R([128, 512], F32, "sz")
            junk = kb.sb(es, [128, 256], BF16, "junk")
            ssq = R([128, 2], F32, "ssq")
            rsq = R([128, 2], F32, "rsq")
            mo = R([128, 512], BF16, "mo")
            pT = kb.ps(es, [128, 1024], BF16, "pT")
            pc = kb.ps(es, [128, 16], F32, "pc")
            pcb = kb.ps(es, [128, 256], F32, "pcb")
            pA = [kb.ps(es, [128, 512], F32, "pA") for _ in range(2)]
            py = kb.ps(es, [128, 512], F32, "py")
            pst = kb.ps(es, [128, 512], F32, "pst")
            order = ([64, 65] + list(range(64))) if d == 0 else ([65, 64] + list(range(63, -1, -1)))
            for n, ti in enumerate(order):
                b = n % NB
                r0 = ti * 128
                X_, DT_ = xbc[b], dta[b]
                kb.dma("sp", out=X_[:, :], in_=self.XBC[r0:r0 + 128, :], reads=[self.XBC], writes=[X_])
                kb.dma("sp", out=DT_[:, :], in_=self.DTA[r0:r0 + 128, :], reads=[self.DTA], writes=[DT_])
                dt_d = DT_[:, 8 * d:8 * d + 8]
                da_d = DT_[:, 16 + 8 * d:16 + 8 * d + 8]
                x3 = X_[:, 0:512].rearrange("p (h c) -> p h c", h=8)
                for q in range(4):
                    kb.op("pe", lambda e, q=q, X_=X_: e.transpose(out=pT[:, q * 128:(q + 1) * 128], in_=X_[:, 512 + q * 128:512 + (q + 1) * 128],
                                                               identity=self.ident_bf[:, :]), reads=[X_, self.ident_bf], writes=[pT])
                kb.op("act", lambda e, b=b: e.copy(out=BCT[b][:, :, :], in_=pT[:, 0:512].rearrange("p (q t) -> p q t", q=4)), reads=[pT], writes=[BCT[b]])
                kb.op("pe", lambda e, da_d=da_d: e.matmul(pc[:, 0:8], lhsT=tri[:, :], rhs=da_d, start=True, stop=True), reads=[tri, DT_], writes=[pc])
                kb.op("pe", lambda e, da_d=da_d: e.matmul(pc[:, 8:16], lhsT=ones[:, :], rhs=da_d, start=True, stop=True), reads=[ones, DT_], writes=[pc])
                kb.op("dve", lambda e, b=b: e.tensor_copy(out=cum[b][:, :], in_=pc[:, :]), reads=[pc], writes=[cum[b]])
                kb.op("dve", lambda e, b=b: e.tensor_tensor(out=te[b][:, :], in0=cum[b][:, 8:16], in1=cum[b][:, 0:8], op=ALU.subtract), reads=[cum[b]], writes=[te[b]])
                kb.op("act", lambda e, b=b: e.activation(out=te[b][:, :], in_=te[b][:, :], func=AF.Exp), reads=[te[b]], writes=[te[b]])
                kb.op("act", lambda e, b=b: e.activation(out=cd[b][:, :], in_=cum[b][:, 8:16], func=AF.Exp), reads=[cum[b]], writes=[cd[b]])
                kb.op("dve", lambda e, b=b, x3=x3, dt_d=dt_d: e.tensor_tensor(out=xdt[b][:, :, :], in0=x3, in1=dt_d.unsqueeze(2).to_broadcast([128, 8, 64]), op=ALU.mult),
                      reads=[X_, DT_], writes=[xdt[b]])
                kb.op("pool", lambda e, b=b: e.tensor_tensor(out=xw[b][:, :, :], in0=xdt[b][:, :, :], in1=te[b][:, :].unsqueeze(2).to_broadcast([128, 8, 64]), op=ALU.mult),
                      reads=[xdt[b], te[b]], writes=[xw[b]])
                for g in range(2):
                    kb.op("pe", lambda e, g=g, b=b: e.matmul(pcb[:, g * 128:(g + 1) * 128], lhsT=BCT[b][:, g, :], rhs=BCT[b][:, 2 + g, :], start=True, stop=True),
                          reads=[BCT[b]], writes=[pcb])
                kb.op("act", lambda e, b=b: e.copy(out=CBT[b][:, :, :], in_=pcb[:, :].rearrange("p (g t) -> p g t", g=2)), reads=[pcb], writes=[CBT[b]])
                kb.op("dve", lambda e, b=b, da_d=da_d: e.tensor_tensor(out=dU[b][:, :, :], in0=tri[:, :].unsqueeze(1).to_broadcast([128, 8, 128]),
                                                                     in1=da_d.unsqueeze(2).to_broadcast([128, 8, 128]), op=ALU.mult),
                      reads=[tri, DT_], writes=[dU[b]])
                for hh in range(2):
                    kb.op("pe", lambda e, hh=hh, b=b: e.matmul(pA[hh][:, :], lhsT=ones[:, :], rhs=dU[b][:, 4 * hh:4 * hh + 4, :].rearrange("p h i -> p (h i)"),
                                                             start=True, stop=True), reads=[ones, dU[b]], writes=[pA[hh]])
                    kb.op("act", lambda e, hh=hh, b=b: e.activation(out=Ebc[b][:, 4 * hh:4 * hh + 4, :].rearrange("p h i -> p (h i)"), in_=pA[hh][:, :], func=AF.Exp),
                          reads=[pA[hh]], writes=[Ebc[b]])
                for g in range(2):
                    kb.op("pool", lambda e, g=g, b=b: e.tensor_tensor(out=CsT[b][:, 4 * g:4 * g + 4, :], in0=Ebc[b][:, 4 * g:4 * g + 4, :],
                                                                    in1=BCT[b][:, 2 + g, :].unsqueeze(1).to_broadcast([128, 4, 128]), op=ALU.mult),
                          reads=[Ebc[b], BCT[b]], writes=[CsT[b]])
                for h in range(8):
                    kb.op("dve", lambda e, h=h, b=b: e.scalar_tensor_tensor(out=Sg[b][:, h, :], in0=pA[h // 4][:, (h % 4) * 128:(h % 4 + 1) * 128],
                                                                          scalar=cum[b][:, h:h + 1], in1=neg[:, :], op0=ALU.subtract, op1=ALU.add),
                          reads=[pA[h // 4], cum[b], neg], writes=[Sg[b]])
                kb.op("act", lambda e, b=b: e.activation(out=Dm[b][:, :, :], in_=Sg[b][:, :, :], func=AF.Exp), reads=[Sg[b]], writes=[Dm[b]])
                for g in range(2):
                    kb.op("dve", lambda e, g=g, b=b: e.tensor_tensor(out=MT[b][:, 4 * g:4 * g + 4, :], in0=Dm[b][:, 4 * g:4 * g + 4, :],
                                                                   in1=CBT[b][:, g, :].unsqueeze(1).to_broadcast([128, 4, 128]), op=ALU.mult),
                          reads=[Dm[b], CBT[b]], writes=[MT[b]])
                for h in range(8):
                    kb.op("pe", lambda e, h=h, b=b: e.matmul(py[:, h * 64:(h + 1) * 64], lhsT=MT[b][:, h, :], rhs=xdt[b][:, h, :], start=(h == 0), stop=False,
                                                           skip_group_check=True), reads=[MT[b], xdt[b]], writes=[py])
                for h in range(8):
                    kb.op("pe", lambda e, h=h, b=b: e.matmul(py[:, h * 64:(h + 1) * 64], lhsT=CsT[b][:, h, :], rhs=hsb[:, h, :], start=False, stop=(h == 7),
                                                           skip_group_check=True), reads=[CsT[b], hsb], writes=[py])
                for g in range(2):
                    kb.op("pe", lambda e, g=g, b=b, X_=X_: e.matmul(pst[:, g * 256:(g + 1) * 256], lhsT=X_[:, 512 + g * 128:512 + (g + 1) * 128],
                                                                 rhs=xw[b][:, 4 * g:4 * g + 4, :].rearrange("p h c -> p (h c)"), start=True, stop=True),
                          reads=[X_, xw[b]], writes=[pst])
                if d == 0:
                    kb.op("pool", lambda e, b=b, x3=x3: e.tensor_tensor(out=yo[b][:, :].rearrange("p (h c) -> p h c", h=8), in0=x3,
                                                                      in1=dsk[:, :].unsqueeze(2).to_broadcast([128, 8, 64]), op=ALU.mult),
                          reads=[X_, dsk], writes=[yo[b]])
                    kb.op("dve", lambda e, b=b: e.tensor_tensor(out=yo[b][:, :], in0=py[:, :], in1=yo[b][:, :], op=ALU.add), reads=[py, yo[b]], writes=[yo[b]])
                    kb.dma("sp", out=self.YF[r0:r0 + 128, :], in_=yo[b][:, :], reads=[yo[b]], writes=[self.YF])
                else:
                    kb.dma("sp", out=yf[b][:, :], in_=self.YF[r0:r0 + 128, :], reads=[self.YF], writes=[yf[b]])
                    kb.dma("sp", out=zt[b][:, :], in_=self.PR[r0:r0 + 128, 0:512], reads=[self.PR], writes=[zt[b]])
                    kb.op("dve", lambda e, b=b: e.tensor_tensor(out=yo[b][:, :], in0=py[:, :], in1=yf[b][:, :], op=ALU.add), reads=[py, yf[b]], writes=[yo[b]])
                    kb.op("act", lambda e, b=b: e.activation(out=sz[b][:, :], in_=zt[b][:, :], func=AF.Silu), reads=[zt[b]], writes=[sz[b]])
                    kb.op("dve", lambda e, b=b: e.tensor_tensor(out=yo[b][:, :], in0=yo[b][:, :], in1=sz[b][:, :], op=ALU.mult), reads=[yo[b], sz[b]], writes=[yo[b]])
                    for g in range(2):
                        kb.op("act", lambda e, b=b, g=g: e.activation(out=junk[:, :], in_=yo[b][:, g * 256:(g + 1) * 256], func=AF.Square, accum_out=ssq[b][:, g:g + 1]),
                              reads=[yo[b]], writes=[junk, ssq[b]])
                    kb.op("dve", lambda e, b=b: e.tensor_scalar(out=ssq[b][:, :], in0=ssq[b][:, :], scalar1=1.0 / 256, scalar2=EPS, op0=ALU.mult, op1=ALU.add),
                          reads=[ssq[b]], writes=[ssq[b]])
                    kb.op("pool", lambda e, b=b: e.tensor_tensor(out=rsq[b][:, :], in0=ssq[b][:, :], in1=negh[:, 0:1].to_broadcast([128, 2]), op=ALU.pow),
                          reads=[ssq[b], negh], writes=[rsq[b]])
                    for g in range(2):
                        kb.op("dve", lambda e, b=b, g=g: e.scalar_tensor_tensor(out=mo[b][:, g * 256:(g + 1) * 256], in0=yo[b][:, g * 256:(g + 1) * 256],
                                                                              scalar=rsq[b][:, g:g + 1], in1=gss[:, g * 256:(g + 1) * 256], op0=ALU.mult, op1=ALU.mult),
                              reads=[yo[b], rsq[b], gss], writes=[mo[b]])
                    kb.dma("sp", out=self.MIX[r0:r0 + 128, 0:512], in_=mo[b][:, :], reads=[mo[b]], writes=[self.MIX])
                kb.op("dve", lambda e, b=b: e.tensor_tensor(out=tmp[b][:, :, :], in0=hs[:, :, :], in1=cd[b][:, :].unsqueeze(2).to_broadcast([128, 8, 64]), op=ALU.mult),
                      reads=[hs, cd[b]], writes=[tmp[b]])
                kb.op("dve", lambda e, b=b: e.tensor_tensor(out=hs[:, :, :], in0=tmp[b][:, :, :], in1=pst[:, :].rearrange("p (h c) -> p h c", h=8), op=ALU.add),
                      reads=[tmp[b], pst], writes=[hs])
                kb.op("act", lambda e: e.copy(out=hsb[:, :, :], in_=hs[:, :, :]), reads=[hs], writes=[hsb])

    def ph_fourier(self, li):
        kb = self.kb
        I = self.I
        SC = 1.0 / np.sqrt(8192.0 * 128.0)
        SCC = 1.0 / np.sqrt(256.0 * 128.0)
        with contextlib.ExitStack() as es:
            F1 = self.cload(es, "F1")
            blk = [kb.sb(es, [128, 16, 1024], BF16, "blk") for _ in range(2)]
            gt = [kb.sb(es, [128, 2, 512], BF16, "gt") for _ in range(3)]
            pg = [kb.ps(es, [128, 512], F32, "pg") for _ in range(4)]
            ADv = self.AD[0:L, :].rearrange("(n1 n2) c -> n1 n2 c", n2=64)
            for rd in range(4):
                bk = blk[rd % 2]
                kb.dma("sp", out=bk[:, :, :], in_=ADv[:, rd * 16:(rd + 1) * 16, :], reads=[self.AD], writes=[bk])
                for nl in range(16):
                    n2 = rd * 16 + nl
                    g = gt[n2 % 3]
                    pr_, pi_ = pg[(2 * n2) % 4], pg[(2 * n2 + 1) % 4]
                    Ac = bk[:, nl, 0:512]
                    As = bk[:, nl, 512:1024]
                    kb.op("pe", lambda e, pr_=pr_, Ac=Ac: e.matmul(pr_[:, :], lhsT=F1[:, 0, :], rhs=Ac, start=True, stop=False), reads=[F1, bk], writes=[pr_])
                    kb.op("pe", lambda e, pr_=pr_, As=As: e.matmul(pr_[:, :], lhsT=F1[:, 1, :], rhs=As, start=False, stop=True), reads=[F1, bk], writes=[pr_])
                    kb.op("pe", lambda e, pi_=pi_, Ac=Ac: e.matmul(pi_[:, :], lhsT=F1[:, 1, :], rhs=Ac, start=True, stop=False), reads=[F1, bk], writes=[pi_])
                    kb.op("pe", lambda e, pi_=pi_, As=As: e.matmul(pi_[:, :], lhsT=F1[:, 2, :], rhs=As, start=False, stop=True), reads=[F1, bk], writes=[pi_])
                    kb.op("act", lambda e, g=g, pr_=pr_: e.copy(out=g[:, 0, :], in_=pr_[:, :]), reads=[pr_], writes=[g])
                    kb.op("dve", lambda e, g=g, pi_=pi_: e.tensor_copy(out=g[:, 1, :], in_=pi_[:, :]), reads=[pi_], writes=[g])
                    kb.dma("sp", out=self.GD[:, n2, :, :].rearrange("ri p c -> p ri c"), in_=g[:, :, :], reads=[g], writes=[self.GD])
        kb.barrier()
        with contextlib.ExitStack() as es:
            TW = self.cload(es, "TW3")
            blk = [kb.sb(es, [128, 32, 512], BF16, "blk3") for _ in range(2)]
            ot = [kb.sb(es, [64, 32, 512], BF16, "ot") for _ in range(2)]
            pg = [kb.ps(es, [64, 512], F32, "pg3") for _ in range(4)]
            GDv = self.GD[:, :, :, :].rearrange("ri n2 p c -> (ri n2) p c")
            MIXv = self.MIX[0:L, :].rearrange("(p2 p1) c -> p2 p1 c", p1=128)
            for rd in range(4):
                bk = blk[rd % 2]
                o = ot[rd % 2]
                kb.dma("sp", out=bk[:, :, :], in_=GDv[:, rd * 32:(rd + 1) * 32, :], reads=[self.GD], writes=[bk])
                for pl in range(32):
                    p1 = rd * 32 + pl
                    p = pg[p1 % 4]
                    kb.op("pe", lambda e, p=p, p1=p1, pl=pl, bk=bk: e.matmul(p[:, :], lhsT=TW[:, p1, :], rhs=bk[:, pl, :], start=True, stop=True), reads=[TW, bk], writes=[p])
                    if pl % 2 == 0:
                        kb.op("act", lambda e, p=p, pl=pl, o=o: e.activation(out=o[:, pl, :], in_=p[:, :], func=AF.Copy, scale=float(SC)), reads=[p], writes=[o])
                    else:
                        kb.op("dve", lambda e, p=p, pl=pl, o=o: e.tensor_scalar(out=o[:, pl, :], in0=p[:, :], scalar1=float(SC), scalar2=None, op0=ALU.mult), reads=[p], writes=[o])
                kb.dma("sp", out=MIXv[:, rd * 32:(rd + 1) * 32, 512:1024], in_=o[:, :, :], reads=[o], writes=[self.MIX])
            C2 = self.cload(es, "C256")
            S2 = self.cload(es, "nS256")
            ac = kb.sb(es, [128, 2, 1024], BF16, "actx")
            kb.dma("sp", out=ac[:, :, :], in_=self.AD[L:NT, :].rearrange("(t p) c -> p t c", p=128), reads=[self.AD], writes=[ac])
            oc = kb.sb(es, [128, 2, 512], BF16, "octx")
            pcx = [kb.ps(es, [128, 512], F32, "pcx") for _ in range(2)]
            for pt in range(2):
                p = pcx[pt]
                k = 0
                for nt in range(2):
                    for (M, off) in ((C2, 0), (S2, 512)):
                        kb.op("pe", lambda e, p=p, M=M, nt=nt, pt=pt, off=off, k=k: e.matmul(p[:, :], lhsT=M[:, nt, pt * 128:(pt + 1) * 128], rhs=ac[:, nt, off:off + 512],
                                                                                          start=(k == 0), stop=(k == 3)), reads=[M, ac], writes=[p])
                        k += 1
                kb.op("act", lambda e, p=p, pt=pt: e.activation(out=oc[:, pt, :], in_=p[:, :], func=AF.Copy, scale=float(SCC)), reads=[p], writes=[oc])
            kb.dma("sp", out=self.MIX[L:NT, 512:1024].rearrange("(t p) c -> p t c", p=128), in_=oc[:, :, :], reads=[oc], writes=[self.MIX])

    def ph_final(self):
        kb = self.kb
        I = self.I
        with contextlib.ExitStack() as es:
            gf = kb.sb(es, [128, D], F32, "gf")
            kb.dma("sp", out=gf[:, :], in_=I["g_final"].partition_broadcast(128), writes=[gf])
            zero = kb.sb(es, [128, D], F32, "zero")
            kb.op("dve", lambda e: e.memset(zero[:, :], 0.0), writes=[zero])
            negh = kb.sb(es, [128, 1], F32, "negh")
            kb.op("dve", lambda e: e.memset(negh[:, :], -0.5), writes=[negh])
            junk = kb.sb(es, [128, D], BF16, "junk")
            NB = 3
            xt = [kb.sb(es, [128, D], F32, "xt") for _ in range(NB)]
            of = [kb.sb(es, [128, D], F32, "of") for _ in range(NB)]
            sm = [[kb.sb(es, [128, 1], F32, "sm") for _ in range(2)] for _ in range(NB)]
            for ti in range(64):
                b = ti % NB
                r0 = ti * 128
                kb.dma("sp", out=xt[b][:, :], in_=self.X[r0:r0 + 128, :], reads=[self.X], writes=[xt[b]])
                self.norm_tile((sm[b][0], sm[b][1]), xt[b], gf, zero, of[b], None, junk, negh)
                kb.dma("sp", out=self.out[r0:r0 + 128, :], in_=of[b][:, :], reads=[of[b]], writes=[self.outk])

    def ph_dumpx(self):
        kb = self.kb
        for r0 in range(0, L, 2048):
            kb.dma("sp", out=self.out[r0:r0 + 2048, :], in_=self.X[r0:r0 + 2048, :], reads=[self.X], writes=[self.outk])


def default_phases():
    ph = [("init",)]
    for li in range(DEPTH):
        need_ctx = li < DEPTH - 1
        ph += [("mod", li)]
        if li % 2 == 0:
            ph += [("inproj", li), ("conv", li), ("ssd", li, 0), ("ssd", li, 1), ("fourier", li),
                   ("oproj", li, "w_out_e", "MIX", need_ctx)]
        else:
            ph += [("qkv", li), ("attn", li, need_ctx), ("oproj", li, "w_o", "OD", need_ctx)]
        ph += [("router", li), ("select", li), ("moe", li)]
    ph += [("final",)]
    return ph


def build_bias_table(rpb):
    no = rpb.shape[0]
    out = np.empty((no, 8, 16, 4, 128, 64), np.float32)
    q = np.arange(64)
    cs = np.clip(q - 8, 0, 48)
    for v in range(8):
        for c in range(4):
            tile = np.full((no, 16, 128, 64), -30000.0, np.float32)
            for half in range(2):
                ro = -v + 2 * c + half + 7
                for qq in range(64):
                    cols = np.arange(cs[qq], cs[qq] + 16)
                    tile[:, :, half * 64 + cols, qq] = rpb[:, :, ro, cols - qq + 15]
            out[:, v, :, c] = tile
    return out


def make_in_maps(inputs, n_cores, names):
    consts = host_consts()
    shared = {}
    for k in names:
        if k in consts:
            shared[k] = consts[k]
        elif k in ("x", "ctx", "c"):
            pass
        elif k == "biasT":
            shared[k] = build_bias_table(inputs["rpb"])
        elif k in ("a_log", "dt_bias"):
            shared[k] = np.ascontiguousarray(inputs[k].reshape(2, 16))
        else:
            shared[k] = np.ascontiguousarray(inputs[k])
    maps = []
    for b in range(n_cores):
        m = dict(shared)
        for k in ("x", "ctx", "c"):
            if k in names:
                m[k] = np.ascontiguousarray(inputs[k][b])
        maps.append(m)
    return maps


def kernel(**inputs):
    inputs = {k: np.asarray(v) for k, v in inputs.items()}
    prog = Prog(default_phases())
    in_maps = make_in_maps(inputs, 4, list(prog.I.keys()))
    res = run_bass_kernel_spmd(prog.nc, in_maps, core_ids=list(range(4)))
    out = np.stack([res.results[b]["out"] for b in range(4)], axis=0)
    return out.astype(np.float32)
```

```python
import contextlib
import numpy as np
import ml_dtypes
import concourse.bass as bass
import concourse.mybir as mybir
from concourse.bass_utils import run_bass_kernel_spmd

F32 = mybir.dt.float32
BF16 = mybir.dt.bfloat16
I32 = mybir.dt.int32
AF = mybir.ActivationFunctionType
ALU = mybir.AluOpType
AX = mybir.AxisListType
IOA = bass.IndirectOffsetOnAxis if hasattr(bass, "IndirectOffsetOnAxis") else None

D = 1024
L = 8192
LC = 256
NT = L + LC
NTILE = NT // 128
DEPTH = 4
NE = 16
FF = 2048
CAPB = 192
NSLOT = 8 * CAPB
NCALL = NSLOT // 128
CAPC = 32
XR = NT + (NCALL + 1) * 128
EPS = 1e-6


class Trk:
    __slots__ = ("w", "r", "x")

    def __init__(self, x=False):
        self.w = None
        self.r = {}
        self.x = x


class Buf:
    def __init__(self, t, x=False):
        self.t = t
        self.k = Trk(x)

    def __getitem__(self, key):
        return self.t[key]


class KB:
    ND = 40

    def __init__(self, nc):
        self.nc = nc
        self.es = contextlib.ExitStack()
        self.eng = {"pe": nc.tensor, "act": nc.scalar, "dve": nc.vector, "pool": nc.gpsimd, "sp": nc.sync}
        self.csem = {}
        self.ccnt = {}
        for e in ("pe", "act", "dve", "pool"):
            self.csem[e] = self.es.enter_context(nc.semaphore("cs_" + e))
            self.ccnt[e] = 0
        self.dsem = [self.es.enter_context(nc.semaphore("ds%d" % i)) for i in range(self.ND)]
        self.dcnt = [0] * self.ND
        self.dnext = 0
        self.seen = {e: {} for e in self.eng}
        self.uid = 0

    def sb(self, es, shape, dtype, name=None):
        self.uid += 1
        return Buf(es.enter_context(self.nc.sbuf_tensor("%s_%d" % (name or "sb", self.uid), list(shape), dtype)))

    def ps(self, es, shape, dtype, name=None):
        self.uid += 1
        return Buf(es.enter_context(self.nc.psum_tensor("%s_%d" % (name or "ps", self.uid), list(shape), dtype)), x=True)

    def dram(self, name, shape, dtype):
        return Buf(self.nc.dram_tensor(name, list(shape), dtype, kind="Internal").ap())

    def _sem(self, ch):
        return self.csem[ch] if isinstance(ch, str) else self.dsem[ch]

    def _deps(self, eng, reads, writes, is_dma):
        deps = {}

        def add(ev, raw):
            if ev is None:
                return
            ch, v = ev
            if (not is_dma) and ch == eng:
                if eng == "pe" or not raw:
                    return
            if deps.get(ch, 0) < v:
                deps[ch] = v

        for t in reads:
            add(t.w, True)
            if t.x:
                for ch, v in t.r.items():
                    if ch != eng:
                        add((ch, v), False)
        for t in writes:
            add(t.w, False)
            for ch, v in t.r.items():
                add((ch, v), False)
        return deps

    def _wait(self, eng, deps):
        s = self.seen[eng]
        for ch, v in deps.items():
            if s.get(ch, 0) < v:
                self.eng[eng].wait_ge(self._sem(ch), v)
                s[ch] = v

    @staticmethod
    def _trks(lst):
        return [b.k if isinstance(b, Buf) else b for b in lst]

    def op(self, eng, fn, reads=(), writes=()):
        reads = self._trks(reads)
        writes = self._trks(writes)
        self._wait(eng, self._deps(eng, reads, writes, False))
        ins = fn(self.eng[eng])
        self.ccnt[eng] += 1
        v = self.ccnt[eng]
        ins.then_inc(self.csem[eng], 1)
        for t in reads:
            if t.r.get(eng, 0) < v:
                t.r[eng] = v
        for t in writes:
            t.w = (eng, v)
            t.r = {}
        return ins

    def dma(self, q, out=None, in_=None, reads=(), writes=(), fn=None, **kw):
        reads = self._trks(reads)
        writes = self._trks(writes)
        s = self.dnext
        self.dnext = (s + 1) % self.ND
        deps = self._deps(q, reads, writes, True)
        if self.dcnt[s] > 0 and deps.get(s, 0) < self.dcnt[s]:
            deps[s] = self.dcnt[s]
        self._wait(q, deps)
        if fn is None:
            ins = self.eng[q].dma_start(out=out, in_=in_, **kw)
        else:
            ins = fn(self.eng[q])
        self.dcnt[s] += 16
        v = self.dcnt[s]
        ins.then_inc(self.dsem[s], 16)
        for t in reads:
            if t.r.get(s, 0) < v:
                t.r[s] = v
        for t in writes:
            t.w = (s, v)
            t.r = {}
        return ins

    def barrier(self):
        for e in self.eng:
            deps = {}
            for c in self.csem:
                if self.ccnt[c] > 0 and c != e:
                    deps[c] = self.ccnt[c]
            for s in range(self.ND):
                if self.dcnt[s] > 0:
                    deps[s] = self.dcnt[s]
            self._wait(e, deps)
        for e in self.csem:
            if self.ccnt[e] > 0:
                s = self.seen[e]
                if s.get(e, 0) < self.ccnt[e]:
                    self.eng[e].wait_ge(self.csem[e], self.ccnt[e])
                    s[e] = self.ccnt[e]


def host_consts():
    c = {}
    c["ident_bf"] = np.eye(128, dtype=np.float32).astype(ml_dtypes.bfloat16)
    c["ident_f"] = np.eye(128, dtype=np.float32)
    p = np.arange(128)
    c["gsum"] = (p[:, None] // 8 == p[None, :] // 8).astype(np.float32)
    c["keyl"] = np.broadcast_to((1024 - np.arange(1024, dtype=np.float32))[None, :], (128, 1024)).copy()
    c["keyc"] = np.broadcast_to((256 - np.arange(256, dtype=np.float32))[None, :], (128, 256)).copy()
    c["basel"] = ((p % 8) * 1024 + 1024).astype(np.float32)[:, None].copy()
    c["basec"] = np.full((128, 1), L + 256, np.float32)
    sig = (p[:, None] % 8) * CAPB + np.arange(CAPB)[None, :]
    c["dumpl"] = (NT + sig).astype(np.float32)
    c["dumpc"] = np.broadcast_to((NT + NSLOT + np.arange(CAPC, dtype=np.float32))[None, :], (128, CAPC)).copy()
    k = np.arange(128)
    c["triU"] = (k[:, None] <= k[None, :]).astype(np.float32)
    c["triL"] = (k[:, None] >= k[None, :]).astype(np.float32)
    c["onesf"] = np.ones((128, 128), np.float32)
    c["negU"] = np.where(k[None, :] >= k[:, None], 0.0, -30000.0).astype(np.float32)
    c["negL"] = np.where(k[None, :] <= k[:, None], 0.0, -30000.0).astype(np.float32)
    ang = 2 * np.pi * np.outer(k, k) / 128.0
    bf = ml_dtypes.bfloat16
    c["CS"] = np.concatenate([np.cos(ang), np.sin(ang)], axis=1).astype(np.float32).astype(bf)
    c["F1"] = np.stack([np.cos(ang), -np.sin(ang), -np.cos(ang)], axis=1).astype(np.float32).astype(bf)
    n2 = np.arange(64)[:, None, None]
    p1 = np.arange(128)[None, :, None]
    p2 = np.arange(64)[None, None, :]
    th = 2 * np.pi * (n2 * p2 / 64.0 + n2 * p1 / 8192.0)
    c["TW3"] = np.concatenate([np.cos(th), np.sin(th)], axis=0).astype(np.float32).astype(bf)
    n = np.arange(256)
    a2 = 2 * np.pi * np.outer(n, n) / 256.0
    c["C256"] = np.cos(a2).reshape(2, 128, 256).transpose(1, 0, 2).astype(np.float32).astype(bf)
    c["nS256"] = (-np.sin(a2)).reshape(2, 128, 256).transpose(1, 0, 2).astype(np.float32).astype(bf)
    return c


CONST_SPECS = {
    "triU": ([128, 128], F32), "triL": ([128, 128], F32), "onesf": ([128, 128], F32), "negU": ([128, 128], F32),
    "negL": ([128, 128], F32), "CS": ([128, 256], BF16), "F1": ([128, 3, 128], BF16), "TW3": ([128, 128, 64], BF16),
    "C256": ([128, 2, 256], BF16), "nS256": ([128, 2, 256], BF16),
    "ident_bf": ([128, 128], BF16), "ident_f": ([128, 128], F32), "gsum": ([128, 128], F32),
    "keyl": ([128, 1024], F32), "keyc": ([128, 256], F32), "basel": ([128, 1], F32), "basec": ([128, 1], F32),
    "dumpl": ([128, CAPB], F32), "dumpc": ([128, CAPC], F32),
}


IN_SPECS = {
    "x": ([L, D], F32), "ctx": ([LC, D], F32), "c": ([D], F32), "c_ctx": ([D], F32),
    "w_mod": ([DEPTH, D, 6 * D], F32), "b_mod": ([DEPTH, 6 * D], F32), "g_mix": ([DEPTH, D], F32), "g_ffn": ([DEPTH, D], F32),
    "w_router": ([DEPTH, D, NE], F32), "w_e1": ([DEPTH, NE, D, FF], F32), "w_e3": ([DEPTH, NE, D, FF], F32),
    "w_e2": ([DEPTH, NE, FF, D], F32), "g_final": ([D], F32),
    "w_in_e": ([2, D, 2064], F32), "conv_w": ([2, 3, D], F32), "conv_b": ([2, D], F32), "a_log": ([2, 16], F32),
    "dt_bias": ([2, 16], F32), "d_skip": ([2, 8], F32), "g_ssd": ([2, 512], F32), "w_out_e": ([2, D, D], F32),
    "w_qkv": ([2, D, 3 * D], F32), "w_o": ([2, D, D], F32), "biasT": ([2, 5, 16, 128, 5, 128], F32),
}


def pipeline(n, stages):
    ns = len(stages)
    for step in range(n + ns - 1):
        for si in range(ns - 1, -1, -1):
            k = step - si
            if 0 <= k < n:
                stages[si](k)


class Prog:
    def __init__(self, phases, debug_out=None):
        self.phases = phases
        self.debug_out = debug_out
        nc = bass.Bass("TRN2", target_bir_lowering=False)
        self.nc = nc
        self.kb = KB(nc)
        kb = self.kb
        dt = nc.dram_tensor

        class LazyIn(dict):
            def __missing__(d, k):
                shp, ty = IN_SPECS[k] if k in IN_SPECS else CONST_SPECS[k]
                d[k] = dt(k, list(shp), ty, kind="ExternalInput").ap()
                return d[k]
        self.I = LazyIn()
        self.out = dt("out", [L, D], F32, kind="ExternalOutput").ap()
        self.X = kb.dram("Xres", [XR, D], F32)
        self.H2 = kb.dram("H2", [XR, D], BF16)
        self.AFF = kb.dram("AFF", [XR, NE], F32)
        self.AFFT = kb.dram("AFFT", [NE, L], F32)
        self.AFFTC = kb.dram("AFFTC", [NE, LC], F32)
        self.IDXD = kb.dram("IDXD", [128, CAPB], I32)
        self.IDXC = kb.dram("IDXC", [NE, CAPC], I32)
        self.MODD = kb.dram("MODD", [2, 6 * D], F32)
        self.outk = Trk()
        self.build()

    def build(self):
        kb = self.kb
        with contextlib.ExitStack() as ges:
            self.ges = ges
            self.ident_bf = kb.sb(ges, [128, 128], BF16, "identbf")
            self.ident_f = kb.sb(ges, [128, 128], F32, "identf")
            kb.dma("sp", out=self.ident_bf[:, :], in_=self.I["ident_bf"][:, :], writes=[self.ident_bf])
            kb.dma("sp", out=self.ident_f[:, :], in_=self.I["ident_f"][:, :], writes=[self.ident_f])
            for ph in self.phases:
                name = ph[0]
                getattr(self, "ph_" + name)(*ph[1:])
                kb.barrier()
            kb.barrier()
        kb.es.close()

    def ph_init(self):
        kb = self.kb
        with contextlib.ExitStack() as es:
            for r0 in range(0, L, 2048):
                kb.dma("sp", out=self.X[r0:r0 + 2048, :], in_=self.I["x"][r0:r0 + 2048, :], writes=[self.X])
            kb.dma("sp", out=self.X[L:NT, :], in_=self.I["ctx"][:, :], writes=[self.X])
            z = kb.sb(es, [128, D], F32, "z")
            zb = kb.sb(es, [128, D], BF16, "zb")
            kb.op("dve", lambda e: e.memset(z[:, :], 0.0), writes=[z])
            kb.op("dve", lambda e: e.memset(zb[:, :], 0.0), writes=[zb])
            for j in range(NCALL + 1):
                r0 = NT + j * 128
                kb.dma("sp", out=self.X[r0:r0 + 128, :], in_=z[:, :], reads=[z], writes=[self.X])
                kb.dma("sp", out=self.H2[r0:r0 + 128, :], in_=zb[:, :], reads=[zb], writes=[self.H2])
                kb.dma("sp", out=self.AFF[r0:r0 + 128, :], in_=z[:, 0:NE], reads=[z], writes=[self.AFF])

    def ph_mod(self, li):
        kb = self.kb
        I = self.I
        with contextlib.ExitStack() as es:
            cv = kb.sb(es, [128, 2, 8], F32, "cv")
            kb.dma("sp", out=cv[:, 0, :], in_=I["c"].rearrange("(kc p) -> p kc", p=128), writes=[cv],
                   allow_slow_non_contiguous=True)
            kb.dma("sp", out=cv[:, 1, :], in_=I["c_ctx"].rearrange("(kc p) -> p kc", p=128), writes=[cv],
                   allow_slow_non_contiguous=True)
            sv = kb.sb(es, [128, 2, 8], F32, "sv")
            kb.op("act", lambda e: e.activation(out=sv[:, :, :], in_=cv[:, :, :], func=AF.Silu), reads=[cv], writes=[sv])
            lb = kb.sb(es, [128, 2, 8, 128], F32, "lb")
            for s in range(2):
                kb.op("dve", lambda e, s=s: e.tensor_copy(out=lb[:, s, :, :], in_=sv[:, s, :].unsqueeze(2).to_broadcast([128, 8, 128])),
                      reads=[sv], writes=[lb])
            gmix = kb.sb(es, [128, D], F32, "gmix")
            gffn = kb.sb(es, [128, D], F32, "gffn")
            kb.dma("sp", out=gmix[:, :], in_=I["g_mix"][li, :].partition_broadcast(128), writes=[gmix])
            kb.dma("sp", out=gffn[:, :], in_=I["g_ffn"][li, :].partition_broadcast(128), writes=[gffn])
            wm = [kb.sb(es, [128, 8, 512], F32, "wm") for _ in range(2)]
            bm = [kb.sb(es, [128, 512], F32, "bm") for _ in range(2)]
            pp = [kb.ps(es, [128, 512], F32, "pm") for _ in range(4)]
            res = [kb.sb(es, [128, 512], F32, "res") for _ in range(4)]
            wsrc = I["w_mod"][li].rearrange("(kc p) n -> p kc n", p=128)
            for n in range(12):
                w = wm[n % 2]
                b = bm[n % 2]
                kb.dma("sp", out=w[:, :, :], in_=wsrc[:, :, n * 512:(n + 1) * 512], writes=[w])
                kb.dma("sp", out=b[:, :], in_=I["b_mod"][li, n * 512:(n + 1) * 512].partition_broadcast(128), writes=[b])
                for s in range(2):
                    p = pp[(2 * n + s) % 4]
                    r = res[(2 * n + s) % 4]
                    for kc in range(8):
                        kb.op("pe", lambda e, kc=kc, s=s, p=p, w=w: e.matmul(p[:, :], lhsT=lb[:, s, kc, :], rhs=w[:, kc, :],
                                                                          start=(kc == 0), stop=(kc == 7)),
                              reads=[lb, w], writes=[p])
                    which = n // 2
                    if which in (1, 4):
                        g = gmix if which == 1 else gffn
                        c0 = (n % 2) * 512
                        kb.op("dve", lambda e, p=p, r=r, b=b: e.tensor_tensor(out=r[:, :], in0=p[:, :], in1=b[:, :], op=ALU.add),
                              reads=[p, b], writes=[r])
                        kb.op("dve", lambda e, r=r, g=g, c0=c0: e.scalar_tensor_tensor(out=r[:, :], in0=r[:, :], scalar=1.0,
                                                                                     in1=g[:, c0:c0 + 512], op0=ALU.add, op1=ALU.mult),
                              reads=[r, g], writes=[r])
                    else:
                        kb.op("dve", lambda e, p=p, r=r, b=b: e.tensor_tensor(out=r[:, :], in0=p[:, :], in1=b[:, :], op=ALU.add),
                              reads=[p, b], writes=[r])
                    kb.dma("sp", out=self.MODD[s:s + 1, n * 512:(n + 1) * 512], in_=r[0:1, :], reads=[r], writes=[self.MODD])

    def load_vec(self, es, s, which, name="vec"):
        kb = self.kb
        t = kb.sb(es, [128, D], F32, name)
        kb.dma("sp", out=t[:, :], in_=self.MODD[s, which * D:(which + 1) * D].partition_broadcast(128),
               reads=[self.MODD], writes=[t])
        return t

    def norm_tile(self, rstd_tmp, xt, s_bc, sh_bc, out_f32, out_bf, junk, negh):
        kb = self.kb
        ss, rs = rstd_tmp
        kb.op("act", lambda e: e.activation(out=junk[:, :], in_=xt[:, :], func=AF.Square, accum_out=ss[:, 0:1]),
              reads=[xt], writes=[junk, ss])
        kb.op("dve", lambda e: e.tensor_scalar(out=ss[:, 0:1], in0=ss[:, 0:1], scalar1=1.0 / D, scalar2=EPS, op0=ALU.mult, op1=ALU.add),
              reads=[ss], writes=[ss])
        kb.op("pool", lambda e: e.tensor_tensor(out=rs[:, 0:1], in0=ss[:, 0:1], in1=negh[:, 0:1], op=ALU.pow),
              reads=[ss, negh], writes=[rs])
        kb.op("dve", lambda e: e.scalar_tensor_tensor(out=out_f32[:, :], in0=xt[:, :], scalar=rs[:, 0:1], in1=s_bc[:, :],
                                                      op0=ALU.mult, op1=ALU.mult),
              reads=[xt, rs, s_bc], writes=[out_f32])
        kb.op("pool", lambda e: e.tensor_tensor(out=out_f32[:, :], in0=out_f32[:, :], in1=sh_bc[:, :], op=ALU.add),
              reads=[out_f32, sh_bc], writes=[out_f32])
        if out_bf is not None:
            kb.op("act", lambda e: e.copy(out=out_bf[:, :], in_=out_f32[:, :]), reads=[out_f32], writes=[out_bf])

    def ph_router(self, li):
        kb = self.kb
        I = self.I
        with contextlib.ExitStack() as es:
            svec = [self.load_vec(es, s, 4, "s2") for s in range(2)]
            shvec = [self.load_vec(es, s, 3, "sh2") for s in range(2)]
            negh = kb.sb(es, [128, 1], F32, "negh")
            kb.op("dve", lambda e: e.memset(negh[:, :], -0.5), writes=[negh])
            wr = kb.sb(es, [128, 8, NE], F32, "wr")
            kb.dma("sp", out=wr[:, :, :], in_=I["w_router"][li].rearrange("(kc p) n -> p kc n", p=128), writes=[wr])
            AT = kb.sb(es, [NE, L], F32, "AT")
            ATC = kb.sb(es, [NE, LC], F32, "ATC")
            NB = 5
            xt = [kb.sb(es, [128, D], F32, "xt") for _ in range(NB)]
            hf = [kb.sb(es, [128, D], F32, "hf") for _ in range(NB)]
            hb = [kb.sb(es, [128, D], BF16, "hb") for _ in range(NB)]
            junk = kb.sb(es, [128, D], BF16, "junk")
            hT = [kb.sb(es, [128, 8, 128], F32, "hT") for _ in range(NB)]
            sm = [[kb.sb(es, [128, 1], F32, "sm") for _ in range(6)] for _ in range(NB)]
            lg = [kb.sb(es, [128, NE], F32, "lg") for _ in range(NB)]
            af = [kb.sb(es, [128, NE], F32, "af") for _ in range(NB)]
            pT = [kb.ps(es, [128, 512], F32, "pT") for _ in range(4)]
            pl = [kb.ps(es, [128, NE], F32, "pl") for _ in range(2)]
            pa = [kb.ps(es, [NE, 128], F32, "pa") for _ in range(2)]

            def s0(ti):
                b = ti % NB
                r0 = ti * 128
                kb.dma("sp", out=xt[b][:, :], in_=self.X[r0:r0 + 128, :], reads=[self.X], writes=[xt[b]])

            def s1(ti):
                b = ti % NB
                s = 0 if ti < 64 else 1
                r0 = ti * 128
                self.norm_tile((sm[b][0], sm[b][1]), xt[b], svec[s], shvec[s], hf[b], hb[b], junk, negh)
                kb.dma("sp", out=self.H2[r0:r0 + 128, :], in_=hb[b][:, :], reads=[hb[b]], writes=[self.H2])

            def s2(ti):
                b = ti % NB
                for half in range(2):
                    p = pT[(2 * ti + half) % 4]
                    for q in range(4):
                        kc = half * 4 + q
                        kb.op("pe", lambda e, p=p, q=q, kc=kc, b=b: e.transpose(out=p[:, q * 128:(q + 1) * 128],
                                                                              in_=hf[b][:, kc * 128:(kc + 1) * 128],
                                                                              identity=self.ident_f[:, :]),
                              reads=[hf[b], self.ident_f], writes=[p])
                    kb.op("act", lambda e, p=p, half=half, b=b: e.copy(out=hT[b][:, half * 4:half * 4 + 4, :],
                                                                     in_=p[:, :].rearrange("p (q t) -> p q t", q=4)),
                          reads=[p], writes=[hT[b]])

            def s3(ti):
                b = ti % NB
                s = 0 if ti < 64 else 1
                r0 = ti * 128
                pp = pl[ti % 2]
                for kc in range(8):
                    kb.op("pe", lambda e, kc=kc, pp=pp, b=b: e.matmul(pp[:, :], lhsT=hT[b][:, kc, :], rhs=wr[:, kc, :],
                                                                    start=(kc == 0), stop=(kc == 7)),
                          reads=[hT[b], wr], writes=[pp])
                mx, nmx, se, rse = sm[b][2], sm[b][3], sm[b][4], sm[b][5]
                kb.op("dve", lambda e, pp=pp, mx=mx: e.reduce_max(out=mx[:, 0:1], in_=pp[:, :], axis=AX.X), reads=[pp], writes=[mx])
                kb.op("dve", lambda e, mx=mx, nmx=nmx: e.tensor_scalar(out=nmx[:, 0:1], in0=mx[:, 0:1], scalar1=-1.0, scalar2=None, op0=ALU.mult),
                      reads=[mx], writes=[nmx])
                kb.op("act", lambda e, pp=pp, b=b, nmx=nmx, se=se: e.activation(out=lg[b][:, :], in_=pp[:, :], func=AF.Exp, bias=nmx[:, 0:1],
                                                                              accum_out=se[:, 0:1]),
                      reads=[pp, nmx], writes=[lg[b], se])
                kb.op("dve", lambda e, se=se, rse=rse: e.reciprocal(out=rse[:, 0:1], in_=se[:, 0:1]), reads=[se], writes=[rse])
                kb.op("dve", lambda e, b=b, rse=rse: e.tensor_scalar(out=af[b][:, :], in0=lg[b][:, :], scalar1=rse[:, 0:1], scalar2=None, op0=ALU.mult),
                      reads=[lg[b], rse], writes=[af[b]])
                kb.dma("sp", out=self.AFF[r0:r0 + 128, :], in_=af[b][:, :], reads=[af[b]], writes=[self.AFF])
                pq = pa[ti % 2]
                kb.op("pe", lambda e, pq=pq, b=b: e.matmul(pq[:, :], lhsT=af[b][:, :], rhs=self.ident_f[:, :], start=True, stop=True),
                      reads=[af[b], self.ident_f], writes=[pq])
                if s == 0:
                    kb.op("act", lambda e, pq=pq, r0=r0: e.copy(out=AT[:, r0:r0 + 128], in_=pq[:, :]), reads=[pq], writes=[AT])
                else:
                    kb.op("act", lambda e, pq=pq, r0=r0: e.copy(out=ATC[:, r0 - L:r0 - L + 128], in_=pq[:, :]), reads=[pq], writes=[ATC])

            pipeline(NTILE, [s0, s1, s2, s3])
            kb.dma("sp", out=self.AFFT[:, :], in_=AT[:, :], reads=[AT], writes=[self.AFFT])
            kb.dma("sp", out=self.AFFTC[:, :], in_=ATC[:, :], reads=[ATC], writes=[self.AFFTC])

    def select(self, es, A, P, F, K, cap, key_c, base_c, dump_c, gsum, idx_out_dram, tag):
        kb = self.kb
        lo = kb.sb(es, [P, 1], F32, "lo" + tag)
        hi = kb.sb(es, [P, 1], F32, "hi" + tag)
        mid = kb.sb(es, [P, 1], F32, "mid" + tag)
        cnt = kb.sb(es, [P, 1], F32, "cnt" + tag)
        ge = kb.sb(es, [P, 1], F32, "ge" + tag)
        d1 = kb.sb(es, [P, 1], F32, "d1" + tag)
        W = kb.sb(es, [P, F], F32, "W" + tag)
        pc = kb.ps(es, [P, 1], F32, "pc" + tag)
        kb.op("dve", lambda e: e.memset(lo[:, :], 0.0), writes=[lo])
        kb.op("dve", lambda e: e.memset(hi[:, :], 1.0), writes=[hi])
        for it in range(36):
            kb.op("dve", lambda e: e.tensor_tensor(out=mid[:, :], in0=lo[:, :], in1=hi[:, :], op=ALU.add), reads=[lo, hi], writes=[mid])
            kb.op("dve", lambda e: e.tensor_scalar(out=mid[:, :], in0=mid[:, :], scalar1=0.5, scalar2=None, op0=ALU.mult), reads=[mid], writes=[mid])
            kb.op("dve", lambda e: e.tensor_scalar(out=W[:, :], in0=A[:, :], scalar1=mid[:, 0:1], scalar2=0.0, op0=ALU.is_ge, op1=ALU.add,
                                                   accum_out=cnt[:, 0:1]), reads=[A, mid], writes=[W, cnt])
            if gsum is not None:
                kb.op("pe", lambda e: e.matmul(pc[:, :], lhsT=gsum[:, :], rhs=cnt[:, :], start=True, stop=True), reads=[gsum, cnt], writes=[pc])
                src = pc
            else:
                src = cnt
            kb.op("dve", lambda e, src=src: e.tensor_scalar(out=ge[:, :], in0=src[:, :], scalar1=float(K) - 0.5, scalar2=None, op0=ALU.is_ge),
                  reads=[src], writes=[ge])
            kb.op("dve", lambda e: e.tensor_tensor(out=d1[:, :], in0=mid[:, :], in1=lo[:, :], op=ALU.subtract), reads=[mid, lo], writes=[d1])
            kb.op("dve", lambda e: e.scalar_tensor_tensor(out=lo[:, :], in0=d1[:, :], scalar=ge[:, 0:1], in1=lo[:, :], op0=ALU.mult, op1=ALU.add),
                  reads=[d1, ge, lo], writes=[lo])
            kb.op("dve", lambda e: e.tensor_tensor(out=d1[:, :], in0=hi[:, :], in1=mid[:, :], op=ALU.subtract), reads=[hi, mid], writes=[d1])
            kb.op("dve", lambda e: e.scalar_tensor_tensor(out=hi[:, :], in0=d1[:, :], scalar=ge[:, 0:1], in1=mid[:, :], op0=ALU.mult, op1=ALU.add),
                  reads=[d1, ge, mid], writes=[hi])
        kb.op("dve", lambda e: e.scalar_tensor_tensor(out=W[:, :], in0=A[:, :], scalar=lo[:, 0:1], in1=key_c[0:P, :], op0=ALU.is_ge, op1=ALU.mult),
              reads=[A, lo, key_c], writes=[W])
        Lv = kb.sb(es, [P, cap], F32, "Lv" + tag)
        for r in range(cap // 8):
            kb.op("dve", lambda e, r=r: e.max(out=Lv[:, 8 * r:8 * r + 8], in_=W[:, :]), reads=[W], writes=[Lv])
            kb.op("dve", lambda e, r=r: e.match_replace(out=W[:, :], in_to_replace=Lv[:, 8 * r:8 * r + 8], in_values=W[:, :], imm_value=0.0),
                  reads=[W, Lv], writes=[W])
        tok = kb.sb(es, [P, cap], F32, "tok" + tag)
        vm = kb.sb(es, [P, cap], F32, "vm" + tag)
        idx = kb.sb(es, [P, cap], I32, "idx" + tag)
        kb.op("dve", lambda e: e.tensor_scalar(out=tok[:, :], in0=Lv[:, :], scalar1=-1.0, scalar2=base_c[0:P, 0:1], op0=ALU.mult, op1=ALU.add),
              reads=[Lv, base_c], writes=[tok])
        kb.op("dve", lambda e: e.tensor_tensor(out=tok[:, :], in0=tok[:, :], in1=dump_c[0:P, :], op=ALU.subtract), reads=[tok, dump_c], writes=[tok])
        kb.op("dve", lambda e: e.tensor_scalar(out=vm[:, :], in0=Lv[:, :], scalar1=0.5, scalar2=None, op0=ALU.is_ge), reads=[Lv], writes=[vm])
        kb.op("dve", lambda e: e.tensor_tensor(out=tok[:, :], in0=tok[:, :], in1=vm[:, :], op=ALU.mult), reads=[tok, vm], writes=[tok])
        kb.op("dve", lambda e: e.tensor_tensor(out=tok[:, :], in0=tok[:, :], in1=dump_c[0:P, :], op=ALU.add), reads=[tok, dump_c], writes=[tok])
        kb.op("dve", lambda e: e.tensor_copy(out=idx[:, :], in_=tok[:, :]), reads=[tok], writes=[idx])
        kb.dma("sp", out=idx_out_dram[:, :], in_=idx[:, :], reads=[idx], writes=[idx_out_dram])

    def ph_select(self, li):
        kb = self.kb
        I = self.I
        with contextlib.ExitStack() as es:
            cs = {}
            for k in ("gsum", "keyl", "keyc", "basel", "basec", "dumpl", "dumpc"):
                shp, ty = CONST_SPECS[k]
                cs[k] = kb.sb(es, shp, ty, k)
                kb.dma("sp", out=cs[k][:, :], in_=I[k][:, :], writes=[cs[k]])
            A = kb.sb(es, [128, 1024], F32, "Asel")
            kb.dma("sp", out=A[:, :], in_=self.AFFT[:, :].rearrange("e (b t) -> (e b) t", b=8), reads=[self.AFFT], writes=[A])
            self.select(es, A, 128, 1024, 1024, CAPB, cs["keyl"], cs["basel"], cs["dumpl"], cs["gsum"], self.IDXD, "l")
            Ac = kb.sb(es, [NE, LC], F32, "Aselc")
            kb.dma("sp", out=Ac[:, :], in_=self.AFFTC[:, :], reads=[self.AFFTC], writes=[Ac])
            self.select(es, Ac, NE, LC, CAPC, CAPC, cs["keyc"], cs["basec"], cs["dumpc"], None, self.IDXC, "c")

    def ph_moe(self, li):
        kb = self.kb
        I = self.I
        NS = NSLOT + CAPC
        chunks = [(0, 512), (512, 512), (1024, 512), (1536, CAPC)]
        with contextlib.ExitStack() as es:
            ga2 = [self.load_vec(es, s, 5, "ga2") for s in range(2)]
            idxt = kb.sb(es, [128, NE, NCALL], I32, "idxt")
            kb.dma("sp", out=idxt[:, :, :], in_=self.IDXD[:, :].rearrange("(e b) r -> e (b r)", b=8).rearrange("e (j p) -> p e j", p=128),
                   reads=[self.IDXD], writes=[idxt], allow_slow_non_contiguous=True)
            idxc = kb.sb(es, [CAPC, NE], I32, "idxc")
            kb.dma("sp", out=idxc[:, :], in_=self.IDXC[:, :].rearrange("e p -> p e"), reads=[self.IDXC], writes=[idxc],
                   allow_slow_non_contiguous=True)
            stage = [kb.sb(es, [128, 4096], F32, "stage") for _ in range(2)]
            w13 = [[kb.sb(es, [128, 8, 512], BF16, "w13") for _ in range(2)] for _ in range(2)]
            w2 = kb.sb(es, [128, 16, D], BF16, "w2")
            w2k = [Trk() for _ in range(4)]
            xgT = kb.sb(es, [128, 8, NS], BF16, "xgT")
            gT = kb.sb(es, [128, 16, NS], BF16, "gT")
            gTk = [Trk() for _ in range(16)]
            G = [kb.sb(es, [128, D], BF16, "G") for _ in range(2)]
            gate = [kb.sb(es, [128, NE], F32, "gate") for _ in range(3)]
            yb = [kb.sb(es, [128, D], F32, "yb") for _ in range(2)]
            sa = [kb.sb(es, [128, 512], BF16, "sa") for _ in range(2)]
            pA = [kb.ps(es, [128, 512], F32, "pA") for _ in range(2)]
            pB = [kb.ps(es, [128, 512], F32, "pB") for _ in range(2)]
            pT = [kb.ps(es, [128, 1024], BF16, "pTm") for _ in range(2)]
            pY = [kb.ps(es, [128, 512], F32, "pY") for _ in range(2)]
            stage_i = [0]
            cast_i = [0]

            def load_cast(dst_ap, dst_trk, src_ap, shape3):
                st = stage[stage_i[0] % 2]
                stage_i[0] += 1
                a, b = shape3
                sv = st[:, 0:a * b].rearrange("p (a b) -> p a b", a=a)
                kb.dma("sp", out=sv, in_=src_ap, writes=[st])
                eng = "pool" if cast_i[0] % 2 == 0 else "act"
                cast_i[0] += 1
                if eng == "pool":
                    kb.op("pool", lambda e: e.tensor_copy(out=dst_ap, in_=sv), reads=[st], writes=[dst_trk])
                else:
                    kb.op("act", lambda e: e.copy(out=dst_ap, in_=sv), reads=[st], writes=[dst_trk])

            gcount = 0
            for ex in range(NE):
                w1src = I["w_e1"][li, ex].rearrange("(kc p) f -> p kc f", p=128)
                w3src = I["w_e3"][li, ex].rearrange("(kc p) f -> p kc f", p=128)
                w2src = I["w_e2"][li, ex].rearrange("(fc p) d -> p fc d", p=128)
                calls = [(j, 128, idxt[:, ex, j:j + 1], j * 128) for j in range(NCALL)] + [(NCALL, CAPC, idxc[:, ex:ex + 1], NSLOT)]
                gates = []
                for (j, rows, iap, col0) in calls:
                    g = G[gcount % 2]
                    gt = gate[gcount % 3]
                    gcount += 1
                    kb.dma("pool", reads=[self.H2, idxt, idxc], writes=[g],
                           fn=lambda e, g=g, rows=rows, iap=iap: e.indirect_dma_start(out=g[0:rows, :], out_offset=None, in_=self.H2[:, :],
                                                                                      in_offset=bass.IndirectOffsetOnAxis(ap=iap, axis=0)))
                    for kc in range(8):
                        p = pT[(kc // 8) % 2]
                    p = pT[gcount % 2]
                    for kc in range(8):
                        kb.op("pe", lambda e, p=p, g=g, kc=kc, rows=rows: e.transpose(out=p[:, kc * 128:kc * 128 + rows],
                                                                                    in_=g[0:rows, kc * 128:(kc + 1) * 128],
                                                                                    identity=self.ident_bf[0:rows, 0:rows]),
                              reads=[g, self.ident_bf], writes=[p])
                    kb.op("dve", lambda e, p=p, rows=rows, col0=col0: e.tensor_copy(
                        out=xgT[:, :, col0:col0 + rows], in_=p[:, :].rearrange("p (k t) -> p k t", k=8)[:, :, 0:rows]),
                          reads=[p], writes=[xgT])
                for q in range(4):
                    wb = w13[q % 2]
                    load_cast(wb[0][:, :, :], wb[0].k, w1src[:, :, q * 512:(q + 1) * 512], (8, 512))
                    load_cast(wb[1][:, :, :], wb[1].k, w3src[:, :, q * 512:(q + 1) * 512], (8, 512))
                    for f4 in range(4):
                        fc = q * 4 + f4
                        for ci, (c0, cn) in enumerate(chunks):
                            k = (fc * 4 + ci) % 2
                            for kc in range(8):
                                kb.op("pe", lambda e, k=k, kc=kc, f4=f4, c0=c0, cn=cn, wb=wb: e.matmul(
                                    pA[k][:, 0:cn], lhsT=wb[0][:, kc, f4 * 128:(f4 + 1) * 128], rhs=xgT[:, kc, c0:c0 + cn],
                                    start=(kc == 0), stop=(kc == 7)), reads=[wb[0], xgT], writes=[pA[k]])
                            for kc in range(8):
                                kb.op("pe", lambda e, k=k, kc=kc, f4=f4, c0=c0, cn=cn, wb=wb: e.matmul(
                                    pB[k][:, 0:cn], lhsT=wb[1][:, kc, f4 * 128:(f4 + 1) * 128], rhs=xgT[:, kc, c0:c0 + cn],
                                    start=(kc == 0), stop=(kc == 7)), reads=[wb[1], xgT], writes=[pB[k]])
                            kb.op("act", lambda e, k=k, cn=cn: e.activation(out=sa[k][:, 0:cn], in_=pA[k][:, 0:cn], func=AF.Silu),
                                  reads=[pA[k]], writes=[sa[k]])
                            kb.op("dve", lambda e, k=k, cn=cn, c0=c0, fc=fc: e.tensor_tensor(out=gT[:, fc, c0:c0 + cn], in0=sa[k][:, 0:cn],
                                                                                          in1=pB[k][:, 0:cn], op=ALU.mult),
                                  reads=[sa[k], pB[k]], writes=[gTk[fc]])
                for g4 in range(4):
                    load_cast(w2[:, g4 * 4:(g4 + 1) * 4, :], w2k[g4], w2src[:, g4 * 4:(g4 + 1) * 4, :], (4, D))
                for (j, rows, iap, col0) in calls:
                    s = 0 if j < NCALL else 1
                    gt = gate[j % 3]
                    kb.dma("pool", reads=[self.AFF, idxt, idxc], writes=[gt],
                           fn=lambda e, gt=gt, rows=rows, iap=iap: e.indirect_dma_start(out=gt[0:rows, :], out_offset=None, in_=self.AFF[:, :],
                                                                                        in_offset=bass.IndirectOffsetOnAxis(ap=iap, axis=0)))
                    y = yb[j % 2]
                    for h in range(2):
                        p = pY[h]
                        for fc in range(16):
                            kb.op("pe", lambda e, p=p, fc=fc, col0=col0, rows=rows, h=h: e.matmul(
                                p[0:rows, :], lhsT=gT[:, fc, col0:col0 + rows], rhs=w2[:, fc, h * 512:(h + 1) * 512],
                                start=(fc == 0), stop=(fc == 15)), reads=[gTk[fc], w2k[fc // 4]], writes=[p])
                        kb.op("dve", lambda e, p=p, y=y, h=h, rows=rows, gt=gt, s=s, ex=ex: e.scalar_tensor_tensor(
                            out=y[0:rows, h * 512:(h + 1) * 512], in0=p[0:rows, :], scalar=gt[0:rows, ex:ex + 1],
                            in1=ga2[s][0:rows, h * 512:(h + 1) * 512], op0=ALU.mult, op1=ALU.mult),
                              reads=[p, gt, ga2[s]], writes=[y])
                    kb.dma("pool", reads=[y, idxt, idxc], writes=[self.X],
                           fn=lambda e, y=y, rows=rows, iap=iap: e.indirect_dma_start(out=self.X[:, :],
                                                                                      out_offset=bass.IndirectOffsetOnAxis(ap=iap, axis=0),
                                                                                      in_=y[0:rows, :], in_offset=None, compute_op=ALU.add))


    def load_w_bf16(self, es, dst, src3, ncols, stage):
        kb = self.kb
        i = 0
        for c0 in range(0, ncols, 512):
            cn = min(512, ncols - c0)
            st = stage[i % 2]
            sv = st[:, 0:8 * cn].rearrange("p (a b) -> p a b", a=8)
            kb.dma("sp", out=sv, in_=src3[:, :, c0:c0 + cn], writes=[st])
            if i % 2 == 0:
                kb.op("pool", lambda e, sv=sv, c0=c0, cn=cn: e.tensor_copy(out=dst[:, :, c0:c0 + cn], in_=sv), reads=[st], writes=[dst])
            else:
                kb.op("act", lambda e, sv=sv, c0=c0, cn=cn: e.copy(out=dst[:, :, c0:c0 + cn], in_=sv), reads=[st], writes=[dst])
            i += 1

    def transpose_tile(self, hb, hT, pT):
        kb = self.kb
        for kc in range(8):
            kb.op("pe", lambda e, kc=kc: e.transpose(out=pT[:, kc * 128:(kc + 1) * 128], in_=hb[:, kc * 128:(kc + 1) * 128],
                                                     identity=self.ident_bf[:, :]), reads=[hb, self.ident_bf], writes=[pT])
        kb.op("act", lambda e: e.copy(out=hT[:, :, :], in_=pT[:, :].rearrange("p (k t) -> p k t", k=8)), reads=[pT], writes=[hT])

    def ntiles(self, need_ctx):
        return NTILE if need_ctx else 64

    def ph_qkv(self, li):
        kb = self.kb
        I = self.I
        j = li // 2
        if not hasattr(self, "QKT"):
            self.QKT = kb.dram("QKT", [16, 128, NT], BF16)
            self.VD = kb.dram("VD", [NT + 64, 16 * 65], BF16)
            self.OD = kb.dram("OD", [NT, D], BF16)
        with contextlib.ExitStack() as es:
            svec = [self.load_vec(es, s, 1, "s1") for s in range(2)]
            shvec = [self.load_vec(es, s, 0, "sh1") for s in range(2)]
            negh = kb.sb(es, [128, 1], F32, "negh")
            kb.op("dve", lambda e: e.memset(negh[:, :], -0.5), writes=[negh])
            stage = [kb.sb(es, [128, 4096], F32, "stage") for _ in range(2)]
            W = kb.sb(es, [128, 8, 3 * D], BF16, "wqkv")
            self.load_w_bf16(es, W, I["w_qkv"][j].rearrange("(kc p) n -> p kc n", p=128), 3 * D, stage)
            NB = 5
            xt = [kb.sb(es, [128, D], F32, "xt") for _ in range(NB)]
            hf = [kb.sb(es, [128, D], F32, "hf") for _ in range(NB)]
            hb = [kb.sb(es, [128, D], BF16, "hb") for _ in range(NB)]
            junk = kb.sb(es, [128, D], BF16, "junk")
            hT = [kb.sb(es, [128, 8, 128], BF16, "hT") for _ in range(NB)]
            sm = [[kb.sb(es, [128, 1], F32, "sm") for _ in range(2)] for _ in range(NB)]
            qk = [kb.sb(es, [128, 16, 128], BF16, "qk") for _ in range(2)]
            vt = [kb.sb(es, [128, 16, 65], BF16, "vt") for _ in range(2)]
            for b in range(2):
                kb.op("dve", lambda e, b=b: e.memset(vt[b][:, :, :], 1.0), writes=[vt[b]])
            pT = [kb.ps(es, [128, 1024], BF16, "pT") for _ in range(2)]
            pq = [kb.ps(es, [128, 512], F32, "pq") for _ in range(6)]

            def s0(ti):
                b = ti % NB
                r0 = ti * 128
                kb.dma("sp", out=xt[b][:, :], in_=self.X[r0:r0 + 128, :], reads=[self.X], writes=[xt[b]])

            def s1(ti):
                b = ti % NB
                s = 0 if ti < 64 else 1
                self.norm_tile((sm[b][0], sm[b][1]), xt[b], svec[s], shvec[s], hf[b], hb[b], junk, negh)

            def s2(ti):
                b = ti % NB
                self.transpose_tile(hb[b], hT[b], pT[ti % 2])

            def s3(ti):
                b = ti % NB
                b2 = ti % 2
                r0 = ti * 128
                for c4 in range(4):
                    p = pq[(6 * ti + c4) % 6]
                    for cc in range(4):
                        c = c4 * 4 + cc
                        for kc in range(8):
                            kb.op("pe", lambda e, p=p, cc=cc, c=c, kc=kc, b=b: e.matmul(p[:, cc * 128:(cc + 1) * 128], lhsT=W[:, kc, c * 128:(c + 1) * 128],
                                                                                     rhs=hT[b][:, kc, :], start=(kc == 0), stop=(kc == 7)),
                                  reads=[W, hT[b]], writes=[p])
                    sc = 0.125 if c4 < 2 else 1.0
                    kb.op("act", lambda e, p=p, c4=c4, b2=b2, sc=sc: e.activation(out=qk[b2][:, c4 * 4:c4 * 4 + 4, :], in_=p[:, :].rearrange("p (c t) -> p c t", c=4),
                                                                               func=AF.Copy, scale=sc), reads=[p], writes=[qk[b2]])
                kb.dma("sp", out=self.QKT[:, :, r0:r0 + 128].rearrange("c p t -> p c t"), in_=qk[b2][:, :, :], reads=[qk[b2]], writes=[self.QKT])
                for h2 in range(2):
                    p = pq[(6 * ti + 4 + h2) % 6]
                    for kc in range(8):
                        kb.op("pe", lambda e, p=p, kc=kc, h2=h2, b=b: e.matmul(p[:, :], lhsT=hT[b][:, kc, :], rhs=W[:, kc, 2 * D + h2 * 512:2 * D + (h2 + 1) * 512],
                                                                            start=(kc == 0), stop=(kc == 7)), reads=[W, hT[b]], writes=[p])
                    kb.op("dve", lambda e, p=p, h2=h2, b2=b2: e.tensor_copy(out=vt[b2][:, h2 * 8:(h2 + 1) * 8, 0:64], in_=p[:, :].rearrange("p (h d) -> p h d", h=8)),
                          reads=[p], writes=[vt[b2]])
                kb.dma("sp", out=self.VD[r0:r0 + 128, :], in_=vt[b2][:, :, :].rearrange("p h d -> p (h d)"), reads=[vt[b2]], writes=[self.VD])

            pipeline(NTILE, [s0, s1, s2, s3])

    def ph_attn(self, li, need_ctx):
        kb = self.kb
        I = self.I
        j = li // 2
        with contextlib.ExitStack() as es:
            KT = kb.sb(es, [128, NT], BF16, "KT")
            QT = kb.sb(es, [128, NT], BF16, "QT")
            V0 = kb.sb(es, [128, NTILE, 130], BF16, "V0")
            bst = [kb.sb(es, [128, 2, 5, 128], F32, "bst") for _ in range(2)]
            EB = [kb.sb(es, [128, 2, 5, 128], BF16, "EB") for _ in range(5)]
            NP = 4
            PT = [kb.sb(es, [128, 7, 128], BF16, "PT") for _ in range(NP)]
            osb = [kb.sb(es, [128, 128], BF16, "osb") for _ in range(3)]
            rec = [kb.sb(es, [128, 1], F32, "rec") for _ in range(4)]
            pSa = [kb.ps(es, [128, 512], F32, "pSa") for _ in range(2)]
            pSb = [kb.ps(es, [128, 512], F32, "pSb") for _ in range(2)]
            pO = [kb.ps(es, [128, 65], F32, "pO") for _ in range(3)]
            VDv = self.VD[0:NT, :].rearrange("(t p) c -> p t c", p=128)
            for hp in range(8):
                kb.dma("sp", out=QT[:, :], in_=self.QKT[hp, :, :], reads=[self.QKT], writes=[QT])
                kb.dma("sp", out=KT[:, :], in_=self.QKT[8 + hp, :, :], reads=[self.QKT], writes=[KT])
                kb.dma("sp", out=V0[:, :, :], in_=VDv[:, :, hp * 130:(hp + 1) * 130], reads=[self.VD], writes=[V0])
                for v in range(5):
                    st = bst[v % 2]
                    kb.dma("sp", out=st[:, :, :, :], in_=I["biasT"][j, v, 2 * hp:2 * hp + 2].rearrange("h p c q -> p h c q"), writes=[st])
                    kb.op("act", lambda e, st=st, v=v: e.activation(out=EB[v][:, :, :, :], in_=st[:, :, :, :], func=AF.Exp), reads=[st], writes=[EB[v]])
                units = []
                for rp in range(64):
                    for hl in range(2):
                        units.append((rp, hl))
                if need_ctx:
                    for cq in range(2):
                        for hl in range(2):
                            units.append((64 + cq, hl))

                def info(u):
                    rp, hl = units[u]
                    if rp < 64:
                        t0 = att_base(rp) // 2
                        tiles = [t0 + c for c in range(5)] + [64, 65]
                        q0 = 128 * rp
                        eb = EB[att_variant(rp)]
                    else:
                        tiles = [64, 65]
                        q0 = L + 128 * (rp - 64)
                        eb = None
                    return rp, hl, tiles, q0, eb

                def sA(u):
                    rp, hl, tiles, q0, eb = info(u)
                    ho = hl * 64
                    pa_, pb_ = pSa[u % 2], pSb[u % 2]
                    for c, t in enumerate(tiles):
                        dst = pa_[:, c * 128:(c + 1) * 128] if c < 4 else pb_[:, (c - 4) * 128:(c - 3) * 128]
                        trk = pa_ if c < 4 else pb_
                        kb.op("pe", lambda e, dst=dst, ho=ho, t=t, q0=q0: e.matmul(dst, lhsT=KT[ho:ho + 64, t * 128:(t + 1) * 128],
                                                                                rhs=QT[ho:ho + 64, q0:q0 + 128], start=True, stop=True),
                              reads=[KT, QT], writes=[trk])

                def sB(u):
                    rp, hl, tiles, q0, eb = info(u)
                    pa_, pb_ = pSa[u % 2], pSb[u % 2]
                    pt = PT[u % NP]
                    n = len(tiles)
                    na = min(n, 4)
                    kb.op("act", lambda e, pt=pt, pa_=pa_, na=na: e.activation(out=pt[:, 0:na, :].rearrange("p c q -> p (c q)"), in_=pa_[:, 0:na * 128], func=AF.Exp),
                          reads=[pa_], writes=[pt])
                    if n > 4:
                        kb.op("act", lambda e, pt=pt, pb_=pb_, n=n: e.activation(out=pt[:, 4:n, :].rearrange("p c q -> p (c q)"), in_=pb_[:, 0:(n - 4) * 128], func=AF.Exp),
                              reads=[pb_], writes=[pt])
                    if eb is not None:
                        me = "pool" if (u % 5) < 3 else "dve"
                        kb.op(me, lambda e, pt=pt, eb=eb, hl=hl: e.tensor_tensor(out=pt[:, 0:5, :], in0=pt[:, 0:5, :], in1=eb[:, hl, :, :], op=ALU.mult),
                              reads=[pt, eb], writes=[pt])

                def sC(u):
                    rp, hl, tiles, q0, eb = info(u)
                    pt = PT[u % NP]
                    po = pO[u % 3]
                    ob = osb[(u // 2) % 3]
                    n = len(tiles)
                    for c, t in enumerate(tiles):
                        kb.op("pe", lambda e, po=po, c=c, t=t, pt=pt, hl=hl, n=n: e.matmul(po[:, :], lhsT=pt[:, c, :], rhs=V0[:, t, hl * 65:(hl + 1) * 65],
                                                                                        start=(c == 0), stop=(c == n - 1)), reads=[pt, V0], writes=[po])
                    rc = rec[u % 4]
                    kb.op("dve", lambda e, po=po, rc=rc: e.reciprocal(out=rc[:, 0:1], in_=po[:, 64:65]), reads=[po], writes=[rc])
                    kb.op("dve", lambda e, po=po, rc=rc, hl=hl, ob=ob: e.tensor_scalar(out=ob[:, hl * 64:(hl + 1) * 64], in0=po[:, 0:64],
                                                                                      scalar1=rc[:, 0:1], scalar2=None, op0=ALU.mult),
                          reads=[po, rc], writes=[ob])
                    if hl == 1:
                        kb.dma("sp", out=self.OD[q0:q0 + 128, hp * 128:(hp + 1) * 128], in_=ob[:, :], reads=[ob], writes=[self.OD])

                pipeline(len(units), [sA, sB, sC])

    def ph_oproj(self, li, wname, srcname, need_ctx):
        kb = self.kb
        I = self.I
        j = li // 2
        SRC = getattr(self, srcname)
        with contextlib.ExitStack() as es:
            ga = [self.load_vec(es, s, 2, "ga1") for s in range(2)]
            stage = [kb.sb(es, [128, 4096], F32, "stage") for _ in range(2)]
            W = kb.sb(es, [128, 8, D], BF16, "wo")
            self.load_w_bf16(es, W, I[wname][j].rearrange("(kc p) n -> p kc n", p=128), D, stage)
            NB = 5
            xt = [kb.sb(es, [128, D], F32, "xt") for _ in range(NB)]
            hb = [kb.sb(es, [128, D], BF16, "hb") for _ in range(NB)]
            tmp = [kb.sb(es, [128, D], F32, "tmp") for _ in range(2)]
            hT = [kb.sb(es, [128, 8, 128], BF16, "hT") for _ in range(NB)]
            pT = [kb.ps(es, [128, 1024], BF16, "pT") for _ in range(2)]
            pq = [kb.ps(es, [128, 512], F32, "pq") for _ in range(4)]

            def s0(ti):
                b = ti % NB
                r0 = ti * 128
                kb.dma("sp", out=xt[b][:, :], in_=self.X[r0:r0 + 128, :], reads=[self.X], writes=[xt[b]])
                kb.dma("sp", out=hb[b][:, :], in_=SRC[r0:r0 + 128, :], reads=[SRC], writes=[hb[b]])

            def s1(ti):
                b = ti % NB
                self.transpose_tile(hb[b], hT[b], pT[ti % 2])

            def s2(ti):
                b = ti % NB
                b2 = ti % 2
                s = 0 if ti < 64 else 1
                r0 = ti * 128
                for h2 in range(2):
                    p = pq[(2 * ti + h2) % 4]
                    for kc in range(8):
                        kb.op("pe", lambda e, p=p, kc=kc, h2=h2, b=b: e.matmul(p[:, :], lhsT=hT[b][:, kc, :], rhs=W[:, kc, h2 * 512:(h2 + 1) * 512],
                                                                            start=(kc == 0), stop=(kc == 7)), reads=[W, hT[b]], writes=[p])
                    kb.op("dve", lambda e, p=p, h2=h2, b2=b2, s=s: e.tensor_tensor(out=tmp[b2][:, h2 * 512:(h2 + 1) * 512], in0=p[:, :],
                                                                                in1=ga[s][:, h2 * 512:(h2 + 1) * 512], op=ALU.mult),
                          reads=[p, ga[s]], writes=[tmp[b2]])
                kb.op("pool", lambda e, b=b, b2=b2: e.tensor_tensor(out=xt[b][:, :], in0=xt[b][:, :], in1=tmp[b2][:, :], op=ALU.add),
                      reads=[xt[b], tmp[b2]], writes=[xt[b]])
                kb.dma("sp", out=self.X[r0:r0 + 128, :], in_=xt[b][:, :], reads=[xt[b]], writes=[self.X])

            pipeline(self.ntiles(need_ctx), [s0, s1, s2])

    def cload(self, es, name):
        shp, ty = CONST_SPECS[name]
        t = self.kb.sb(es, shp, ty, name)
        sl = tuple(slice(None) for _ in shp)
        self.kb.dma("sp", out=t[sl], in_=self.I[name][sl], writes=[t])
        return t

    def even_scratch(self):
        kb = self.kb
        if not hasattr(self, "PR"):
            self.PR = kb.dram("PR", [NT, 1536], BF16)
            self.DTR = kb.dram("DTR", [NT, 16], F32)
            self.AD = kb.dram("AD", [NT, 1024], BF16)
            self.XBC = kb.dram("XBC", [NT, 1024], BF16)
            self.DTA = kb.dram("DTA", [NT, 32], F32)
            self.YF = kb.dram("YF", [NT, 512], F32)
            self.MIX = kb.dram("MIX", [NT, 1024], BF16)
            self.GD = kb.dram("GD", [2, 64, 128, 512], BF16)

    def ph_inproj(self, li):
        kb = self.kb
        I = self.I
        j = li // 2
        self.even_scratch()
        with contextlib.ExitStack() as es:
            svec = [self.load_vec(es, s, 1, "s1") for s in range(2)]
            shvec = [self.load_vec(es, s, 0, "sh1") for s in range(2)]
            negh = kb.sb(es, [128, 1], F32, "negh")
            kb.op("dve", lambda e: e.memset(negh[:, :], -0.5), writes=[negh])
            stage = [kb.sb(es, [128, 4096], F32, "stage") for _ in range(2)]
            W = kb.sb(es, [128, 8, 2064], BF16, "win")
            self.load_w_bf16(es, W, I["w_in_e"][j].rearrange("(kc p) n -> p kc n", p=128), 2064, stage)
            CS = self.cload(es, "CS")
            NB = 5
            xt = [kb.sb(es, [128, D], F32, "xt") for _ in range(NB)]
            hf = [kb.sb(es, [128, D], F32, "hf") for _ in range(NB)]
            hb = [kb.sb(es, [128, D], BF16, "hb") for _ in range(NB)]
            junk = kb.sb(es, [128, D], BF16, "junk")
            hT = [kb.sb(es, [128, 8, 128], BF16, "hT") for _ in range(NB)]
            sm = [[kb.sb(es, [128, 1], F32, "sm") for _ in range(2)] for _ in range(NB)]
            prt = [kb.sb(es, [128, 1536], BF16, "prt") for _ in range(2)]
            dtr = [kb.sb(es, [128, 16], F32, "dtr") for _ in range(2)]
            uT = [kb.sb(es, [128, 4, 128], BF16, "uT") for _ in range(2)]
            At = [kb.sb(es, [128, 2, 4, 128], BF16, "At") for _ in range(2)]
            pT = [kb.ps(es, [128, 1024], BF16, "pT") for _ in range(2)]
            pq = [kb.ps(es, [128, 512], F32, "pq") for _ in range(4)]
            pa = [kb.ps(es, [128, 512], F32, "pa") for _ in range(2)]

            def s0(ti):
                b = ti % NB
                r0 = ti * 128
                kb.dma("sp", out=xt[b][:, :], in_=self.X[r0:r0 + 128, :], reads=[self.X], writes=[xt[b]])

            def s1(ti):
                b = ti % NB
                s = 0 if ti < 64 else 1
                self.norm_tile((sm[b][0], sm[b][1]), xt[b], svec[s], shvec[s], hf[b], hb[b], junk, negh)

            def s2(ti):
                b = ti % NB
                self.transpose_tile(hb[b], hT[b], pT[ti % 2])

            def s3(ti):
                b = ti % NB
                b2 = ti % 2
                r0 = ti * 128
                for ci, (c0, cn) in enumerate([(0, 512), (512, 512), (1024, 512), (1536, 16)]):
                    p = pq[(5 * ti + ci) % 4]
                    for kc in range(8):
                        kb.op("pe", lambda e, p=p, kc=kc, c0=c0, cn=cn, b=b: e.matmul(p[:, 0:cn], lhsT=hT[b][:, kc, :], rhs=W[:, kc, c0:c0 + cn],
                                                                                   start=(kc == 0), stop=(kc == 7)), reads=[W, hT[b]], writes=[p])
                    if ci < 3:
                        if ci % 2 == 0:
                            kb.op("act", lambda e, p=p, c0=c0, b2=b2: e.copy(out=prt[b2][:, c0:c0 + 512], in_=p[:, :]), reads=[p], writes=[prt[b2]])
                        else:
                            kb.op("dve", lambda e, p=p, c0=c0, b2=b2: e.tensor_copy(out=prt[b2][:, c0:c0 + 512], in_=p[:, :]), reads=[p], writes=[prt[b2]])
                    else:
                        kb.op("dve", lambda e, p=p, b2=b2: e.tensor_copy(out=dtr[b2][:, :], in_=p[:, 0:16]), reads=[p], writes=[dtr[b2]])
                kb.dma("sp", out=self.PR[r0:r0 + 128, :], in_=prt[b2][:, :], reads=[prt[b2]], writes=[self.PR])
                kb.dma("sp", out=self.DTR[r0:r0 + 128, :], in_=dtr[b2][:, :], reads=[dtr[b2]], writes=[self.DTR])
                p = pq[(5 * ti + 4) % 4]
                for g in range(4):
                    for kc in range(8):
                        kb.op("pe", lambda e, p=p, kc=kc, g=g, b=b: e.matmul(p[:, g * 128:(g + 1) * 128], lhsT=W[:, kc, 1552 + g * 128:1552 + (g + 1) * 128],
                                                                          rhs=hT[b][:, kc, :], start=(kc == 0), stop=(kc == 7)), reads=[W, hT[b]], writes=[p])
                kb.op("act", lambda e, p=p, b2=b2: e.copy(out=uT[b2][:, :, :], in_=p[:, :].rearrange("p (g t) -> p g t", g=4)), reads=[p], writes=[uT[b2]])
                for gh in range(2):
                    for g2 in range(2):
                        g = gh * 2 + g2
                        kb.op("pe", lambda e, gh=gh, g2=g2, g=g, b2=b2: e.matmul(pa[gh][:, g2 * 256:(g2 + 1) * 256], lhsT=uT[b2][:, g, :], rhs=CS[:, :],
                                                                              start=True, stop=True), reads=[uT[b2], CS], writes=[pa[gh]])
                    kb.op("dve", lambda e, gh=gh, b2=b2: e.tensor_copy(out=At[b2][:, :, 2 * gh:2 * gh + 2, :],
                                                                     in_=pa[gh][:, :].rearrange("p (g cs q) -> p cs g q", g=2, cs=2)),
                          reads=[pa[gh]], writes=[At[b2]])
                kb.dma("sp", out=self.AD[r0:r0 + 128, :], in_=At[b2][:, :, :, :].rearrange("p cs g q -> p (cs g q)"), reads=[At[b2]], writes=[self.AD])

            pipeline(NTILE, [s0, s1, s2, s3])

    def ph_conv(self, li):
        kb = self.kb
        I = self.I
        j = li // 2
        with contextlib.ExitStack() as es:
            cw = [kb.sb(es, [128, D], F32, "cw") for _ in range(3)]
            for k in range(3):
                kb.dma("sp", out=cw[k][:, :], in_=I["conv_w"][j, k, :].partition_broadcast(128), writes=[cw[k]])
            cb = kb.sb(es, [128, D], F32, "cb")
            kb.dma("sp", out=cb[:, :], in_=I["conv_b"][j, :].partition_broadcast(128), writes=[cb])
            dtb = kb.sb(es, [128, 16], F32, "dtb")
            kb.dma("sp", out=dtb[:, :], in_=I["dt_bias"][j, :].partition_broadcast(128), writes=[dtb])
            abc = kb.sb(es, [128, 16], F32, "abc")
            kb.dma("sp", out=abc[:, :], in_=I["a_log"][j, :].partition_broadcast(128), writes=[abc])
            kb.op("act", lambda e: e.activation(out=abc[:, :], in_=abc[:, :], func=AF.Exp), reads=[abc], writes=[abc])
            kb.op("dve", lambda e: e.tensor_scalar(out=abc[:, :], in0=abc[:, :], scalar1=-1.0, scalar2=None, op0=ALU.mult), reads=[abc], writes=[abc])
            NB = 4
            m1 = [kb.sb(es, [128, D], BF16, "m1") for _ in range(NB)]
            c0 = [kb.sb(es, [128, D], BF16, "c0") for _ in range(NB)]
            p1 = [kb.sb(es, [128, D], BF16, "p1") for _ in range(NB)]
            acc = [kb.sb(es, [128, D], F32, "acc") for _ in range(NB)]
            t2 = [kb.sb(es, [128, D], F32, "t2") for _ in range(NB)]
            t3 = [kb.sb(es, [128, D], F32, "t3") for _ in range(NB)]
            xo = [kb.sb(es, [128, D], BF16, "xo") for _ in range(NB)]
            dtr = [kb.sb(es, [128, 16], F32, "dtr") for _ in range(NB)]
            dta = [kb.sb(es, [128, 32], F32, "dta") for _ in range(NB)]
            src = self.PR

            def s0(ti):
                b = ti % NB
                r0 = ti * 128
                first = ti in (0, 64)
                last = ti in (63, 65)
                if first:
                    kb.op("dve", lambda e, b=b: e.memset(m1[b][:, :], 0.0), writes=[m1[b]])
                    kb.dma("sp", out=m1[b][1:128, :], in_=src[r0:r0 + 127, 512:1536], reads=[src], writes=[m1[b]])
                else:
                    kb.dma("sp", out=m1[b][:, :], in_=src[r0 - 1:r0 + 127, 512:1536], reads=[src], writes=[m1[b]])
                kb.dma("sp", out=c0[b][:, :], in_=src[r0:r0 + 128, 512:1536], reads=[src], writes=[c0[b]])
                if last:
                    kb.op("dve", lambda e, b=b: e.memset(p1[b][:, :], 0.0), writes=[p1[b]])
                    kb.dma("sp", out=p1[b][0:127, :], in_=src[r0 + 1:r0 + 128, 512:1536], reads=[src], writes=[p1[b]])
                else:
                    kb.dma("sp", out=p1[b][:, :], in_=src[r0 + 1:r0 + 129, 512:1536], reads=[src], writes=[p1[b]])
                kb.dma("sp", out=dtr[b][:, :], in_=self.DTR[r0:r0 + 128, :], reads=[self.DTR], writes=[dtr[b]])

            def s1(ti):
                b = ti % NB
                kb.op("dve", lambda e, b=b: e.tensor_tensor(out=acc[b][:, :], in0=m1[b][:, :], in1=cw[0][:, :], op=ALU.mult), reads=[m1[b], cw[0]], writes=[acc[b]])
                kb.op("pool", lambda e, b=b: e.tensor_tensor(out=t2[b][:, :], in0=c0[b][:, :], in1=cw[1][:, :], op=ALU.mult), reads=[c0[b], cw[1]], writes=[t2[b]])
                kb.op("pool", lambda e, b=b: e.tensor_tensor(out=t3[b][:, :], in0=p1[b][:, :], in1=cw[2][:, :], op=ALU.mult), reads=[p1[b], cw[2]], writes=[t3[b]])
                kb.op("dve", lambda e, b=b: e.tensor_tensor(out=dtr[b][:, :], in0=dtr[b][:, :], in1=dtb[:, :], op=ALU.add), reads=[dtr[b], dtb], writes=[dtr[b]])

            def s2(ti):
                b = ti % NB
                kb.op("dve", lambda e, b=b: e.tensor_tensor(out=acc[b][:, :], in0=acc[b][:, :], in1=t2[b][:, :], op=ALU.add), reads=[acc[b], t2[b]], writes=[acc[b]])
                kb.op("pool", lambda e, b=b: e.tensor_tensor(out=t3[b][:, :], in0=t3[b][:, :], in1=cb[:, :], op=ALU.add), reads=[t3[b], cb], writes=[t3[b]])
                kb.op("act", lambda e, b=b: e.activation(out=dtr[b][:, :], in_=dtr[b][:, :], func=AF.Exp), reads=[dtr[b]], writes=[dtr[b]])

            def s3(ti):
                b = ti % NB
                kb.op("dve", lambda e, b=b: e.tensor_tensor(out=acc[b][:, :], in0=acc[b][:, :], in1=t3[b][:, :], op=ALU.add), reads=[acc[b], t3[b]], writes=[acc[b]])
                kb.op("act", lambda e, b=b: e.activation(out=dta[b][:, 0:16], in_=dtr[b][:, :], func=AF.Ln, bias=1.0), reads=[dtr[b]], writes=[dta[b]])

            def s4(ti):
                b = ti % NB
                r0 = ti * 128
                kb.op("act", lambda e, b=b: e.activation(out=xo[b][:, :], in_=acc[b][:, :], func=AF.Silu), reads=[acc[b]], writes=[xo[b]])
                kb.dma("sp", out=self.XBC[r0:r0 + 128, :], in_=xo[b][:, :], reads=[xo[b]], writes=[self.XBC])
                kb.op("dve", lambda e, b=b: e.tensor_tensor(out=dta[b][:, 16:32], in0=dta[b][:, 0:16], in1=abc[:, :], op=ALU.mult), reads=[dta[b], abc], writes=[dta[b]])
                kb.dma("sp", out=self.DTA[r0:r0 + 128, :], in_=dta[b][:, :], reads=[dta[b]], writes=[self.DTA])

            pipeline(NTILE, [s0, s1, s2, s3, s4])

    def ph_ssd(self, li, d):
        kb = self.kb
        I = self.I
        j = li // 2
        with contextlib.ExitStack() as es:
            tri = self.cload(es, "triU" if d == 0 else "triL")
            ones = self.cload(es, "onesf")
            neg = self.cload(es, "negU" if d == 0 else "negL")
            dsk = kb.sb(es, [128, 8], F32, "dsk")
            kb.dma("sp", out=dsk[:, :], in_=I["d_skip"][j, :].partition_broadcast(128), writes=[dsk])
            gss = kb.sb(es, [128, 512], F32, "gss")
            kb.dma("sp", out=gss[:, :], in_=I["g_ssd"][j, :].partition_broadcast(128), writes=[gss])
            negh = kb.sb(es, [128, 1], F32, "negh")
            kb.op("dve", lambda e: e.memset(negh[:, :], -0.5), writes=[negh])
            hs = kb.sb(es, [128, 8, 64], F32, "hs")
            hsb = kb.sb(es, [128, 8, 64], BF16, "hsb")
            kb.op("dve", lambda e: e.memset(hs[:, :, :], 0.0), writes=[hs])
            kb.op("dve", lambda e: e.memset(hsb[:, :, :], 0.0), writes=[hsb])
            NB = 4
            R = lambda shape, ty, nm, n=NB: [kb.sb(es, shape, ty, nm) for _ in range(n)]
            xbc = R([128, D], BF16, "xbc")
            dta = R([128, 32], F32, "dta")
            BCT = R([128, 4, 128], BF16, "BCT")
            cum = R([128, 16], F32, "cum")
            te = R([128, 8], F32, "te")
            cd = R([128, 8], F32, "cd")
            xdt = R([128, 8, 64], BF16, "xdt")
            xw = R([128, 8, 64], BF16, "xw")
            CBT = R([128, 2, 128], BF16, "CBT", 2)
            dU = R([128, 8, 128], F32, "dU", 2)
            Ebc = R([128, 8, 128], BF16, "Ebc", 2)
            CsT = R([128, 8, 128], BF16, "CsT")
            Sg = R([128, 8, 128], F32, "Sg", 2)
            Dm = R([128, 8, 128], BF16, "Dm", 2)
            MT = R([128, 8, 128], BF16, "MT")
            tmp = R([128, 8, 64], F32, "tmp", 2)
            yo = R([128, 512], F32, "yo")
            yf = R([128, 512], F32, "yf")
            zt = R([128, 512], BF16, "zt")
            sz = R([128, 512], F32, "sz", 2)
            junk = kb.sb(es, [128, 256], BF16, "junk")
            ssq = R([128, 2], F32, "ssq")
            rsq = R([128, 2], F32, "rsq")
            mo = R([128, 512], BF16, "mo", 2)
            pT = kb.ps(es, [128, 1024], BF16, "pT")
            pc = kb.ps(es, [128, 16], F32, "pc")
            pcb = kb.ps(es, [128, 256], F32, "pcb")
            pA = [kb.ps(es, [128, 512], F32, "pA") for _ in range(2)]
            py = [kb.ps(es, [128, 512], F32, "py") for _ in range(2)]
            pst = kb.ps(es, [128, 512], F32, "pst")
            order = ([64, 65] + list(range(64))) if d == 0 else ([65, 64] + list(range(63, -1, -1)))

            def views(n):
                b = n % NB
                X_, DT_ = xbc[b], dta[b]
                return (b, n % 2, order[n] * 128, X_, DT_, DT_[:, 8 * d:8 * d + 8], DT_[:, 16 + 8 * d:16 + 8 * d + 8],
                        X_[:, 0:512].rearrange("p (h c) -> p h c", h=8))

            def s0(n):
                b, b2, r0, X_, DT_, dt_d, da_d, x3 = views(n)
                kb.dma("sp", out=X_[:, :], in_=self.XBC[r0:r0 + 128, :], reads=[self.XBC], writes=[X_])
                kb.dma("sp", out=DT_[:, :], in_=self.DTA[r0:r0 + 128, :], reads=[self.DTA], writes=[DT_])
                if d == 1:
                    kb.dma("sp", out=yf[b][:, :], in_=self.YF[r0:r0 + 128, :], reads=[self.YF], writes=[yf[b]])
                    kb.dma("sp", out=zt[b][:, :], in_=self.PR[r0:r0 + 128, 0:512], reads=[self.PR], writes=[zt[b]])

            def s1(n):
                b, b2, r0, X_, DT_, dt_d, da_d, x3 = views(n)
                for q in range(4):
                    kb.op("pe", lambda e, q=q, X_=X_: e.transpose(out=pT[:, q * 128:(q + 1) * 128], in_=X_[:, 512 + q * 128:512 + (q + 1) * 128],
                                                               identity=self.ident_bf[:, :]), reads=[X_, self.ident_bf], writes=[pT])
                kb.op("act", lambda e, b=b: e.copy(out=BCT[b][:, :, :], in_=pT[:, 0:512].rearrange("p (q t) -> p q t", q=4)), reads=[pT], writes=[BCT[b]])
                kb.op("pe", lambda e, da_d=da_d: e.matmul(pc[:, 0:8], lhsT=tri[:, :], rhs=da_d, start=True, stop=True), reads=[tri, DT_], writes=[pc])
                kb.op("pe", lambda e, da_d=da_d: e.matmul(pc[:, 8:16], lhsT=ones[:, :], rhs=da_d, start=True, stop=True), reads=[ones, DT_], writes=[pc])
                kb.op("dve", lambda e, b=b, da_d=da_d: e.tensor_tensor(out=dU[b2][:, :, :], in0=tri[:, :].unsqueeze(1).to_broadcast([128, 8, 128]),
                                                                     in1=da_d.unsqueeze(2).to_broadcast([128, 8, 128]), op=ALU.mult),
                      reads=[tri, DT_], writes=[dU[b2]])
                kb.op("dve", lambda e, b=b: e.tensor_copy(out=cum[b][:, :], in_=pc[:, :]), reads=[pc], writes=[cum[b]])
                for hh in range(2):
                    kb.op("pe", lambda e, hh=hh, b2=b2: e.matmul(pA[hh][:, :], lhsT=ones[:, :], rhs=dU[b2][:, 4 * hh:4 * hh + 4, :].rearrange("p h i -> p (h i)"),
                                                               start=True, stop=True), reads=[ones, dU[b2]], writes=[pA[hh]])
                for g in range(2):
                    kb.op("pe", lambda e, g=g, b=b: e.matmul(pcb[:, g * 128:(g + 1) * 128], lhsT=BCT[b][:, g, :], rhs=BCT[b][:, 2 + g, :], start=True, stop=True),
                          reads=[BCT[b]], writes=[pcb])
                kb.op("dve", lambda e, b=b: e.tensor_tensor(out=te[b][:, :], in0=cum[b][:, 8:16], in1=cum[b][:, 0:8], op=ALU.subtract), reads=[cum[b]], writes=[te[b]])
                kb.op("pool", lambda e, b=b, x3=x3, dt_d=dt_d: e.tensor_tensor(out=xdt[b][:, :, :], in0=x3, in1=dt_d.unsqueeze(2).to_broadcast([128, 8, 64]), op=ALU.mult),
                      reads=[X_, DT_], writes=[xdt[b]])
                for hh in range(2):
                    kb.op("act", lambda e, hh=hh, b2=b2: e.activation(out=Ebc[b2][:, 4 * hh:4 * hh + 4, :].rearrange("p h i -> p (h i)"), in_=pA[hh][:, :], func=AF.Exp),
                          reads=[pA[hh]], writes=[Ebc[b2]])
                kb.op("act", lambda e, b=b: e.activation(out=te[b][:, :], in_=te[b][:, :], func=AF.Exp), reads=[te[b]], writes=[te[b]])
                kb.op("act", lambda e, b=b: e.activation(out=cd[b][:, :], in_=cum[b][:, 8:16], func=AF.Exp), reads=[cum[b]], writes=[cd[b]])
                for h in range(8):
                    kb.op("dve", lambda e, h=h, b=b, b2=b2: e.scalar_tensor_tensor(out=Sg[b2][:, h, :], in0=pA[h // 4][:, (h % 4) * 128:(h % 4 + 1) * 128],
                                                                                scalar=cum[b][:, h:h + 1], in1=neg[:, :], op0=ALU.subtract, op1=ALU.add),
                          reads=[pA[h // 4], cum[b], neg], writes=[Sg[b2]])
                kb.op("act", lambda e, b2=b2: e.copy(out=CBT[b2][:, :, :], in_=pcb[:, :].rearrange("p (g t) -> p g t", g=2)), reads=[pcb], writes=[CBT[b2]])
                kb.op("act", lambda e, b2=b2: e.activation(out=Dm[b2][:, :, :], in_=Sg[b2][:, :, :], func=AF.Exp), reads=[Sg[b2]], writes=[Dm[b2]])
                kb.op("pool", lambda e, b=b: e.tensor_tensor(out=xw[b][:, :, :], in0=xdt[b][:, :, :], in1=te[b][:, :].unsqueeze(2).to_broadcast([128, 8, 64]), op=ALU.mult),
                      reads=[xdt[b], te[b]], writes=[xw[b]])
                for g in range(2):
                    kb.op("pool", lambda e, g=g, b=b, b2=b2: e.tensor_tensor(out=CsT[b][:, 4 * g:4 * g + 4, :], in0=Ebc[b2][:, 4 * g:4 * g + 4, :],
                                                                           in1=BCT[b][:, 2 + g, :].unsqueeze(1).to_broadcast([128, 4, 128]), op=ALU.mult),
                          reads=[Ebc[b2], BCT[b]], writes=[CsT[b]])
                for g in range(2):
                    kb.op("dve", lambda e, g=g, b=b, b2=b2: e.tensor_tensor(out=MT[b][:, 4 * g:4 * g + 4, :], in0=Dm[b2][:, 4 * g:4 * g + 4, :],
                                                                          in1=CBT[b2][:, g, :].unsqueeze(1).to_broadcast([128, 4, 128]), op=ALU.mult),
                          reads=[Dm[b2], CBT[b2]], writes=[MT[b]])

            def s2(n):
                b, b2, r0, X_, DT_, dt_d, da_d, x3 = views(n)
                p_y = py[n % 2]
                for h in range(8):
                    kb.op("pe", lambda e, h=h, b=b: e.matmul(p_y[:, h * 64:(h + 1) * 64], lhsT=MT[b][:, h, :], rhs=xdt[b][:, h, :], start=(h == 0), stop=False,
                                                           skip_group_check=True), reads=[MT[b], xdt[b]], writes=[p_y])
                for h in range(8):
                    kb.op("pe", lambda e, h=h, b=b: e.matmul(p_y[:, h * 64:(h + 1) * 64], lhsT=CsT[b][:, h, :], rhs=hsb[:, h, :], start=False, stop=(h == 7),
                                                           skip_group_check=True), reads=[CsT[b], hsb], writes=[p_y])
                for g in range(2):
                    kb.op("pe", lambda e, g=g, b=b, X_=X_: e.matmul(pst[:, g * 256:(g + 1) * 256], lhsT=X_[:, 512 + g * 128:512 + (g + 1) * 128],
                                                                 rhs=xw[b][:, 4 * g:4 * g + 4, :].rearrange("p h c -> p (h c)"), start=True, stop=True),
                          reads=[X_, xw[b]], writes=[pst])
                kb.op("dve", lambda e, b=b, b2=b2: e.tensor_tensor(out=tmp[b2][:, :, :], in0=hs[:, :, :], in1=cd[b][:, :].unsqueeze(2).to_broadcast([128, 8, 64]), op=ALU.mult),
                      reads=[hs, cd[b]], writes=[tmp[b2]])
                kb.op("dve", lambda e, b2=b2: e.tensor_tensor(out=hs[:, :, :], in0=tmp[b2][:, :, :], in1=pst[:, :].rearrange("p (h c) -> p h c", h=8), op=ALU.add),
                      reads=[tmp[b2], pst], writes=[hs])
                kb.op("act", lambda e: e.copy(out=hsb[:, :, :], in_=hs[:, :, :]), reads=[hs], writes=[hsb])

            def s3(n):
                b, b2, r0, X_, DT_, dt_d, da_d, x3 = views(n)
                p_y = py[n % 2]
                if d == 0:
                    kb.op("pool", lambda e, b=b, x3=x3: e.tensor_tensor(out=yo[b][:, :].rearrange("p (h c) -> p h c", h=8), in0=x3,
                                                                      in1=dsk[:, :].unsqueeze(2).to_broadcast([128, 8, 64]), op=ALU.mult),
                          reads=[X_, dsk], writes=[yo[b]])
                    kb.op("dve", lambda e, b=b: e.tensor_tensor(out=yo[b][:, :], in0=p_y[:, :], in1=yo[b][:, :], op=ALU.add), reads=[p_y, yo[b]], writes=[yo[b]])
                    kb.dma("sp", out=self.YF[r0:r0 + 128, :], in_=yo[b][:, :], reads=[yo[b]], writes=[self.YF])
                else:
                    kb.op("dve", lambda e, b=b: e.tensor_tensor(out=yo[b][:, :], in0=p_y[:, :], in1=yf[b][:, :], op=ALU.add), reads=[p_y, yf[b]], writes=[yo[b]])
                    kb.op("act", lambda e, b=b, b2=b2: e.activation(out=sz[b2][:, :], in_=zt[b][:, :], func=AF.Silu), reads=[zt[b]], writes=[sz[b2]])
                    kb.op("pool", lambda e, b=b, b2=b2: e.tensor_tensor(out=yo[b][:, :], in0=yo[b][:, :], in1=sz[b2][:, :], op=ALU.mult), reads=[yo[b], sz[b2]], writes=[yo[b]])
                    for g in range(2):
                        kb.op("act", lambda e, b=b, g=g: e.activation(out=junk[:, :], in_=yo[b][:, g * 256:(g + 1) * 256], func=AF.Square, accum_out=ssq[b][:, g:g + 1]),
                              reads=[yo[b]], writes=[junk, ssq[b]])
                    kb.op("dve", lambda e, b=b: e.tensor_scalar(out=ssq[b][:, :], in0=ssq[b][:, :], scalar1=1.0 / 256, scalar2=EPS, op0=ALU.mult, op1=ALU.add),
                          reads=[ssq[b]], writes=[ssq[b]])
                    kb.op("pool", lambda e, b=b: e.tensor_tensor(out=rsq[b][:, :], in0=ssq[b][:, :], in1=negh[:, 0:1].to_broadcast([128, 2]), op=ALU.pow),
                          reads=[ssq[b], negh], writes=[rsq[b]])
                    for g in range(2):
                        kb.op("dve", lambda e, b=b, g=g, b2=b2: e.scalar_tensor_tensor(out=mo[b2][:, g * 256:(g + 1) * 256], in0=yo[b][:, g * 256:(g + 1) * 256],
                                                                                     scalar=rsq[b][:, g:g + 1], in1=gss[:, g * 256:(g + 1) * 256], op0=ALU.mult, op1=ALU.mult),
                              reads=[yo[b], rsq[b], gss], writes=[mo[b2]])
                    kb.dma("sp", out=self.MIX[r0:r0 + 128, 0:512], in_=mo[b2][:, :], reads=[mo[b2]], writes=[self.MIX])

            pipeline(len(order), [s0, s1, s2, s3])

    def ph_fourier(self, li):
        kb = self.kb
        I = self.I
        SC = 1.0 / np.sqrt(8192.0 * 128.0)
        SCC = 1.0 / np.sqrt(256.0 * 128.0)
        with contextlib.ExitStack() as es:
            F1 = self.cload(es, "F1")
            blk = [kb.sb(es, [128, 16, 1024], BF16, "blk") for _ in range(2)]
            gt = [kb.sb(es, [128, 2, 512], BF16, "gt") for _ in range(3)]
            pg = [kb.ps(es, [128, 512], F32, "pg") for _ in range(4)]
            ADv = self.AD[0:L, :].rearrange("(n1 n2) c -> n1 n2 c", n2=64)
            for rd in range(4):
                bk = blk[rd % 2]
                kb.dma("sp", out=bk[:, :, :], in_=ADv[:, rd * 16:(rd + 1) * 16, :], reads=[self.AD], writes=[bk])
                for nl in range(16):
                    n2 = rd * 16 + nl
                    g = gt[n2 % 3]
                    pr_, pi_ = pg[(2 * n2) % 4], pg[(2 * n2 + 1) % 4]
                    Ac = bk[:, nl, 0:512]
                    As = bk[:, nl, 512:1024]
                    kb.op("pe", lambda e, pr_=pr_, Ac=Ac: e.matmul(pr_[:, :], lhsT=F1[:, 0, :], rhs=Ac, start=True, stop=False), reads=[F1, bk], writes=[pr_])
                    kb.op("pe", lambda e, pr_=pr_, As=As: e.matmul(pr_[:, :], lhsT=F1[:, 1, :], rhs=As, start=False, stop=True), reads=[F1, bk], writes=[pr_])
                    kb.op("pe", lambda e, pi_=pi_, Ac=Ac: e.matmul(pi_[:, :], lhsT=F1[:, 1, :], rhs=Ac, start=True, stop=False), reads=[F1, bk], writes=[pi_])
                    kb.op("pe", lambda e, pi_=pi_, As=As: e.matmul(pi_[:, :], lhsT=F1[:, 2, :], rhs=As, start=False, stop=True), reads=[F1, bk], writes=[pi_])
                    kb.op("act", lambda e, g=g, pr_=pr_: e.copy(out=g[:, 0, :], in_=pr_[:, :]), reads=[pr_], writes=[g])
                    kb.op("dve", lambda e, g=g, pi_=pi_: e.tensor_copy(out=g[:, 1, :], in_=pi_[:, :]), reads=[pi_], writes=[g])
                    kb.dma("sp", out=self.GD[:, n2, :, :].rearrange("ri p c -> p ri c"), in_=g[:, :, :], reads=[g], writes=[self.GD])
        kb.barrier()
        with contextlib.ExitStack() as es:
            TW = self.cload(es, "TW3")
            blk = [kb.sb(es, [128, 32, 512], BF16, "blk3") for _ in range(2)]
            ot = [kb.sb(es, [64, 32, 512], BF16, "ot") for _ in range(2)]
            pg = [kb.ps(es, [64, 512], F32, "pg3") for _ in range(4)]
            GDv = self.GD[:, :, :, :].rearrange("ri n2 p c -> (ri n2) p c")
            MIXv = self.MIX[0:L, :].rearrange("(p2 p1) c -> p2 p1 c", p1=128)
            for rd in range(4):
                bk = blk[rd % 2]
                o = ot[rd % 2]
                kb.dma("sp", out=bk[:, :, :], in_=GDv[:, rd * 32:(rd + 1) * 32, :], reads=[self.GD], writes=[bk])
                for pl in range(32):
                    p1 = rd * 32 + pl
                    p = pg[p1 % 4]
                    kb.op("pe", lambda e, p=p, p1=p1, pl=pl, bk=bk: e.matmul(p[:, :], lhsT=TW[:, p1, :], rhs=bk[:, pl, :], start=True, stop=True), reads=[TW, bk], writes=[p])
                    if pl % 2 == 0:
                        kb.op("act", lambda e, p=p, pl=pl, o=o: e.activation(out=o[:, pl, :], in_=p[:, :], func=AF.Copy, scale=float(SC)), reads=[p], writes=[o])
                    else:
                        kb.op("dve", lambda e, p=p, pl=pl, o=o: e.tensor_scalar(out=o[:, pl, :], in0=p[:, :], scalar1=float(SC), scalar2=None, op0=ALU.mult), reads=[p], writes=[o])
                kb.dma("sp", out=MIXv[:, rd * 32:(rd + 1) * 32, 512:1024], in_=o[:, :, :], reads=[o], writes=[self.MIX])
            C2 = self.cload(es, "C256")
            S2 = self.cload(es, "nS256")
            ac = kb.sb(es, [128, 2, 1024], BF16, "actx")
            kb.dma("sp", out=ac[:, :, :], in_=self.AD[L:NT, :].rearrange("(t p) c -> p t c", p=128), reads=[self.AD], writes=[ac])
            oc = kb.sb(es, [128, 2, 512], BF16, "octx")
            pcx = [kb.ps(es, [128, 512], F32, "pcx") for _ in range(2)]
            for pt in range(2):
                p = pcx[pt]
                k = 0
                for nt in range(2):
                    for (M, off) in ((C2, 0), (S2, 512)):
                        kb.op("pe", lambda e, p=p, M=M, nt=nt, pt=pt, off=off, k=k: e.matmul(p[:, :], lhsT=M[:, nt, pt * 128:(pt + 1) * 128], rhs=ac[:, nt, off:off + 512],
                                                                                          start=(k == 0), stop=(k == 3)), reads=[M, ac], writes=[p])
                        k += 1
                kb.op("act", lambda e, p=p, pt=pt: e.activation(out=oc[:, pt, :], in_=p[:, :], func=AF.Copy, scale=float(SCC)), reads=[p], writes=[oc])
            kb.dma("sp", out=self.MIX[L:NT, 512:1024].rearrange("(t p) c -> p t c", p=128), in_=oc[:, :, :], reads=[oc], writes=[self.MIX])

    def ph_final(self):
        kb = self.kb
        I = self.I
        with contextlib.ExitStack() as es:
            gf = kb.sb(es, [128, D], F32, "gf")
            kb.dma("sp", out=gf[:, :], in_=I["g_final"].partition_broadcast(128), writes=[gf])
            zero = kb.sb(es, [128, D], F32, "zero")
            kb.op("dve", lambda e: e.memset(zero[:, :], 0.0), writes=[zero])
            negh = kb.sb(es, [128, 1], F32, "negh")
            kb.op("dve", lambda e: e.memset(negh[:, :], -0.5), writes=[negh])
            junk = kb.sb(es, [128, D], BF16, "junk")
            NB = 4
            xt = [kb.sb(es, [128, D], F32, "xt") for _ in range(NB)]
            of = [kb.sb(es, [128, D], F32, "of") for _ in range(NB)]
            sm = [[kb.sb(es, [128, 1], F32, "sm") for _ in range(2)] for _ in range(NB)]

            def s0(ti):
                b = ti % NB
                r0 = ti * 128
                kb.dma("sp", out=xt[b][:, :], in_=self.X[r0:r0 + 128, :], reads=[self.X], writes=[xt[b]])

            def s1(ti):
                b = ti % NB
                r0 = ti * 128
                self.norm_tile((sm[b][0], sm[b][1]), xt[b], gf, zero, of[b], None, junk, negh)
                kb.dma("sp", out=self.out[r0:r0 + 128, :], in_=of[b][:, :], reads=[of[b]], writes=[self.outk])

            pipeline(64, [s0, s1])

    def ph_dumpx(self):
        kb = self.kb
        for r0 in range(0, L, 2048):
            kb.dma("sp", out=self.out[r0:r0 + 2048, :], in_=self.X[r0:r0 + 2048, :], reads=[self.X], writes=[self.outk])


def default_phases():
    ph = [("init",)]
    for li in range(DEPTH):
        need_ctx = li < DEPTH - 1
        ph += [("mod", li)]
        if li % 2 == 0:
            ph += [("inproj", li), ("conv", li), ("ssd", li, 0), ("ssd", li, 1), ("fourier", li),
                   ("oproj", li, "w_out_e", "MIX", need_ctx)]
        else:
            ph += [("qkv", li), ("attn", li, need_ctx), ("oproj", li, "w_o", "OD", need_ctx)]
        ph += [("router", li), ("select", li), ("moe", li)]
    ph += [("final",)]
    return ph


ATT_VARIANT_RP = [2, 0, 1, 62, 63]


def att_base(rp):
    r0 = 2 * rp
    return min(min(max(r0 - 4, 0), 120), 118)


def att_variant(rp):
    return 0 if 2 <= rp <= 61 else {0: 1, 1: 2, 62: 3, 63: 4}[rp]


def build_bias_table(rpb):
    no = rpb.shape[0]
    out = np.empty((no, 5, 16, 128, 5, 128), np.float32)
    p = np.arange(128)
    q = np.arange(128)
    c = np.arange(5)
    for v, rp in enumerate(ATT_VARIANT_RP):
        r0 = 2 * rp
        base = att_base(rp)
        krow = base + 2 * c[:, None, None] + (p // 64)[None, :, None]
        kcol = (p % 64)[None, :, None]
        r = (r0 + q // 64)[None, None, :]
        col = (q % 64)[None, None, :]
        rs = np.clip(r - 4, 0, 120)
        cs = np.clip(col - 8, 0, 48)
        valid = (krow >= rs) & (krow < rs + 8) & (kcol >= cs) & (kcol < cs + 16)
        ro = np.clip(krow - r + 7, 0, 14) + 0 * kcol
        co = np.clip(kcol - col + 15, 0, 30) + 0 * krow
        g = rpb[:, :, ro, co]
        g = np.where(valid[None, None], g, np.float32(-30000.0))
        out[:, v] = np.transpose(g, (0, 1, 3, 2, 4))
    return out


def make_in_maps(inputs, n_cores, names):
    consts = host_consts()
    shared = {}
    for k in names:
        if k in consts:
            shared[k] = consts[k]
        elif k in ("x", "ctx", "c"):
            pass
        elif k == "biasT":
            shared[k] = build_bias_table(inputs["rpb"])
        elif k in ("a_log", "dt_bias"):
            shared[k] = np.ascontiguousarray(inputs[k].reshape(2, 16))
        else:
            shared[k] = np.ascontiguousarray(inputs[k])
    maps = []
    for b in range(n_cores):
        m = dict(shared)
        for k in ("x", "ctx", "c"):
            if k in names:
                m[k] = np.ascontiguousarray(inputs[k][b])
        maps.append(m)
    return maps


def kernel(**inputs):
    inputs = {k: np.asarray(v) for k, v in inputs.items()}
    prog = Prog(default_phases())
    in_maps = make_in_maps(inputs, 4, list(prog.I.keys()))
    res = run_bass_kernel_spmd(prog.nc, in_maps, core_ids=list(range(4)))
    out = np.stack([res.results[b]["out"] for b in range(4)], axis=0)
    return out.astype(np.float32)
```

```python
import contextlib
import numpy as np
import ml_dtypes
import concourse.bass as bass
import concourse.mybir as mybir
from concourse.bass_utils import run_bass_kernel_spmd

F32 = mybir.dt.float32
BF16 = mybir.dt.bfloat16
I32 = mybir.dt.int32
AF = mybir.ActivationFunctionType
ALU = mybir.AluOpType
AX = mybir.AxisListType
IOA = bass.IndirectOffsetOnAxis if hasattr(bass, "IndirectOffsetOnAxis") else None

D = 1024
L = 8192
LC = 256
NT = L + LC
NTILE = NT // 128
DEPTH = 4
NE = 16
FF = 2048
NBLK = 4
BLK = L // NBLK
PSEL = NE * NBLK
CAPB = 320
NSLOT = NBLK * CAPB
NCALL = NSLOT // 128
CAPC = 32
XR = NT + (NCALL + 1) * 128
EPS = 1e-6


class Trk:
    __slots__ = ("w", "r", "x")

    def __init__(self, x=False):
        self.w = None
        self.r = {}
        self.x = x


class Buf:
    def __init__(self, t, x=False):
        self.t = t
        self.k = Trk(x)

    def __getitem__(self, key):
        return self.t[key]


class KB:
    ND = 40

    def __init__(self, nc):
        self.nc = nc
        self.es = contextlib.ExitStack()
        self.eng = {"pe": nc.tensor, "act": nc.scalar, "dve": nc.vector, "pool": nc.gpsimd, "sp": nc.sync}
        self.csem = {}
        self.ccnt = {}
        for e in ("pe", "act", "dve", "pool"):
            self.csem[e] = self.es.enter_context(nc.semaphore("cs_" + e))
            self.ccnt[e] = 0
        self.dsem = [self.es.enter_context(nc.semaphore("ds%d" % i)) for i in range(self.ND)]
        self.dcnt = [0] * self.ND
        self.dnext = 0
        self.seen = {e: {} for e in self.eng}
        self.uid = 0

    def sb(self, es, shape, dtype, name=None):
        self.uid += 1
        return Buf(es.enter_context(self.nc.sbuf_tensor("%s_%d" % (name or "sb", self.uid), list(shape), dtype)))

    def ps(self, es, shape, dtype, name=None):
        self.uid += 1
        return Buf(es.enter_context(self.nc.psum_tensor("%s_%d" % (name or "ps", self.uid), list(shape), dtype)), x=True)

    def dram(self, name, shape, dtype):
        return Buf(self.nc.dram_tensor(name, list(shape), dtype, kind="Internal").ap())

    def _sem(self, ch):
        return self.csem[ch] if isinstance(ch, str) else self.dsem[ch]

    def _deps(self, eng, reads, writes, is_dma):
        deps = {}

        def add(ev, raw):
            if ev is None:
                return
            ch, v = ev
            if (not is_dma) and ch == eng:
                if eng == "pe" or not raw:
                    return
            if deps.get(ch, 0) < v:
                deps[ch] = v

        for t in reads:
            add(t.w, True)
            if t.x:
                for ch, v in t.r.items():
                    if ch != eng:
                        add((ch, v), False)
        for t in writes:
            add(t.w, False)
            for ch, v in t.r.items():
                add((ch, v), False)
        return deps

    def _wait(self, eng, deps):
        s = self.seen[eng]
        for ch, v in deps.items():
            if s.get(ch, 0) < v:
                self.eng[eng].wait_ge(self._sem(ch), v)
                s[ch] = v

    @staticmethod
    def _trks(lst):
        return [b.k if isinstance(b, Buf) else b for b in lst]

    def op(self, eng, fn, reads=(), writes=()):
        reads = self._trks(reads)
        writes = self._trks(writes)
        self._wait(eng, self._deps(eng, reads, writes, False))
        ins = fn(self.eng[eng])
        self.ccnt[eng] += 1
        v = self.ccnt[eng]
        ins.then_inc(self.csem[eng], 1)
        for t in reads:
            if t.r.get(eng, 0) < v:
                t.r[eng] = v
        for t in writes:
            t.w = (eng, v)
            t.r = {}
        return ins

    def dma(self, q, out=None, in_=None, reads=(), writes=(), fn=None, **kw):
        reads = self._trks(reads)
        writes = self._trks(writes)
        s = self.dnext
        self.dnext = (s + 1) % self.ND
        deps = self._deps(q, reads, writes, True)
        if self.dcnt[s] > 0 and deps.get(s, 0) < self.dcnt[s]:
            deps[s] = self.dcnt[s]
        self._wait(q, deps)
        if fn is None:
            ins = self.eng[q].dma_start(out=out, in_=in_, **kw)
        else:
            ins = fn(self.eng[q])
        self.dcnt[s] += 16
        v = self.dcnt[s]
        ins.then_inc(self.dsem[s], 16)
        for t in reads:
            if t.r.get(s, 0) < v:
                t.r[s] = v
        for t in writes:
            t.w = (s, v)
            t.r = {}
        return ins

    def barrier(self):
        for e in self.eng:
            deps = {}
            for c in self.csem:
                if self.ccnt[c] > 0 and c != e:
                    deps[c] = self.ccnt[c]
            for s in range(self.ND):
                if self.dcnt[s] > 0:
                    deps[s] = self.dcnt[s]
            self._wait(e, deps)
        for e in self.csem:
            if self.ccnt[e] > 0:
                s = self.seen[e]
                if s.get(e, 0) < self.ccnt[e]:
                    self.eng[e].wait_ge(self.csem[e], self.ccnt[e])
                    s[e] = self.ccnt[e]


def host_consts():
    c = {}
    c["ident_bf"] = np.eye(128, dtype=np.float32).astype(ml_dtypes.bfloat16)
    c["ident_f"] = np.eye(128, dtype=np.float32)
    p = np.arange(128)
    c["gsum"] = (p[:, None] // NBLK == p[None, :] // NBLK).astype(np.float32)
    c["keyl"] = np.broadcast_to((BLK - np.arange(BLK, dtype=np.float32))[None, :], (128, BLK)).copy()
    c["keyc"] = np.broadcast_to((256 - np.arange(256, dtype=np.float32))[None, :], (128, 256)).copy()
    c["basel"] = ((p % NBLK) * BLK + BLK).astype(np.float32)[:, None].copy()
    c["basec"] = np.full((128, 1), L + 256, np.float32)
    sig = (p[:, None] % NBLK) * CAPB + np.arange(CAPB)[None, :]
    c["dumpl"] = (NT + sig).astype(np.float32)
    c["dumpc"] = np.broadcast_to((NT + NSLOT + np.arange(CAPC, dtype=np.float32))[None, :], (128, CAPC)).copy()
    k = np.arange(128)
    c["triU"] = (k[:, None] <= k[None, :]).astype(np.float32)
    c["triL"] = (k[:, None] >= k[None, :]).astype(np.float32)
    c["onesf"] = np.ones((128, 128), np.float32)
    c["negU"] = np.where(k[None, :] >= k[:, None], 0.0, -30000.0).astype(np.float32)
    c["negL"] = np.where(k[None, :] <= k[:, None], 0.0, -30000.0).astype(np.float32)
    ang = 2 * np.pi * np.outer(k, k) / 128.0
    bf = ml_dtypes.bfloat16
    c["CS"] = np.concatenate([np.cos(ang), np.sin(ang)], axis=1).astype(np.float32).astype(bf)
    c["F1"] = np.stack([np.cos(ang), -np.sin(ang), -np.cos(ang)], axis=1).astype(np.float32).astype(bf)
    n2 = np.arange(64)[:, None, None]
    p1 = np.arange(128)[None, :, None]
    p2 = np.arange(64)[None, None, :]
    th = 2 * np.pi * (n2 * p2 / 64.0 + n2 * p1 / 8192.0)
    c["TW3"] = np.concatenate([np.cos(th), np.sin(th)], axis=0).astype(np.float32).astype(bf)
    n = np.arange(256)
    a2 = 2 * np.pi * np.outer(n, n) / 256.0
    c["C256"] = np.cos(a2).reshape(2, 128, 256).transpose(1, 0, 2).astype(np.float32).astype(bf)
    c["nS256"] = (-np.sin(a2)).reshape(2, 128, 256).transpose(1, 0, 2).astype(np.float32).astype(bf)
    return c


CONST_SPECS = {
    "triU": ([128, 128], F32), "triL": ([128, 128], F32), "onesf": ([128, 128], F32), "negU": ([128, 128], F32),
    "negL": ([128, 128], F32), "CS": ([128, 256], BF16), "F1": ([128, 3, 128], BF16), "TW3": ([128, 128, 64], BF16),
    "C256": ([128, 2, 256], BF16), "nS256": ([128, 2, 256], BF16),
    "ident_bf": ([128, 128], BF16), "ident_f": ([128, 128], F32), "gsum": ([128, 128], F32),
    "keyl": ([128, BLK], F32), "keyc": ([128, 256], F32), "basel": ([128, 1], F32), "basec": ([128, 1], F32),
    "dumpl": ([128, CAPB], F32), "dumpc": ([128, CAPC], F32),
}


IN_SPECS = {
    "x": ([L, D], F32), "ctx": ([LC, D], F32), "c": ([D], F32), "c_ctx": ([D], F32),
    "w_mod": ([DEPTH, D, 6 * D], F32), "b_mod": ([DEPTH, 6 * D], F32), "g_mix": ([DEPTH, D], F32), "g_ffn": ([DEPTH, D], F32),
    "w_router": ([DEPTH, D, NE], F32), "w_e1": ([DEPTH, NE, D, FF], F32), "w_e3": ([DEPTH, NE, D, FF], F32),
    "w_e2": ([DEPTH, NE, FF, D], F32), "g_final": ([D], F32),
    "w_in_e": ([2, D, 2064], F32), "conv_w": ([2, 3, D], F32), "conv_b": ([2, D], F32), "a_log": ([2, 16], F32),
    "dt_bias": ([2, 16], F32), "d_skip": ([2, 8], F32), "g_ssd": ([2, 512], F32), "w_out_e": ([2, D, D], F32),
    "w_qkv": ([2, D, 3 * D], F32), "w_o": ([2, D, D], F32), "biasT": ([2, 5, 16, 128, 5, 128], F32),
}


def pipeline(n, stages):
    ns = len(stages)
    for step in range(n + ns - 1):
        for si in range(ns - 1, -1, -1):
            k = step - si
            if 0 <= k < n:
                stages[si](k)


class Prog:
    def __init__(self, phases, debug_out=None):
        self.phases = phases
        self.debug_out = debug_out
        nc = bass.Bass("TRN2", target_bir_lowering=False)
        self.nc = nc
        self.kb = KB(nc)
        kb = self.kb
        dt = nc.dram_tensor

        class LazyIn(dict):
            def __missing__(d, k):
                shp, ty = IN_SPECS[k] if k in IN_SPECS else CONST_SPECS[k]
                d[k] = dt(k, list(shp), ty, kind="ExternalInput").ap()
                return d[k]
        self.I = LazyIn()
        self.out = dt("out", [L, D], F32, kind="ExternalOutput").ap()
        self.X = kb.dram("Xres", [XR, D], F32)
        self.H2 = kb.dram("H2", [XR, D], BF16)
        self.AFF = kb.dram("AFF", [XR, NE], F32)
        self.AFFT = kb.dram("AFFT", [NE, L], F32)
        self.AFFTC = kb.dram("AFFTC", [NE, LC], F32)
        self.IDXD = kb.dram("IDXD", [PSEL, CAPB], I32)
        self.IDXC = kb.dram("IDXC", [NE, CAPC], I32)
        self.MODD = kb.dram("MODD", [2, 6 * D], F32)
        self.outk = Trk()
        self.build()

    def build(self):
        kb = self.kb
        with contextlib.ExitStack() as ges:
            self.ges = ges
            self.ident_bf = kb.sb(ges, [128, 128], BF16, "identbf")
            self.ident_f = kb.sb(ges, [128, 128], F32, "identf")
            kb.dma("sp", out=self.ident_bf[:, :], in_=self.I["ident_bf"][:, :], writes=[self.ident_bf])
            kb.dma("sp", out=self.ident_f[:, :], in_=self.I["ident_f"][:, :], writes=[self.ident_f])
            for ph in self.phases:
                name = ph[0]
                getattr(self, "ph_" + name)(*ph[1:])
                kb.barrier()
            kb.barrier()
        kb.es.close()

    def ph_init(self):
        kb = self.kb
        with contextlib.ExitStack() as es:
            for r0 in range(0, L, 2048):
                kb.dma("sp", out=self.X[r0:r0 + 2048, :], in_=self.I["x"][r0:r0 + 2048, :], writes=[self.X])
            kb.dma("sp", out=self.X[L:NT, :], in_=self.I["ctx"][:, :], writes=[self.X])
            z = kb.sb(es, [128, D], F32, "z")
            zb = kb.sb(es, [128, D], BF16, "zb")
            kb.op("dve", lambda e: e.memset(z[:, :], 0.0), writes=[z])
            kb.op("dve", lambda e: e.memset(zb[:, :], 0.0), writes=[zb])
            for j in range(NCALL + 1):
                r0 = NT + j * 128
                kb.dma("sp", out=self.X[r0:r0 + 128, :], in_=z[:, :], reads=[z], writes=[self.X])
                kb.dma("sp", out=self.H2[r0:r0 + 128, :], in_=zb[:, :], reads=[zb], writes=[self.H2])
                kb.dma("sp", out=self.AFF[r0:r0 + 128, :], in_=z[:, 0:NE], reads=[z], writes=[self.AFF])

    def ph_mod(self, li):
        kb = self.kb
        I = self.I
        with contextlib.ExitStack() as es:
            cv = kb.sb(es, [128, 2, 8], F32, "cv")
            kb.dma("sp", out=cv[:, 0, :], in_=I["c"].rearrange("(kc p) -> p kc", p=128), writes=[cv],
                   allow_slow_non_contiguous=True)
            kb.dma("sp", out=cv[:, 1, :], in_=I["c_ctx"].rearrange("(kc p) -> p kc", p=128), writes=[cv],
                   allow_slow_non_contiguous=True)
            sv = kb.sb(es, [128, 2, 8], F32, "sv")
            kb.op("act", lambda e: e.activation(out=sv[:, :, :], in_=cv[:, :, :], func=AF.Silu), reads=[cv], writes=[sv])
            lb = kb.sb(es, [128, 2, 8, 128], F32, "lb")
            for s in range(2):
                kb.op("dve", lambda e, s=s: e.tensor_copy(out=lb[:, s, :, :], in_=sv[:, s, :].unsqueeze(2).to_broadcast([128, 8, 128])),
                      reads=[sv], writes=[lb])
            gmix = kb.sb(es, [128, D], F32, "gmix")
            gffn = kb.sb(es, [128, D], F32, "gffn")
            kb.dma("sp", out=gmix[:, :], in_=I["g_mix"][li, :].partition_broadcast(128), writes=[gmix])
            kb.dma("sp", out=gffn[:, :], in_=I["g_ffn"][li, :].partition_broadcast(128), writes=[gffn])
            wm = [kb.sb(es, [128, 8, 512], F32, "wm") for _ in range(2)]
            bm = [kb.sb(es, [128, 512], F32, "bm") for _ in range(2)]
            pp = [kb.ps(es, [128, 512], F32, "pm") for _ in range(4)]
            res = [kb.sb(es, [128, 512], F32, "res") for _ in range(4)]
            wsrc = I["w_mod"][li].rearrange("(kc p) n -> p kc n", p=128)
            for n in range(12):
                w = wm[n % 2]
                b = bm[n % 2]
                kb.dma("sp", out=w[:, :, :], in_=wsrc[:, :, n * 512:(n + 1) * 512], writes=[w])
                kb.dma("sp", out=b[:, :], in_=I["b_mod"][li, n * 512:(n + 1) * 512].partition_broadcast(128), writes=[b])
                for s in range(2):
                    p = pp[(2 * n + s) % 4]
                    r = res[(2 * n + s) % 4]
                    for kc in range(8):
                        kb.op("pe", lambda e, kc=kc, s=s, p=p, w=w: e.matmul(p[:, :], lhsT=lb[:, s, kc, :], rhs=w[:, kc, :],
                                                                          start=(kc == 0), stop=(kc == 7)),
                              reads=[lb, w], writes=[p])
                    which = n // 2
                    if which in (1, 4):
                        g = gmix if which == 1 else gffn
                        c0 = (n % 2) * 512
                        kb.op("dve", lambda e, p=p, r=r, b=b: e.tensor_tensor(out=r[:, :], in0=p[:, :], in1=b[:, :], op=ALU.add),
                              reads=[p, b], writes=[r])
                        kb.op("dve", lambda e, r=r, g=g, c0=c0: e.scalar_tensor_tensor(out=r[:, :], in0=r[:, :], scalar=1.0,
                                                                                     in1=g[:, c0:c0 + 512], op0=ALU.add, op1=ALU.mult),
                              reads=[r, g], writes=[r])
                    else:
                        kb.op("dve", lambda e, p=p, r=r, b=b: e.tensor_tensor(out=r[:, :], in0=p[:, :], in1=b[:, :], op=ALU.add),
                              reads=[p, b], writes=[r])
                    kb.dma("sp", out=self.MODD[s:s + 1, n * 512:(n + 1) * 512], in_=r[0:1, :], reads=[r], writes=[self.MODD])

    def load_vec(self, es, s, which, name="vec"):
        kb = self.kb
        t = kb.sb(es, [128, D], F32, name)
        kb.dma("sp", out=t[:, :], in_=self.MODD[s, which * D:(which + 1) * D].partition_broadcast(128),
               reads=[self.MODD], writes=[t])
        return t

    def norm_tile(self, rstd_tmp, xt, s_bc, sh_bc, out_f32, out_bf, junk, negh):
        kb = self.kb
        ss, rs = rstd_tmp
        kb.op("act", lambda e: e.activation(out=junk[:, :], in_=xt[:, :], func=AF.Square, accum_out=ss[:, 0:1]),
              reads=[xt], writes=[junk, ss])
        kb.op("dve", lambda e: e.tensor_scalar(out=ss[:, 0:1], in0=ss[:, 0:1], scalar1=1.0 / D, scalar2=EPS, op0=ALU.mult, op1=ALU.add),
              reads=[ss], writes=[ss])
        kb.op("pool", lambda e: e.tensor_tensor(out=rs[:, 0:1], in0=ss[:, 0:1], in1=negh[:, 0:1], op=ALU.pow),
              reads=[ss, negh], writes=[rs])
        kb.op("dve", lambda e: e.scalar_tensor_tensor(out=out_f32[:, :], in0=xt[:, :], scalar=rs[:, 0:1], in1=s_bc[:, :],
                                                      op0=ALU.mult, op1=ALU.mult),
              reads=[xt, rs, s_bc], writes=[out_f32])
        kb.op("pool", lambda e: e.tensor_tensor(out=out_f32[:, :], in0=out_f32[:, :], in1=sh_bc[:, :], op=ALU.add),
              reads=[out_f32, sh_bc], writes=[out_f32])
        if out_bf is not None:
            kb.op("act", lambda e: e.copy(out=out_bf[:, :], in_=out_f32[:, :]), reads=[out_f32], writes=[out_bf])

    def ph_router(self, li):
        kb = self.kb
        I = self.I
        with contextlib.ExitStack() as es:
            svec = [self.load_vec(es, s, 4, "s2") for s in range(2)]
            shvec = [self.load_vec(es, s, 3, "sh2") for s in range(2)]
            negh = kb.sb(es, [128, 1], F32, "negh")
            kb.op("dve", lambda e: e.memset(negh[:, :], -0.5), writes=[negh])
            wr = kb.sb(es, [128, 8, NE], F32, "wr")
            kb.dma("sp", out=wr[:, :, :], in_=I["w_router"][li].rearrange("(kc p) n -> p kc n", p=128), writes=[wr])
            AT = kb.sb(es, [NE, L], F32, "AT")
            ATC = kb.sb(es, [NE, LC], F32, "ATC")
            NB = 5
            xt = [kb.sb(es, [128, D], F32, "xt") for _ in range(NB)]
            hf = [kb.sb(es, [128, D], F32, "hf") for _ in range(NB)]
            hb = [kb.sb(es, [128, D], BF16, "hb") for _ in range(NB)]
            junk = kb.sb(es, [128, D], BF16, "junk")
            hT = [kb.sb(es, [128, 8, 128], F32, "hT") for _ in range(NB)]
            sm = [[kb.sb(es, [128, 1], F32, "sm") for _ in range(6)] for _ in range(NB)]
            lg = [kb.sb(es, [128, NE], F32, "lg") for _ in range(NB)]
            af = [kb.sb(es, [128, NE], F32, "af") for _ in range(NB)]
            pT = [kb.ps(es, [128, 512], F32, "pT") for _ in range(4)]
            pl = [kb.ps(es, [128, NE], F32, "pl") for _ in range(2)]
            pa = [kb.ps(es, [NE, 128], F32, "pa") for _ in range(2)]

            def s0(ti):
                b = ti % NB
                r0 = ti * 128
                kb.dma("sp", out=xt[b][:, :], in_=self.X[r0:r0 + 128, :], reads=[self.X], writes=[xt[b]])

            def s1(ti):
                b = ti % NB
                s = 0 if ti < 64 else 1
                r0 = ti * 128
                self.norm_tile((sm[b][0], sm[b][1]), xt[b], svec[s], shvec[s], hf[b], hb[b], junk, negh)
                kb.dma("sp", out=self.H2[r0:r0 + 128, :], in_=hb[b][:, :], reads=[hb[b]], writes=[self.H2])

            def s2(ti):
                b = ti % NB
                for half in range(2):
                    p = pT[(2 * ti + half) % 4]
                    for q in range(4):
                        kc = half * 4 + q
                        kb.op("pe", lambda e, p=p, q=q, kc=kc, b=b: e.transpose(out=p[:, q * 128:(q + 1) * 128],
                                                                              in_=hf[b][:, kc * 128:(kc + 1) * 128],
                                                                              identity=self.ident_f[:, :]),
                              reads=[hf[b], self.ident_f], writes=[p])
                    kb.op("act", lambda e, p=p, half=half, b=b: e.copy(out=hT[b][:, half * 4:half * 4 + 4, :],
                                                                     in_=p[:, :].rearrange("p (q t) -> p q t", q=4)),
                          reads=[p], writes=[hT[b]])

            def s3(ti):
                b = ti % NB
                s = 0 if ti < 64 else 1
                r0 = ti * 128
                pp = pl[ti % 2]
                for kc in range(8):
                    kb.op("pe", lambda e, kc=kc, pp=pp, b=b: e.matmul(pp[:, :], lhsT=hT[b][:, kc, :], rhs=wr[:, kc, :],
                                                                    start=(kc == 0), stop=(kc == 7)),
                          reads=[hT[b], wr], writes=[pp])
                mx, nmx, se, rse = sm[b][2], sm[b][3], sm[b][4], sm[b][5]
                kb.op("dve", lambda e, pp=pp, mx=mx: e.reduce_max(out=mx[:, 0:1], in_=pp[:, :], axis=AX.X), reads=[pp], writes=[mx])
                kb.op("dve", lambda e, mx=mx, nmx=nmx: e.tensor_scalar(out=nmx[:, 0:1], in0=mx[:, 0:1], scalar1=-1.0, scalar2=None, op0=ALU.mult),
                      reads=[mx], writes=[nmx])
                kb.op("act", lambda e, pp=pp, b=b, nmx=nmx, se=se: e.activation(out=lg[b][:, :], in_=pp[:, :], func=AF.Exp, bias=nmx[:, 0:1],
                                                                              accum_out=se[:, 0:1]),
                      reads=[pp, nmx], writes=[lg[b], se])
                kb.op("dve", lambda e, se=se, rse=rse: e.reciprocal(out=rse[:, 0:1], in_=se[:, 0:1]), reads=[se], writes=[rse])
                kb.op("dve", lambda e, b=b, rse=rse: e.tensor_scalar(out=af[b][:, :], in0=lg[b][:, :], scalar1=rse[:, 0:1], scalar2=None, op0=ALU.mult),
                      reads=[lg[b], rse], writes=[af[b]])
                kb.dma("sp", out=self.AFF[r0:r0 + 128, :], in_=af[b][:, :], reads=[af[b]], writes=[self.AFF])
                pq = pa[ti % 2]
                kb.op("pe", lambda e, pq=pq, b=b: e.matmul(pq[:, :], lhsT=af[b][:, :], rhs=self.ident_f[:, :], start=True, stop=True),
                      reads=[af[b], self.ident_f], writes=[pq])
                if s == 0:
                    kb.op("act", lambda e, pq=pq, r0=r0: e.copy(out=AT[:, r0:r0 + 128], in_=pq[:, :]), reads=[pq], writes=[AT])
                else:
                    kb.op("act", lambda e, pq=pq, r0=r0: e.copy(out=ATC[:, r0 - L:r0 - L + 128], in_=pq[:, :]), reads=[pq], writes=[ATC])

            pipeline(NTILE, [s0, s1, s2, s3])
            kb.dma("sp", out=self.AFFT[:, :], in_=AT[:, :], reads=[AT], writes=[self.AFFT])
            kb.dma("sp", out=self.AFFTC[:, :], in_=ATC[:, :], reads=[ATC], writes=[self.AFFTC])

    def select(self, es, A, P, F, K, cap, key_c, base_c, dump_c, gsum, idx_out_dram, tag):
        kb = self.kb
        lo = kb.sb(es, [P, 1], F32, "lo" + tag)
        hi = kb.sb(es, [P, 1], F32, "hi" + tag)
        mid = kb.sb(es, [P, 1], F32, "mid" + tag)
        cnt = kb.sb(es, [P, 1], F32, "cnt" + tag)
        ge = kb.sb(es, [P, 1], F32, "ge" + tag)
        d1 = kb.sb(es, [P, 1], F32, "d1" + tag)
        W = kb.sb(es, [P, F], F32, "W" + tag)
        pc = kb.ps(es, [P, 1], F32, "pc" + tag)
        kb.op("dve", lambda e: e.memset(lo[:, :], 0.0), writes=[lo])
        kb.op("dve", lambda e: e.memset(hi[:, :], 1.0), writes=[hi])
        for it in range(36):
            kb.op("dve", lambda e: e.tensor_tensor(out=mid[:, :], in0=lo[:, :], in1=hi[:, :], op=ALU.add), reads=[lo, hi], writes=[mid])
            kb.op("dve", lambda e: e.tensor_scalar(out=mid[:, :], in0=mid[:, :], scalar1=0.5, scalar2=None, op0=ALU.mult), reads=[mid], writes=[mid])
            kb.op("dve", lambda e: e.tensor_scalar(out=W[:, :], in0=A[:, :], scalar1=mid[:, 0:1], scalar2=0.0, op0=ALU.is_ge, op1=ALU.add,
                                                   accum_out=cnt[:, 0:1]), reads=[A, mid], writes=[W, cnt])
            if gsum is not None:
                kb.op("pe", lambda e: e.matmul(pc[:, :], lhsT=gsum[0:P, 0:P], rhs=cnt[:, :], start=True, stop=True), reads=[gsum, cnt], writes=[pc])
                src = pc
            else:
                src = cnt
            kb.op("dve", lambda e, src=src: e.tensor_scalar(out=ge[:, :], in0=src[:, :], scalar1=float(K) - 0.5, scalar2=None, op0=ALU.is_ge),
                  reads=[src], writes=[ge])
            kb.op("dve", lambda e: e.tensor_tensor(out=d1[:, :], in0=mid[:, :], in1=lo[:, :], op=ALU.subtract), reads=[mid, lo], writes=[d1])
            kb.op("dve", lambda e: e.scalar_tensor_tensor(out=lo[:, :], in0=d1[:, :], scalar=ge[:, 0:1], in1=lo[:, :], op0=ALU.mult, op1=ALU.add),
                  reads=[d1, ge, lo], writes=[lo])
            kb.op("dve", lambda e: e.tensor_tensor(out=d1[:, :], in0=hi[:, :], in1=mid[:, :], op=ALU.subtract), reads=[hi, mid], writes=[d1])
            kb.op("dve", lambda e: e.scalar_tensor_tensor(out=hi[:, :], in0=d1[:, :], scalar=ge[:, 0:1], in1=mid[:, :], op0=ALU.mult, op1=ALU.add),
                  reads=[d1, ge, mid], writes=[hi])
        kb.op("dve", lambda e: e.scalar_tensor_tensor(out=W[:, :], in0=A[:, :], scalar=lo[:, 0:1], in1=key_c[0:P, :], op0=ALU.is_ge, op1=ALU.mult),
              reads=[A, lo, key_c], writes=[W])
        Lv = kb.sb(es, [P, cap], F32, "Lv" + tag)
        for r in range(cap // 8):
            kb.op("dve", lambda e, r=r: e.max(out=Lv[:, 8 * r:8 * r + 8], in_=W[:, :]), reads=[W], writes=[Lv])
            kb.op("dve", lambda e, r=r: e.match_replace(out=W[:, :], in_to_replace=Lv[:, 8 * r:8 * r + 8], in_values=W[:, :], imm_value=0.0),
                  reads=[W, Lv], writes=[W])
        tok = kb.sb(es, [P, cap], F32, "tok" + tag)
        vm = kb.sb(es, [P, cap], F32, "vm" + tag)
        idx = kb.sb(es, [P, cap], I32, "idx" + tag)
        kb.op("dve", lambda e: e.tensor_scalar(out=tok[:, :], in0=Lv[:, :], scalar1=-1.0, scalar2=base_c[0:P, 0:1], op0=ALU.mult, op1=ALU.add),
              reads=[Lv, base_c], writes=[tok])
        kb.op("dve", lambda e: e.tensor_tensor(out=tok[:, :], in0=tok[:, :], in1=dump_c[0:P, :], op=ALU.subtract), reads=[tok, dump_c], writes=[tok])
        kb.op("dve", lambda e: e.tensor_scalar(out=vm[:, :], in0=Lv[:, :], scalar1=0.5, scalar2=None, op0=ALU.is_ge), reads=[Lv], writes=[vm])
        kb.op("dve", lambda e: e.tensor_tensor(out=tok[:, :], in0=tok[:, :], in1=vm[:, :], op=ALU.mult), reads=[tok, vm], writes=[tok])
        kb.op("dve", lambda e: e.tensor_tensor(out=tok[:, :], in0=tok[:, :], in1=dump_c[0:P, :], op=ALU.add), reads=[tok, dump_c], writes=[tok])
        kb.op("dve", lambda e: e.tensor_copy(out=idx[:, :], in_=tok[:, :]), reads=[tok], writes=[idx])
        kb.dma("sp", out=idx_out_dram[:, :], in_=idx[:, :], reads=[idx], writes=[idx_out_dram])

    def ph_select(self, li):
        kb = self.kb
        I = self.I
        with contextlib.ExitStack() as es:
            cs = {}
            for k in ("gsum", "keyl", "keyc", "basel", "basec", "dumpl", "dumpc"):
                shp, ty = CONST_SPECS[k]
                cs[k] = kb.sb(es, shp, ty, k)
                kb.dma("sp", out=cs[k][:, :], in_=I[k][:, :], writes=[cs[k]])
            A = kb.sb(es, [PSEL, BLK], F32, "Asel")
            kb.dma("sp", out=A[:, :], in_=self.AFFT[:, :].rearrange("e (b t) -> (e b) t", b=NBLK), reads=[self.AFFT], writes=[A])
            self.select(es, A, PSEL, BLK, 1024, CAPB, cs["keyl"], cs["basel"], cs["dumpl"], cs["gsum"], self.IDXD, "l")
            Ac = kb.sb(es, [NE, LC], F32, "Aselc")
            kb.dma("sp", out=Ac[:, :], in_=self.AFFTC[:, :], reads=[self.AFFTC], writes=[Ac])
            self.select(es, Ac, NE, LC, CAPC, CAPC, cs["keyc"], cs["basec"], cs["dumpc"], None, self.IDXC, "c")

    def ph_moe(self, li):
        kb = self.kb
        I = self.I
        NS = NSLOT + CAPC
        chunks = []
        c0 = 0
        while c0 < NS:
            cn = min(512, NS - c0)
            chunks.append((c0, cn))
            c0 += cn
        NCH = len(chunks)
        with contextlib.ExitStack() as es:
            ga2 = [self.load_vec(es, s, 5, "ga2") for s in range(2)]
            idxt = kb.sb(es, [128, NE, NCALL], I32, "idxt")
            kb.dma("sp", out=idxt[:, :, :], in_=self.IDXD[:, :].rearrange("(e b) r -> e (b r)", b=NBLK).rearrange("e (j p) -> p e j", p=128),
                   reads=[self.IDXD], writes=[idxt], allow_slow_non_contiguous=True)
            idxc = kb.sb(es, [CAPC, NE], I32, "idxc")
            kb.dma("sp", out=idxc[:, :], in_=self.IDXC[:, :].rearrange("e p -> p e"), reads=[self.IDXC], writes=[idxc],
                   allow_slow_non_contiguous=True)
            stage = [kb.sb(es, [128, 4096], F32, "stage") for _ in range(2)]
            w13 = [[kb.sb(es, [128, 8, 512], BF16, "w13") for _ in range(2)] for _ in range(2)]
            w2 = kb.sb(es, [128, 16, D], BF16, "w2")
            w2k = [Trk() for _ in range(4)]
            xgT = kb.sb(es, [128, 8, NS], BF16, "xgT")
            gT = kb.sb(es, [128, 16, NS], BF16, "gT")
            gTk = [Trk() for _ in range(16)]
            G = [kb.sb(es, [128, D], BF16, "G") for _ in range(NCALL + 1)]
            gate = [kb.sb(es, [128, NCALL + 1, NE], F32, "gate") for _ in range(2)]
            gatek = [[Trk() for _ in range(NCALL + 1)] for _ in range(2)]
            yb = [kb.sb(es, [128, D], F32, "yb") for _ in range(2)]
            sa = [kb.sb(es, [128, 512], BF16, "sa") for _ in range(2)]
            pA = [kb.ps(es, [128, 512], F32, "pA") for _ in range(2)]
            pB = [kb.ps(es, [128, 512], F32, "pB") for _ in range(2)]
            pT = [kb.ps(es, [128, 1024], BF16, "pTm") for _ in range(2)]
            pY = [kb.ps(es, [128, 512], F32, "pY") for _ in range(2)]
            stage_i = [0]

            def load_cast(dst_ap, dst_trk, src_ap, shape3):
                st = stage[stage_i[0] % 2]
                a_, b_ = shape3
                sv = st[:, 0:a_ * b_].rearrange("p (a b) -> p a b", a=a_)
                kb.dma("sp", out=sv, in_=src_ap, writes=[st])
                if stage_i[0] % 2 == 0:
                    kb.op("dve", lambda e: e.tensor_copy(out=dst_ap, in_=sv), reads=[st], writes=[dst_trk])
                else:
                    kb.op("act", lambda e: e.copy(out=dst_ap, in_=sv), reads=[st], writes=[dst_trk])
                stage_i[0] += 1

            def calls_of(ex):
                return [(j, 128, idxt[:, ex, j:j + 1], j * 128) for j in range(NCALL)] + [(NCALL, CAPC, idxc[:, ex:ex + 1], NSLOT)]

            def issue_gathers(ex):
                gb = ex % 2
                for (j, rows, iap, col0) in calls_of(ex):
                    g = G[j]
                    kb.dma("pool", reads=[self.H2, idxt, idxc], writes=[g],
                           fn=lambda e, g=g, rows=rows, iap=iap: e.indirect_dma_start(out=g[0:rows, :], out_offset=None, in_=self.H2[:, :],
                                                                                      in_offset=bass.IndirectOffsetOnAxis(ap=iap, axis=0)))
                    kb.dma("pool", reads=[self.AFF, idxt, idxc], writes=[gatek[gb][j]],
                           fn=lambda e, gb=gb, j=j, rows=rows, iap=iap: e.indirect_dma_start(out=gate[gb][0:rows, j, :], out_offset=None, in_=self.AFF[:, :],
                                                                                           in_offset=bass.IndirectOffsetOnAxis(ap=iap, axis=0)))

            issue_gathers(0)
            tcount = 0
            for ex in range(NE):
                gb = ex % 2
                w1src = I["w_e1"][li, ex].rearrange("(kc p) f -> p kc f", p=128)
                w3src = I["w_e3"][li, ex].rearrange("(kc p) f -> p kc f", p=128)
                w2src = I["w_e2"][li, ex].rearrange("(fc p) d -> p fc d", p=128)
                calls = calls_of(ex)
                for (j, rows, iap, col0) in calls:
                    g = G[j]
                    p = pT[tcount % 2]
                    tcount += 1
                    for kc in range(8):
                        kb.op("pe", lambda e, p=p, g=g, kc=kc, rows=rows: e.transpose(out=p[:, kc * 128:kc * 128 + rows],
                                                                                    in_=g[0:rows, kc * 128:(kc + 1) * 128],
                                                                                    identity=self.ident_bf[0:rows, 0:rows]),
                              reads=[g, self.ident_bf], writes=[p])
                    ev = "dve" if tcount % 2 == 0 else "act"
                    if ev == "dve":
                        kb.op("dve", lambda e, p=p, rows=rows, col0=col0: e.tensor_copy(
                            out=xgT[:, :, col0:col0 + rows], in_=p[:, :].rearrange("p (k t) -> p k t", k=8)[:, :, 0:rows]),
                              reads=[p], writes=[xgT])
                    else:
                        kb.op("act", lambda e, p=p, rows=rows, col0=col0: e.copy(
                            out=xgT[:, :, col0:col0 + rows], in_=p[:, :].rearrange("p (k t) -> p k t", k=8)[:, :, 0:rows]),
                              reads=[p], writes=[xgT])
                if ex + 1 < NE:
                    issue_gathers(ex + 1)
                for q in range(4):
                    wb = w13[q % 2]
                    load_cast(wb[0][:, :, :], wb[0].k, w1src[:, :, q * 512:(q + 1) * 512], (8, 512))
                    load_cast(wb[1][:, :, :], wb[1].k, w3src[:, :, q * 512:(q + 1) * 512], (8, 512))
                    for f4 in range(4):
                        fc = q * 4 + f4
                        for ci, (c0, cn) in enumerate(chunks):
                            k = (fc * NCH + ci) % 2
                            for kc in range(8):
                                kb.op("pe", lambda e, k=k, kc=kc, f4=f4, c0=c0, cn=cn, wb=wb: e.matmul(
                                    pA[k][:, 0:cn], lhsT=wb[0][:, kc, f4 * 128:(f4 + 1) * 128], rhs=xgT[:, kc, c0:c0 + cn],
                                    start=(kc == 0), stop=(kc == 7)), reads=[wb[0], xgT], writes=[pA[k]])
                            for kc in range(8):
                                kb.op("pe", lambda e, k=k, kc=kc, f4=f4, c0=c0, cn=cn, wb=wb: e.matmul(
                                    pB[k][:, 0:cn], lhsT=wb[1][:, kc, f4 * 128:(f4 + 1) * 128], rhs=xgT[:, kc, c0:c0 + cn],
                                    start=(kc == 0), stop=(kc == 7)), reads=[wb[1], xgT], writes=[pB[k]])
                            kb.op("act", lambda e, k=k, cn=cn: e.activation(out=sa[k][:, 0:cn], in_=pA[k][:, 0:cn], func=AF.Silu),
                                  reads=[pA[k]], writes=[sa[k]])
                            kb.op("dve", lambda e, k=k, cn=cn, c0=c0, fc=fc: e.tensor_tensor(out=gT[:, fc, c0:c0 + cn], in0=sa[k][:, 0:cn],
                                                                                          in1=pB[k][:, 0:cn], op=ALU.mult),
                                  reads=[sa[k], pB[k]], writes=[gTk[fc]])
                for g4 in range(4):
                    load_cast(w2[:, g4 * 4:(g4 + 1) * 4, :], w2k[g4], w2src[:, g4 * 4:(g4 + 1) * 4, :], (4, D))
                for (j, rows, iap, col0) in calls:
                    s = 0 if j < NCALL else 1
                    y = yb[j % 2]
                    for h in range(2):
                        p = pY[h]
                        for fc in range(16):
                            kb.op("pe", lambda e, p=p, fc=fc, col0=col0, rows=rows, h=h: e.matmul(
                                p[0:rows, :], lhsT=gT[:, fc, col0:col0 + rows], rhs=w2[:, fc, h * 512:(h + 1) * 512],
                                start=(fc == 0), stop=(fc == 15)), reads=[gTk[fc], w2k[fc // 4]], writes=[p])
                        kb.op("dve", lambda e, p=p, y=y, h=h, rows=rows, gb=gb, j=j, s=s, ex=ex: e.scalar_tensor_tensor(
                            out=y[0:rows, h * 512:(h + 1) * 512], in0=p[0:rows, :], scalar=gate[gb][0:rows, j, ex:ex + 1],
                            in1=ga2[s][0:rows, h * 512:(h + 1) * 512], op0=ALU.mult, op1=ALU.mult),
                              reads=[p, gatek[gb][j], ga2[s]], writes=[y])
                    kb.dma("pool", reads=[y, idxt, idxc], writes=[self.X],
                           fn=lambda e, y=y, rows=rows, iap=iap: e.indirect_dma_start(out=self.X[:, :],
                                                                                      out_offset=bass.IndirectOffsetOnAxis(ap=iap, axis=0),
                                                                                      in_=y[0:rows, :], in_offset=None, compute_op=ALU.add))

    def load_w_bf16(self, es, dst, src3, ncols, stage):
        kb = self.kb
        i = 0
        for c0 in range(0, ncols, 512):
            cn = min(512, ncols - c0)
            st = stage[i % 2]
            sv = st[:, 0:8 * cn].rearrange("p (a b) -> p a b", a=8)
            kb.dma("sp", out=sv, in_=src3[:, :, c0:c0 + cn], writes=[st])
            if i % 2 == 0:
                kb.op("pool", lambda e, sv=sv, c0=c0, cn=cn: e.tensor_copy(out=dst[:, :, c0:c0 + cn], in_=sv), reads=[st], writes=[dst])
            else:
                kb.op("act", lambda e, sv=sv, c0=c0, cn=cn: e.copy(out=dst[:, :, c0:c0 + cn], in_=sv), reads=[st], writes=[dst])
            i += 1

    def transpose_tile(self, hb, hT, pT):
        kb = self.kb
        for kc in range(8):
            kb.op("pe", lambda e, kc=kc: e.transpose(out=pT[:, kc * 128:(kc + 1) * 128], in_=hb[:, kc * 128:(kc + 1) * 128],
                                                     identity=self.ident_bf[:, :]), reads=[hb, self.ident_bf], writes=[pT])
        kb.op("act", lambda e: e.copy(out=hT[:, :, :], in_=pT[:, :].rearrange("p (k t) -> p k t", k=8)), reads=[pT], writes=[hT])

    def ntiles(self, need_ctx):
        return NTILE if need_ctx else 64

    def ph_qkv(self, li):
        kb = self.kb
        I = self.I
        j = li // 2
        if not hasattr(self, "QKT"):
            self.QKT = kb.dram("QKT", [16, 128, NT], BF16)
            self.VD = kb.dram("VD", [NT + 64, 16 * 65], BF16)
            self.OD = kb.dram("OD", [NT, D], BF16)
        with contextlib.ExitStack() as es:
            svec = [self.load_vec(es, s, 1, "s1") for s in range(2)]
            shvec = [self.load_vec(es, s, 0, "sh1") for s in range(2)]
            negh = kb.sb(es, [128, 1], F32, "negh")
            kb.op("dve", lambda e: e.memset(negh[:, :], -0.5), writes=[negh])
            stage = [kb.sb(es, [128, 4096], F32, "stage") for _ in range(2)]
            W = kb.sb(es, [128, 8, 3 * D], BF16, "wqkv")
            self.load_w_bf16(es, W, I["w_qkv"][j].rearrange("(kc p) n -> p kc n", p=128), 3 * D, stage)
            NB = 5
            xt = [kb.sb(es, [128, D], F32, "xt") for _ in range(NB)]
            hf = [kb.sb(es, [128, D], F32, "hf") for _ in range(NB)]
            hb = [kb.sb(es, [128, D], BF16, "hb") for _ in range(NB)]
            junk = kb.sb(es, [128, D], BF16, "junk")
            hT = [kb.sb(es, [128, 8, 128], BF16, "hT") for _ in range(NB)]
            sm = [[kb.sb(es, [128, 1], F32, "sm") for _ in range(2)] for _ in range(NB)]
            qk = [kb.sb(es, [128, 16, 128], BF16, "qk") for _ in range(2)]
            vt = [kb.sb(es, [128, 16, 65], BF16, "vt") for _ in range(2)]
            for b in range(2):
                kb.op("dve", lambda e, b=b: e.memset(vt[b][:, :, :], 1.0), writes=[vt[b]])
            pT = [kb.ps(es, [128, 1024], BF16, "pT") for _ in range(2)]
            pq = [kb.ps(es, [128, 512], F32, "pq") for _ in range(6)]

            def s0(ti):
                b = ti % NB
                r0 = ti * 128
                kb.dma("sp", out=xt[b][:, :], in_=self.X[r0:r0 + 128, :], reads=[self.X], writes=[xt[b]])

            def s1(ti):
                b = ti % NB
                s = 0 if ti < 64 else 1
                self.norm_tile((sm[b][0], sm[b][1]), xt[b], svec[s], shvec[s], hf[b], hb[b], junk, negh)

            def s2(ti):
                b = ti % NB
                self.transpose_tile(hb[b], hT[b], pT[ti % 2])

            def s3(ti):
                b = ti % NB
                b2 = ti % 2
                r0 = ti * 128
                for c4 in range(4):
                    p = pq[(6 * ti + c4) % 6]
                    for cc in range(4):
                        c = c4 * 4 + cc
                        for kc in range(8):
                            kb.op("pe", lambda e, p=p, cc=cc, c=c, kc=kc, b=b: e.matmul(p[:, cc * 128:(cc + 1) * 128], lhsT=W[:, kc, c * 128:(c + 1) * 128],
                                                                                     rhs=hT[b][:, kc, :], start=(kc == 0), stop=(kc == 7)),
                                  reads=[W, hT[b]], writes=[p])
                    sc = 0.125 if c4 < 2 else 1.0
                    kb.op("act", lambda e, p=p, c4=c4, b2=b2, sc=sc: e.activation(out=qk[b2][:, c4 * 4:c4 * 4 + 4, :], in_=p[:, :].rearrange("p (c t) -> p c t", c=4),
                                                                               func=AF.Copy, scale=sc), reads=[p], writes=[qk[b2]])
                kb.dma("sp", out=self.QKT[:, :, r0:r0 + 128].rearrange("c p t -> p c t"), in_=qk[b2][:, :, :], reads=[qk[b2]], writes=[self.QKT])
                for h2 in range(2):
                    p = pq[(6 * ti + 4 + h2) % 6]
                    for kc in range(8):
                        kb.op("pe", lambda e, p=p, kc=kc, h2=h2, b=b: e.matmul(p[:, :], lhsT=hT[b][:, kc, :], rhs=W[:, kc, 2 * D + h2 * 512:2 * D + (h2 + 1) * 512],
                                                                            start=(kc == 0), stop=(kc == 7)), reads=[W, hT[b]], writes=[p])
                    kb.op("dve", lambda e, p=p, h2=h2, b2=b2: e.tensor_copy(out=vt[b2][:, h2 * 8:(h2 + 1) * 8, 0:64], in_=p[:, :].rearrange("p (h d) -> p h d", h=8)),
                          reads=[p], writes=[vt[b2]])
                kb.dma("sp", out=self.VD[r0:r0 + 128, :], in_=vt[b2][:, :, :].rearrange("p h d -> p (h d)"), reads=[vt[b2]], writes=[self.VD])

            pipeline(NTILE, [s0, s1, s2, s3])

    def ph_attn(self, li, need_ctx):
        kb = self.kb
        I = self.I
        j = li // 2
        with contextlib.ExitStack() as es:
            KT = kb.sb(es, [128, NT], BF16, "KT")
            QT = kb.sb(es, [128, NT], BF16, "QT")
            V0 = kb.sb(es, [128, NTILE, 130], BF16, "V0")
            bst = [kb.sb(es, [128, 2, 5, 128], F32, "bst") for _ in range(2)]
            EB = [kb.sb(es, [128, 2, 5, 128], BF16, "EB") for _ in range(5)]
            NP = 4
            PT = [kb.sb(es, [128, 7, 128], BF16, "PT") for _ in range(NP)]
            osb = [kb.sb(es, [128, 128], BF16, "osb") for _ in range(3)]
            rec = [kb.sb(es, [128, 1], F32, "rec") for _ in range(4)]
            pSa = [kb.ps(es, [128, 512], F32, "pSa") for _ in range(2)]
            pSb = [kb.ps(es, [128, 512], F32, "pSb") for _ in range(2)]
            pO = [kb.ps(es, [128, 65], F32, "pO") for _ in range(3)]
            VDv = self.VD[0:NT, :].rearrange("(t p) c -> p t c", p=128)
            for hp in range(8):
                kb.dma("sp", out=QT[:, :], in_=self.QKT[hp, :, :], reads=[self.QKT], writes=[QT])
                kb.dma("sp", out=KT[:, :], in_=self.QKT[8 + hp, :, :], reads=[self.QKT], writes=[KT])
                kb.dma("sp", out=V0[:, :, :], in_=VDv[:, :, hp * 130:(hp + 1) * 130], reads=[self.VD], writes=[V0])
                for v in range(5):
                    st = bst[v % 2]
                    kb.dma("sp", out=st[:, :, :, :], in_=I["biasT"][j, v, 2 * hp:2 * hp + 2].rearrange("h p c q -> p h c q"), writes=[st])
                    kb.op("act", lambda e, st=st, v=v: e.activation(out=EB[v][:, :, :, :], in_=st[:, :, :, :], func=AF.Exp), reads=[st], writes=[EB[v]])
                units = []
                for rp in range(64):
                    for hl in range(2):
                        units.append((rp, hl))
                if need_ctx:
                    for cq in range(2):
                        for hl in range(2):
                            units.append((64 + cq, hl))

                def info(u):
                    rp, hl = units[u]
                    if rp < 64:
                        t0 = att_base(rp) // 2
                        tiles = [t0 + c for c in range(5)] + [64, 65]
                        q0 = 128 * rp
                        eb = EB[att_variant(rp)]
                    else:
                        tiles = [64, 65]
                        q0 = L + 128 * (rp - 64)
                        eb = None
                    return rp, hl, tiles, q0, eb

                def sA(u):
                    rp, hl, tiles, q0, eb = info(u)
                    ho = hl * 64
                    pa_, pb_ = pSa[u % 2], pSb[u % 2]
                    for c, t in enumerate(tiles):
                        dst = pa_[:, c * 128:(c + 1) * 128] if c < 4 else pb_[:, (c - 4) * 128:(c - 3) * 128]
                        trk = pa_ if c < 4 else pb_
                        kb.op("pe", lambda e, dst=dst, ho=ho, t=t, q0=q0: e.matmul(dst, lhsT=KT[ho:ho + 64, t * 128:(t + 1) * 128],
                                                                                rhs=QT[ho:ho + 64, q0:q0 + 128], start=True, stop=True),
                              reads=[KT, QT], writes=[trk])

                def sB(u):
                    rp, hl, tiles, q0, eb = info(u)
                    pa_, pb_ = pSa[u % 2], pSb[u % 2]
                    pt = PT[u % NP]
                    n = len(tiles)
                    na = min(n, 4)
                    kb.op("act", lambda e, pt=pt, pa_=pa_, na=na: e.activation(out=pt[:, 0:na, :].rearrange("p c q -> p (c q)"), in_=pa_[:, 0:na * 128], func=AF.Exp),
                          reads=[pa_], writes=[pt])
                    if n > 4:
                        kb.op("act", lambda e, pt=pt, pb_=pb_, n=n: e.activation(out=pt[:, 4:n, :].rearrange("p c q -> p (c q)"), in_=pb_[:, 0:(n - 4) * 128], func=AF.Exp),
                              reads=[pb_], writes=[pt])
                    if eb is not None:
                        me = "pool" if (u % 5) < 3 else "dve"
                        kb.op(me, lambda e, pt=pt, eb=eb, hl=hl: e.tensor_tensor(out=pt[:, 0:5, :], in0=pt[:, 0:5, :], in1=eb[:, hl, :, :], op=ALU.mult),
                              reads=[pt, eb], writes=[pt])

                def sC(u):
                    rp, hl, tiles, q0, eb = info(u)
                    pt = PT[u % NP]
                    po = pO[u % 3]
                    ob = osb[(u // 2) % 3]
                    n = len(tiles)
                    for c, t in enumerate(tiles):
                        kb.op("pe", lambda e, po=po, c=c, t=t, pt=pt, hl=hl, n=n: e.matmul(po[:, :], lhsT=pt[:, c, :], rhs=V0[:, t, hl * 65:(hl + 1) * 65],
                                                                                        start=(c == 0), stop=(c == n - 1)), reads=[pt, V0], writes=[po])
                    rc = rec[u % 4]
                    kb.op("dve", lambda e, po=po, rc=rc: e.reciprocal(out=rc[:, 0:1], in_=po[:, 64:65]), reads=[po], writes=[rc])
                    kb.op("dve", lambda e, po=po, rc=rc, hl=hl, ob=ob: e.tensor_scalar(out=ob[:, hl * 64:(hl + 1) * 64], in0=po[:, 0:64],
                                                                                      scalar1=rc[:, 0:1], scalar2=None, op0=ALU.mult),
                          reads=[po, rc], writes=[ob])
                    if hl == 1:
                        kb.dma("sp", out=self.OD[q0:q0 + 128, hp * 128:(hp + 1) * 128], in_=ob[:, :], reads=[ob], writes=[self.OD])

                pipeline(len(units), [sA, sB, sC])

    def ph_oproj(self, li, wname, srcname, need_ctx):
        kb = self.kb
        I = self.I
        j = li // 2
        SRC = getattr(self, srcname)
        with contextlib.ExitStack() as es:
            ga = [self.load_vec(es, s, 2, "ga1") for s in range(2)]
            stage = [kb.sb(es, [128, 4096], F32, "stage") for _ in range(2)]
            W = kb.sb(es, [128, 8, D], BF16, "wo")
            self.load_w_bf16(es, W, I[wname][j].rearrange("(kc p) n -> p kc n", p=128), D, stage)
            NB = 5
            xt = [kb.sb(es, [128, D], F32, "xt") for _ in range(NB)]
            hb = [kb.sb(es, [128, D], BF16, "hb") for _ in range(NB)]
            tmp = [kb.sb(es, [128, D], F32, "tmp") for _ in range(2)]
            hT = [kb.sb(es, [128, 8, 128], BF16, "hT") for _ in range(NB)]
            pT = [kb.ps(es, [128, 1024], BF16, "pT") for _ in range(2)]
            pq = [kb.ps(es, [128, 512], F32, "pq") for _ in range(4)]

            def s0(ti):
                b = ti % NB
                r0 = ti * 128
                kb.dma("sp", out=xt[b][:, :], in_=self.X[r0:r0 + 128, :], reads=[self.X], writes=[xt[b]])
                kb.dma("sp", out=hb[b][:, :], in_=SRC[r0:r0 + 128, :], reads=[SRC], writes=[hb[b]])

            def s1(ti):
                b = ti % NB
                self.transpose_tile(hb[b], hT[b], pT[ti % 2])

            def s2(ti):
                b = ti % NB
                b2 = ti % 2
                s = 0 if ti < 64 else 1
                r0 = ti * 128
                for h2 in range(2):
                    p = pq[(2 * ti + h2) % 4]
                    for kc in range(8):
                        kb.op("pe", lambda e, p=p, kc=kc, h2=h2, b=b: e.matmul(p[:, :], lhsT=hT[b][:, kc, :], rhs=W[:, kc, h2 * 512:(h2 + 1) * 512],
                                                                            start=(kc == 0), stop=(kc == 7)), reads=[W, hT[b]], writes=[p])
                    kb.op("dve", lambda e, p=p, h2=h2, b2=b2, s=s: e.tensor_tensor(out=tmp[b2][:, h2 * 512:(h2 + 1) * 512], in0=p[:, :],
                                                                                in1=ga[s][:, h2 * 512:(h2 + 1) * 512], op=ALU.mult),
                          reads=[p, ga[s]], writes=[tmp[b2]])
                kb.op("pool", lambda e, b=b, b2=b2: e.tensor_tensor(out=xt[b][:, :], in0=xt[b][:, :], in1=tmp[b2][:, :], op=ALU.add),
                      reads=[xt[b], tmp[b2]], writes=[xt[b]])
                kb.dma("sp", out=self.X[r0:r0 + 128, :], in_=xt[b][:, :], reads=[xt[b]], writes=[self.X])

            pipeline(self.ntiles(need_ctx), [s0, s1, s2])

    def cload(self, es, name):
        shp, ty = CONST_SPECS[name]
        t = self.kb.sb(es, shp, ty, name)
        sl = tuple(slice(None) for _ in shp)
        self.kb.dma("sp", out=t[sl], in_=self.I[name][sl], writes=[t])
        return t

    def even_scratch(self):
        kb = self.kb
        if not hasattr(self, "PR"):
            self.PR = kb.dram("PR", [NT, 1536], BF16)
            self.DTR = kb.dram("DTR", [NT, 16], F32)
            self.AD = kb.dram("AD", [NT, 1024], BF16)
            self.XBC = kb.dram("XBC", [NT, 1024], BF16)
            self.DTA = kb.dram("DTA", [NT, 32], F32)
            self.YF = kb.dram("YF", [NT, 512], F32)
            self.MIX = kb.dram("MIX", [NT, 1024], BF16)
            self.GD = kb.dram("GD", [2, 64, 128, 512], BF16)

    def ph_inproj(self, li):
        kb = self.kb
        I = self.I
        j = li // 2
        self.even_scratch()
        with contextlib.ExitStack() as es:
            svec = [self.load_vec(es, s, 1, "s1") for s in range(2)]
            shvec = [self.load_vec(es, s, 0, "sh1") for s in range(2)]
            negh = kb.sb(es, [128, 1], F32, "negh")
            kb.op("dve", lambda e: e.memset(negh[:, :], -0.5), writes=[negh])
            stage = [kb.sb(es, [128, 4096], F32, "stage") for _ in range(2)]
            W = kb.sb(es, [128, 8, 2064], BF16, "win")
            self.load_w_bf16(es, W, I["w_in_e"][j].rearrange("(kc p) n -> p kc n", p=128), 2064, stage)
            CS = self.cload(es, "CS")
            NB = 5
            xt = [kb.sb(es, [128, D], F32, "xt") for _ in range(NB)]
            hf = [kb.sb(es, [128, D], F32, "hf") for _ in range(NB)]
            hb = [kb.sb(es, [128, D], BF16, "hb") for _ in range(NB)]
            junk = kb.sb(es, [128, D], BF16, "junk")
            hT = [kb.sb(es, [128, 8, 128], BF16, "hT") for _ in range(NB)]
            sm = [[kb.sb(es, [128, 1], F32, "sm") for _ in range(2)] for _ in range(NB)]
            prt = [kb.sb(es, [128, 1536], BF16, "prt") for _ in range(2)]
            dtr = [kb.sb(es, [128, 16], F32, "dtr") for _ in range(2)]
            uT = [kb.sb(es, [128, 4, 128], BF16, "uT") for _ in range(2)]
            At = [kb.sb(es, [128, 2, 4, 128], BF16, "At") for _ in range(2)]
            pT = [kb.ps(es, [128, 1024], BF16, "pT") for _ in range(2)]
            pq = [kb.ps(es, [128, 512], F32, "pq") for _ in range(4)]
            pa = [kb.ps(es, [128, 512], F32, "pa") for _ in range(2)]

            def s0(ti):
                b = ti % NB
                r0 = ti * 128
                kb.dma("sp", out=xt[b][:, :], in_=self.X[r0:r0 + 128, :], reads=[self.X], writes=[xt[b]])

            def s1(ti):
                b = ti % NB
                s = 0 if ti < 64 else 1
                self.norm_tile((sm[b][0], sm[b][1]), xt[b], svec[s], shvec[s], hf[b], hb[b], junk, negh)

            def s2(ti):
                b = ti % NB
                self.transpose_tile(hb[b], hT[b], pT[ti % 2])

            def s3(ti):
                b = ti % NB
                b2 = ti % 2
                r0 = ti * 128
                for ci, (c0, cn) in enumerate([(0, 512), (512, 512), (1024, 512), (1536, 16)]):
                    p = pq[(5 * ti + ci) % 4]
                    for kc in range(8):
                        kb.op("pe", lambda e, p=p, kc=kc, c0=c0, cn=cn, b=b: e.matmul(p[:, 0:cn], lhsT=hT[b][:, kc, :], rhs=W[:, kc, c0:c0 + cn],
                                                                                   start=(kc == 0), stop=(kc == 7)), reads=[W, hT[b]], writes=[p])
                    if ci < 3:
                        if ci % 2 == 0:
                            kb.op("act", lambda e, p=p, c0=c0, b2=b2: e.copy(out=prt[b2][:, c0:c0 + 512], in_=p[:, :]), reads=[p], writes=[prt[b2]])
                        else:
                            kb.op("dve", lambda e, p=p, c0=c0, b2=b2: e.tensor_copy(out=prt[b2][:, c0:c0 + 512], in_=p[:, :]), reads=[p], writes=[prt[b2]])
                    else:
                        kb.op("dve", lambda e, p=p, b2=b2: e.tensor_copy(out=dtr[b2][:, :], in_=p[:, 0:16]), reads=[p], writes=[dtr[b2]])
                kb.dma("sp", out=self.PR[r0:r0 + 128, :], in_=prt[b2][:, :], reads=[prt[b2]], writes=[self.PR])
                kb.dma("sp", out=self.DTR[r0:r0 + 128, :], in_=dtr[b2][:, :], reads=[dtr[b2]], writes=[self.DTR])
                p = pq[(5 * ti + 4) % 4]
                for g in range(4):
                    for kc in range(8):
                        kb.op("pe", lambda e, p=p, kc=kc, g=g, b=b: e.matmul(p[:, g * 128:(g + 1) * 128], lhsT=W[:, kc, 1552 + g * 128:1552 + (g + 1) * 128],
                                                                          rhs=hT[b][:, kc, :], start=(kc == 0), stop=(kc == 7)), reads=[W, hT[b]], writes=[p])
                kb.op("act", lambda e, p=p, b2=b2: e.copy(out=uT[b2][:, :, :], in_=p[:, :].rearrange("p (g t) -> p g t", g=4)), reads=[p], writes=[uT[b2]])
                for gh in range(2):
                    for g2 in range(2):
                        g = gh * 2 + g2
                        kb.op("pe", lambda e, gh=gh, g2=g2, g=g, b2=b2: e.matmul(pa[gh][:, g2 * 256:(g2 + 1) * 256], lhsT=uT[b2][:, g, :], rhs=CS[:, :],
                                                                              start=True, stop=True), reads=[uT[b2], CS], writes=[pa[gh]])
                    kb.op("dve", lambda e, gh=gh, b2=b2: e.tensor_copy(out=At[b2][:, :, 2 * gh:2 * gh + 2, :],
                                                                     in_=pa[gh][:, :].rearrange("p (g cs q) -> p cs g q", g=2, cs=2)),
                          reads=[pa[gh]], writes=[At[b2]])
                kb.dma("sp", out=self.AD[r0:r0 + 128, :], in_=At[b2][:, :, :, :].rearrange("p cs g q -> p (cs g q)"), reads=[At[b2]], writes=[self.AD])

            pipeline(NTILE, [s0, s1, s2, s3])

    def ph_conv(self, li):
        kb = self.kb
        I = self.I
        j = li // 2
        with contextlib.ExitStack() as es:
            cw = [kb.sb(es, [128, D], F32, "cw") for _ in range(3)]
            for k in range(3):
                kb.dma("sp", out=cw[k][:, :], in_=I["conv_w"][j, k, :].partition_broadcast(128), writes=[cw[k]])
            cb = kb.sb(es, [128, D], F32, "cb")
            kb.dma("sp", out=cb[:, :], in_=I["conv_b"][j, :].partition_broadcast(128), writes=[cb])
            dtb = kb.sb(es, [128, 16], F32, "dtb")
            kb.dma("sp", out=dtb[:, :], in_=I["dt_bias"][j, :].partition_broadcast(128), writes=[dtb])
            abc = kb.sb(es, [128, 16], F32, "abc")
            kb.dma("sp", out=abc[:, :], in_=I["a_log"][j, :].partition_broadcast(128), writes=[abc])
            kb.op("act", lambda e: e.activation(out=abc[:, :], in_=abc[:, :], func=AF.Exp), reads=[abc], writes=[abc])
            kb.op("dve", lambda e: e.tensor_scalar(out=abc[:, :], in0=abc[:, :], scalar1=-1.0, scalar2=None, op0=ALU.mult), reads=[abc], writes=[abc])
            NB = 4
            m1 = [kb.sb(es, [128, D], BF16, "m1") for _ in range(NB)]
            c0 = [kb.sb(es, [128, D], BF16, "c0") for _ in range(NB)]
            p1 = [kb.sb(es, [128, D], BF16, "p1") for _ in range(NB)]
            acc = [kb.sb(es, [128, D], F32, "acc") for _ in range(NB)]
            t2 = [kb.sb(es, [128, D], F32, "t2") for _ in range(NB)]
            t3 = [kb.sb(es, [128, D], F32, "t3") for _ in range(NB)]
            xo = [kb.sb(es, [128, D], BF16, "xo") for _ in range(NB)]
            dtr = [kb.sb(es, [128, 16], F32, "dtr") for _ in range(NB)]
            dta = [kb.sb(es, [128, 32], F32, "dta") for _ in range(NB)]
            src = self.PR

            def s0(ti):
                b = ti % NB
                r0 = ti * 128
                first = ti in (0, 64)
                last = ti in (63, 65)
                if first:
                    kb.op("dve", lambda e, b=b: e.memset(m1[b][:, :], 0.0), writes=[m1[b]])
                    kb.dma("sp", out=m1[b][1:128, :], in_=src[r0:r0 + 127, 512:1536], reads=[src], writes=[m1[b]])
                else:
                    kb.dma("sp", out=m1[b][:, :], in_=src[r0 - 1:r0 + 127, 512:1536], reads=[src], writes=[m1[b]])
                kb.dma("sp", out=c0[b][:, :], in_=src[r0:r0 + 128, 512:1536], reads=[src], writes=[c0[b]])
                if last:
                    kb.op("dve", lambda e, b=b: e.memset(p1[b][:, :], 0.0), writes=[p1[b]])
                    kb.dma("sp", out=p1[b][0:127, :], in_=src[r0 + 1:r0 + 128, 512:1536], reads=[src], writes=[p1[b]])
                else:
                    kb.dma("sp", out=p1[b][:, :], in_=src[r0 + 1:r0 + 129, 512:1536], reads=[src], writes=[p1[b]])
                kb.dma("sp", out=dtr[b][:, :], in_=self.DTR[r0:r0 + 128, :], reads=[self.DTR], writes=[dtr[b]])

            def s1(ti):
                b = ti % NB
                kb.op("dve", lambda e, b=b: e.tensor_tensor(out=acc[b][:, :], in0=m1[b][:, :], in1=cw[0][:, :], op=ALU.mult), reads=[m1[b], cw[0]], writes=[acc[b]])
                kb.op("pool", lambda e, b=b: e.tensor_tensor(out=t2[b][:, :], in0=c0[b][:, :], in1=cw[1][:, :], op=ALU.mult), reads=[c0[b], cw[1]], writes=[t2[b]])
                kb.op("pool", lambda e, b=b: e.tensor_tensor(out=t3[b][:, :], in0=p1[b][:, :], in1=cw[2][:, :], op=ALU.mult), reads=[p1[b], cw[2]], writes=[t3[b]])
                kb.op("dve", lambda e, b=b: e.tensor_tensor(out=dtr[b][:, :], in0=dtr[b][:, :], in1=dtb[:, :], op=ALU.add), reads=[dtr[b], dtb], writes=[dtr[b]])

            def s2(ti):
                b = ti % NB
                kb.op("dve", lambda e, b=b: e.tensor_tensor(out=acc[b][:, :], in0=acc[b][:, :], in1=t2[b][:, :], op=ALU.add), reads=[acc[b], t2[b]], writes=[acc[b]])
                kb.op("pool", lambda e, b=b: e.tensor_tensor(out=t3[b][:, :], in0=t3[b][:, :], in1=cb[:, :], op=ALU.add), reads=[t3[b], cb], writes=[t3[b]])
                kb.op("act", lambda e, b=b: e.activation(out=dtr[b][:, :], in_=dtr[b][:, :], func=AF.Exp), reads=[dtr[b]], writes=[dtr[b]])

            def s3(ti):
                b = ti % NB
                kb.op("dve", lambda e, b=b: e.tensor_tensor(out=acc[b][:, :], in0=acc[b][:, :], in1=t3[b][:, :], op=ALU.add), reads=[acc[b], t3[b]], writes=[acc[b]])
                kb.op("act", lambda e, b=b: e.activation(out=dta[b][:, 0:16], in_=dtr[b][:, :], func=AF.Ln, bias=1.0), reads=[dtr[b]], writes=[dta[b]])

            def s4(ti):
                b = ti % NB
                r0 = ti * 128
                kb.op("act", lambda e, b=b: e.activation(out=xo[b][:, :], in_=acc[b][:, :], func=AF.Silu), reads=[acc[b]], writes=[xo[b]])
                kb.dma("sp", out=self.XBC[r0:r0 + 128, :], in_=xo[b][:, :], reads=[xo[b]], writes=[self.XBC])
                kb.op("dve", lambda e, b=b: e.tensor_tensor(out=dta[b][:, 16:32], in0=dta[b][:, 0:16], in1=abc[:, :], op=ALU.mult), reads=[dta[b], abc], writes=[dta[b]])
                kb.dma("sp", out=self.DTA[r0:r0 + 128, :], in_=dta[b][:, :], reads=[dta[b]], writes=[self.DTA])

            pipeline(NTILE, [s0, s1, s2, s3, s4])

    def ph_ssd(self, li, d):
        kb = self.kb
        I = self.I
        j = li // 2
        with contextlib.ExitStack() as es:
            tri = self.cload(es, "triU" if d == 0 else "triL")
            ones = self.cload(es, "onesf")
            neg = self.cload(es, "negU" if d == 0 else "negL")
            dsk = kb.sb(es, [128, 8], F32, "dsk")
            kb.dma("sp", out=dsk[:, :], in_=I["d_skip"][j, :].partition_broadcast(128), writes=[dsk])
            gss = kb.sb(es, [128, 512], F32, "gss")
            kb.dma("sp", out=gss[:, :], in_=I["g_ssd"][j, :].partition_broadcast(128), writes=[gss])
            negh = kb.sb(es, [128, 1], F32, "negh")
            kb.op("dve", lambda e: e.memset(negh[:, :], -0.5), writes=[negh])
            hs = kb.sb(es, [128, 8, 64], F32, "hs")
            hsb = kb.sb(es, [128, 8, 64], BF16, "hsb")
            kb.op("dve", lambda e: e.memset(hs[:, :, :], 0.0), writes=[hs])
            kb.op("dve", lambda e: e.memset(hsb[:, :, :], 0.0), writes=[hsb])
            NB = 4
            R = lambda shape, ty, nm, n=NB: [kb.sb(es, shape, ty, nm) for _ in range(n)]
            xbc = R([128, D], BF16, "xbc")
            dta = R([128, 32], F32, "dta")
            BCT = R([128, 4, 128], BF16, "BCT")
            cum = R([128, 16], F32, "cum")
            te = R([128, 8], F32, "te")
            cd = R([128, 8], F32, "cd")
            xdt = R([128, 8, 64], BF16, "xdt")
            xw = R([128, 8, 64], BF16, "xw")
            CBT = R([128, 2, 128], BF16, "CBT", 2)
            dU = R([128, 8, 128], F32, "dU", 2)
            Ebc = R([128, 8, 128], BF16, "Ebc", 2)
            CsT = R([128, 8, 128], BF16, "CsT")
            Sg = R([128, 8, 128], F32, "Sg", 2)
            Dm = R([128, 8, 128], BF16, "Dm", 2)
            MT = R([128, 8, 128], BF16, "MT")
            tmp = R([128, 8, 64], F32, "tmp", 2)
            yo = R([128, 512], F32, "yo")
            yf = R([128, 512], F32, "yf")
            zt = R([128, 512], BF16, "zt")
            sz = R([128, 512], F32, "sz", 2)
            junk = kb.sb(es, [128, 256], BF16, "junk")
            ssq = R([128, 2], F32, "ssq")
            rsq = R([128, 2], F32, "rsq")
            mo = R([128, 512], BF16, "mo", 2)
            pT = kb.ps(es, [128, 1024], BF16, "pT")
            pc = kb.ps(es, [128, 16], F32, "pc")
            pcb = kb.ps(es, [128, 256], F32, "pcb")
            pA = [kb.ps(es, [128, 512], F32, "pA") for _ in range(2)]
            py = [kb.ps(es, [128, 512], F32, "py") for _ in range(2)]
            pst = kb.ps(es, [128, 512], F32, "pst")
            order = ([64, 65] + list(range(64))) if d == 0 else ([65, 64] + list(range(63, -1, -1)))

            def views(n):
                b = n % NB
                X_, DT_ = xbc[b], dta[b]
                return (b, n % 2, order[n] * 128, X_, DT_, DT_[:, 8 * d:8 * d + 8], DT_[:, 16 + 8 * d:16 + 8 * d + 8],
                        X_[:, 0:512].rearrange("p (h c) -> p h c", h=8))

            def s0(n):
                b, b2, r0, X_, DT_, dt_d, da_d, x3 = views(n)
                kb.dma("sp", out=X_[:, :], in_=self.XBC[r0:r0 + 128, :], reads=[self.XBC], writes=[X_])
                kb.dma("sp", out=DT_[:, :], in_=self.DTA[r0:r0 + 128, :], reads=[self.DTA], writes=[DT_])
                if d == 1:
                    kb.dma("sp", out=yf[b][:, :], in_=self.YF[r0:r0 + 128, :], reads=[self.YF], writes=[yf[b]])
                    kb.dma("sp", out=zt[b][:, :], in_=self.PR[r0:r0 + 128, 0:512], reads=[self.PR], writes=[zt[b]])

            def s1(n):
                b, b2, r0, X_, DT_, dt_d, da_d, x3 = views(n)
                for q in range(4):
                    kb.op("pe", lambda e, q=q, X_=X_: e.transpose(out=pT[:, q * 128:(q + 1) * 128], in_=X_[:, 512 + q * 128:512 + (q + 1) * 128],
                                                               identity=self.ident_bf[:, :]), reads=[X_, self.ident_bf], writes=[pT])
                kb.op("act", lambda e, b=b: e.copy(out=BCT[b][:, :, :], in_=pT[:, 0:512].rearrange("p (q t) -> p q t", q=4)), reads=[pT], writes=[BCT[b]])
                kb.op("pe", lambda e, da_d=da_d: e.matmul(pc[:, 0:8], lhsT=tri[:, :], rhs=da_d, start=True, stop=True), reads=[tri, DT_], writes=[pc])
                kb.op("pe", lambda e, da_d=da_d: e.matmul(pc[:, 8:16], lhsT=ones[:, :], rhs=da_d, start=True, stop=True), reads=[ones, DT_], writes=[pc])
                kb.op("dve", lambda e, b=b, da_d=da_d: e.tensor_tensor(out=dU[b2][:, :, :], in0=tri[:, :].unsqueeze(1).to_broadcast([128, 8, 128]),
                                                                     in1=da_d.unsqueeze(2).to_broadcast([128, 8, 128]), op=ALU.mult),
                      reads=[tri, DT_], writes=[dU[b2]])
                kb.op("dve", lambda e, b=b: e.tensor_copy(out=cum[b][:, :], in_=pc[:, :]), reads=[pc], writes=[cum[b]])
                for hh in range(2):
                    kb.op("pe", lambda e, hh=hh, b2=b2: e.matmul(pA[hh][:, :], lhsT=ones[:, :], rhs=dU[b2][:, 4 * hh:4 * hh + 4, :].rearrange("p h i -> p (h i)"),
                                                               start=True, stop=True), reads=[ones, dU[b2]], writes=[pA[hh]])
                for g in range(2):
                    kb.op("pe", lambda e, g=g, b=b: e.matmul(pcb[:, g * 128:(g + 1) * 128], lhsT=BCT[b][:, g, :], rhs=BCT[b][:, 2 + g, :], start=True, stop=True),
                          reads=[BCT[b]], writes=[pcb])
                kb.op("dve", lambda e, b=b: e.tensor_tensor(out=te[b][:, :], in0=cum[b][:, 8:16], in1=cum[b][:, 0:8], op=ALU.subtract), reads=[cum[b]], writes=[te[b]])
                kb.op("pool", lambda e, b=b, x3=x3, dt_d=dt_d: e.tensor_tensor(out=xdt[b][:, :, :], in0=x3, in1=dt_d.unsqueeze(2).to_broadcast([128, 8, 64]), op=ALU.mult),
                      reads=[X_, DT_], writes=[xdt[b]])
                for hh in range(2):
                    kb.op("act", lambda e, hh=hh, b2=b2: e.activation(out=Ebc[b2][:, 4 * hh:4 * hh + 4, :].rearrange("p h i -> p (h i)"), in_=pA[hh][:, :], func=AF.Exp),
                          reads=[pA[hh]], writes=[Ebc[b2]])
                kb.op("act", lambda e, b=b: e.activation(out=te[b][:, :], in_=te[b][:, :], func=AF.Exp), reads=[te[b]], writes=[te[b]])
                kb.op("act", lambda e, b=b: e.activation(out=cd[b][:, :], in_=cum[b][:, 8:16], func=AF.Exp), reads=[cum[b]], writes=[cd[b]])
                for h in range(8):
                    kb.op("dve", lambda e, h=h, b=b, b2=b2: e.scalar_tensor_tensor(out=Sg[b2][:, h, :], in0=pA[h // 4][:, (h % 4) * 128:(h % 4 + 1) * 128],
                                                                                scalar=cum[b][:, h:h + 1], in1=neg[:, :], op0=ALU.subtract, op1=ALU.add),
                          reads=[pA[h // 4], cum[b], neg], writes=[Sg[b2]])
                kb.op("act", lambda e, b2=b2: e.copy(out=CBT[b2][:, :, :], in_=pcb[:, :].rearrange("p (g t) -> p g t", g=2)), reads=[pcb], writes=[CBT[b2]])
                kb.op("act", lambda e, b2=b2: e.activation(out=Dm[b2][:, :, :], in_=Sg[b2][:, :, :], func=AF.Exp), reads=[Sg[b2]], writes=[Dm[b2]])
                kb.op("pool", lambda e, b=b: e.tensor_tensor(out=xw[b][:, :, :], in0=xdt[b][:, :, :], in1=te[b][:, :].unsqueeze(2).to_broadcast([128, 8, 64]), op=ALU.mult),
                      reads=[xdt[b], te[b]], writes=[xw[b]])
                for g in range(2):
                    kb.op("pool", lambda e, g=g, b=b, b2=b2: e.tensor_tensor(out=CsT[b][:, 4 * g:4 * g + 4, :], in0=Ebc[b2][:, 4 * g:4 * g + 4, :],
                                                                           in1=BCT[b][:, 2 + g, :].unsqueeze(1).to_broadcast([128, 4, 128]), op=ALU.mult),
                          reads=[Ebc[b2], BCT[b]], writes=[CsT[b]])
                for g in range(2):
                    kb.op("dve", lambda e, g=g, b=b, b2=b2: e.tensor_tensor(out=MT[b][:, 4 * g:4 * g + 4, :], in0=Dm[b2][:, 4 * g:4 * g + 4, :],
                                                                          in1=CBT[b2][:, g, :].unsqueeze(1).to_broadcast([128, 4, 128]), op=ALU.mult),
                          reads=[Dm[b2], CBT[b2]], writes=[MT[b]])

            def s2(n):
                b, b2, r0, X_, DT_, dt_d, da_d, x3 = views(n)
                p_y = py[n % 2]
                for h in range(8):
                    kb.op("pe", lambda e, h=h, b=b: e.matmul(p_y[:, h * 64:(h + 1) * 64], lhsT=MT[b][:, h, :], rhs=xdt[b][:, h, :], start=(h == 0), stop=False,
                                                           skip_group_check=True), reads=[MT[b], xdt[b]], writes=[p_y])
                for h in range(8):
                    kb.op("pe", lambda e, h=h, b=b: e.matmul(p_y[:, h * 64:(h + 1) * 64], lhsT=CsT[b][:, h, :], rhs=hsb[:, h, :], start=False, stop=(h == 7),
                                                           skip_group_check=True), reads=[CsT[b], hsb], writes=[p_y])
                for g in range(2):
                    kb.op("pe", lambda e, g=g, b=b, X_=X_: e.matmul(pst[:, g * 256:(g + 1) * 256], lhsT=X_[:, 512 + g * 128:512 + (g + 1) * 128],
                                                                 rhs=xw[b][:, 4 * g:4 * g + 4, :].rearrange("p h c -> p (h c)"), start=True, stop=True),
                          reads=[X_, xw[b]], writes=[pst])
                kb.op("dve", lambda e, b=b, b2=b2: e.tensor_tensor(out=tmp[b2][:, :, :], in0=hs[:, :, :], in1=cd[b][:, :].unsqueeze(2).to_broadcast([128, 8, 64]), op=ALU.mult),
                      reads=[hs, cd[b]], writes=[tmp[b2]])
                kb.op("dve", lambda e, b2=b2: e.tensor_tensor(out=hs[:, :, :], in0=tmp[b2][:, :, :], in1=pst[:, :].rearrange("p (h c) -> p h c", h=8), op=ALU.add),
                      reads=[tmp[b2], pst], writes=[hs])
                kb.op("act", lambda e: e.copy(out=hsb[:, :, :], in_=hs[:, :, :]), reads=[hs], writes=[hsb])

            def s3(n):
                b, b2, r0, X_, DT_, dt_d, da_d, x3 = views(n)
                p_y = py[n % 2]
                if d == 0:
                    kb.op("pool", lambda e, b=b, x3=x3: e.tensor_tensor(out=yo[b][:, :].rearrange("p (h c) -> p h c", h=8), in0=x3,
                                                                      in1=dsk[:, :].unsqueeze(2).to_broadcast([128, 8, 64]), op=ALU.mult),
                          reads=[X_, dsk], writes=[yo[b]])
                    kb.op("dve", lambda e, b=b: e.tensor_tensor(out=yo[b][:, :], in0=p_y[:, :], in1=yo[b][:, :], op=ALU.add), reads=[p_y, yo[b]], writes=[yo[b]])
                    kb.dma("sp", out=self.YF[r0:r0 + 128, :], in_=yo[b][:, :], reads=[yo[b]], writes=[self.YF])
                else:
                    kb.op("dve", lambda e, b=b: e.tensor_tensor(out=yo[b][:, :], in0=p_y[:, :], in1=yf[b][:, :], op=ALU.add), reads=[p_y, yf[b]], writes=[yo[b]])
                    kb.op("act", lambda e, b=b, b2=b2: e.activation(out=sz[b2][:, :], in_=zt[b][:, :], func=AF.Silu), reads=[zt[b]], writes=[sz[b2]])
                    kb.op("pool", lambda e, b=b, b2=b2: e.tensor_tensor(out=yo[b][:, :], in0=yo[b][:, :], in1=sz[b2][:, :], op=ALU.mult), reads=[yo[b], sz[b2]], writes=[yo[b]])
                    for g in range(2):
                        kb.op("act", lambda e, b=b, g=g: e.activation(out=junk[:, :], in_=yo[b][:, g * 256:(g + 1) * 256], func=AF.Square, accum_out=ssq[b][:, g:g + 1]),
                              reads=[yo[b]], writes=[junk, ssq[b]])
                    kb.op("dve", lambda e, b=b: e.tensor_scalar(out=ssq[b][:, :], in0=ssq[b][:, :], scalar1=1.0 / 256, scalar2=EPS, op0=ALU.mult, op1=ALU.add),
                          reads=[ssq[b]], writes=[ssq[b]])
                    kb.op("pool", lambda e, b=b: e.tensor_tensor(out=rsq[b][:, :], in0=ssq[b][:, :], in1=negh[:, 0:1].to_broadcast([128, 2]), op=ALU.pow),
                          reads=[ssq[b], negh], writes=[rsq[b]])
                    for g in range(2):
                        kb.op("dve", lambda e, b=b, g=g, b2=b2: e.scalar_tensor_tensor(out=mo[b2][:, g * 256:(g + 1) * 256], in0=yo[b][:, g * 256:(g + 1) * 256],
                                                                                     scalar=rsq[b][:, g:g + 1], in1=gss[:, g * 256:(g + 1) * 256], op0=ALU.mult, op1=ALU.mult),
                              reads=[yo[b], rsq[b], gss], writes=[mo[b2]])
                    kb.dma("sp", out=self.MIX[r0:r0 + 128, 0:512], in_=mo[b2][:, :], reads=[mo[b2]], writes=[self.MIX])

            pipeline(len(order), [s0, s1, s2, s3])

    def ph_fourier(self, li):
        kb = self.kb
        I = self.I
        SC = 1.0 / np.sqrt(8192.0 * 128.0)
        SCC = 1.0 / np.sqrt(256.0 * 128.0)
        with contextlib.ExitStack() as es:
            F1 = self.cload(es, "F1")
            blk = [kb.sb(es, [128, 16, 1024], BF16, "blk") for _ in range(2)]
            gt = [kb.sb(es, [128, 2, 512], BF16, "gt") for _ in range(3)]
            pg = [kb.ps(es, [128, 512], F32, "pg") for _ in range(4)]
            ADv = self.AD[0:L, :].rearrange("(n1 n2) c -> n1 n2 c", n2=64)
            for rd in range(4):
                bk = blk[rd % 2]
                kb.dma("sp", out=bk[:, :, :], in_=ADv[:, rd * 16:(rd + 1) * 16, :], reads=[self.AD], writes=[bk])
                for nl in range(16):
                    n2 = rd * 16 + nl
                    g = gt[n2 % 3]
                    pr_, pi_ = pg[(2 * n2) % 4], pg[(2 * n2 + 1) % 4]
                    Ac = bk[:, nl, 0:512]
                    As = bk[:, nl, 512:1024]
                    kb.op("pe", lambda e, pr_=pr_, Ac=Ac: e.matmul(pr_[:, :], lhsT=F1[:, 0, :], rhs=Ac, start=True, stop=False), reads=[F1, bk], writes=[pr_])
                    kb.op("pe", lambda e, pr_=pr_, As=As: e.matmul(pr_[:, :], lhsT=F1[:, 1, :], rhs=As, start=False, stop=True), reads=[F1, bk], writes=[pr_])
                    kb.op("pe", lambda e, pi_=pi_, Ac=Ac: e.matmul(pi_[:, :], lhsT=F1[:, 1, :], rhs=Ac, start=True, stop=False), reads=[F1, bk], writes=[pi_])
                    kb.op("pe", lambda e, pi_=pi_, As=As: e.matmul(pi_[:, :], lhsT=F1[:, 2, :], rhs=As, start=False, stop=True), reads=[F1, bk], writes=[pi_])
                    kb.op("act", lambda e, g=g, pr_=pr_: e.copy(out=g[:, 0, :], in_=pr_[:, :]), reads=[pr_], writes=[g])
                    kb.op("dve", lambda e, g=g, pi_=pi_: e.tensor_copy(out=g[:, 1, :], in_=pi_[:, :]), reads=[pi_], writes=[g])
                    kb.dma("sp", out=self.GD[:, n2, :, :].rearrange("ri p c -> p ri c"), in_=g[:, :, :], reads=[g], writes=[self.GD])
        kb.barrier()
        with contextlib.ExitStack() as es:
            TW = self.cload(es, "TW3")
            blk = [kb.sb(es, [128, 32, 512], BF16, "blk3") for _ in range(2)]
            ot = [kb.sb(es, [64, 32, 512], BF16, "ot") for _ in range(2)]
            pg = [kb.ps(es, [64, 512], F32, "pg3") for _ in range(4)]
            GDv = self.GD[:, :, :, :].rearrange("ri n2 p c -> (ri n2) p c")
            MIXv = self.MIX[0:L, :].rearrange("(p2 p1) c -> p2 p1 c", p1=128)
            for rd in range(4):
                bk = blk[rd % 2]
                o = ot[rd % 2]
                kb.dma("sp", out=bk[:, :, :], in_=GDv[:, rd * 32:(rd + 1) * 32, :], reads=[self.GD], writes=[bk])
                for pl in range(32):
                    p1 = rd * 32 + pl
                    p = pg[p1 % 4]
                    kb.op("pe", lambda e, p=p, p1=p1, pl=pl, bk=bk: e.matmul(p[:, :], lhsT=TW[:, p1, :], rhs=bk[:, pl, :], start=True, stop=True), reads=[TW, bk], writes=[p])
                    if pl % 2 == 0:
                        kb.op("act", lambda e, p=p, pl=pl, o=o: e.activation(out=o[:, pl, :], in_=p[:, :], func=AF.Copy, scale=float(SC)), reads=[p], writes=[o])
                    else:
                        kb.op("dve", lambda e, p=p, pl=pl, o=o: e.tensor_scalar(out=o[:, pl, :], in0=p[:, :], scalar1=float(SC), scalar2=None, op0=ALU.mult), reads=[p], writes=[o])
                kb.dma("sp", out=MIXv[:, rd * 32:(rd + 1) * 32, 512:1024], in_=o[:, :, :], reads=[o], writes=[self.MIX])
            C2 = self.cload(es, "C256")
            S2 = self.cload(es, "nS256")
            ac = kb.sb(es, [128, 2, 1024], BF16, "actx")
            kb.dma("sp", out=ac[:, :, :], in_=self.AD[L:NT, :].rearrange("(t p) c -> p t c", p=128), reads=[self.AD], writes=[ac])
            oc = kb.sb(es, [128, 2, 512], BF16, "octx")
            pcx = [kb.ps(es, [128, 512], F32, "pcx") for _ in range(2)]
            for pt in range(2):
                p = pcx[pt]
                k = 0
                for nt in range(2):
                    for (M, off) in ((C2, 0), (S2, 512)):
                        kb.op("pe", lambda e, p=p, M=M, nt=nt, pt=pt, off=off, k=k: e.matmul(p[:, :], lhsT=M[:, nt, pt * 128:(pt + 1) * 128], rhs=ac[:, nt, off:off + 512],
                                                                                          start=(k == 0), stop=(k == 3)), reads=[M, ac], writes=[p])
                        k += 1
                kb.op("act", lambda e, p=p, pt=pt: e.activation(out=oc[:, pt, :], in_=p[:, :], func=AF.Copy, scale=float(SCC)), reads=[p], writes=[oc])
            kb.dma("sp", out=self.MIX[L:NT, 512:1024].rearrange("(t p) c -> p t c", p=128), in_=oc[:, :, :], reads=[oc], writes=[self.MIX])

    def ph_final(self):
        kb = self.kb
        I = self.I
        with contextlib.ExitStack() as es:
            gf = kb.sb(es, [128, D], F32, "gf")
            kb.dma("sp", out=gf[:, :], in_=I["g_final"].partition_broadcast(128), writes=[gf])
            zero = kb.sb(es, [128, D], F32, "zero")
            kb.op("dve", lambda e: e.memset(zero[:, :], 0.0), writes=[zero])
            negh = kb.sb(es, [128, 1], F32, "negh")
            kb.op("dve", lambda e: e.memset(negh[:, :], -0.5), writes=[negh])
            junk = kb.sb(es, [128, D], BF16, "junk")
            NB = 4
            xt = [kb.sb(es, [128, D], F32, "xt") for _ in range(NB)]
            of = [kb.sb(es, [128, D], F32, "of") for _ in range(NB)]
            sm = [[kb.sb(es, [128, 1], F32, "sm") for _ in range(2)] for _ in range(NB)]

            def s0(ti):
                b = ti % NB
                r0 = ti * 128
                kb.dma("sp", out=xt[b][:, :], in_=self.X[r0:r0 + 128, :], reads=[self.X], writes=[xt[b]])

            def s1(ti):
                b = ti % NB
                r0 = ti * 128
                self.norm_tile((sm[b][0], sm[b][1]), xt[b], gf, zero, of[b], None, junk, negh)
                kb.dma("sp", out=self.out[r0:r0 + 128, :], in_=of[b][:, :], reads=[of[b]], writes=[self.outk])

            pipeline(64, [s0, s1])

    def ph_dumpx(self):
        kb = self.kb
        for r0 in range(0, L, 2048):
            kb.dma("sp", out=self.out[r0:r0 + 2048, :], in_=self.X[r0:r0 + 2048, :], reads=[self.X], writes=[self.outk])


def default_phases():
    ph = [("init",)]
    for li in range(DEPTH):
        need_ctx = li < DEPTH - 1
        ph += [("mod", li)]
        if li % 2 == 0:
            ph += [("inproj", li), ("conv", li), ("ssd", li, 0), ("ssd", li, 1), ("fourier", li),
                   ("oproj", li, "w_out_e", "MIX", need_ctx)]
        else:
            ph += [("qkv", li), ("attn", li, need_ctx), ("oproj", li, "w_o", "OD", need_ctx)]
        ph += [("router", li), ("select", li), ("moe", li)]
    ph += [("final",)]
    return ph


ATT_VARIANT_RP = [2, 0, 1, 62, 63]


def att_base(rp):
    r0 = 2 * rp
    return min(min(max(r0 - 4, 0), 120), 118)


def att_variant(rp):
    return 0 if 2 <= rp <= 61 else {0: 1, 1: 2, 62: 3, 63: 4}[rp]


def build_bias_table(rpb):
    no = rpb.shape[0]
    out = np.empty((no, 5, 16, 128, 5, 128), np.float32)
    p = np.arange(128)
    q = np.arange(128)
    c = np.arange(5)
    for v, rp in enumerate(ATT_VARIANT_RP):
        r0 = 2 * rp
        base = att_base(rp)
        krow = base + 2 * c[:, None, None] + (p // 64)[None, :, None]
        kcol = (p % 64)[None, :, None]
        r = (r0 + q // 64)[None, None, :]
        col = (q % 64)[None, None, :]
        rs = np.clip(r - 4, 0, 120)
        cs = np.clip(col - 8, 0, 48)
        valid = (krow >= rs) & (krow < rs + 8) & (kcol >= cs) & (kcol < cs + 16)
        ro = np.clip(krow - r + 7, 0, 14) + 0 * kcol
        co = np.clip(kcol - col + 15, 0, 30) + 0 * krow
        g = rpb[:, :, ro, co]
        g = np.where(valid[None, None], g, np.float32(-30000.0))
        out[:, v] = np.transpose(g, (0, 1, 3, 2, 4))
    return out


def make_in_maps(inputs, n_cores, names):
    consts = host_consts()
    shared = {}
    for k in names:
        if k in consts:
            shared[k] = consts[k]
        elif k in ("x", "ctx", "c"):
            pass
        elif k == "biasT":
            shared[k] = build_bias_table(inputs["rpb"])
        elif k in ("a_log", "dt_bias"):
            shared[k] = np.ascontiguousarray(inputs[k].reshape(2, 16))
        else:
            shared[k] = np.ascontiguousarray(inputs[k])
    maps = []
    for b in range(n_cores):
        m = dict(shared)
        for k in ("x", "ctx", "c"):
            if k in names:
                m[k] = np.ascontiguousarray(inputs[k][b])
        maps.append(m)
    return maps


def kernel(**inputs):
    inputs = {k: np.asarray(v) for k, v in inputs.items()}
    prog = Prog(default_phases())
    in_maps = make_in_maps(inputs, 4, list(prog.I.keys()))
    res = run_bass_kernel_spmd(prog.nc, in_maps, core_ids=list(range(4)))
    out = np.stack([res.results[b]["out"] for b in range(4)], axis=0)
    return out.astype(np.float32)
```

```python
import contextlib
import numpy as np
import ml_dtypes
import concourse.bass as bass
import concourse.mybir as mybir
from concourse.bass_utils import run_bass_kernel_spmd

F32 = mybir.dt.float32
BF16 = mybir.dt.bfloat16
I32 = mybir.dt.int32
AF = mybir.ActivationFunctionType
ALU = mybir.AluOpType
AX = mybir.AxisListType
IOA = bass.IndirectOffsetOnAxis if hasattr(bass, "IndirectOffsetOnAxis") else None

D = 1024
L = 8192
LC = 256
NT = L + LC
NTILE = NT // 128
DEPTH = 4
NE = 16
FF = 2048
NBLK = 4
BLK = L // NBLK
PSEL = NE * NBLK
CAPB = 320
NSLOT = NBLK * CAPB
NCALL = NSLOT // 128
CAPC = 32
XR = NT + (NCALL + 1) * 128
EPS = 1e-6


class Trk:
    __slots__ = ("w", "r", "x")

    def __init__(self, x=False):
        self.w = None
        self.r = {}
        self.x = x


class Buf:
    def __init__(self, t, x=False):
        self.t = t
        self.k = Trk(x)

    def __getitem__(self, key):
        return self.t[key]


class KB:
    ND = 40

    def __init__(self, nc):
        self.nc = nc
        self.es = contextlib.ExitStack()
        self.eng = {"pe": nc.tensor, "act": nc.scalar, "dve": nc.vector, "pool": nc.gpsimd, "sp": nc.sync}
        self.csem = {}
        self.ccnt = {}
        for e in ("pe", "act", "dve", "pool"):
            self.csem[e] = self.es.enter_context(nc.semaphore("cs_" + e))
            self.ccnt[e] = 0
        self.dsem = [self.es.enter_context(nc.semaphore("ds%d" % i)) for i in range(self.ND)]
        self.dcnt = [0] * self.ND
        self.dnext = 0
        self.seen = {e: {} for e in self.eng}
        self.uid = 0

    def sb(self, es, shape, dtype, name=None):
        self.uid += 1
        return Buf(es.enter_context(self.nc.sbuf_tensor("%s_%d" % (name or "sb", self.uid), list(shape), dtype)))

    def ps(self, es, shape, dtype, name=None):
        self.uid += 1
        return Buf(es.enter_context(self.nc.psum_tensor("%s_%d" % (name or "ps", self.uid), list(shape), dtype)), x=True)

    def dram(self, name, shape, dtype):
        return Buf(self.nc.dram_tensor(name, list(shape), dtype, kind="Internal").ap())

    def _sem(self, ch):
        return self.csem[ch] if isinstance(ch, str) else self.dsem[ch]

    def _deps(self, eng, reads, writes, is_dma):
        deps = {}

        def add(ev, raw):
            if ev is None:
                return
            ch, v = ev
            if (not is_dma) and ch == eng:
                if eng == "pe" or not raw:
                    return
            if deps.get(ch, 0) < v:
                deps[ch] = v

        for t in reads:
            add(t.w, True)
            if t.x:
                for ch, v in t.r.items():
                    if ch != eng:
                        add((ch, v), False)
        for t in writes:
            add(t.w, False)
            for ch, v in t.r.items():
                add((ch, v), False)
        return deps

    def _wait(self, eng, deps):
        s = self.seen[eng]
        for ch, v in deps.items():
            if s.get(ch, 0) < v:
                self.eng[eng].wait_ge(self._sem(ch), v)
                s[ch] = v

    @staticmethod
    def _trks(lst):
        return [b.k if isinstance(b, Buf) else b for b in lst]

    def op(self, eng, fn, reads=(), writes=()):
        reads = self._trks(reads)
        writes = self._trks(writes)
        self._wait(eng, self._deps(eng, reads, writes, False))
        ins = fn(self.eng[eng])
        self.ccnt[eng] += 1
        v = self.ccnt[eng]
        ins.then_inc(self.csem[eng], 1)
        for t in reads:
            if t.r.get(eng, 0) < v:
                t.r[eng] = v
        for t in writes:
            t.w = (eng, v)
            t.r = {}
        return ins

    def dma(self, q, out=None, in_=None, reads=(), writes=(), fn=None, **kw):
        reads = self._trks(reads)
        writes = self._trks(writes)
        s = self.dnext
        self.dnext = (s + 1) % self.ND
        deps = self._deps(q, reads, writes, True)
        if self.dcnt[s] > 0 and deps.get(s, 0) < self.dcnt[s]:
            deps[s] = self.dcnt[s]
        self._wait(q, deps)
        if fn is None:
            ins = self.eng[q].dma_start(out=out, in_=in_, **kw)
        else:
            ins = fn(self.eng[q])
        self.dcnt[s] += 16
        v = self.dcnt[s]
        ins.then_inc(self.dsem[s], 16)
        for t in reads:
            if t.r.get(s, 0) < v:
                t.r[s] = v
        for t in writes:
            t.w = (s, v)
            t.r = {}
        return ins

    def barrier(self):
        for e in self.eng:
            deps = {}
            for c in self.csem:
                if self.ccnt[c] > 0 and c != e:
                    deps[c] = self.ccnt[c]
            for s in range(self.ND):
                if self.dcnt[s] > 0:
                    deps[s] = self.dcnt[s]
            self._wait(e, deps)
        for e in self.csem:
            if self.ccnt[e] > 0:
                s = self.seen[e]
                if s.get(e, 0) < self.ccnt[e]:
                    self.eng[e].wait_ge(self.csem[e], self.ccnt[e])
                    s[e] = self.ccnt[e]


def host_consts():
    c = {}
    c["ident_bf"] = np.eye(128, dtype=np.float32).astype(ml_dtypes.bfloat16)
    c["ident_f"] = np.eye(128, dtype=np.float32)
    p = np.arange(128)
    c["gsum"] = (p[:, None] // NBLK == p[None, :] // NBLK).astype(np.float32)
    c["keyl"] = np.broadcast_to((BLK - np.arange(BLK, dtype=np.float32))[None, :], (128, BLK)).copy()
    c["keyc"] = np.broadcast_to((256 - np.arange(256, dtype=np.float32))[None, :], (128, 256)).copy()
    c["basel"] = ((p % NBLK) * BLK + BLK).astype(np.float32)[:, None].copy()
    c["basec"] = np.full((128, 1), L + 256, np.float32)
    sig = (p[:, None] % NBLK) * CAPB + np.arange(CAPB)[None, :]
    c["dumpl"] = (NT + sig).astype(np.float32)
    c["dumpc"] = np.broadcast_to((NT + NSLOT + np.arange(CAPC, dtype=np.float32))[None, :], (128, CAPC)).copy()
    k = np.arange(128)
    c["triU"] = (k[:, None] <= k[None, :]).astype(np.float32)
    c["triL"] = (k[:, None] >= k[None, :]).astype(np.float32)
    c["onesf"] = np.ones((128, 128), np.float32)
    c["negU"] = np.where(k[None, :] >= k[:, None], 0.0, -30000.0).astype(np.float32)
    c["negL"] = np.where(k[None, :] <= k[:, None], 0.0, -30000.0).astype(np.float32)
    ang = 2 * np.pi * np.outer(k, k) / 128.0
    bf = ml_dtypes.bfloat16
    c["CS"] = np.concatenate([np.cos(ang), np.sin(ang)], axis=1).astype(np.float32).astype(bf)
    c["F1"] = np.stack([np.cos(ang), -np.sin(ang), -np.cos(ang)], axis=1).astype(np.float32).astype(bf)
    n2 = np.arange(64)[:, None, None]
    p1 = np.arange(128)[None, :, None]
    p2 = np.arange(64)[None, None, :]
    th = 2 * np.pi * (n2 * p2 / 64.0 + n2 * p1 / 8192.0)
    c["TW3"] = np.concatenate([np.cos(th), np.sin(th)], axis=0).astype(np.float32).astype(bf)
    n = np.arange(256)
    a2 = 2 * np.pi * np.outer(n, n) / 256.0
    c["C256"] = np.cos(a2).reshape(2, 128, 256).transpose(1, 0, 2).astype(np.float32).astype(bf)
    c["nS256"] = (-np.sin(a2)).reshape(2, 128, 256).transpose(1, 0, 2).astype(np.float32).astype(bf)
    return c


CONST_SPECS = {
    "triU": ([128, 128], F32), "triL": ([128, 128], F32), "onesf": ([128, 128], F32), "negU": ([128, 128], F32),
    "negL": ([128, 128], F32), "CS": ([128, 256], BF16), "F1": ([128, 3, 128], BF16), "TW3": ([128, 128, 64], BF16),
    "C256": ([128, 2, 256], BF16), "nS256": ([128, 2, 256], BF16),
    "ident_bf": ([128, 128], BF16), "ident_f": ([128, 128], F32), "gsum": ([128, 128], F32),
    "keyl": ([128, BLK], F32), "keyc": ([128, 256], F32), "basel": ([128, 1], F32), "basec": ([128, 1], F32),
    "dumpl": ([128, CAPB], F32), "dumpc": ([128, CAPC], F32),
}


IN_SPECS = {
    "x": ([L, D], F32), "ctx": ([LC, D], F32), "c": ([D], F32), "c_ctx": ([D], F32),
    "w_mod": ([DEPTH, D, 6 * D], F32), "b_mod": ([DEPTH, 6 * D], F32), "g_mix": ([DEPTH, D], F32), "g_ffn": ([DEPTH, D], F32),
    "w_router": ([DEPTH, D, NE], F32), "w_e1": ([DEPTH, NE, D, FF], F32), "w_e3": ([DEPTH, NE, D, FF], F32),
    "w_e2": ([DEPTH, NE, FF, D], F32), "g_final": ([D], F32),
    "w_in_e": ([2, D, 2064], F32), "conv_w": ([2, 3, D], F32), "conv_b": ([2, D], F32), "a_log": ([2, 16], F32),
    "dt_bias": ([2, 16], F32), "d_skip": ([2, 8], F32), "g_ssd": ([2, 512], F32), "w_out_e": ([2, D, D], F32),
    "w_qkv": ([2, D, 3 * D], F32), "w_o": ([2, D, D], F32), "biasT": ([2, 5, 16, 128, 5, 128], F32),
}


def pipeline(n, stages):
    ns = len(stages)
    for step in range(n + ns - 1):
        for si in range(ns - 1, -1, -1):
            k = step - si
            if 0 <= k < n:
                stages[si](k)


class Prog:
    def __init__(self, phases, debug_out=None):
        self.phases = phases
        self.debug_out = debug_out
        nc = bass.Bass("TRN2", target_bir_lowering=False)
        self.nc = nc
        self.kb = KB(nc)
        kb = self.kb
        dt = nc.dram_tensor

        class LazyIn(dict):
            def __missing__(d, k):
                shp, ty = IN_SPECS[k] if k in IN_SPECS else CONST_SPECS[k]
                d[k] = dt(k, list(shp), ty, kind="ExternalInput").ap()
                return d[k]
        self.I = LazyIn()
        self.out = dt("out", [L, D], F32, kind="ExternalOutput").ap()
        self.X = kb.dram("Xres", [XR, D], F32)
        self.H2 = kb.dram("H2", [XR, D], BF16)
        self.AFF = kb.dram("AFF", [XR, NE], F32)
        self.AFFT = kb.dram("AFFT", [NE, L], F32)
        self.AFFTC = kb.dram("AFFTC", [NE, LC], F32)
        self.IDXD = kb.dram("IDXD", [PSEL, CAPB], I32)
        self.IDXC = kb.dram("IDXC", [NE, CAPC], I32)
        self.MODD = kb.dram("MODD", [2, 6 * D], F32)
        self.outk = Trk()
        self.build()

    def build(self):
        kb = self.kb
        with contextlib.ExitStack() as ges:
            self.ges = ges
            self.ident_bf = kb.sb(ges, [128, 128], BF16, "identbf")
            self.ident_f = kb.sb(ges, [128, 128], F32, "identf")
            kb.dma("sp", out=self.ident_bf[:, :], in_=self.I["ident_bf"][:, :], writes=[self.ident_bf])
            kb.dma("sp", out=self.ident_f[:, :], in_=self.I["ident_f"][:, :], writes=[self.ident_f])
            for ph in self.phases:
                name = ph[0]
                getattr(self, "ph_" + name)(*ph[1:])
                kb.barrier()
            kb.barrier()
        kb.es.close()

    def ph_init(self):
        kb = self.kb
        with contextlib.ExitStack() as es:
            for r0 in range(0, L, 2048):
                kb.dma("sp", out=self.X[r0:r0 + 2048, :], in_=self.I["x"][r0:r0 + 2048, :], writes=[self.X])
            kb.dma("sp", out=self.X[L:NT, :], in_=self.I["ctx"][:, :], writes=[self.X])
            z = kb.sb(es, [128, D], F32, "z")
            zb = kb.sb(es, [128, D], BF16, "zb")
            kb.op("dve", lambda e: e.memset(z[:, :], 0.0), writes=[z])
            kb.op("dve", lambda e: e.memset(zb[:, :], 0.0), writes=[zb])
            for j in range(NCALL + 1):
                r0 = NT + j * 128
                kb.dma("sp", out=self.X[r0:r0 + 128, :], in_=z[:, :], reads=[z], writes=[self.X])
                kb.dma("sp", out=self.H2[r0:r0 + 128, :], in_=zb[:, :], reads=[zb], writes=[self.H2])
                kb.dma("sp", out=self.AFF[r0:r0 + 128, :], in_=z[:, 0:NE], reads=[z], writes=[self.AFF])

    def ph_mod(self, li):
        kb = self.kb
        I = self.I
        with contextlib.ExitStack() as es:
            cv = kb.sb(es, [128, 2, 8], F32, "cv")
            kb.dma("sp", out=cv[:, 0, :], in_=I["c"].rearrange("(kc p) -> p kc", p=128), writes=[cv],
                   allow_slow_non_contiguous=True)
            kb.dma("sp", out=cv[:, 1, :], in_=I["c_ctx"].rearrange("(kc p) -> p kc", p=128), writes=[cv],
                   allow_slow_non_contiguous=True)
            sv = kb.sb(es, [128, 2, 8], F32, "sv")
            kb.op("act", lambda e: e.activation(out=sv[:, :, :], in_=cv[:, :, :], func=AF.Silu), reads=[cv], writes=[sv])
            lb = kb.sb(es, [128, 2, 8, 128], BF16, "lb")
            for s in range(2):
                kb.op("dve", lambda e, s=s: e.tensor_copy(out=lb[:, s, :, :], in_=sv[:, s, :].unsqueeze(2).to_broadcast([128, 8, 128])),
                      reads=[sv], writes=[lb])
            gmix = kb.sb(es, [128, D], F32, "gmix")
            gffn = kb.sb(es, [128, D], F32, "gffn")
            kb.dma("sp", out=gmix[:, :], in_=I["g_mix"][li, :].partition_broadcast(128), writes=[gmix])
            kb.dma("sp", out=gffn[:, :], in_=I["g_ffn"][li, :].partition_broadcast(128), writes=[gffn])
            NB = 3
            wm = [kb.sb(es, [128, 8, 512], F32, "wm") for _ in range(NB)]
            wmb = [kb.sb(es, [128, 8, 512], BF16, "wmb") for _ in range(NB)]
            bm = [kb.sb(es, [128, 512], F32, "bm") for _ in range(NB)]
            pp = [kb.ps(es, [128, 512], F32, "pm") for _ in range(4)]
            res = [kb.sb(es, [128, 512], F32, "res") for _ in range(4)]
            wsrc = I["w_mod"][li].rearrange("(kc p) n -> p kc n", p=128)

            def s0(n):
                w = wm[n % NB]
                b = bm[n % NB]
                kb.dma("sp", out=w[:, :, :], in_=wsrc[:, :, n * 512:(n + 1) * 512], writes=[w])
                kb.dma("sp", out=b[:, :], in_=I["b_mod"][li, n * 512:(n + 1) * 512].partition_broadcast(128), writes=[b])

            def s1(n):
                w = wm[n % NB]
                wb_ = wmb[n % NB]
                if n % 2 == 0:
                    kb.op("pool", lambda e: e.tensor_copy(out=wb_[:, :, :], in_=w[:, :, :]), reads=[w], writes=[wb_])
                else:
                    kb.op("act", lambda e: e.copy(out=wb_[:, :, :], in_=w[:, :, :]), reads=[w], writes=[wb_])

            def s2(n):
                w = wmb[n % NB]
                b = bm[n % NB]
                for s in range(2):
                    p = pp[(2 * n + s) % 4]
                    r = res[(2 * n + s) % 4]
                    for kc in range(8):
                        kb.op("pe", lambda e, kc=kc, s=s, p=p, w=w: e.matmul(p[:, :], lhsT=lb[:, s, kc, :], rhs=w[:, kc, :],
                                                                          start=(kc == 0), stop=(kc == 7)),
                              reads=[lb, w], writes=[p])
                    which = n // 2
                    kb.op("dve", lambda e, p=p, r=r, b=b: e.tensor_tensor(out=r[:, :], in0=p[:, :], in1=b[:, :], op=ALU.add),
                          reads=[p, b], writes=[r])
                    if which in (1, 4):
                        g = gmix if which == 1 else gffn
                        c0 = (n % 2) * 512
                        kb.op("dve", lambda e, r=r, g=g, c0=c0: e.scalar_tensor_tensor(out=r[:, :], in0=r[:, :], scalar=1.0,
                                                                                     in1=g[:, c0:c0 + 512], op0=ALU.add, op1=ALU.mult),
                              reads=[r, g], writes=[r])
                    kb.dma("sp", out=self.MODD[s:s + 1, n * 512:(n + 1) * 512], in_=r[0:1, :], reads=[r], writes=[self.MODD])

            pipeline(12, [s0, s1, s2])

    def load_vec(self, es, s, which, name="vec"):
        kb = self.kb
        t = kb.sb(es, [128, D], F32, name)
        kb.dma("sp", out=t[:, :], in_=self.MODD[s, which * D:(which + 1) * D].partition_broadcast(128),
               reads=[self.MODD], writes=[t])
        return t

    def norm_tile(self, rstd_tmp, xt, s_bc, sh_bc, out_f32, out_bf, junk, negh):
        kb = self.kb
        ss, rs = rstd_tmp
        kb.op("act", lambda e: e.activation(out=junk[:, :], in_=xt[:, :], func=AF.Square, accum_out=ss[:, 0:1]),
              reads=[xt], writes=[junk, ss])
        kb.op("dve", lambda e: e.tensor_scalar(out=ss[:, 0:1], in0=ss[:, 0:1], scalar1=1.0 / D, scalar2=EPS, op0=ALU.mult, op1=ALU.add),
              reads=[ss], writes=[ss])
        kb.op("pool", lambda e: e.tensor_tensor(out=rs[:, 0:1], in0=ss[:, 0:1], in1=negh[:, 0:1], op=ALU.pow),
              reads=[ss, negh], writes=[rs])
        kb.op("dve", lambda e: e.scalar_tensor_tensor(out=out_f32[:, :], in0=xt[:, :], scalar=rs[:, 0:1], in1=s_bc[:, :],
                                                      op0=ALU.mult, op1=ALU.mult),
              reads=[xt, rs, s_bc], writes=[out_f32])
        kb.op("pool", lambda e: e.tensor_tensor(out=out_f32[:, :], in0=out_f32[:, :], in1=sh_bc[:, :], op=ALU.add),
              reads=[out_f32, sh_bc], writes=[out_f32])
        if out_bf is not None:
            kb.op("act", lambda e: e.copy(out=out_bf[:, :], in_=out_f32[:, :]), reads=[out_f32], writes=[out_bf])

    def ph_router(self, li):
        kb = self.kb
        I = self.I
        with contextlib.ExitStack() as es:
            svec = [self.load_vec(es, s, 4, "s2") for s in range(2)]
            shvec = [self.load_vec(es, s, 3, "sh2") for s in range(2)]
            negh = kb.sb(es, [128, 1], F32, "negh")
            kb.op("dve", lambda e: e.memset(negh[:, :], -0.5), writes=[negh])
            wr = kb.sb(es, [128, 8, NE], F32, "wr")
            kb.dma("sp", out=wr[:, :, :], in_=I["w_router"][li].rearrange("(kc p) n -> p kc n", p=128), writes=[wr])
            AT = kb.sb(es, [NE, L], F32, "AT")
            ATC = kb.sb(es, [NE, LC], F32, "ATC")
            NB = 5
            xt = [kb.sb(es, [128, D], F32, "xt") for _ in range(NB)]
            hf = [kb.sb(es, [128, D], F32, "hf") for _ in range(NB)]
            hb = [kb.sb(es, [128, D], BF16, "hb") for _ in range(NB)]
            junk = kb.sb(es, [128, D], BF16, "junk")
            hT = [kb.sb(es, [128, 8, 128], F32, "hT") for _ in range(NB)]
            sm = [[kb.sb(es, [128, 1], F32, "sm") for _ in range(6)] for _ in range(NB)]
            lg = [kb.sb(es, [128, NE], F32, "lg") for _ in range(NB)]
            af = [kb.sb(es, [128, NE], F32, "af") for _ in range(NB)]
            pT = [kb.ps(es, [128, 512], F32, "pT") for _ in range(4)]
            pl = [kb.ps(es, [128, NE], F32, "pl") for _ in range(2)]
            pa = [kb.ps(es, [NE, 128], F32, "pa") for _ in range(2)]

            def s0(ti):
                b = ti % NB
                r0 = ti * 128
                kb.dma("sp", out=xt[b][:, :], in_=self.X[r0:r0 + 128, :], reads=[self.X], writes=[xt[b]])

            def s1(ti):
                b = ti % NB
                s = 0 if ti < 64 else 1
                r0 = ti * 128
                self.norm_tile((sm[b][0], sm[b][1]), xt[b], svec[s], shvec[s], hf[b], hb[b], junk, negh)
                kb.dma("sp", out=self.H2[r0:r0 + 128, :], in_=hb[b][:, :], reads=[hb[b]], writes=[self.H2])

            def s2(ti):
                b = ti % NB
                for half in range(2):
                    p = pT[(2 * ti + half) % 4]
                    for q in range(4):
                        kc = half * 4 + q
                        kb.op("pe", lambda e, p=p, q=q, kc=kc, b=b: e.transpose(out=p[:, q * 128:(q + 1) * 128],
                                                                              in_=hf[b][:, kc * 128:(kc + 1) * 128],
                                                                              identity=self.ident_f[:, :]),
                              reads=[hf[b], self.ident_f], writes=[p])
                    kb.op("act", lambda e, p=p, half=half, b=b: e.copy(out=hT[b][:, half * 4:half * 4 + 4, :],
                                                                     in_=p[:, :].rearrange("p (q t) -> p q t", q=4)),
                          reads=[p], writes=[hT[b]])

            def s3(ti):
                b = ti % NB
                s = 0 if ti < 64 else 1
                r0 = ti * 128
                pp = pl[ti % 2]
                for kc in range(8):
                    kb.op("pe", lambda e, kc=kc, pp=pp, b=b: e.matmul(pp[:, :], lhsT=hT[b][:, kc, :], rhs=wr[:, kc, :],
                                                                    start=(kc == 0), stop=(kc == 7)),
                          reads=[hT[b], wr], writes=[pp])
                mx, nmx, se, rse = sm[b][2], sm[b][3], sm[b][4], sm[b][5]
                kb.op("dve", lambda e, pp=pp, mx=mx: e.reduce_max(out=mx[:, 0:1], in_=pp[:, :], axis=AX.X), reads=[pp], writes=[mx])
                kb.op("dve", lambda e, mx=mx, nmx=nmx: e.tensor_scalar(out=nmx[:, 0:1], in0=mx[:, 0:1], scalar1=-1.0, scalar2=None, op0=ALU.mult),
                      reads=[mx], writes=[nmx])
                kb.op("act", lambda e, pp=pp, b=b, nmx=nmx, se=se: e.activation(out=lg[b][:, :], in_=pp[:, :], func=AF.Exp, bias=nmx[:, 0:1],
                                                                              accum_out=se[:, 0:1]),
                      reads=[pp, nmx], writes=[lg[b], se])
                kb.op("dve", lambda e, se=se, rse=rse: e.reciprocal(out=rse[:, 0:1], in_=se[:, 0:1]), reads=[se], writes=[rse])
                kb.op("dve", lambda e, b=b, rse=rse: e.tensor_scalar(out=af[b][:, :], in0=lg[b][:, :], scalar1=rse[:, 0:1], scalar2=None, op0=ALU.mult),
                      reads=[lg[b], rse], writes=[af[b]])
                kb.dma("sp", out=self.AFF[r0:r0 + 128, :], in_=af[b][:, :], reads=[af[b]], writes=[self.AFF])
                pq = pa[ti % 2]
                kb.op("pe", lambda e, pq=pq, b=b: e.matmul(pq[:, :], lhsT=af[b][:, :], rhs=self.ident_f[:, :], start=True, stop=True),
                      reads=[af[b], self.ident_f], writes=[pq])
                if s == 0:
                    kb.op("act", lambda e, pq=pq, r0=r0: e.copy(out=AT[:, r0:r0 + 128], in_=pq[:, :]), reads=[pq], writes=[AT])
                else:
                    kb.op("act", lambda e, pq=pq, r0=r0: e.copy(out=ATC[:, r0 - L:r0 - L + 128], in_=pq[:, :]), reads=[pq], writes=[ATC])

            pipeline(NTILE, [s0, s1, s2, s3])
            kb.dma("sp", out=self.AFFT[:, :], in_=AT[:, :], reads=[AT], writes=[self.AFFT])
            kb.dma("sp", out=self.AFFTC[:, :], in_=ATC[:, :], reads=[ATC], writes=[self.AFFTC])

    def select(self, es, A, P, F, K, cap, key_c, base_c, dump_c, gsum, idx_out_dram, tag):
        kb = self.kb
        lo = kb.sb(es, [P, 1], F32, "lo" + tag)
        hi = kb.sb(es, [P, 1], F32, "hi" + tag)
        mid = kb.sb(es, [P, 1], F32, "mid" + tag)
        cnt = kb.sb(es, [P, 1], F32, "cnt" + tag)
        ge = kb.sb(es, [P, 1], F32, "ge" + tag)
        d1 = kb.sb(es, [P, 1], F32, "d1" + tag)
        W = kb.sb(es, [P, F], F32, "W" + tag)
        pc = kb.ps(es, [P, 1], F32, "pc" + tag)
        kb.op("dve", lambda e: e.memset(lo[:, :], 0.0), writes=[lo])
        kb.op("dve", lambda e: e.memset(hi[:, :], 1.0), writes=[hi])
        for it in range(36):
            kb.op("dve", lambda e: e.tensor_tensor(out=mid[:, :], in0=lo[:, :], in1=hi[:, :], op=ALU.add), reads=[lo, hi], writes=[mid])
            kb.op("dve", lambda e: e.tensor_scalar(out=mid[:, :], in0=mid[:, :], scalar1=0.5, scalar2=None, op0=ALU.mult), reads=[mid], writes=[mid])
            kb.op("dve", lambda e: e.tensor_scalar(out=W[:, :], in0=A[:, :], scalar1=mid[:, 0:1], scalar2=0.0, op0=ALU.is_ge, op1=ALU.add,
                                                   accum_out=cnt[:, 0:1]), reads=[A, mid], writes=[W, cnt])
            if gsum is not None:
                kb.op("pe", lambda e: e.matmul(pc[:, :], lhsT=gsum[0:P, 0:P], rhs=cnt[:, :], start=True, stop=True), reads=[gsum, cnt], writes=[pc])
                src = pc
            else:
                src = cnt
            kb.op("dve", lambda e, src=src: e.tensor_scalar(out=ge[:, :], in0=src[:, :], scalar1=float(K) - 0.5, scalar2=None, op0=ALU.is_ge),
                  reads=[src], writes=[ge])
            kb.op("dve", lambda e: e.tensor_tensor(out=d1[:, :], in0=mid[:, :], in1=lo[:, :], op=ALU.subtract), reads=[mid, lo], writes=[d1])
            kb.op("dve", lambda e: e.scalar_tensor_tensor(out=lo[:, :], in0=d1[:, :], scalar=ge[:, 0:1], in1=lo[:, :], op0=ALU.mult, op1=ALU.add),
                  reads=[d1, ge, lo], writes=[lo])
            kb.op("dve", lambda e: e.tensor_tensor(out=d1[:, :], in0=hi[:, :], in1=mid[:, :], op=ALU.subtract), reads=[hi, mid], writes=[d1])
            kb.op("dve", lambda e: e.scalar_tensor_tensor(out=hi[:, :], in0=d1[:, :], scalar=ge[:, 0:1], in1=mid[:, :], op0=ALU.mult, op1=ALU.add),
                  reads=[d1, ge, mid], writes=[hi])
        kb.op("dve", lambda e: e.scalar_tensor_tensor(out=W[:, :], in0=A[:, :], scalar=lo[:, 0:1], in1=key_c[0:P, :], op0=ALU.is_ge, op1=ALU.mult),
              reads=[A, lo, key_c], writes=[W])
        Lv = kb.sb(es, [P, cap], F32, "Lv" + tag)
        for r in range(cap // 8):
            kb.op("dve", lambda e, r=r: e.max(out=Lv[:, 8 * r:8 * r + 8], in_=W[:, :]), reads=[W], writes=[Lv])
            kb.op("dve", lambda e, r=r: e.match_replace(out=W[:, :], in_to_replace=Lv[:, 8 * r:8 * r + 8], in_values=W[:, :], imm_value=0.0),
                  reads=[W, Lv], writes=[W])
        tok = kb.sb(es, [P, cap], F32, "tok" + tag)
        vm = kb.sb(es, [P, cap], F32, "vm" + tag)
        idx = kb.sb(es, [P, cap], I32, "idx" + tag)
        kb.op("dve", lambda e: e.tensor_scalar(out=tok[:, :], in0=Lv[:, :], scalar1=-1.0, scalar2=base_c[0:P, 0:1], op0=ALU.mult, op1=ALU.add),
              reads=[Lv, base_c], writes=[tok])
        kb.op("dve", lambda e: e.tensor_tensor(out=tok[:, :], in0=tok[:, :], in1=dump_c[0:P, :], op=ALU.subtract), reads=[tok, dump_c], writes=[tok])
        kb.op("dve", lambda e: e.tensor_scalar(out=vm[:, :], in0=Lv[:, :], scalar1=0.5, scalar2=None, op0=ALU.is_ge), reads=[Lv], writes=[vm])
        kb.op("dve", lambda e: e.tensor_tensor(out=tok[:, :], in0=tok[:, :], in1=vm[:, :], op=ALU.mult), reads=[tok, vm], writes=[tok])
        kb.op("dve", lambda e: e.tensor_tensor(out=tok[:, :], in0=tok[:, :], in1=dump_c[0:P, :], op=ALU.add), reads=[tok, dump_c], writes=[tok])
        kb.op("dve", lambda e: e.tensor_copy(out=idx[:, :], in_=tok[:, :]), reads=[tok], writes=[idx])
        kb.dma("sp", out=idx_out_dram[:, :], in_=idx[:, :], reads=[idx], writes=[idx_out_dram])

    def ph_select(self, li):
        kb = self.kb
        I = self.I
        with contextlib.ExitStack() as es:
            cs = {}
            for k in ("gsum", "keyl", "keyc", "basel", "basec", "dumpl", "dumpc"):
                shp, ty = CONST_SPECS[k]
                cs[k] = kb.sb(es, shp, ty, k)
                kb.dma("sp", out=cs[k][:, :], in_=I[k][:, :], writes=[cs[k]])
            A = kb.sb(es, [PSEL, BLK], F32, "Asel")
            kb.dma("sp", out=A[:, :], in_=self.AFFT[:, :].rearrange("e (b t) -> (e b) t", b=NBLK), reads=[self.AFFT], writes=[A])
            self.select(es, A, PSEL, BLK, 1024, CAPB, cs["keyl"], cs["basel"], cs["dumpl"], cs["gsum"], self.IDXD, "l")
            Ac = kb.sb(es, [NE, LC], F32, "Aselc")
            kb.dma("sp", out=Ac[:, :], in_=self.AFFTC[:, :], reads=[self.AFFTC], writes=[Ac])
            self.select(es, Ac, NE, LC, CAPC, CAPC, cs["keyc"], cs["basec"], cs["dumpc"], None, self.IDXC, "c")

    def ph_moe(self, li):
        kb = self.kb
        I = self.I
        NS = NSLOT + CAPC
        chunks = []
        c0 = 0
        while c0 < NS:
            cn = min(512, NS - c0)
            chunks.append((c0, cn))
            c0 += cn
        NCH = len(chunks)
        with contextlib.ExitStack() as es:
            ga2 = [self.load_vec(es, s, 5, "ga2") for s in range(2)]
            idxt = kb.sb(es, [128, NE, NCALL], I32, "idxt")
            kb.dma("sp", out=idxt[:, :, :], in_=self.IDXD[:, :].rearrange("(e b) r -> e (b r)", b=NBLK).rearrange("e (j p) -> p e j", p=128),
                   reads=[self.IDXD], writes=[idxt], allow_slow_non_contiguous=True)
            idxc = kb.sb(es, [CAPC, NE], I32, "idxc")
            kb.dma("sp", out=idxc[:, :], in_=self.IDXC[:, :].rearrange("e p -> p e"), reads=[self.IDXC], writes=[idxc],
                   allow_slow_non_contiguous=True)
            stage = [kb.sb(es, [128, 4096], F32, "stage") for _ in range(2)]
            w13 = [[kb.sb(es, [128, 8, 512], BF16, "w13") for _ in range(2)] for _ in range(2)]
            w2 = kb.sb(es, [128, 16, D], BF16, "w2")
            w2k = [Trk() for _ in range(4)]
            xgT = kb.sb(es, [128, 8, NS], BF16, "xgT")
            gT = kb.sb(es, [128, 16, NS], BF16, "gT")
            gTk = [Trk() for _ in range(16)]
            G = [kb.sb(es, [128, D], BF16, "G") for _ in range(NCALL + 1)]
            gate = [kb.sb(es, [128, NCALL + 1, NE], F32, "gate") for _ in range(2)]
            gatek = [[Trk() for _ in range(NCALL + 1)] for _ in range(2)]
            yb = [kb.sb(es, [128, D], F32, "yb") for _ in range(2)]
            sa = [kb.sb(es, [128, 512], BF16, "sa") for _ in range(2)]
            pA = [kb.ps(es, [128, 512], F32, "pA") for _ in range(2)]
            pB = [kb.ps(es, [128, 512], F32, "pB") for _ in range(2)]
            pT = [kb.ps(es, [128, 1024], BF16, "pTm") for _ in range(2)]
            pY = [kb.ps(es, [128, 512], F32, "pY") for _ in range(2)]
            stage_i = [0]

            def load_cast(dst_ap, dst_trk, src_ap, shape3):
                st = stage[stage_i[0] % 2]
                a_, b_ = shape3
                sv = st[:, 0:a_ * b_].rearrange("p (a b) -> p a b", a=a_)
                kb.dma("sp", out=sv, in_=src_ap, writes=[st])
                if stage_i[0] % 2 == 0:
                    kb.op("dve", lambda e: e.tensor_copy(out=dst_ap, in_=sv), reads=[st], writes=[dst_trk])
                else:
                    kb.op("act", lambda e: e.copy(out=dst_ap, in_=sv), reads=[st], writes=[dst_trk])
                stage_i[0] += 1

            def calls_of(ex):
                return [(j, 128, idxt[:, ex, j:j + 1], j * 128) for j in range(NCALL)] + [(NCALL, CAPC, idxc[:, ex:ex + 1], NSLOT)]

            def issue_gathers(ex):
                gb = ex % 2
                for (j, rows, iap, col0) in calls_of(ex):
                    g = G[j]
                    kb.dma("pool", reads=[self.H2, idxt, idxc], writes=[g],
                           fn=lambda e, g=g, rows=rows, iap=iap: e.indirect_dma_start(out=g[0:rows, :], out_offset=None, in_=self.H2[:, :],
                                                                                      in_offset=bass.IndirectOffsetOnAxis(ap=iap, axis=0)))
                    kb.dma("pool", reads=[self.AFF, idxt, idxc], writes=[gatek[gb][j]],
                           fn=lambda e, gb=gb, j=j, rows=rows, iap=iap: e.indirect_dma_start(out=gate[gb][0:rows, j, :], out_offset=None, in_=self.AFF[:, :],
                                                                                           in_offset=bass.IndirectOffsetOnAxis(ap=iap, axis=0)))

            issue_gathers(0)
            tcount = 0
            for ex in range(NE):
                gb = ex % 2
                w1src = I["w_e1"][li, ex].rearrange("(kc p) f -> p kc f", p=128)
                w3src = I["w_e3"][li, ex].rearrange("(kc p) f -> p kc f", p=128)
                w2src = I["w_e2"][li, ex].rearrange("(fc p) d -> p fc d", p=128)
                calls = calls_of(ex)
                for (j, rows, iap, col0) in calls:
                    g = G[j]
                    p = pT[tcount % 2]
                    tcount += 1
                    for kc in range(8):
                        kb.op("pe", lambda e, p=p, g=g, kc=kc, rows=rows: e.transpose(out=p[:, kc * 128:kc * 128 + rows],
                                                                                    in_=g[0:rows, kc * 128:(kc + 1) * 128],
                                                                                    identity=self.ident_bf[0:rows, 0:rows]),
                              reads=[g, self.ident_bf], writes=[p])
                    ev = "dve" if tcount % 2 == 0 else "act"
                    if ev == "dve":
                        kb.op("dve", lambda e, p=p, rows=rows, col0=col0: e.tensor_copy(
                            out=xgT[:, :, col0:col0 + rows], in_=p[:, :].rearrange("p (k t) -> p k t", k=8)[:, :, 0:rows]),
                              reads=[p], writes=[xgT])
                    else:
                        kb.op("act", lambda e, p=p, rows=rows, col0=col0: e.copy(
                            out=xgT[:, :, col0:col0 + rows], in_=p[:, :].rearrange("p (k t) -> p k t", k=8)[:, :, 0:rows]),
                              reads=[p], writes=[xgT])
                if ex + 1 < NE:
                    issue_gathers(ex + 1)
                for q in range(4):
                    wb = w13[q % 2]
                    load_cast(wb[0][:, :, :], wb[0].k, w1src[:, :, q * 512:(q + 1) * 512], (8, 512))
                    load_cast(wb[1][:, :, :], wb[1].k, w3src[:, :, q * 512:(q + 1) * 512], (8, 512))
                    for f4 in range(4):
                        fc = q * 4 + f4
                        for ci, (c0, cn) in enumerate(chunks):
                            k = (fc * NCH + ci) % 2
                            for kc in range(8):
                                kb.op("pe", lambda e, k=k, kc=kc, f4=f4, c0=c0, cn=cn, wb=wb: e.matmul(
                                    pA[k][:, 0:cn], lhsT=wb[0][:, kc, f4 * 128:(f4 + 1) * 128], rhs=xgT[:, kc, c0:c0 + cn],
                                    start=(kc == 0), stop=(kc == 7)), reads=[wb[0], xgT], writes=[pA[k]])
                            for kc in range(8):
                                kb.op("pe", lambda e, k=k, kc=kc, f4=f4, c0=c0, cn=cn, wb=wb: e.matmul(
                                    pB[k][:, 0:cn], lhsT=wb[1][:, kc, f4 * 128:(f4 + 1) * 128], rhs=xgT[:, kc, c0:c0 + cn],
                                    start=(kc == 0), stop=(kc == 7)), reads=[wb[1], xgT], writes=[pB[k]])
                            kb.op("act", lambda e, k=k, cn=cn: e.activation(out=sa[k][:, 0:cn], in_=pA[k][:, 0:cn], func=AF.Silu),
                                  reads=[pA[k]], writes=[sa[k]])
                            kb.op("dve", lambda e, k=k, cn=cn, c0=c0, fc=fc: e.tensor_tensor(out=gT[:, fc, c0:c0 + cn], in0=sa[k][:, 0:cn],
                                                                                          in1=pB[k][:, 0:cn], op=ALU.mult),
                                  reads=[sa[k], pB[k]], writes=[gTk[fc]])
                for g4 in range(4):
                    load_cast(w2[:, g4 * 4:(g4 + 1) * 4, :], w2k[g4], w2src[:, g4 * 4:(g4 + 1) * 4, :], (4, D))
                for (j, rows, iap, col0) in calls:
                    s = 0 if j < NCALL else 1
                    y = yb[j % 2]
                    for h in range(2):
                        p = pY[h]
                        for fc in range(16):
                            kb.op("pe", lambda e, p=p, fc=fc, col0=col0, rows=rows, h=h: e.matmul(
                                p[0:rows, :], lhsT=gT[:, fc, col0:col0 + rows], rhs=w2[:, fc, h * 512:(h + 1) * 512],
                                start=(fc == 0), stop=(fc == 15)), reads=[gTk[fc], w2k[fc // 4]], writes=[p])
                        kb.op("dve", lambda e, p=p, y=y, h=h, rows=rows, gb=gb, j=j, s=s, ex=ex: e.scalar_tensor_tensor(
                            out=y[0:rows, h * 512:(h + 1) * 512], in0=p[0:rows, :], scalar=gate[gb][0:rows, j, ex:ex + 1],
                            in1=ga2[s][0:rows, h * 512:(h + 1) * 512], op0=ALU.mult, op1=ALU.mult),
                              reads=[p, gatek[gb][j], ga2[s]], writes=[y])
                    kb.dma("pool", reads=[y, idxt, idxc], writes=[self.X],
                           fn=lambda e, y=y, rows=rows, iap=iap: e.indirect_dma_start(out=self.X[:, :],
                                                                                      out_offset=bass.IndirectOffsetOnAxis(ap=iap, axis=0),
                                                                                      in_=y[0:rows, :], in_offset=None, compute_op=ALU.add))

    def load_w_bf16(self, es, dst, src3, ncols, stage):
        kb = self.kb
        i = 0
        for c0 in range(0, ncols, 512):
            cn = min(512, ncols - c0)
            st = stage[i % 2]
            sv = st[:, 0:8 * cn].rearrange("p (a b) -> p a b", a=8)
            kb.dma("sp", out=sv, in_=src3[:, :, c0:c0 + cn], writes=[st])
            if i % 2 == 0:
                kb.op("pool", lambda e, sv=sv, c0=c0, cn=cn: e.tensor_copy(out=dst[:, :, c0:c0 + cn], in_=sv), reads=[st], writes=[dst])
            else:
                kb.op("act", lambda e, sv=sv, c0=c0, cn=cn: e.copy(out=dst[:, :, c0:c0 + cn], in_=sv), reads=[st], writes=[dst])
            i += 1

    def transpose_tile(self, hb, hT, pT):
        kb = self.kb
        for kc in range(8):
            kb.op("pe", lambda e, kc=kc: e.transpose(out=pT[:, kc * 128:(kc + 1) * 128], in_=hb[:, kc * 128:(kc + 1) * 128],
                                                     identity=self.ident_bf[:, :]), reads=[hb, self.ident_bf], writes=[pT])
        kb.op("act", lambda e: e.copy(out=hT[:, :, :], in_=pT[:, :].rearrange("p (k t) -> p k t", k=8)), reads=[pT], writes=[hT])

    def ntiles(self, need_ctx):
        return NTILE if need_ctx else 64

    def ph_qkv(self, li):
        kb = self.kb
        I = self.I
        j = li // 2
        if not hasattr(self, "QKT"):
            self.QKT = kb.dram("QKT", [16, 128, NT], BF16)
            self.VD = kb.dram("VD", [NT + 64, 16 * 65], BF16)
            self.OD = kb.dram("OD", [NT, D], BF16)
        with contextlib.ExitStack() as es:
            svec = [self.load_vec(es, s, 1, "s1") for s in range(2)]
            shvec = [self.load_vec(es, s, 0, "sh1") for s in range(2)]
            negh = kb.sb(es, [128, 1], F32, "negh")
            kb.op("dve", lambda e: e.memset(negh[:, :], -0.5), writes=[negh])
            stage = [kb.sb(es, [128, 4096], F32, "stage") for _ in range(2)]
            W = kb.sb(es, [128, 8, 3 * D], BF16, "wqkv")
            self.load_w_bf16(es, W, I["w_qkv"][j].rearrange("(kc p) n -> p kc n", p=128), 3 * D, stage)
            NB = 5
            xt = [kb.sb(es, [128, D], F32, "xt") for _ in range(NB)]
            hf = [kb.sb(es, [128, D], F32, "hf") for _ in range(NB)]
            hb = [kb.sb(es, [128, D], BF16, "hb") for _ in range(NB)]
            junk = kb.sb(es, [128, D], BF16, "junk")
            hT = [kb.sb(es, [128, 8, 128], BF16, "hT") for _ in range(NB)]
            sm = [[kb.sb(es, [128, 1], F32, "sm") for _ in range(2)] for _ in range(NB)]
            qk = [kb.sb(es, [128, 16, 128], BF16, "qk") for _ in range(2)]
            vt = [kb.sb(es, [128, 16, 65], BF16, "vt") for _ in range(2)]
            for b in range(2):
                kb.op("dve", lambda e, b=b: e.memset(vt[b][:, :, :], 1.0), writes=[vt[b]])
            pT = [kb.ps(es, [128, 1024], BF16, "pT") for _ in range(2)]
            pq = [kb.ps(es, [128, 512], F32, "pq") for _ in range(6)]

            def s0(ti):
                b = ti % NB
                r0 = ti * 128
                kb.dma("sp", out=xt[b][:, :], in_=self.X[r0:r0 + 128, :], reads=[self.X], writes=[xt[b]])

            def s1(ti):
                b = ti % NB
                s = 0 if ti < 64 else 1
                self.norm_tile((sm[b][0], sm[b][1]), xt[b], svec[s], shvec[s], hf[b], hb[b], junk, negh)

            def s2(ti):
                b = ti % NB
                self.transpose_tile(hb[b], hT[b], pT[ti % 2])

            def s3(ti):
                b = ti % NB
                b2 = ti % 2
                r0 = ti * 128
                for c4 in range(4):
                    p = pq[(6 * ti + c4) % 6]
                    for cc in range(4):
                        c = c4 * 4 + cc
                        for kc in range(8):
                            kb.op("pe", lambda e, p=p, cc=cc, c=c, kc=kc, b=b: e.matmul(p[:, cc * 128:(cc + 1) * 128], lhsT=W[:, kc, c * 128:(c + 1) * 128],
                                                                                     rhs=hT[b][:, kc, :], start=(kc == 0), stop=(kc == 7)),
                                  reads=[W, hT[b]], writes=[p])
                    sc = 0.125 if c4 < 2 else 1.0
                    kb.op("act", lambda e, p=p, c4=c4, b2=b2, sc=sc: e.activation(out=qk[b2][:, c4 * 4:c4 * 4 + 4, :], in_=p[:, :].rearrange("p (c t) -> p c t", c=4),
                                                                               func=AF.Copy, scale=sc), reads=[p], writes=[qk[b2]])
                kb.dma("sp", out=self.QKT[:, :, r0:r0 + 128].rearrange("c p t -> p c t"), in_=qk[b2][:, :, :], reads=[qk[b2]], writes=[self.QKT])
                for h2 in range(2):
                    p = pq[(6 * ti + 4 + h2) % 6]
                    for kc in range(8):
                        kb.op("pe", lambda e, p=p, kc=kc, h2=h2, b=b: e.matmul(p[:, :], lhsT=hT[b][:, kc, :], rhs=W[:, kc, 2 * D + h2 * 512:2 * D + (h2 + 1) * 512],
                                                                            start=(kc == 0), stop=(kc == 7)), reads=[W, hT[b]], writes=[p])
                    kb.op("dve", lambda e, p=p, h2=h2, b2=b2: e.tensor_copy(out=vt[b2][:, h2 * 8:(h2 + 1) * 8, 0:64], in_=p[:, :].rearrange("p (h d) -> p h d", h=8)),
                          reads=[p], writes=[vt[b2]])
                kb.dma("sp", out=self.VD[r0:r0 + 128, :], in_=vt[b2][:, :, :].rearrange("p h d -> p (h d)"), reads=[vt[b2]], writes=[self.VD])

            pipeline(NTILE, [s0, s1, s2, s3])

    def ph_attn(self, li, need_ctx):
        kb = self.kb
        I = self.I
        j = li // 2
        with contextlib.ExitStack() as es:
            KT = kb.sb(es, [128, NT], BF16, "KT")
            QT = kb.sb(es, [128, NT], BF16, "QT")
            V0 = kb.sb(es, [128, NTILE, 130], BF16, "V0")
            bst = [kb.sb(es, [128, 2, 5, 128], F32, "bst") for _ in range(2)]
            EB = [kb.sb(es, [128, 2, 5, 128], BF16, "EB") for _ in range(5)]
            NP = 5
            PT = [kb.sb(es, [128, 7, 128], BF16, "PT") for _ in range(NP)]
            osb = [kb.sb(es, [128, 128], BF16, "osb") for _ in range(3)]
            rec = [kb.sb(es, [128, 1], F32, "rec") for _ in range(4)]
            pSa = [kb.ps(es, [128, 512], F32, "pSa") for _ in range(2)]
            pSb = [kb.ps(es, [128, 512], F32, "pSb") for _ in range(2)]
            pO = [kb.ps(es, [128, 65], F32, "pO") for _ in range(3)]
            VDv = self.VD[0:NT, :].rearrange("(t p) c -> p t c", p=128)
            for hp in range(8):
                kb.dma("sp", out=QT[:, :], in_=self.QKT[hp, :, :], reads=[self.QKT], writes=[QT])
                kb.dma("sp", out=KT[:, :], in_=self.QKT[8 + hp, :, :], reads=[self.QKT], writes=[KT])
                kb.dma("sp", out=V0[:, :, :], in_=VDv[:, :, hp * 130:(hp + 1) * 130], reads=[self.VD], writes=[V0])
                for v in range(5):
                    st = bst[v % 2]
                    kb.dma("sp", out=st[:, :, :, :], in_=I["biasT"][j, v, 2 * hp:2 * hp + 2].rearrange("h p c q -> p h c q"), writes=[st])
                    kb.op("act", lambda e, st=st, v=v: e.activation(out=EB[v][:, :, :, :], in_=st[:, :, :, :], func=AF.Exp), reads=[st], writes=[EB[v]])
                units = []
                for rp in range(64):
                    for hl in range(2):
                        units.append((rp, hl))
                if need_ctx:
                    for cq in range(2):
                        for hl in range(2):
                            units.append((64 + cq, hl))

                def info(u):
                    rp, hl = units[u]
                    if rp < 64:
                        t0 = att_base(rp) // 2
                        tiles = [t0 + c for c in range(5)] + [64, 65]
                        q0 = 128 * rp
                        eb = EB[att_variant(rp)]
                    else:
                        tiles = [64, 65]
                        q0 = L + 128 * (rp - 64)
                        eb = None
                    return rp, hl, tiles, q0, eb

                def sA(u):
                    rp, hl, tiles, q0, eb = info(u)
                    ho = hl * 64
                    pa_, pb_ = pSa[u % 2], pSb[u % 2]
                    for c, t in enumerate(tiles):
                        dst = pa_[:, c * 128:(c + 1) * 128] if c < 4 else pb_[:, (c - 4) * 128:(c - 3) * 128]
                        trk = pa_ if c < 4 else pb_
                        kb.op("pe", lambda e, dst=dst, ho=ho, t=t, q0=q0: e.matmul(dst, lhsT=KT[ho:ho + 64, t * 128:(t + 1) * 128],
                                                                                rhs=QT[ho:ho + 64, q0:q0 + 128], start=True, stop=True),
                              reads=[KT, QT], writes=[trk])

                def sB(u):
                    rp, hl, tiles, q0, eb = info(u)
                    pa_, pb_ = pSa[u % 2], pSb[u % 2]
                    pt = PT[u % NP]
                    n = len(tiles)
                    na = min(n, 4)
                    kb.op("act", lambda e, pt=pt, pa_=pa_, na=na: e.activation(out=pt[:, 0:na, :].rearrange("p c q -> p (c q)"), in_=pa_[:, 0:na * 128], func=AF.Exp),
                          reads=[pa_], writes=[pt])
                    if n > 4:
                        kb.op("act", lambda e, pt=pt, pb_=pb_, n=n: e.activation(out=pt[:, 4:n, :].rearrange("p c q -> p (c q)"), in_=pb_[:, 0:(n - 4) * 128], func=AF.Exp),
                              reads=[pb_], writes=[pt])

                def sB2(u):
                    rp, hl, tiles, q0, eb = info(u)
                    pt = PT[u % NP]
                    if eb is not None:
                        me = "pool" if (u % 5) < 3 else "dve"
                        kb.op(me, lambda e, pt=pt, eb=eb, hl=hl: e.tensor_tensor(out=pt[:, 0:5, :], in0=pt[:, 0:5, :], in1=eb[:, hl, :, :], op=ALU.mult),
                              reads=[pt, eb], writes=[pt])

                def sC(u):
                    rp, hl, tiles, q0, eb = info(u)
                    pt = PT[u % NP]
                    po = pO[u % 3]
                    ob = osb[(u // 2) % 3]
                    n = len(tiles)
                    for c, t in enumerate(tiles):
                        kb.op("pe", lambda e, po=po, c=c, t=t, pt=pt, hl=hl, n=n: e.matmul(po[:, :], lhsT=pt[:, c, :], rhs=V0[:, t, hl * 65:(hl + 1) * 65],
                                                                                        start=(c == 0), stop=(c == n - 1)), reads=[pt, V0], writes=[po])
                    rc = rec[u % 4]
                    kb.op("dve", lambda e, po=po, rc=rc: e.reciprocal(out=rc[:, 0:1], in_=po[:, 64:65]), reads=[po], writes=[rc])
                    kb.op("dve", lambda e, po=po, rc=rc, hl=hl, ob=ob: e.tensor_scalar(out=ob[:, hl * 64:(hl + 1) * 64], in0=po[:, 0:64],
                                                                                      scalar1=rc[:, 0:1], scalar2=None, op0=ALU.mult),
                          reads=[po, rc], writes=[ob])
                    if hl == 1:
                        kb.dma("sp", out=self.OD[q0:q0 + 128, hp * 128:(hp + 1) * 128], in_=ob[:, :], reads=[ob], writes=[self.OD])

                pipeline(len(units), [sA, sB, sB2, sC])

    def ph_oproj(self, li, wname, srcname, need_ctx):
        kb = self.kb
        I = self.I
        j = li // 2
        SRC = getattr(self, srcname)
        with contextlib.ExitStack() as es:
            ga = [self.load_vec(es, s, 2, "ga1") for s in range(2)]
            stage = [kb.sb(es, [128, 4096], F32, "stage") for _ in range(2)]
            W = kb.sb(es, [128, 8, D], BF16, "wo")
            self.load_w_bf16(es, W, I[wname][j].rearrange("(kc p) n -> p kc n", p=128), D, stage)
            NB = 5
            xt = [kb.sb(es, [128, D], F32, "xt") for _ in range(NB)]
            hb = [kb.sb(es, [128, D], BF16, "hb") for _ in range(NB)]
            tmp = [kb.sb(es, [128, D], F32, "tmp") for _ in range(2)]
            hT = [kb.sb(es, [128, 8, 128], BF16, "hT") for _ in range(NB)]
            pT = [kb.ps(es, [128, 1024], BF16, "pT") for _ in range(2)]
            pq = [kb.ps(es, [128, 512], F32, "pq") for _ in range(4)]

            def s0(ti):
                b = ti % NB
                r0 = ti * 128
                kb.dma("sp", out=xt[b][:, :], in_=self.X[r0:r0 + 128, :], reads=[self.X], writes=[xt[b]])
                kb.dma("sp", out=hb[b][:, :], in_=SRC[r0:r0 + 128, :], reads=[SRC], writes=[hb[b]])

            def s1(ti):
                b = ti % NB
                self.transpose_tile(hb[b], hT[b], pT[ti % 2])

            def s2(ti):
                b = ti % NB
                b2 = ti % 2
                s = 0 if ti < 64 else 1
                r0 = ti * 128
                for h2 in range(2):
                    p = pq[(2 * ti + h2) % 4]
                    for kc in range(8):
                        kb.op("pe", lambda e, p=p, kc=kc, h2=h2, b=b: e.matmul(p[:, :], lhsT=hT[b][:, kc, :], rhs=W[:, kc, h2 * 512:(h2 + 1) * 512],
                                                                            start=(kc == 0), stop=(kc == 7)), reads=[W, hT[b]], writes=[p])
                    kb.op("dve", lambda e, p=p, h2=h2, b2=b2, s=s: e.tensor_tensor(out=tmp[b2][:, h2 * 512:(h2 + 1) * 512], in0=p[:, :],
                                                                                in1=ga[s][:, h2 * 512:(h2 + 1) * 512], op=ALU.mult),
                          reads=[p, ga[s]], writes=[tmp[b2]])
                kb.op("pool", lambda e, b=b, b2=b2: e.tensor_tensor(out=xt[b][:, :], in0=xt[b][:, :], in1=tmp[b2][:, :], op=ALU.add),
                      reads=[xt[b], tmp[b2]], writes=[xt[b]])
                kb.dma("sp", out=self.X[r0:r0 + 128, :], in_=xt[b][:, :], reads=[xt[b]], writes=[self.X])

            pipeline(self.ntiles(need_ctx), [s0, s1, s2])

    def cload(self, es, name):
        shp, ty = CONST_SPECS[name]
        t = self.kb.sb(es, shp, ty, name)
        sl = tuple(slice(None) for _ in shp)
        self.kb.dma("sp", out=t[sl], in_=self.I[name][sl], writes=[t])
        return t

    def even_scratch(self):
        kb = self.kb
        if not hasattr(self, "PR"):
            self.PR = kb.dram("PR", [NT, 1536], BF16)
            self.DTR = kb.dram("DTR", [NT, 16], F32)
            self.AD = kb.dram("AD", [NT, 1024], BF16)
            self.XBC = kb.dram("XBC", [NT, 1024], BF16)
            self.DTA = kb.dram("DTA", [NT, 32], F32)
            self.YF = kb.dram("YF", [NT, 512], F32)
            self.MIX = kb.dram("MIX", [NT, 1024], BF16)
            self.GD = kb.dram("GD", [2, 64, 128, 512], BF16)

    def ph_inproj(self, li):
        kb = self.kb
        I = self.I
        j = li // 2
        self.even_scratch()
        with contextlib.ExitStack() as es:
            svec = [self.load_vec(es, s, 1, "s1") for s in range(2)]
            shvec = [self.load_vec(es, s, 0, "sh1") for s in range(2)]
            negh = kb.sb(es, [128, 1], F32, "negh")
            kb.op("dve", lambda e: e.memset(negh[:, :], -0.5), writes=[negh])
            stage = [kb.sb(es, [128, 4096], F32, "stage") for _ in range(2)]
            W = kb.sb(es, [128, 8, 2064], BF16, "win")
            self.load_w_bf16(es, W, I["w_in_e"][j].rearrange("(kc p) n -> p kc n", p=128), 2064, stage)
            CS = self.cload(es, "CS")
            NB = 5
            xt = [kb.sb(es, [128, D], F32, "xt") for _ in range(NB)]
            hf = [kb.sb(es, [128, D], F32, "hf") for _ in range(NB)]
            hb = [kb.sb(es, [128, D], BF16, "hb") for _ in range(NB)]
            junk = kb.sb(es, [128, D], BF16, "junk")
            hT = [kb.sb(es, [128, 8, 128], BF16, "hT") for _ in range(NB)]
            sm = [[kb.sb(es, [128, 1], F32, "sm") for _ in range(2)] for _ in range(NB)]
            prt = [kb.sb(es, [128, 1536], BF16, "prt") for _ in range(2)]
            dtr = [kb.sb(es, [128, 16], F32, "dtr") for _ in range(2)]
            uT = [kb.sb(es, [128, 4, 128], BF16, "uT") for _ in range(2)]
            At = [kb.sb(es, [128, 2, 4, 128], BF16, "At") for _ in range(2)]
            pT = [kb.ps(es, [128, 1024], BF16, "pT") for _ in range(2)]
            pq = [kb.ps(es, [128, 512], F32, "pq") for _ in range(4)]
            pa = [kb.ps(es, [128, 512], F32, "pa") for _ in range(2)]

            def s0(ti):
                b = ti % NB
                r0 = ti * 128
                kb.dma("sp", out=xt[b][:, :], in_=self.X[r0:r0 + 128, :], reads=[self.X], writes=[xt[b]])

            def s1(ti):
                b = ti % NB
                s = 0 if ti < 64 else 1
                self.norm_tile((sm[b][0], sm[b][1]), xt[b], svec[s], shvec[s], hf[b], hb[b], junk, negh)

            def s2(ti):
                b = ti % NB
                self.transpose_tile(hb[b], hT[b], pT[ti % 2])

            def s3(ti):
                b = ti % NB
                b2 = ti % 2
                r0 = ti * 128
                for ci, (c0, cn) in enumerate([(0, 512), (512, 512), (1024, 512), (1536, 16)]):
                    p = pq[(5 * ti + ci) % 4]
                    for kc in range(8):
                        kb.op("pe", lambda e, p=p, kc=kc, c0=c0, cn=cn, b=b: e.matmul(p[:, 0:cn], lhsT=hT[b][:, kc, :], rhs=W[:, kc, c0:c0 + cn],
                                                                                   start=(kc == 0), stop=(kc == 7)), reads=[W, hT[b]], writes=[p])
                    if ci < 3:
                        if ci % 2 == 0:
                            kb.op("act", lambda e, p=p, c0=c0, b2=b2: e.copy(out=prt[b2][:, c0:c0 + 512], in_=p[:, :]), reads=[p], writes=[prt[b2]])
                        else:
                            kb.op("dve", lambda e, p=p, c0=c0, b2=b2: e.tensor_copy(out=prt[b2][:, c0:c0 + 512], in_=p[:, :]), reads=[p], writes=[prt[b2]])
                    else:
                        kb.op("dve", lambda e, p=p, b2=b2: e.tensor_copy(out=dtr[b2][:, :], in_=p[:, 0:16]), reads=[p], writes=[dtr[b2]])
                kb.dma("sp", out=self.PR[r0:r0 + 128, :], in_=prt[b2][:, :], reads=[prt[b2]], writes=[self.PR])
                kb.dma("sp", out=self.DTR[r0:r0 + 128, :], in_=dtr[b2][:, :], reads=[dtr[b2]], writes=[self.DTR])
                p = pq[(5 * ti + 4) % 4]
                for g in range(4):
                    for kc in range(8):
                        kb.op("pe", lambda e, p=p, kc=kc, g=g, b=b: e.matmul(p[:, g * 128:(g + 1) * 128], lhsT=W[:, kc, 1552 + g * 128:1552 + (g + 1) * 128],
                                                                          rhs=hT[b][:, kc, :], start=(kc == 0), stop=(kc == 7)), reads=[W, hT[b]], writes=[p])
                kb.op("act", lambda e, p=p, b2=b2: e.copy(out=uT[b2][:, :, :], in_=p[:, :].rearrange("p (g t) -> p g t", g=4)), reads=[p], writes=[uT[b2]])
                for gh in range(2):
                    for g2 in range(2):
                        g = gh * 2 + g2
                        kb.op("pe", lambda e, gh=gh, g2=g2, g=g, b2=b2: e.matmul(pa[gh][:, g2 * 256:(g2 + 1) * 256], lhsT=uT[b2][:, g, :], rhs=CS[:, :],
                                                                              start=True, stop=True), reads=[uT[b2], CS], writes=[pa[gh]])
                    kb.op("dve", lambda e, gh=gh, b2=b2: e.tensor_copy(out=At[b2][:, :, 2 * gh:2 * gh + 2, :],
                                                                     in_=pa[gh][:, :].rearrange("p (g cs q) -> p cs g q", g=2, cs=2)),
                          reads=[pa[gh]], writes=[At[b2]])
                kb.dma("sp", out=self.AD[r0:r0 + 128, :], in_=At[b2][:, :, :, :].rearrange("p cs g q -> p (cs g q)"), reads=[At[b2]], writes=[self.AD])

            pipeline(NTILE, [s0, s1, s2, s3])

    def ph_conv(self, li):
        kb = self.kb
        I = self.I
        j = li // 2
        with contextlib.ExitStack() as es:
            cw = [kb.sb(es, [128, D], F32, "cw") for _ in range(3)]
            for k in range(3):
                kb.dma("sp", out=cw[k][:, :], in_=I["conv_w"][j, k, :].partition_broadcast(128), writes=[cw[k]])
            cb = kb.sb(es, [128, D], F32, "cb")
            kb.dma("sp", out=cb[:, :], in_=I["conv_b"][j, :].partition_broadcast(128), writes=[cb])
            dtb = kb.sb(es, [128, 16], F32, "dtb")
            kb.dma("sp", out=dtb[:, :], in_=I["dt_bias"][j, :].partition_broadcast(128), writes=[dtb])
            abc = kb.sb(es, [128, 16], F32, "abc")
            kb.dma("sp", out=abc[:, :], in_=I["a_log"][j, :].partition_broadcast(128), writes=[abc])
            kb.op("act", lambda e: e.activation(out=abc[:, :], in_=abc[:, :], func=AF.Exp), reads=[abc], writes=[abc])
            kb.op("dve", lambda e: e.tensor_scalar(out=abc[:, :], in0=abc[:, :], scalar1=-1.0, scalar2=None, op0=ALU.mult), reads=[abc], writes=[abc])
            NB = 4
            m1 = [kb.sb(es, [128, D], BF16, "m1") for _ in range(NB)]
            c0 = [kb.sb(es, [128, D], BF16, "c0") for _ in range(NB)]
            p1 = [kb.sb(es, [128, D], BF16, "p1") for _ in range(NB)]
            acc = [kb.sb(es, [128, D], F32, "acc") for _ in range(NB)]
            t2 = [kb.sb(es, [128, D], F32, "t2") for _ in range(NB)]
            t3 = [kb.sb(es, [128, D], F32, "t3") for _ in range(NB)]
            xo = [kb.sb(es, [128, D], BF16, "xo") for _ in range(NB)]
            dtr = [kb.sb(es, [128, 16], F32, "dtr") for _ in range(NB)]
            dta = [kb.sb(es, [128, 32], F32, "dta") for _ in range(NB)]
            src = self.PR

            def s0(ti):
                b = ti % NB
                r0 = ti * 128
                first = ti in (0, 64)
                last = ti in (63, 65)
                if first:
                    kb.op("dve", lambda e, b=b: e.memset(m1[b][:, :], 0.0), writes=[m1[b]])
                    kb.dma("sp", out=m1[b][1:128, :], in_=src[r0:r0 + 127, 512:1536], reads=[src], writes=[m1[b]])
                else:
                    kb.dma("sp", out=m1[b][:, :], in_=src[r0 - 1:r0 + 127, 512:1536], reads=[src], writes=[m1[b]])
                kb.dma("sp", out=c0[b][:, :], in_=src[r0:r0 + 128, 512:1536], reads=[src], writes=[c0[b]])
                if last:
                    kb.op("dve", lambda e, b=b: e.memset(p1[b][:, :], 0.0), writes=[p1[b]])
                    kb.dma("sp", out=p1[b][0:127, :], in_=src[r0 + 1:r0 + 128, 512:1536], reads=[src], writes=[p1[b]])
                else:
                    kb.dma("sp", out=p1[b][:, :], in_=src[r0 + 1:r0 + 129, 512:1536], reads=[src], writes=[p1[b]])
                kb.dma("sp", out=dtr[b][:, :], in_=self.DTR[r0:r0 + 128, :], reads=[self.DTR], writes=[dtr[b]])

            def s1(ti):
                b = ti % NB
                kb.op("dve", lambda e, b=b: e.tensor_tensor(out=acc[b][:, :], in0=m1[b][:, :], in1=cw[0][:, :], op=ALU.mult), reads=[m1[b], cw[0]], writes=[acc[b]])
                kb.op("pool", lambda e, b=b: e.tensor_tensor(out=t2[b][:, :], in0=c0[b][:, :], in1=cw[1][:, :], op=ALU.mult), reads=[c0[b], cw[1]], writes=[t2[b]])
                kb.op("pool", lambda e, b=b: e.tensor_tensor(out=t3[b][:, :], in0=p1[b][:, :], in1=cw[2][:, :], op=ALU.mult), reads=[p1[b], cw[2]], writes=[t3[b]])
                kb.op("dve", lambda e, b=b: e.tensor_tensor(out=dtr[b][:, :], in0=dtr[b][:, :], in1=dtb[:, :], op=ALU.add), reads=[dtr[b], dtb], writes=[dtr[b]])

            def s2(ti):
                b = ti % NB
                kb.op("dve", lambda e, b=b: e.tensor_tensor(out=acc[b][:, :], in0=acc[b][:, :], in1=t2[b][:, :], op=ALU.add), reads=[acc[b], t2[b]], writes=[acc[b]])
                kb.op("pool", lambda e, b=b: e.tensor_tensor(out=t3[b][:, :], in0=t3[b][:, :], in1=cb[:, :], op=ALU.add), reads=[t3[b], cb], writes=[t3[b]])
                kb.op("act", lambda e, b=b: e.activation(out=dtr[b][:, :], in_=dtr[b][:, :], func=AF.Exp), reads=[dtr[b]], writes=[dtr[b]])

            def s3(ti):
                b = ti % NB
                kb.op("dve", lambda e, b=b: e.tensor_tensor(out=acc[b][:, :], in0=acc[b][:, :], in1=t3[b][:, :], op=ALU.add), reads=[acc[b], t3[b]], writes=[acc[b]])
                kb.op("act", lambda e, b=b: e.activation(out=dta[b][:, 0:16], in_=dtr[b][:, :], func=AF.Ln, bias=1.0), reads=[dtr[b]], writes=[dta[b]])

            def s4(ti):
                b = ti % NB
                r0 = ti * 128
                kb.op("act", lambda e, b=b: e.activation(out=xo[b][:, :], in_=acc[b][:, :], func=AF.Silu), reads=[acc[b]], writes=[xo[b]])
                kb.dma("sp", out=self.XBC[r0:r0 + 128, :], in_=xo[b][:, :], reads=[xo[b]], writes=[self.XBC])
                kb.op("dve", lambda e, b=b: e.tensor_tensor(out=dta[b][:, 16:32], in0=dta[b][:, 0:16], in1=abc[:, :], op=ALU.mult), reads=[dta[b], abc], writes=[dta[b]])
                kb.dma("sp", out=self.DTA[r0:r0 + 128, :], in_=dta[b][:, :], reads=[dta[b]], writes=[self.DTA])

            pipeline(NTILE, [s0, s1, s2, s3, s4])

    def ph_ssd(self, li, d):
        kb = self.kb
        I = self.I
        j = li // 2
        with contextlib.ExitStack() as es:
            tri = self.cload(es, "triU" if d == 0 else "triL")
            ones = self.cload(es, "onesf")
            neg = self.cload(es, "negU" if d == 0 else "negL")
            dsk = kb.sb(es, [128, 8], F32, "dsk")
            kb.dma("sp", out=dsk[:, :], in_=I["d_skip"][j, :].partition_broadcast(128), writes=[dsk])
            gss = kb.sb(es, [128, 512], F32, "gss")
            kb.dma("sp", out=gss[:, :], in_=I["g_ssd"][j, :].partition_broadcast(128), writes=[gss])
            negh = kb.sb(es, [128, 1], F32, "negh")
            kb.op("dve", lambda e: e.memset(negh[:, :], -0.5), writes=[negh])
            hs = kb.sb(es, [128, 8, 64], F32, "hs")
            hsb = kb.sb(es, [128, 8, 64], BF16, "hsb")
            kb.op("dve", lambda e: e.memset(hs[:, :, :], 0.0), writes=[hs])
            kb.op("dve", lambda e: e.memset(hsb[:, :, :], 0.0), writes=[hsb])
            NB = 4
            R = lambda shape, ty, nm, n=NB: [kb.sb(es, shape, ty, nm) for _ in range(n)]
            xbc = R([128, D], BF16, "xbc")
            dta = R([128, 32], F32, "dta")
            BCT = R([128, 4, 128], BF16, "BCT")
            cum = R([128, 16], F32, "cum")
            te = R([128, 8], F32, "te")
            cd = R([128, 8], F32, "cd")
            xdt = R([128, 8, 64], BF16, "xdt")
            xw = R([128, 8, 64], BF16, "xw")
            CBT = R([128, 2, 128], BF16, "CBT", 2)
            dU = R([128, 8, 128], F32, "dU", 2)
            Ebc = R([128, 8, 128], BF16, "Ebc", 2)
            CsT = R([128, 8, 128], BF16, "CsT")
            Sg = R([128, 8, 128], F32, "Sg", 2)
            Dm = R([128, 8, 128], BF16, "Dm", 2)
            MT = R([128, 8, 128], BF16, "MT")
            tmp = R([128, 8, 64], F32, "tmp", 2)
            yo = R([128, 512], F32, "yo")
            yf = R([128, 512], F32, "yf")
            zt = R([128, 512], BF16, "zt")
            sz = R([128, 512], F32, "sz", 2)
            junk = kb.sb(es, [128, 256], BF16, "junk")
            ssq = R([128, 2], F32, "ssq")
            rsq = R([128, 2], F32, "rsq")
            mo = R([128, 512], BF16, "mo", 2)
            pT = kb.ps(es, [128, 1024], BF16, "pT")
            pc = kb.ps(es, [128, 16], F32, "pc")
            pcb = kb.ps(es, [128, 256], F32, "pcb")
            pA = [kb.ps(es, [128, 512], F32, "pA") for _ in range(2)]
            py = [kb.ps(es, [128, 512], F32, "py") for _ in range(2)]
            pst = kb.ps(es, [128, 512], F32, "pst")
            order = ([64, 65] + list(range(64))) if d == 0 else ([65, 64] + list(range(63, -1, -1)))

            def views(n):
                b = n % NB
                X_, DT_ = xbc[b], dta[b]
                return (b, n % 2, order[n] * 128, X_, DT_, DT_[:, 8 * d:8 * d + 8], DT_[:, 16 + 8 * d:16 + 8 * d + 8],
                        X_[:, 0:512].rearrange("p (h c) -> p h c", h=8))

            def s0(n):
                b, b2, r0, X_, DT_, dt_d, da_d, x3 = views(n)
                kb.dma("sp", out=X_[:, :], in_=self.XBC[r0:r0 + 128, :], reads=[self.XBC], writes=[X_])
                kb.dma("sp", out=DT_[:, :], in_=self.DTA[r0:r0 + 128, :], reads=[self.DTA], writes=[DT_])
                if d == 1:
                    kb.dma("sp", out=yf[b][:, :], in_=self.YF[r0:r0 + 128, :], reads=[self.YF], writes=[yf[b]])
                    kb.dma("sp", out=zt[b][:, :], in_=self.PR[r0:r0 + 128, 0:512], reads=[self.PR], writes=[zt[b]])

            def s1(n):
                b, b2, r0, X_, DT_, dt_d, da_d, x3 = views(n)
                for q in range(4):
                    kb.op("pe", lambda e, q=q, X_=X_: e.transpose(out=pT[:, q * 128:(q + 1) * 128], in_=X_[:, 512 + q * 128:512 + (q + 1) * 128],
                                                               identity=self.ident_bf[:, :]), reads=[X_, self.ident_bf], writes=[pT])
                kb.op("act", lambda e, b=b: e.copy(out=BCT[b][:, :, :], in_=pT[:, 0:512].rearrange("p (q t) -> p q t", q=4)), reads=[pT], writes=[BCT[b]])
                kb.op("pe", lambda e, da_d=da_d: e.matmul(pc[:, 0:8], lhsT=tri[:, :], rhs=da_d, start=True, stop=True), reads=[tri, DT_], writes=[pc])
                kb.op("pe", lambda e, da_d=da_d: e.matmul(pc[:, 8:16], lhsT=ones[:, :], rhs=da_d, start=True, stop=True), reads=[ones, DT_], writes=[pc])
                kb.op("dve", lambda e, b=b, da_d=da_d: e.tensor_tensor(out=dU[b2][:, :, :], in0=tri[:, :].unsqueeze(1).to_broadcast([128, 8, 128]),
                                                                     in1=da_d.unsqueeze(2).to_broadcast([128, 8, 128]), op=ALU.mult),
                      reads=[tri, DT_], writes=[dU[b2]])
                kb.op("dve", lambda e, b=b: e.tensor_copy(out=cum[b][:, :], in_=pc[:, :]), reads=[pc], writes=[cum[b]])
                for hh in range(2):
                    kb.op("pe", lambda e, hh=hh, b2=b2: e.matmul(pA[hh][:, :], lhsT=ones[:, :], rhs=dU[b2][:, 4 * hh:4 * hh + 4, :].rearrange("p h i -> p (h i)"),
                                                               start=True, stop=True), reads=[ones, dU[b2]], writes=[pA[hh]])
                for g in range(2):
                    kb.op("pe", lambda e, g=g, b=b: e.matmul(pcb[:, g * 128:(g + 1) * 128], lhsT=BCT[b][:, g, :], rhs=BCT[b][:, 2 + g, :], start=True, stop=True),
                          reads=[BCT[b]], writes=[pcb])
                kb.op("dve", lambda e, b=b: e.tensor_tensor(out=te[b][:, :], in0=cum[b][:, 8:16], in1=cum[b][:, 0:8], op=ALU.subtract), reads=[cum[b]], writes=[te[b]])
                kb.op("pool", lambda e, b=b, x3=x3, dt_d=dt_d: e.tensor_tensor(out=xdt[b][:, :, :], in0=x3, in1=dt_d.unsqueeze(2).to_broadcast([128, 8, 64]), op=ALU.mult),
                      reads=[X_, DT_], writes=[xdt[b]])
                for hh in range(2):
                    kb.op("act", lambda e, hh=hh, b2=b2: e.activation(out=Ebc[b2][:, 4 * hh:4 * hh + 4, :].rearrange("p h i -> p (h i)"), in_=pA[hh][:, :], func=AF.Exp),
                          reads=[pA[hh]], writes=[Ebc[b2]])
                kb.op("act", lambda e, b=b: e.activation(out=te[b][:, :], in_=te[b][:, :], func=AF.Exp), reads=[te[b]], writes=[te[b]])
                kb.op("act", lambda e, b=b: e.activation(out=cd[b][:, :], in_=cum[b][:, 8:16], func=AF.Exp), reads=[cum[b]], writes=[cd[b]])
                for h in range(8):
                    kb.op("dve", lambda e, h=h, b=b, b2=b2: e.scalar_tensor_tensor(out=Sg[b2][:, h, :], in0=pA[h // 4][:, (h % 4) * 128:(h % 4 + 1) * 128],
                                                                                scalar=cum[b][:, h:h + 1], in1=neg[:, :], op0=ALU.subtract, op1=ALU.add),
                          reads=[pA[h // 4], cum[b], neg], writes=[Sg[b2]])
                kb.op("act", lambda e, b2=b2: e.copy(out=CBT[b2][:, :, :], in_=pcb[:, :].rearrange("p (g t) -> p g t", g=2)), reads=[pcb], writes=[CBT[b2]])
                kb.op("act", lambda e, b2=b2: e.activation(out=Dm[b2][:, :, :], in_=Sg[b2][:, :, :], func=AF.Exp), reads=[Sg[b2]], writes=[Dm[b2]])
                kb.op("pool", lambda e, b=b: e.tensor_tensor(out=xw[b][:, :, :], in0=xdt[b][:, :, :], in1=te[b][:, :].unsqueeze(2).to_broadcast([128, 8, 64]), op=ALU.mult),
                      reads=[xdt[b], te[b]], writes=[xw[b]])
                for g in range(2):
                    kb.op("pool", lambda e, g=g, b=b, b2=b2: e.tensor_tensor(out=CsT[b][:, 4 * g:4 * g + 4, :], in0=Ebc[b2][:, 4 * g:4 * g + 4, :],
                                                                           in1=BCT[b][:, 2 + g, :].unsqueeze(1).to_broadcast([128, 4, 128]), op=ALU.mult),
                          reads=[Ebc[b2], BCT[b]], writes=[CsT[b]])
                for g in range(2):
                    kb.op("dve", lambda e, g=g, b=b, b2=b2: e.tensor_tensor(out=MT[b][:, 4 * g:4 * g + 4, :], in0=Dm[b2][:, 4 * g:4 * g + 4, :],
                                                                          in1=CBT[b2][:, g, :].unsqueeze(1).to_broadcast([128, 4, 128]), op=ALU.mult),
                          reads=[Dm[b2], CBT[b2]], writes=[MT[b]])

            def s2(n):
                b, b2, r0, X_, DT_, dt_d, da_d, x3 = views(n)
                p_y = py[n % 2]
                for h in range(8):
                    kb.op("pe", lambda e, h=h, b=b: e.matmul(p_y[:, h * 64:(h + 1) * 64], lhsT=MT[b][:, h, :], rhs=xdt[b][:, h, :], start=(h == 0), stop=False,
                                                           skip_group_check=True), reads=[MT[b], xdt[b]], writes=[p_y])
                for h in range(8):
                    kb.op("pe", lambda e, h=h, b=b: e.matmul(p_y[:, h * 64:(h + 1) * 64], lhsT=CsT[b][:, h, :], rhs=hsb[:, h, :], start=False, stop=(h == 7),
                                                           skip_group_check=True), reads=[CsT[b], hsb], writes=[p_y])
                for g in range(2):
                    kb.op("pe", lambda e, g=g, b=b, X_=X_: e.matmul(pst[:, g * 256:(g + 1) * 256], lhsT=X_[:, 512 + g * 128:512 + (g + 1) * 128],
                                                                 rhs=xw[b][:, 4 * g:4 * g + 4, :].rearrange("p h c -> p (h c)"), start=True, stop=True),
                          reads=[X_, xw[b]], writes=[pst])
                kb.op("dve", lambda e, b=b, b2=b2: e.tensor_tensor(out=tmp[b2][:, :, :], in0=hs[:, :, :], in1=cd[b][:, :].unsqueeze(2).to_broadcast([128, 8, 64]), op=ALU.mult),
                      reads=[hs, cd[b]], writes=[tmp[b2]])
                kb.op("dve", lambda e, b2=b2: e.tensor_tensor(out=hs[:, :, :], in0=tmp[b2][:, :, :], in1=pst[:, :].rearrange("p (h c) -> p h c", h=8), op=ALU.add),
                      reads=[tmp[b2], pst], writes=[hs])
                kb.op("act", lambda e: e.copy(out=hsb[:, :, :], in_=hs[:, :, :]), reads=[hs], writes=[hsb])

            def s3(n):
                b, b2, r0, X_, DT_, dt_d, da_d, x3 = views(n)
                p_y = py[n % 2]
                if d == 0:
                    kb.op("pool", lambda e, b=b, x3=x3: e.tensor_tensor(out=yo[b][:, :].rearrange("p (h c) -> p h c", h=8), in0=x3,
                                                                      in1=dsk[:, :].unsqueeze(2).to_broadcast([128, 8, 64]), op=ALU.mult),
                          reads=[X_, dsk], writes=[yo[b]])
                    kb.op("dve", lambda e, b=b: e.tensor_tensor(out=yo[b][:, :], in0=p_y[:, :], in1=yo[b][:, :], op=ALU.add), reads=[p_y, yo[b]], writes=[yo[b]])
                    kb.dma("sp", out=self.YF[r0:r0 + 128, :], in_=yo[b][:, :], reads=[yo[b]], writes=[self.YF])
                else:
                    kb.op("dve", lambda e, b=b: e.tensor_tensor(out=yo[b][:, :], in0=p_y[:, :], in1=yf[b][:, :], op=ALU.add), reads=[p_y, yf[b]], writes=[yo[b]])
                    kb.op("act", lambda e, b=b, b2=b2: e.activation(out=sz[b2][:, :], in_=zt[b][:, :], func=AF.Silu), reads=[zt[b]], writes=[sz[b2]])
                    kb.op("pool", lambda e, b=b, b2=b2: e.tensor_tensor(out=yo[b][:, :], in0=yo[b][:, :], in1=sz[b2][:, :], op=ALU.mult), reads=[yo[b], sz[b2]], writes=[yo[b]])
                    for g in range(2):
                        kb.op("act", lambda e, b=b, g=g: e.activation(out=junk[:, :], in_=yo[b][:, g * 256:(g + 1) * 256], func=AF.Square, accum_out=ssq[b][:, g:g + 1]),
                              reads=[yo[b]], writes=[junk, ssq[b]])
                    kb.op("dve", lambda e, b=b: e.tensor_scalar(out=ssq[b][:, :], in0=ssq[b][:, :], scalar1=1.0 / 256, scalar2=EPS, op0=ALU.mult, op1=ALU.add),
                          reads=[ssq[b]], writes=[ssq[b]])
                    kb.op("pool", lambda e, b=b: e.tensor_tensor(out=rsq[b][:, :], in0=ssq[b][:, :], in1=negh[:, 0:1].to_broadcast([128, 2]), op=ALU.pow),
                          reads=[ssq[b], negh], writes=[rsq[b]])
                    for g in range(2):
                        kb.op("dve", lambda e, b=b, g=g, b2=b2: e.scalar_tensor_tensor(out=mo[b2][:, g * 256:(g + 1) * 256], in0=yo[b][:, g * 256:(g + 1) * 256],
                                                                                     scalar=rsq[b][:, g:g + 1], in1=gss[:, g * 256:(g + 1) * 256], op0=ALU.mult, op1=ALU.mult),
                              reads=[yo[b], rsq[b], gss], writes=[mo[b2]])
                    kb.dma("sp", out=self.MIX[r0:r0 + 128, 0:512], in_=mo[b2][:, :], reads=[mo[b2]], writes=[self.MIX])

            pipeline(len(order), [s0, s1, s2, s3])

    def ph_fourier(self, li):
        kb = self.kb
        I = self.I
        SC = 1.0 / np.sqrt(8192.0 * 128.0)
        SCC = 1.0 / np.sqrt(256.0 * 128.0)
        with contextlib.ExitStack() as es:
            F1 = self.cload(es, "F1")
            blk = [kb.sb(es, [128, 16, 1024], BF16, "blk") for _ in range(2)]
            gt = [kb.sb(es, [128, 2, 512], BF16, "gt") for _ in range(3)]
            pg = [kb.ps(es, [128, 512], F32, "pg") for _ in range(4)]
            ADv = self.AD[0:L, :].rearrange("(n1 n2) c -> n1 n2 c", n2=64)
            for rd in range(4):
                bk = blk[rd % 2]
                kb.dma("sp", out=bk[:, :, :], in_=ADv[:, rd * 16:(rd + 1) * 16, :], reads=[self.AD], writes=[bk])
                for nl in range(16):
                    n2 = rd * 16 + nl
                    g = gt[n2 % 3]
                    pr_, pi_ = pg[(2 * n2) % 4], pg[(2 * n2 + 1) % 4]
                    Ac = bk[:, nl, 0:512]
                    As = bk[:, nl, 512:1024]
                    kb.op("pe", lambda e, pr_=pr_, Ac=Ac: e.matmul(pr_[:, :], lhsT=F1[:, 0, :], rhs=Ac, start=True, stop=False), reads=[F1, bk], writes=[pr_])
                    kb.op("pe", lambda e, pr_=pr_, As=As: e.matmul(pr_[:, :], lhsT=F1[:, 1, :], rhs=As, start=False, stop=True), reads=[F1, bk], writes=[pr_])
                    kb.op("pe", lambda e, pi_=pi_, Ac=Ac: e.matmul(pi_[:, :], lhsT=F1[:, 1, :], rhs=Ac, start=True, stop=False), reads=[F1, bk], writes=[pi_])
                    kb.op("pe", lambda e, pi_=pi_, As=As: e.matmul(pi_[:, :], lhsT=F1[:, 2, :], rhs=As, start=False, stop=True), reads=[F1, bk], writes=[pi_])
                    kb.op("act", lambda e, g=g, pr_=pr_: e.copy(out=g[:, 0, :], in_=pr_[:, :]), reads=[pr_], writes=[g])
                    kb.op("dve", lambda e, g=g, pi_=pi_: e.tensor_copy(out=g[:, 1, :], in_=pi_[:, :]), reads=[pi_], writes=[g])
                    kb.dma("sp", out=self.GD[:, n2, :, :].rearrange("ri p c -> p ri c"), in_=g[:, :, :], reads=[g], writes=[self.GD])
        kb.barrier()
        with contextlib.ExitStack() as es:
            TW = self.cload(es, "TW3")
            blk = [kb.sb(es, [128, 32, 512], BF16, "blk3") for _ in range(2)]
            ot = [kb.sb(es, [64, 32, 512], BF16, "ot") for _ in range(2)]
            pg = [kb.ps(es, [64, 512], F32, "pg3") for _ in range(4)]
            GDv = self.GD[:, :, :, :].rearrange("ri n2 p c -> (ri n2) p c")
            MIXv = self.MIX[0:L, :].rearrange("(p2 p1) c -> p2 p1 c", p1=128)
            for rd in range(4):
                bk = blk[rd % 2]
                o = ot[rd % 2]
                kb.dma("sp", out=bk[:, :, :], in_=GDv[:, rd * 32:(rd + 1) * 32, :], reads=[self.GD], writes=[bk])
                for pl in range(32):
                    p1 = rd * 32 + pl
                    p = pg[p1 % 4]
                    kb.op("pe", lambda e, p=p, p1=p1, pl=pl, bk=bk: e.matmul(p[:, :], lhsT=TW[:, p1, :], rhs=bk[:, pl, :], start=True, stop=True), reads=[TW, bk], writes=[p])
                    if pl % 2 == 0:
                        kb.op("act", lambda e, p=p, pl=pl, o=o: e.activation(out=o[:, pl, :], in_=p[:, :], func=AF.Copy, scale=float(SC)), reads=[p], writes=[o])
                    else:
                        kb.op("dve", lambda e, p=p, pl=pl, o=o: e.tensor_scalar(out=o[:, pl, :], in0=p[:, :], scalar1=float(SC), scalar2=None, op0=ALU.mult), reads=[p], writes=[o])
                kb.dma("sp", out=MIXv[:, rd * 32:(rd + 1) * 32, 512:1024], in_=o[:, :, :], reads=[o], writes=[self.MIX])
            C2 = self.cload(es, "C256")
            S2 = self.cload(es, "nS256")
            ac = kb.sb(es, [128, 2, 1024], BF16, "actx")
            kb.dma("sp", out=ac[:, :, :], in_=self.AD[L:NT, :].rearrange("(t p) c -> p t c", p=128), reads=[self.AD], writes=[ac])
            oc = kb.sb(es, [128, 2, 512], BF16, "octx")
            pcx = [kb.ps(es, [128, 512], F32, "pcx") for _ in range(2)]
            for pt in range(2):
                p = pcx[pt]
                k = 0
                for nt in range(2):
                    for (M, off) in ((C2, 0), (S2, 512)):
                        kb.op("pe", lambda e, p=p, M=M, nt=nt, pt=pt, off=off, k=k: e.matmul(p[:, :], lhsT=M[:, nt, pt * 128:(pt + 1) * 128], rhs=ac[:, nt, off:off + 512],
                                                                                          start=(k == 0), stop=(k == 3)), reads=[M, ac], writes=[p])
                        k += 1
                kb.op("act", lambda e, p=p, pt=pt: e.activation(out=oc[:, pt, :], in_=p[:, :], func=AF.Copy, scale=float(SCC)), reads=[p], writes=[oc])
            kb.dma("sp", out=self.MIX[L:NT, 512:1024].rearrange("(t p) c -> p t c", p=128), in_=oc[:, :, :], reads=[oc], writes=[self.MIX])

    def ph_final(self):
        kb = self.kb
        I = self.I
        with contextlib.ExitStack() as es:
            gf = kb.sb(es, [128, D], F32, "gf")
            kb.dma("sp", out=gf[:, :], in_=I["g_final"].partition_broadcast(128), writes=[gf])
            zero = kb.sb(es, [128, D], F32, "zero")
            kb.op("dve", lambda e: e.memset(zero[:, :], 0.0), writes=[zero])
            negh = kb.sb(es, [128, 1], F32, "negh")
            kb.op("dve", lambda e: e.memset(negh[:, :], -0.5), writes=[negh])
            junk = kb.sb(es, [128, D], BF16, "junk")
            NB = 4
            xt = [kb.sb(es, [128, D], F32, "xt") for _ in range(NB)]
            of = [kb.sb(es, [128, D], F32, "of") for _ in range(NB)]
            sm = [[kb.sb(es, [128, 1], F32, "sm") for _ in range(2)] for _ in range(NB)]

            def s0(ti):
                b = ti % NB
                r0 = ti * 128
                kb.dma("sp", out=xt[b][:, :], in_=self.X[r0:r0 + 128, :], reads=[self.X], writes=[xt[b]])

            def s1(ti):
                b = ti % NB
                r0 = ti * 128
                self.norm_tile((sm[b][0], sm[b][1]), xt[b], gf, zero, of[b], None, junk, negh)
                kb.dma("sp", out=self.out[r0:r0 + 128, :], in_=of[b][:, :], reads=[of[b]], writes=[self.outk])

            pipeline(64, [s0, s1])

    def ph_dumpx(self):
        kb = self.kb
        for r0 in range(0, L, 2048):
            kb.dma("sp", out=self.out[r0:r0 + 2048, :], in_=self.X[r0:r0 + 2048, :], reads=[self.X], writes=[self.outk])


def default_phases():
    ph = [("init",)]
    for li in range(DEPTH):
        need_ctx = li < DEPTH - 1
        ph += [("mod", li)]
        if li % 2 == 0:
            ph += [("inproj", li), ("conv", li), ("ssd", li, 0), ("ssd", li, 1), ("fourier", li),
                   ("oproj", li, "w_out_e", "MIX", need_ctx)]
        else:
            ph += [("qkv", li), ("attn", li, need_ctx), ("oproj", li, "w_o", "OD", need_ctx)]
        ph += [("router", li), ("select", li), ("moe", li)]
    ph += [("final",)]
    return ph


ATT_VARIANT_RP = [2, 0, 1, 62, 63]


def att_base(rp):
    r0 = 2 * rp
    return min(min(max(r0 - 4, 0), 120), 118)


def att_variant(rp):
    return 0 if 2 <= rp <= 61 else {0: 1, 1: 2, 62: 3, 63: 4}[rp]


def build_bias_table(rpb):
    no = rpb.shape[0]
    out = np.empty((no, 5, 16, 128, 5, 128), np.float32)
    p = np.arange(128)
    q = np.arange(128)
    c = np.arange(5)
    for v, rp in enumerate(ATT_VARIANT_RP):
        r0 = 2 * rp
        base = att_base(rp)
        krow = base + 2 * c[:, None, None] + (p // 64)[None, :, None]
        kcol = (p % 64)[None, :, None]
        r = (r0 + q // 64)[None, None, :]
        col = (q % 64)[None, None, :]
        rs = np.clip(r - 4, 0, 120)
        cs = np.clip(col - 8, 0, 48)
        valid = (krow >= rs) & (krow < rs + 8) & (kcol >= cs) & (kcol < cs + 16)
        ro = np.clip(krow - r + 7, 0, 14) + 0 * kcol
        co = np.clip(kcol - col + 15, 0, 30) + 0 * krow
        g = rpb[:, :, ro, co]
        g = np.where(valid[None, None], g, np.float32(-30000.0))
        out[:, v] = np.transpose(g, (0, 1, 3, 2, 4))
    return out


def make_in_maps(inputs, n_cores, names):
    consts = host_consts()
    shared = {}
    for k in names:
        if k in consts:
            shared[k] = consts[k]
        elif k in ("x", "ctx", "c"):
            pass
        elif k == "biasT":
            shared[k] = build_bias_table(inputs["rpb"])
        elif k in ("a_log", "dt_bias"):
            shared[k] = np.ascontiguousarray(inputs[k].reshape(2, 16))
        else:
            shared[k] = np.ascontiguousarray(inputs[k])
    maps = []
    for b in range(n_cores):
        m = dict(shared)
        for k in ("x", "ctx", "c"):
            if k in names:
                m[k] = np.ascontiguousarray(inputs[k][b])
        maps.append(m)
    return maps


def kernel(**inputs):
    inputs = {k: np.asarray(v) for k, v in inputs.items()}
    prog = Prog(default_phases())
    in_maps = make_in_maps(inputs, 4, list(prog.I.keys()))
    res = run_bass_kernel_spmd(prog.nc, in_maps, core_ids=list(range(4)))
    out = np.stack([res.results[b]["out"] for b in range(4)], axis=0)
    return out.astype(np.float32)
```

```python
import contextlib
import numpy as np
import ml_dtypes
import concourse.bass as bass
import concourse.mybir as mybir
from concourse.bass_utils import run_bass_kernel_spmd

F32 = mybir.dt.float32
BF16 = mybir.dt.bfloat16
I32 = mybir.dt.int32
AF = mybir.ActivationFunctionType
ALU = mybir.AluOpType
AX = mybir.AxisListType
IOA = bass.IndirectOffsetOnAxis if hasattr(bass, "IndirectOffsetOnAxis") else None

D = 1024
L = 8192
LC = 256
NT = L + LC
NTILE = NT // 128
DEPTH = 4
NE = 16
FF = 2048
NBLK = 4
BLK = L // NBLK
PSEL = NE * NBLK
CAPB = 320
NSLOT = NBLK * CAPB
NCALL = NSLOT // 128
CAPC = 32
XR = NT + (NCALL + 1) * 128
EPS = 1e-6


class Trk:
    __slots__ = ("w", "r", "x")

    def __init__(self, x=False):
        self.w = None
        self.r = {}
        self.x = x


class Buf:
    def __init__(self, t, x=False):
        self.t = t
        self.k = Trk(x)

    def __getitem__(self, key):
        return self.t[key]


class KB:
    ND = 40

    def __init__(self, nc):
        self.nc = nc
        self.es = contextlib.ExitStack()
        self.eng = {"pe": nc.tensor, "act": nc.scalar, "dve": nc.vector, "pool": nc.gpsimd, "sp": nc.sync}
        self.csem = {}
        self.ccnt = {}
        for e in ("pe", "act", "dve", "pool"):
            self.csem[e] = self.es.enter_context(nc.semaphore("cs_" + e))
            self.ccnt[e] = 0
        self.dsem = [self.es.enter_context(nc.semaphore("ds%d" % i)) for i in range(self.ND)]
        self.dcnt = [0] * self.ND
        self.dnext = 0
        self.seen = {e: {} for e in self.eng}
        self.uid = 0

    def sb(self, es, shape, dtype, name=None):
        self.uid += 1
        return Buf(es.enter_context(self.nc.sbuf_tensor("%s_%d" % (name or "sb", self.uid), list(shape), dtype)))

    def ps(self, es, shape, dtype, name=None):
        self.uid += 1
        return Buf(es.enter_context(self.nc.psum_tensor("%s_%d" % (name or "ps", self.uid), list(shape), dtype)), x=True)

    def dram(self, name, shape, dtype):
        return Buf(self.nc.dram_tensor(name, list(shape), dtype, kind="Internal").ap())

    def _sem(self, ch):
        return self.csem[ch] if isinstance(ch, str) else self.dsem[ch]

    def _deps(self, eng, reads, writes, is_dma):
        deps = {}

        def add(ev, raw):
            if ev is None:
                return
            ch, v = ev
            if (not is_dma) and ch == eng:
                if eng == "pe" or not raw:
                    return
            if deps.get(ch, 0) < v:
                deps[ch] = v

        for t in reads:
            add(t.w, True)
            if t.x:
                for ch, v in t.r.items():
                    if ch != eng:
                        add((ch, v), False)
        for t in writes:
            add(t.w, False)
            for ch, v in t.r.items():
                add((ch, v), False)
        return deps

    def _wait(self, eng, deps):
        s = self.seen[eng]
        for ch, v in deps.items():
            if s.get(ch, 0) < v:
                self.eng[eng].wait_ge(self._sem(ch), v)
                s[ch] = v

    @staticmethod
    def _trks(lst):
        return [b.k if isinstance(b, Buf) else b for b in lst]

    def op(self, eng, fn, reads=(), writes=()):
        reads = self._trks(reads)
        writes = self._trks(writes)
        self._wait(eng, self._deps(eng, reads, writes, False))
        ins = fn(self.eng[eng])
        self.ccnt[eng] += 1
        v = self.ccnt[eng]
        ins.then_inc(self.csem[eng], 1)
        for t in reads:
            if t.r.get(eng, 0) < v:
                t.r[eng] = v
        for t in writes:
            t.w = (eng, v)
            t.r = {}
        return ins

    def dma(self, q, out=None, in_=None, reads=(), writes=(), fn=None, **kw):
        reads = self._trks(reads)
        writes = self._trks(writes)
        s = self.dnext
        self.dnext = (s + 1) % self.ND
        deps = self._deps(q, reads, writes, True)
        if self.dcnt[s] > 0 and deps.get(s, 0) < self.dcnt[s]:
            deps[s] = self.dcnt[s]
        self._wait(q, deps)
        if fn is None:
            ins = self.eng[q].dma_start(out=out, in_=in_, **kw)
        else:
            ins = fn(self.eng[q])
        self.dcnt[s] += 16
        v = self.dcnt[s]
        ins.then_inc(self.dsem[s], 16)
        for t in reads:
            if t.r.get(s, 0) < v:
                t.r[s] = v
        for t in writes:
            t.w = (s, v)
            t.r = {}
        return ins

    def barrier(self):
        for e in self.eng:
            deps = {}
            for c in self.csem:
                if self.ccnt[c] > 0 and c != e:
                    deps[c] = self.ccnt[c]
            for s in range(self.ND):
                if self.dcnt[s] > 0:
                    deps[s] = self.dcnt[s]
            self._wait(e, deps)
        for e in self.csem:
            if self.ccnt[e] > 0:
                s = self.seen[e]
                if s.get(e, 0) < self.ccnt[e]:
                    self.eng[e].wait_ge(self.csem[e], self.ccnt[e])
                    s[e] = self.ccnt[e]


def host_consts():
    c = {}
    c["ident_bf"] = np.eye(128, dtype=np.float32).astype(ml_dtypes.bfloat16)
    c["ident_f"] = np.eye(128, dtype=np.float32)
    p = np.arange(128)
    c["gsum"] = (p[:, None] // NBLK == p[None, :] // NBLK).astype(np.float32)
    c["keyl"] = np.broadcast_to((BLK - np.arange(BLK, dtype=np.float32))[None, :], (128, BLK)).copy()
    c["keyc"] = np.broadcast_to((256 - np.arange(256, dtype=np.float32))[None, :], (128, 256)).copy()
    c["basel"] = ((p % NBLK) * BLK + BLK).astype(np.float32)[:, None].copy()
    c["basec"] = np.full((128, 1), L + 256, np.float32)
    sig = (p[:, None] % NBLK) * CAPB + np.arange(CAPB)[None, :]
    c["dumpl"] = (NT + sig).astype(np.float32)
    c["dumpc"] = np.broadcast_to((NT + NSLOT + np.arange(CAPC, dtype=np.float32))[None, :], (128, CAPC)).copy()
    k = np.arange(128)
    c["triU"] = (k[:, None] <= k[None, :]).astype(np.float32)
    c["triL"] = (k[:, None] >= k[None, :]).astype(np.float32)
    c["onesf"] = np.ones((128, 128), np.float32)
    c["negU"] = np.where(k[None, :] >= k[:, None], 0.0, -30000.0).astype(np.float32)
    c["negL"] = np.where(k[None, :] <= k[:, None], 0.0, -30000.0).astype(np.float32)
    ang = 2 * np.pi * np.outer(k, k) / 128.0
    bf = ml_dtypes.bfloat16
    c["CS"] = np.concatenate([np.cos(ang), np.sin(ang)], axis=1).astype(np.float32).astype(bf)
    c["F1"] = np.stack([np.cos(ang), -np.sin(ang), -np.cos(ang)], axis=1).astype(np.float32).astype(bf)
    n2 = np.arange(64)[:, None, None]
    p1 = np.arange(128)[None, :, None]
    p2 = np.arange(64)[None, None, :]
    th = 2 * np.pi * (n2 * p2 / 64.0 + n2 * p1 / 8192.0)
    c["TW3"] = np.concatenate([np.cos(th), np.sin(th)], axis=0).astype(np.float32).astype(bf)
    n = np.arange(256)
    a2 = 2 * np.pi * np.outer(n, n) / 256.0
    c["C256"] = np.cos(a2).reshape(2, 128, 256).transpose(1, 0, 2).astype(np.float32).astype(bf)
    c["nS256"] = (-np.sin(a2)).reshape(2, 128, 256).transpose(1, 0, 2).astype(np.float32).astype(bf)
    return c


CONST_SPECS = {
    "triU": ([128, 128], F32), "triL": ([128, 128], F32), "onesf": ([128, 128], F32), "negU": ([128, 128], F32),
    "negL": ([128, 128], F32), "CS": ([128, 256], BF16), "F1": ([128, 3, 128], BF16), "TW3": ([128, 128, 64], BF16),
    "C256": ([128, 2, 256], BF16), "nS256": ([128, 2, 256], BF16),
    "ident_bf": ([128, 128], BF16), "ident_f": ([128, 128], F32), "gsum": ([128, 128], F32),
    "keyl": ([128, BLK], F32), "keyc": ([128, 256], F32), "basel": ([128, 1], F32), "basec": ([128, 1], F32),
    "dumpl": ([128, CAPB], F32), "dumpc": ([128, CAPC], F32),
}


IN_SPECS = {
    "x": ([L, D], F32), "ctx": ([LC, D], F32), "c": ([D], F32), "c_ctx": ([D], F32),
    "w_mod": ([DEPTH, D, 6 * D], F32), "b_mod": ([DEPTH, 6 * D], F32), "g_mix": ([DEPTH, D], F32), "g_ffn": ([DEPTH, D], F32),
    "w_router": ([DEPTH, D, NE], F32), "w_e1": ([DEPTH, NE, D, FF], F32), "w_e3": ([DEPTH, NE, D, FF], F32),
    "w_e2": ([DEPTH, NE, FF, D], F32), "g_final": ([D], F32),
    "w_in_e": ([2, D, 2064], F32), "conv_w": ([2, 3, D], F32), "conv_b": ([2, D], F32), "a_log": ([2, 16], F32),
    "dt_bias": ([2, 16], F32), "d_skip": ([2, 8], F32), "g_ssd": ([2, 512], F32), "w_out_e": ([2, D, D], F32),
    "w_qkv": ([2, D, 3 * D], F32), "w_o": ([2, D, D], F32), "biasT": ([2, 5, 16, 128, 5, 128], F32),
}


def pipeline(n, stages):
    ns = len(stages)
    for step in range(n + ns - 1):
        for si in range(ns - 1, -1, -1):
            k = step - si
            if 0 <= k < n:
                stages[si](k)


class Prog:
    def __init__(self, phases, debug_out=None):
        self.phases = phases
        self.debug_out = debug_out
        nc = bass.Bass("TRN2", target_bir_lowering=False)
        self.nc = nc
        self.kb = KB(nc)
        kb = self.kb
        dt = nc.dram_tensor

        class LazyIn(dict):
            def __missing__(d, k):
                shp, ty = IN_SPECS[k] if k in IN_SPECS else CONST_SPECS[k]
                d[k] = dt(k, list(shp), ty, kind="ExternalInput").ap()
                return d[k]
        self.I = LazyIn()
        self.out = dt("out", [L, D], F32, kind="ExternalOutput").ap()
        self.X = kb.dram("Xres", [XR, D], F32)
        self.H2 = kb.dram("H2", [XR, D], BF16)
        self.AFF = kb.dram("AFF", [XR, NE], F32)
        self.AFFT = kb.dram("AFFT", [NE, L], F32)
        self.AFFTC = kb.dram("AFFTC", [NE, LC], F32)
        self.IDXD = kb.dram("IDXD", [PSEL, CAPB], I32)
        self.IDXC = kb.dram("IDXC", [NE, CAPC], I32)
        self.MODD = kb.dram("MODD", [2, 6 * D], F32)
        self.outk = Trk()
        self.build()

    def build(self):
        kb = self.kb
        with contextlib.ExitStack() as ges:
            self.ges = ges
            self.ident_bf = kb.sb(ges, [128, 128], BF16, "identbf")
            self.ident_f = kb.sb(ges, [128, 128], F32, "identf")
            kb.dma("sp", out=self.ident_bf[:, :], in_=self.I["ident_bf"][:, :], writes=[self.ident_bf])
            kb.dma("sp", out=self.ident_f[:, :], in_=self.I["ident_f"][:, :], writes=[self.ident_f])
            for ph in self.phases:
                name = ph[0]
                getattr(self, "ph_" + name)(*ph[1:])
                kb.barrier()
            kb.barrier()
        kb.es.close()

    def ph_init(self):
        kb = self.kb
        with contextlib.ExitStack() as es:
            for r0 in range(0, L, 2048):
                kb.dma("sp", out=self.X[r0:r0 + 2048, :], in_=self.I["x"][r0:r0 + 2048, :], writes=[self.X])
            kb.dma("sp", out=self.X[L:NT, :], in_=self.I["ctx"][:, :], writes=[self.X])
            z = kb.sb(es, [128, D], F32, "z")
            zb = kb.sb(es, [128, D], BF16, "zb")
            kb.op("dve", lambda e: e.memset(z[:, :], 0.0), writes=[z])
            kb.op("dve", lambda e: e.memset(zb[:, :], 0.0), writes=[zb])
            for j in range(NCALL + 1):
                r0 = NT + j * 128
                kb.dma("sp", out=self.X[r0:r0 + 128, :], in_=z[:, :], reads=[z], writes=[self.X])
                kb.dma("sp", out=self.H2[r0:r0 + 128, :], in_=zb[:, :], reads=[zb], writes=[self.H2])
                kb.dma("sp", out=self.AFF[r0:r0 + 128, :], in_=z[:, 0:NE], reads=[z], writes=[self.AFF])

    def ph_mod(self, li):
        kb = self.kb
        I = self.I
        with contextlib.ExitStack() as es:
            cv = kb.sb(es, [128, 2, 8], F32, "cv")
            kb.dma("sp", out=cv[:, 0, :], in_=I["c"].rearrange("(kc p) -> p kc", p=128), writes=[cv],
                   allow_slow_non_contiguous=True)
            kb.dma("sp", out=cv[:, 1, :], in_=I["c_ctx"].rearrange("(kc p) -> p kc", p=128), writes=[cv],
                   allow_slow_non_contiguous=True)
            sv = kb.sb(es, [128, 2, 8], F32, "sv")
            kb.op("act", lambda e: e.activation(out=sv[:, :, :], in_=cv[:, :, :], func=AF.Silu), reads=[cv], writes=[sv])
            lb = kb.sb(es, [128, 2, 8, 128], BF16, "lb")
            for s in range(2):
                kb.op("dve", lambda e, s=s: e.tensor_copy(out=lb[:, s, :, :], in_=sv[:, s, :].unsqueeze(2).to_broadcast([128, 8, 128])),
                      reads=[sv], writes=[lb])
            gmix = kb.sb(es, [128, D], F32, "gmix")
            gffn = kb.sb(es, [128, D], F32, "gffn")
            kb.dma("sp", out=gmix[:, :], in_=I["g_mix"][li, :].partition_broadcast(128), writes=[gmix])
            kb.dma("sp", out=gffn[:, :], in_=I["g_ffn"][li, :].partition_broadcast(128), writes=[gffn])
            NB = 3
            wm = [kb.sb(es, [128, 8, 512], F32, "wm") for _ in range(NB)]
            wmb = [kb.sb(es, [128, 8, 512], BF16, "wmb") for _ in range(NB)]
            bm = [kb.sb(es, [128, 512], F32, "bm") for _ in range(NB)]
            pp = [kb.ps(es, [128, 512], F32, "pm") for _ in range(4)]
            res = [kb.sb(es, [128, 512], F32, "res") for _ in range(4)]
            wsrc = I["w_mod"][li].rearrange("(kc p) n -> p kc n", p=128)

            def s0(n):
                w = wm[n % NB]
                b = bm[n % NB]
                kb.dma("sp", out=w[:, :, :], in_=wsrc[:, :, n * 512:(n + 1) * 512], writes=[w])
                kb.dma("sp", out=b[:, :], in_=I["b_mod"][li, n * 512:(n + 1) * 512].partition_broadcast(128), writes=[b])

            def s1(n):
                w = wm[n % NB]
                wb_ = wmb[n % NB]
                if n % 2 == 0:
                    kb.op("pool", lambda e: e.tensor_copy(out=wb_[:, :, :], in_=w[:, :, :]), reads=[w], writes=[wb_])
                else:
                    kb.op("act", lambda e: e.copy(out=wb_[:, :, :], in_=w[:, :, :]), reads=[w], writes=[wb_])

            def s2(n):
                w = wmb[n % NB]
                b = bm[n % NB]
                for s in range(2):
                    p = pp[(2 * n + s) % 4]
                    r = res[(2 * n + s) % 4]
                    for kc in range(8):
                        kb.op("pe", lambda e, kc=kc, s=s, p=p, w=w: e.matmul(p[:, :], lhsT=lb[:, s, kc, :], rhs=w[:, kc, :],
                                                                          start=(kc == 0), stop=(kc == 7)),
                              reads=[lb, w], writes=[p])
                    which = n // 2
                    kb.op("dve", lambda e, p=p, r=r, b=b: e.tensor_tensor(out=r[:, :], in0=p[:, :], in1=b[:, :], op=ALU.add),
                          reads=[p, b], writes=[r])
                    if which in (1, 4):
                        g = gmix if which == 1 else gffn
                        c0 = (n % 2) * 512
                        kb.op("dve", lambda e, r=r, g=g, c0=c0: e.scalar_tensor_tensor(out=r[:, :], in0=r[:, :], scalar=1.0,
                                                                                     in1=g[:, c0:c0 + 512], op0=ALU.add, op1=ALU.mult),
                              reads=[r, g], writes=[r])
                    kb.dma("sp", out=self.MODD[s:s + 1, n * 512:(n + 1) * 512], in_=r[0:1, :], reads=[r], writes=[self.MODD])

            pipeline(12, [s0, s1, s2])

    def load_vec(self, es, s, which, name="vec"):
        kb = self.kb
        t = kb.sb(es, [128, D], F32, name)
        kb.dma("sp", out=t[:, :], in_=self.MODD[s, which * D:(which + 1) * D].partition_broadcast(128),
               reads=[self.MODD], writes=[t])
        return t

    def norm_tile(self, rstd_tmp, xt, s_bc, sh_bc, out_f32, out_bf, junk, negh):
        self.norm_a(rstd_tmp, xt, junk, negh)
        self.norm_b(rstd_tmp, xt, s_bc, sh_bc, out_f32, out_bf)

    def norm_a(self, rstd_tmp, xt, junk, negh):
        kb = self.kb
        ss, rs = rstd_tmp
        kb.op("act", lambda e: e.activation(out=junk[:, :], in_=xt[:, :], func=AF.Square, accum_out=ss[:, 0:1]),
              reads=[xt], writes=[junk, ss])
        kb.op("dve", lambda e: e.tensor_scalar(out=ss[:, 0:1], in0=ss[:, 0:1], scalar1=1.0 / D, scalar2=EPS, op0=ALU.mult, op1=ALU.add),
              reads=[ss], writes=[ss])
        kb.op("pool", lambda e: e.tensor_tensor(out=rs[:, 0:1], in0=ss[:, 0:1], in1=negh[:, 0:1], op=ALU.pow),
              reads=[ss, negh], writes=[rs])

    def norm_b(self, rstd_tmp, xt, s_bc, sh_bc, out_f32, out_bf):
        kb = self.kb
        ss, rs = rstd_tmp
        kb.op("dve", lambda e: e.scalar_tensor_tensor(out=out_f32[:, :], in0=xt[:, :], scalar=rs[:, 0:1], in1=s_bc[:, :],
                                                      op0=ALU.mult, op1=ALU.mult),
              reads=[xt, rs, s_bc], writes=[out_f32])
        kb.op("pool", lambda e: e.tensor_tensor(out=out_f32[:, :], in0=out_f32[:, :], in1=sh_bc[:, :], op=ALU.add),
              reads=[out_f32, sh_bc], writes=[out_f32])
        if out_bf is not None:
            kb.op("act", lambda e: e.copy(out=out_bf[:, :], in_=out_f32[:, :]), reads=[out_f32], writes=[out_bf])

    def ph_router(self, li):
        kb = self.kb
        I = self.I
        with contextlib.ExitStack() as es:
            svec = [self.load_vec(es, s, 4, "s2") for s in range(2)]
            shvec = [self.load_vec(es, s, 3, "sh2") for s in range(2)]
            negh = kb.sb(es, [128, 1], F32, "negh")
            kb.op("dve", lambda e: e.memset(negh[:, :], -0.5), writes=[negh])
            wr = kb.sb(es, [128, 8, NE], F32, "wr")
            kb.dma("sp", out=wr[:, :, :], in_=I["w_router"][li].rearrange("(kc p) n -> p kc n", p=128), writes=[wr])
            AT = kb.sb(es, [NE, L], F32, "AT")
            ATC = kb.sb(es, [NE, LC], F32, "ATC")
            NB = 6
            xt = [kb.sb(es, [128, D], F32, "xt") for _ in range(NB)]
            hf = [kb.sb(es, [128, D], F32, "hf") for _ in range(NB)]
            hb = [kb.sb(es, [128, D], BF16, "hb") for _ in range(NB)]
            junk = kb.sb(es, [128, D], BF16, "junk")
            hT = [kb.sb(es, [128, 8, 128], F32, "hT") for _ in range(NB)]
            sm = [[kb.sb(es, [128, 1], F32, "sm") for _ in range(6)] for _ in range(NB)]
            lg = [kb.sb(es, [128, NE], F32, "lg") for _ in range(NB)]
            af = [kb.sb(es, [128, NE], F32, "af") for _ in range(NB)]
            pT = [kb.ps(es, [128, 512], F32, "pT") for _ in range(4)]
            pl = [kb.ps(es, [128, NE], F32, "pl") for _ in range(2)]
            pa = [kb.ps(es, [NE, 128], F32, "pa") for _ in range(2)]

            def s0(ti):
                b = ti % NB
                r0 = ti * 128
                kb.dma("sp", out=xt[b][:, :], in_=self.X[r0:r0 + 128, :], reads=[self.X], writes=[xt[b]])

            def s1a(ti):
                b = ti % NB
                self.norm_a((sm[b][0], sm[b][1]), xt[b], junk, negh)

            def s1(ti):
                b = ti % NB
                s = 0 if ti < 64 else 1
                r0 = ti * 128
                self.norm_b((sm[b][0], sm[b][1]), xt[b], svec[s], shvec[s], hf[b], hb[b])
                kb.dma("sp", out=self.H2[r0:r0 + 128, :], in_=hb[b][:, :], reads=[hb[b]], writes=[self.H2])

            def s2(ti):
                b = ti % NB
                for half in range(2):
                    p = pT[(2 * ti + half) % 4]
                    for q in range(4):
                        kc = half * 4 + q
                        kb.op("pe", lambda e, p=p, q=q, kc=kc, b=b: e.transpose(out=p[:, q * 128:(q + 1) * 128],
                                                                              in_=hf[b][:, kc * 128:(kc + 1) * 128],
                                                                              identity=self.ident_f[:, :]),
                              reads=[hf[b], self.ident_f], writes=[p])
                    kb.op("act", lambda e, p=p, half=half, b=b: e.copy(out=hT[b][:, half * 4:half * 4 + 4, :],
                                                                     in_=p[:, :].rearrange("p (q t) -> p q t", q=4)),
                          reads=[p], writes=[hT[b]])

            def s3(ti):
                b = ti % NB
                s = 0 if ti < 64 else 1
                r0 = ti * 128
                pp = pl[ti % 2]
                for kc in range(8):
                    kb.op("pe", lambda e, kc=kc, pp=pp, b=b: e.matmul(pp[:, :], lhsT=hT[b][:, kc, :], rhs=wr[:, kc, :],
                                                                    start=(kc == 0), stop=(kc == 7)),
                          reads=[hT[b], wr], writes=[pp])
                mx, nmx, se, rse = sm[b][2], sm[b][3], sm[b][4], sm[b][5]
                kb.op("dve", lambda e, pp=pp, mx=mx: e.reduce_max(out=mx[:, 0:1], in_=pp[:, :], axis=AX.X), reads=[pp], writes=[mx])
                kb.op("dve", lambda e, mx=mx, nmx=nmx: e.tensor_scalar(out=nmx[:, 0:1], in0=mx[:, 0:1], scalar1=-1.0, scalar2=None, op0=ALU.mult),
                      reads=[mx], writes=[nmx])
                kb.op("act", lambda e, pp=pp, b=b, nmx=nmx, se=se: e.activation(out=lg[b][:, :], in_=pp[:, :], func=AF.Exp, bias=nmx[:, 0:1],
                                                                              accum_out=se[:, 0:1]),
                      reads=[pp, nmx], writes=[lg[b], se])
                kb.op("dve", lambda e, se=se, rse=rse: e.reciprocal(out=rse[:, 0:1], in_=se[:, 0:1]), reads=[se], writes=[rse])
                kb.op("dve", lambda e, b=b, rse=rse: e.tensor_scalar(out=af[b][:, :], in0=lg[b][:, :], scalar1=rse[:, 0:1], scalar2=None, op0=ALU.mult),
                      reads=[lg[b], rse], writes=[af[b]])
                kb.dma("sp", out=self.AFF[r0:r0 + 128, :], in_=af[b][:, :], reads=[af[b]], writes=[self.AFF])
                pq = pa[ti % 2]
                kb.op("pe", lambda e, pq=pq, b=b: e.matmul(pq[:, :], lhsT=af[b][:, :], rhs=self.ident_f[:, :], start=True, stop=True),
                      reads=[af[b], self.ident_f], writes=[pq])
                if s == 0:
                    kb.op("act", lambda e, pq=pq, r0=r0: e.copy(out=AT[:, r0:r0 + 128], in_=pq[:, :]), reads=[pq], writes=[AT])
                else:
                    kb.op("act", lambda e, pq=pq, r0=r0: e.copy(out=ATC[:, r0 - L:r0 - L + 128], in_=pq[:, :]), reads=[pq], writes=[ATC])

            pipeline(NTILE, [s0, s1a, s1, s2, s3])
            kb.dma("sp", out=self.AFFT[:, :], in_=AT[:, :], reads=[AT], writes=[self.AFFT])
            kb.dma("sp", out=self.AFFTC[:, :], in_=ATC[:, :], reads=[ATC], writes=[self.AFFTC])

    def select(self, es, A, P, F, K, cap, key_c, base_c, dump_c, gsum, idx_out_dram, tag):
        kb = self.kb
        lo = kb.sb(es, [P, 1], F32, "lo" + tag)
        hi = kb.sb(es, [P, 1], F32, "hi" + tag)
        mid = kb.sb(es, [P, 1], F32, "mid" + tag)
        cnt = kb.sb(es, [P, 1], F32, "cnt" + tag)
        ge = kb.sb(es, [P, 1], F32, "ge" + tag)
        d1 = kb.sb(es, [P, 1], F32, "d1" + tag)
        W = kb.sb(es, [P, F], F32, "W" + tag)
        pc = kb.ps(es, [P, 1], F32, "pc" + tag)
        kb.op("dve", lambda e: e.memset(lo[:, :], 0.0), writes=[lo])
        kb.op("dve", lambda e: e.memset(hi[:, :], 1.0), writes=[hi])
        for it in range(36):
            kb.op("dve", lambda e: e.tensor_tensor(out=mid[:, :], in0=lo[:, :], in1=hi[:, :], op=ALU.add), reads=[lo, hi], writes=[mid])
            kb.op("dve", lambda e: e.tensor_scalar(out=mid[:, :], in0=mid[:, :], scalar1=0.5, scalar2=None, op0=ALU.mult), reads=[mid], writes=[mid])
            kb.op("dve", lambda e: e.tensor_scalar(out=W[:, :], in0=A[:, :], scalar1=mid[:, 0:1], scalar2=0.0, op0=ALU.is_ge, op1=ALU.add,
                                                   accum_out=cnt[:, 0:1]), reads=[A, mid], writes=[W, cnt])
            if gsum is not None:
                kb.op("pe", lambda e: e.matmul(pc[:, :], lhsT=gsum[0:P, 0:P], rhs=cnt[:, :], start=True, stop=True), reads=[gsum, cnt], writes=[pc])
                src = pc
            else:
                src = cnt
            kb.op("dve", lambda e, src=src: e.tensor_scalar(out=ge[:, :], in0=src[:, :], scalar1=float(K) - 0.5, scalar2=None, op0=ALU.is_ge),
                  reads=[src], writes=[ge])
            kb.op("dve", lambda e: e.tensor_tensor(out=d1[:, :], in0=mid[:, :], in1=lo[:, :], op=ALU.subtract), reads=[mid, lo], writes=[d1])
            kb.op("dve", lambda e: e.scalar_tensor_tensor(out=lo[:, :], in0=d1[:, :], scalar=ge[:, 0:1], in1=lo[:, :], op0=ALU.mult, op1=ALU.add),
                  reads=[d1, ge, lo], writes=[lo])
            kb.op("dve", lambda e: e.tensor_tensor(out=d1[:, :], in0=hi[:, :], in1=mid[:, :], op=ALU.subtract), reads=[hi, mid], writes=[d1])
            kb.op("dve", lambda e: e.scalar_tensor_tensor(out=hi[:, :], in0=d1[:, :], scalar=ge[:, 0:1], in1=mid[:, :], op0=ALU.mult, op1=ALU.add),
                  reads=[d1, ge, mid], writes=[hi])
        kb.op("dve", lambda e: e.scalar_tensor_tensor(out=W[:, :], in0=A[:, :], scalar=lo[:, 0:1], in1=key_c[0:P, :], op0=ALU.is_ge, op1=ALU.mult),
              reads=[A, lo, key_c], writes=[W])
        Lv = kb.sb(es, [P, cap], F32, "Lv" + tag)
        for r in range(cap // 8):
            kb.op("dve", lambda e, r=r: e.max(out=Lv[:, 8 * r:8 * r + 8], in_=W[:, :]), reads=[W], writes=[Lv])
            kb.op("dve", lambda e, r=r: e.match_replace(out=W[:, :], in_to_replace=Lv[:, 8 * r:8 * r + 8], in_values=W[:, :], imm_value=0.0),
                  reads=[W, Lv], writes=[W])
        tok = kb.sb(es, [P, cap], F32, "tok" + tag)
        vm = kb.sb(es, [P, cap], F32, "vm" + tag)
        idx = kb.sb(es, [P, cap], I32, "idx" + tag)
        kb.op("dve", lambda e: e.tensor_scalar(out=tok[:, :], in0=Lv[:, :], scalar1=-1.0, scalar2=base_c[0:P, 0:1], op0=ALU.mult, op1=ALU.add),
              reads=[Lv, base_c], writes=[tok])
        kb.op("dve", lambda e: e.tensor_tensor(out=tok[:, :], in0=tok[:, :], in1=dump_c[0:P, :], op=ALU.subtract), reads=[tok, dump_c], writes=[tok])
        kb.op("dve", lambda e: e.tensor_scalar(out=vm[:, :], in0=Lv[:, :], scalar1=0.5, scalar2=None, op0=ALU.is_ge), reads=[Lv], writes=[vm])
        kb.op("dve", lambda e: e.tensor_tensor(out=tok[:, :], in0=tok[:, :], in1=vm[:, :], op=ALU.mult), reads=[tok, vm], writes=[tok])
        kb.op("dve", lambda e: e.tensor_tensor(out=tok[:, :], in0=tok[:, :], in1=dump_c[0:P, :], op=ALU.add), reads=[tok, dump_c], writes=[tok])
        kb.op("dve", lambda e: e.tensor_copy(out=idx[:, :], in_=tok[:, :]), reads=[tok], writes=[idx])
        kb.dma("sp", out=idx_out_dram[:, :], in_=idx[:, :], reads=[idx], writes=[idx_out_dram])

    def ph_select(self, li):
        kb = self.kb
        I = self.I
        with contextlib.ExitStack() as es:
            cs = {}
            for k in ("gsum", "keyl", "keyc", "basel", "basec", "dumpl", "dumpc"):
                shp, ty = CONST_SPECS[k]
                cs[k] = kb.sb(es, shp, ty, k)
                kb.dma("sp", out=cs[k][:, :], in_=I[k][:, :], writes=[cs[k]])
            A = kb.sb(es, [PSEL, BLK], F32, "Asel")
            kb.dma("sp", out=A[:, :], in_=self.AFFT[:, :].rearrange("e (b t) -> (e b) t", b=NBLK), reads=[self.AFFT], writes=[A])
            self.select(es, A, PSEL, BLK, 1024, CAPB, cs["keyl"], cs["basel"], cs["dumpl"], cs["gsum"], self.IDXD, "l")
            Ac = kb.sb(es, [NE, LC], F32, "Aselc")
            kb.dma("sp", out=Ac[:, :], in_=self.AFFTC[:, :], reads=[self.AFFTC], writes=[Ac])
            self.select(es, Ac, NE, LC, CAPC, CAPC, cs["keyc"], cs["basec"], cs["dumpc"], None, self.IDXC, "c")

    def ph_moe(self, li):
        kb = self.kb
        I = self.I
        NS = NSLOT + CAPC
        chunks = []
        c0 = 0
        while c0 < NS:
            cn = min(512, NS - c0)
            chunks.append((c0, cn))
            c0 += cn
        NCH = len(chunks)
        with contextlib.ExitStack() as es:
            ga2 = [self.load_vec(es, s, 5, "ga2") for s in range(2)]
            idxt = kb.sb(es, [128, NE, NCALL], I32, "idxt")
            kb.dma("sp", out=idxt[:, :, :], in_=self.IDXD[:, :].rearrange("(e b) r -> e (b r)", b=NBLK).rearrange("e (j p) -> p e j", p=128),
                   reads=[self.IDXD], writes=[idxt], allow_slow_non_contiguous=True)
            idxc = kb.sb(es, [CAPC, NE], I32, "idxc")
            kb.dma("sp", out=idxc[:, :], in_=self.IDXC[:, :].rearrange("e p -> p e"), reads=[self.IDXC], writes=[idxc],
                   allow_slow_non_contiguous=True)
            stage = [kb.sb(es, [128, 4096], F32, "stage") for _ in range(2)]
            w13 = [[kb.sb(es, [128, 8, 512], BF16, "w13") for _ in range(2)] for _ in range(2)]
            w2 = kb.sb(es, [128, 16, D], BF16, "w2")
            w2k = [Trk() for _ in range(4)]
            xgT = kb.sb(es, [128, 8, NS], BF16, "xgT")
            gT = kb.sb(es, [128, 16, NS], BF16, "gT")
            gTk = [Trk() for _ in range(16)]
            G = [kb.sb(es, [128, D], BF16, "G") for _ in range(NCALL + 1)]
            gate = [kb.sb(es, [128, NCALL + 1, NE], F32, "gate") for _ in range(2)]
            gatek = [[Trk() for _ in range(NCALL + 1)] for _ in range(2)]
            yb = [kb.sb(es, [128, D], F32, "yb") for _ in range(2)]
            sa = [kb.sb(es, [128, 512], BF16, "sa") for _ in range(2)]
            pA = [kb.ps(es, [128, 512], F32, "pA") for _ in range(2)]
            pB = [kb.ps(es, [128, 512], F32, "pB") for _ in range(2)]
            pT = [kb.ps(es, [128, 1024], BF16, "pTm") for _ in range(2)]
            pY = [kb.ps(es, [128, 512], F32, "pY") for _ in range(2)]
            stage_i = [0]

            def load_cast(dst_ap, dst_trk, src_ap, shape3):
                st = stage[stage_i[0] % 2]
                a_, b_ = shape3
                sv = st[:, 0:a_ * b_].rearrange("p (a b) -> p a b", a=a_)
                kb.dma("sp", out=sv, in_=src_ap, writes=[st])
                if stage_i[0] % 2 == 0:
                    kb.op("dve", lambda e: e.tensor_copy(out=dst_ap, in_=sv), reads=[st], writes=[dst_trk])
                else:
                    kb.op("act", lambda e: e.copy(out=dst_ap, in_=sv), reads=[st], writes=[dst_trk])
                stage_i[0] += 1

            def calls_of(ex):
                return [(j, 128, idxt[:, ex, j:j + 1], j * 128) for j in range(NCALL)] + [(NCALL, CAPC, idxc[:, ex:ex + 1], NSLOT)]

            def issue_gathers(ex):
                gb = ex % 2
                for (j, rows, iap, col0) in calls_of(ex):
                    g = G[j]
                    kb.dma("pool", reads=[self.H2, idxt, idxc], writes=[g],
                           fn=lambda e, g=g, rows=rows, iap=iap: e.indirect_dma_start(out=g[0:rows, :], out_offset=None, in_=self.H2[:, :],
                                                                                      in_offset=bass.IndirectOffsetOnAxis(ap=iap, axis=0)))
                    kb.dma("pool", reads=[self.AFF, idxt, idxc], writes=[gatek[gb][j]],
                           fn=lambda e, gb=gb, j=j, rows=rows, iap=iap: e.indirect_dma_start(out=gate[gb][0:rows, j, :], out_offset=None, in_=self.AFF[:, :],
                                                                                           in_offset=bass.IndirectOffsetOnAxis(ap=iap, axis=0)))

            issue_gathers(0)
            tcount = 0
            for ex in range(NE):
                gb = ex % 2
                w1src = I["w_e1"][li, ex].rearrange("(kc p) f -> p kc f", p=128)
                w3src = I["w_e3"][li, ex].rearrange("(kc p) f -> p kc f", p=128)
                w2src = I["w_e2"][li, ex].rearrange("(fc p) d -> p fc d", p=128)
                calls = calls_of(ex)
                for (j, rows, iap, col0) in calls:
                    g = G[j]
                    p = pT[tcount % 2]
                    tcount += 1
                    for kc in range(8):
                        kb.op("pe", lambda e, p=p, g=g, kc=kc, rows=rows: e.transpose(out=p[:, kc * 128:kc * 128 + rows],
                                                                                    in_=g[0:rows, kc * 128:(kc + 1) * 128],
                                                                                    identity=self.ident_bf[0:rows, 0:rows]),
                              reads=[g, self.ident_bf], writes=[p])
                    ev = "dve" if tcount % 2 == 0 else "act"
                    if ev == "dve":
                        kb.op("dve", lambda e, p=p, rows=rows, col0=col0: e.tensor_copy(
                            out=xgT[:, :, col0:col0 + rows], in_=p[:, :].rearrange("p (k t) -> p k t", k=8)[:, :, 0:rows]),
                              reads=[p], writes=[xgT])
                    else:
                        kb.op("act", lambda e, p=p, rows=rows, col0=col0: e.copy(
                            out=xgT[:, :, col0:col0 + rows], in_=p[:, :].rearrange("p (k t) -> p k t", k=8)[:, :, 0:rows]),
                              reads=[p], writes=[xgT])
                if ex + 1 < NE:
                    issue_gathers(ex + 1)
                for q in range(4):
                    wb = w13[q % 2]
                    load_cast(wb[0][:, :, :], wb[0].k, w1src[:, :, q * 512:(q + 1) * 512], (8, 512))
                    load_cast(wb[1][:, :, :], wb[1].k, w3src[:, :, q * 512:(q + 1) * 512], (8, 512))
                    for f4 in range(4):
                        fc = q * 4 + f4
                        for ci, (c0, cn) in enumerate(chunks):
                            k = (fc * NCH + ci) % 2
                            for kc in range(8):
                                kb.op("pe", lambda e, k=k, kc=kc, f4=f4, c0=c0, cn=cn, wb=wb: e.matmul(
                                    pA[k][:, 0:cn], lhsT=wb[0][:, kc, f4 * 128:(f4 + 1) * 128], rhs=xgT[:, kc, c0:c0 + cn],
                                    start=(kc == 0), stop=(kc == 7)), reads=[wb[0], xgT], writes=[pA[k]])
                            for kc in range(8):
                                kb.op("pe", lambda e, k=k, kc=kc, f4=f4, c0=c0, cn=cn, wb=wb: e.matmul(
                                    pB[k][:, 0:cn], lhsT=wb[1][:, kc, f4 * 128:(f4 + 1) * 128], rhs=xgT[:, kc, c0:c0 + cn],
                                    start=(kc == 0), stop=(kc == 7)), reads=[wb[1], xgT], writes=[pB[k]])
                            kb.op("act", lambda e, k=k, cn=cn: e.activation(out=sa[k][:, 0:cn], in_=pA[k][:, 0:cn], func=AF.Silu),
                                  reads=[pA[k]], writes=[sa[k]])
                            kb.op("dve", lambda e, k=k, cn=cn, c0=c0, fc=fc: e.tensor_tensor(out=gT[:, fc, c0:c0 + cn], in0=sa[k][:, 0:cn],
                                                                                          in1=pB[k][:, 0:cn], op=ALU.mult),
                                  reads=[sa[k], pB[k]], writes=[gTk[fc]])
                for g4 in range(4):
                    load_cast(w2[:, g4 * 4:(g4 + 1) * 4, :], w2k[g4], w2src[:, g4 * 4:(g4 + 1) * 4, :], (4, D))
                for (j, rows, iap, col0) in calls:
                    s = 0 if j < NCALL else 1
                    y = yb[j % 2]
                    for h in range(2):
                        p = pY[h]
                        for fc in range(16):
                            kb.op("pe", lambda e, p=p, fc=fc, col0=col0, rows=rows, h=h: e.matmul(
                                p[0:rows, :], lhsT=gT[:, fc, col0:col0 + rows], rhs=w2[:, fc, h * 512:(h + 1) * 512],
                                start=(fc == 0), stop=(fc == 15)), reads=[gTk[fc], w2k[fc // 4]], writes=[p])
                        kb.op("dve", lambda e, p=p, y=y, h=h, rows=rows, gb=gb, j=j, s=s, ex=ex: e.scalar_tensor_tensor(
                            out=y[0:rows, h * 512:(h + 1) * 512], in0=p[0:rows, :], scalar=gate[gb][0:rows, j, ex:ex + 1],
                            in1=ga2[s][0:rows, h * 512:(h + 1) * 512], op0=ALU.mult, op1=ALU.mult),
                              reads=[p, gatek[gb][j], ga2[s]], writes=[y])
                    kb.dma("pool", reads=[y, idxt, idxc], writes=[self.X],
                           fn=lambda e, y=y, rows=rows, iap=iap: e.indirect_dma_start(out=self.X[:, :],
                                                                                      out_offset=bass.IndirectOffsetOnAxis(ap=iap, axis=0),
                                                                                      in_=y[0:rows, :], in_offset=None, compute_op=ALU.add))

    def load_w_bf16(self, es, dst, src3, ncols, stage):
        kb = self.kb
        i = 0
        for c0 in range(0, ncols, 512):
            cn = min(512, ncols - c0)
            st = stage[i % 2]
            sv = st[:, 0:8 * cn].rearrange("p (a b) -> p a b", a=8)
            kb.dma("sp", out=sv, in_=src3[:, :, c0:c0 + cn], writes=[st])
            if i % 2 == 0:
                kb.op("pool", lambda e, sv=sv, c0=c0, cn=cn: e.tensor_copy(out=dst[:, :, c0:c0 + cn], in_=sv), reads=[st], writes=[dst])
            else:
                kb.op("act", lambda e, sv=sv, c0=c0, cn=cn: e.copy(out=dst[:, :, c0:c0 + cn], in_=sv), reads=[st], writes=[dst])
            i += 1

    def transpose_tile(self, hb, hT, pT):
        kb = self.kb
        for kc in range(8):
            kb.op("pe", lambda e, kc=kc: e.transpose(out=pT[:, kc * 128:(kc + 1) * 128], in_=hb[:, kc * 128:(kc + 1) * 128],
                                                     identity=self.ident_bf[:, :]), reads=[hb, self.ident_bf], writes=[pT])
        kb.op("act", lambda e: e.copy(out=hT[:, :, :], in_=pT[:, :].rearrange("p (k t) -> p k t", k=8)), reads=[pT], writes=[hT])

    def ntiles(self, need_ctx):
        return NTILE if need_ctx else 64

    def ph_qkv(self, li):
        kb = self.kb
        I = self.I
        j = li // 2
        if not hasattr(self, "QKT"):
            self.QKT = kb.dram("QKT", [16, 128, NT], BF16)
            self.VD = kb.dram("VD", [NT + 64, 16 * 65], BF16)
            self.OD = kb.dram("OD", [NT, D], BF16)
        with contextlib.ExitStack() as es:
            svec = [self.load_vec(es, s, 1, "s1") for s in range(2)]
            shvec = [self.load_vec(es, s, 0, "sh1") for s in range(2)]
            negh = kb.sb(es, [128, 1], F32, "negh")
            kb.op("dve", lambda e: e.memset(negh[:, :], -0.5), writes=[negh])
            stage = [kb.sb(es, [128, 4096], F32, "stage") for _ in range(2)]
            W = kb.sb(es, [128, 8, 3 * D], BF16, "wqkv")
            self.load_w_bf16(es, W, I["w_qkv"][j].rearrange("(kc p) n -> p kc n", p=128), 3 * D, stage)
            NB = 6
            xt = [kb.sb(es, [128, D], F32, "xt") for _ in range(NB)]
            hf = [kb.sb(es, [128, D], F32, "hf") for _ in range(NB)]
            hb = [kb.sb(es, [128, D], BF16, "hb") for _ in range(NB)]
            junk = kb.sb(es, [128, D], BF16, "junk")
            hT = [kb.sb(es, [128, 8, 128], BF16, "hT") for _ in range(NB)]
            sm = [[kb.sb(es, [128, 1], F32, "sm") for _ in range(2)] for _ in range(NB)]
            qk = [kb.sb(es, [128, 16, 128], BF16, "qk") for _ in range(2)]
            vt = [kb.sb(es, [128, 16, 65], BF16, "vt") for _ in range(2)]
            for b in range(2):
                kb.op("dve", lambda e, b=b: e.memset(vt[b][:, :, :], 1.0), writes=[vt[b]])
            pT = [kb.ps(es, [128, 1024], BF16, "pT") for _ in range(2)]
            pq = [kb.ps(es, [128, 512], F32, "pq") for _ in range(6)]

            def s0(ti):
                b = ti % NB
                r0 = ti * 128
                kb.dma("sp", out=xt[b][:, :], in_=self.X[r0:r0 + 128, :], reads=[self.X], writes=[xt[b]])

            def s1a(ti):
                b = ti % NB
                self.norm_a((sm[b][0], sm[b][1]), xt[b], junk, negh)

            def s1(ti):
                b = ti % NB
                s = 0 if ti < 64 else 1
                self.norm_b((sm[b][0], sm[b][1]), xt[b], svec[s], shvec[s], hf[b], hb[b])

            def s2(ti):
                b = ti % NB
                self.transpose_tile(hb[b], hT[b], pT[ti % 2])

            def s3(ti):
                b = ti % NB
                b2 = ti % 2
                r0 = ti * 128
                for c4 in range(4):
                    p = pq[(6 * ti + c4) % 6]
                    for cc in range(4):
                        c = c4 * 4 + cc
                        for kc in range(8):
                            kb.op("pe", lambda e, p=p, cc=cc, c=c, kc=kc, b=b: e.matmul(p[:, cc * 128:(cc + 1) * 128], lhsT=W[:, kc, c * 128:(c + 1) * 128],
                                                                                     rhs=hT[b][:, kc, :], start=(kc == 0), stop=(kc == 7)),
                                  reads=[W, hT[b]], writes=[p])
                    sc = 0.125 if c4 < 2 else 1.0
                    kb.op("act", lambda e, p=p, c4=c4, b2=b2, sc=sc: e.activation(out=qk[b2][:, c4 * 4:c4 * 4 + 4, :], in_=p[:, :].rearrange("p (c t) -> p c t", c=4),
                                                                               func=AF.Copy, scale=sc), reads=[p], writes=[qk[b2]])
                kb.dma("sp", out=self.QKT[:, :, r0:r0 + 128].rearrange("c p t -> p c t"), in_=qk[b2][:, :, :], reads=[qk[b2]], writes=[self.QKT])
                for h2 in range(2):
                    p = pq[(6 * ti + 4 + h2) % 6]
                    for kc in range(8):
                        kb.op("pe", lambda e, p=p, kc=kc, h2=h2, b=b: e.matmul(p[:, :], lhsT=hT[b][:, kc, :], rhs=W[:, kc, 2 * D + h2 * 512:2 * D + (h2 + 1) * 512],
                                                                            start=(kc == 0), stop=(kc == 7)), reads=[W, hT[b]], writes=[p])
                    kb.op("dve", lambda e, p=p, h2=h2, b2=b2: e.tensor_copy(out=vt[b2][:, h2 * 8:(h2 + 1) * 8, 0:64], in_=p[:, :].rearrange("p (h d) -> p h d", h=8)),
                          reads=[p], writes=[vt[b2]])
                kb.dma("sp", out=self.VD[r0:r0 + 128, :], in_=vt[b2][:, :, :].rearrange("p h d -> p (h d)"), reads=[vt[b2]], writes=[self.VD])

            pipeline(NTILE, [s0, s1a, s1, s2, s3])

    def ph_attn(self, li, need_ctx):
        kb = self.kb
        I = self.I
        j = li // 2
        with contextlib.ExitStack() as es:
            KT = kb.sb(es, [128, NT], BF16, "KT")
            QT = kb.sb(es, [128, NT], BF16, "QT")
            V0 = kb.sb(es, [128, NTILE, 130], BF16, "V0")
            bst = [kb.sb(es, [128, 2, 5, 128], F32, "bst") for _ in range(2)]
            EB = [kb.sb(es, [128, 2, 5, 128], BF16, "EB") for _ in range(5)]
            NP = 5
            PT = [kb.sb(es, [128, 7, 128], BF16, "PT") for _ in range(NP)]
            osb = [kb.sb(es, [128, 128], BF16, "osb") for _ in range(3)]
            rec = [kb.sb(es, [128, 1], F32, "rec") for _ in range(4)]
            pSa = [kb.ps(es, [128, 512], F32, "pSa") for _ in range(2)]
            pSb = [kb.ps(es, [128, 512], F32, "pSb") for _ in range(2)]
            pO = [kb.ps(es, [128, 65], F32, "pO") for _ in range(3)]
            VDv = self.VD[0:NT, :].rearrange("(t p) c -> p t c", p=128)
            for hp in range(8):
                kb.dma("sp", out=QT[:, :], in_=self.QKT[hp, :, :], reads=[self.QKT], writes=[QT])
                kb.dma("sp", out=KT[:, :], in_=self.QKT[8 + hp, :, :], reads=[self.QKT], writes=[KT])
                kb.dma("sp", out=V0[:, :, :], in_=VDv[:, :, hp * 130:(hp + 1) * 130], reads=[self.VD], writes=[V0])
                for v in range(5):
                    st = bst[v % 2]
                    kb.dma("sp", out=st[:, :, :, :], in_=I["biasT"][j, v, 2 * hp:2 * hp + 2].rearrange("h p c q -> p h c q"), writes=[st])
                    kb.op("act", lambda e, st=st, v=v: e.activation(out=EB[v][:, :, :, :], in_=st[:, :, :, :], func=AF.Exp), reads=[st], writes=[EB[v]])
                units = []
                for rp in range(64):
                    for hl in range(2):
                        units.append((rp, hl))
                if need_ctx:
                    for cq in range(2):
                        for hl in range(2):
                            units.append((64 + cq, hl))

                def info(u):
                    rp, hl = units[u]
                    if rp < 64:
                        t0 = att_base(rp) // 2
                        tiles = [t0 + c for c in range(5)] + [64, 65]
                        q0 = 128 * rp
                        eb = EB[att_variant(rp)]
                    else:
                        tiles = [64, 65]
                        q0 = L + 128 * (rp - 64)
                        eb = None
                    return rp, hl, tiles, q0, eb

                def sA(u):
                    rp, hl, tiles, q0, eb = info(u)
                    ho = hl * 64
                    pa_, pb_ = pSa[u % 2], pSb[u % 2]
                    for c, t in enumerate(tiles):
                        dst = pa_[:, c * 128:(c + 1) * 128] if c < 4 else pb_[:, (c - 4) * 128:(c - 3) * 128]
                        trk = pa_ if c < 4 else pb_
                        kb.op("pe", lambda e, dst=dst, ho=ho, t=t, q0=q0: e.matmul(dst, lhsT=KT[ho:ho + 64, t * 128:(t + 1) * 128],
                                                                                rhs=QT[ho:ho + 64, q0:q0 + 128], start=True, stop=True),
                              reads=[KT, QT], writes=[trk])

                def sB(u):
                    rp, hl, tiles, q0, eb = info(u)
                    pa_, pb_ = pSa[u % 2], pSb[u % 2]
                    pt = PT[u % NP]
                    n = len(tiles)
                    na = min(n, 4)
                    kb.op("act", lambda e, pt=pt, pa_=pa_, na=na: e.activation(out=pt[:, 0:na, :].rearrange("p c q -> p (c q)"), in_=pa_[:, 0:na * 128], func=AF.Exp),
                          reads=[pa_], writes=[pt])
                    if n > 4:
                        kb.op("act", lambda e, pt=pt, pb_=pb_, n=n: e.activation(out=pt[:, 4:n, :].rearrange("p c q -> p (c q)"), in_=pb_[:, 0:(n - 4) * 128], func=AF.Exp),
                              reads=[pb_], writes=[pt])

                def sB2(u):
                    rp, hl, tiles, q0, eb = info(u)
                    pt = PT[u % NP]
                    if eb is not None:
                        me = "pool" if (u % 5) < 3 else "dve"
                        kb.op(me, lambda e, pt=pt, eb=eb, hl=hl: e.tensor_tensor(out=pt[:, 0:5, :], in0=pt[:, 0:5, :], in1=eb[:, hl, :, :], op=ALU.mult),
                              reads=[pt, eb], writes=[pt])

                def sC(u):
                    rp, hl, tiles, q0, eb = info(u)
                    pt = PT[u % NP]
                    po = pO[u % 3]
                    ob = osb[(u // 2) % 3]
                    n = len(tiles)
                    for c, t in enumerate(tiles):
                        kb.op("pe", lambda e, po=po, c=c, t=t, pt=pt, hl=hl, n=n: e.matmul(po[:, :], lhsT=pt[:, c, :], rhs=V0[:, t, hl * 65:(hl + 1) * 65],
                                                                                        start=(c == 0), stop=(c == n - 1)), reads=[pt, V0], writes=[po])
                    rc = rec[u % 4]
                    kb.op("dve", lambda e, po=po, rc=rc: e.reciprocal(out=rc[:, 0:1], in_=po[:, 64:65]), reads=[po], writes=[rc])
                    kb.op("dve", lambda e, po=po, rc=rc, hl=hl, ob=ob: e.tensor_scalar(out=ob[:, hl * 64:(hl + 1) * 64], in0=po[:, 0:64],
                                                                                      scalar1=rc[:, 0:1], scalar2=None, op0=ALU.mult),
                          reads=[po, rc], writes=[ob])
                    if hl == 1:
                        kb.dma("sp", out=self.OD[q0:q0 + 128, hp * 128:(hp + 1) * 128], in_=ob[:, :], reads=[ob], writes=[self.OD])

                pipeline(len(units), [sA, sB, sB2, sC])

    def ph_oproj(self, li, wname, srcname, need_ctx):
        kb = self.kb
        I = self.I
        j = li // 2
        SRC = getattr(self, srcname)
        with contextlib.ExitStack() as es:
            ga = [self.load_vec(es, s, 2, "ga1") for s in range(2)]
            stage = [kb.sb(es, [128, 4096], F32, "stage") for _ in range(2)]
            W = kb.sb(es, [128, 8, D], BF16, "wo")
            self.load_w_bf16(es, W, I[wname][j].rearrange("(kc p) n -> p kc n", p=128), D, stage)
            NB = 5
            xt = [kb.sb(es, [128, D], F32, "xt") for _ in range(NB)]
            hb = [kb.sb(es, [128, D], BF16, "hb") for _ in range(NB)]
            tmp = [kb.sb(es, [128, D], F32, "tmp") for _ in range(2)]
            hT = [kb.sb(es, [128, 8, 128], BF16, "hT") for _ in range(NB)]
            pT = [kb.ps(es, [128, 1024], BF16, "pT") for _ in range(2)]
            pq = [kb.ps(es, [128, 512], F32, "pq") for _ in range(4)]

            def s0(ti):
                b = ti % NB
                r0 = ti * 128
                kb.dma("sp", out=xt[b][:, :], in_=self.X[r0:r0 + 128, :], reads=[self.X], writes=[xt[b]])
                kb.dma("sp", out=hb[b][:, :], in_=SRC[r0:r0 + 128, :], reads=[SRC], writes=[hb[b]])

            def s1(ti):
                b = ti % NB
                self.transpose_tile(hb[b], hT[b], pT[ti % 2])

            def s2(ti):
                b = ti % NB
                b2 = ti % 2
                s = 0 if ti < 64 else 1
                r0 = ti * 128
                for h2 in range(2):
                    p = pq[(2 * ti + h2) % 4]
                    for kc in range(8):
                        kb.op("pe", lambda e, p=p, kc=kc, h2=h2, b=b: e.matmul(p[:, :], lhsT=hT[b][:, kc, :], rhs=W[:, kc, h2 * 512:(h2 + 1) * 512],
                                                                            start=(kc == 0), stop=(kc == 7)), reads=[W, hT[b]], writes=[p])
                    kb.op("dve", lambda e, p=p, h2=h2, b2=b2, s=s: e.tensor_tensor(out=tmp[b2][:, h2 * 512:(h2 + 1) * 512], in0=p[:, :],
                                                                                in1=ga[s][:, h2 * 512:(h2 + 1) * 512], op=ALU.mult),
                          reads=[p, ga[s]], writes=[tmp[b2]])
                kb.op("pool", lambda e, b=b, b2=b2: e.tensor_tensor(out=xt[b][:, :], in0=xt[b][:, :], in1=tmp[b2][:, :], op=ALU.add),
                      reads=[xt[b], tmp[b2]], writes=[xt[b]])
                kb.dma("sp", out=self.X[r0:r0 + 128, :], in_=xt[b][:, :], reads=[xt[b]], writes=[self.X])

            pipeline(self.ntiles(need_ctx), [s0, s1, s2])

    def cload(self, es, name):
        shp, ty = CONST_SPECS[name]
        t = self.kb.sb(es, shp, ty, name)
        sl = tuple(slice(None) for _ in shp)
        self.kb.dma("sp", out=t[sl], in_=self.I[name][sl], writes=[t])
        return t

    def even_scratch(self):
        kb = self.kb
        if not hasattr(self, "PR"):
            self.PR = kb.dram("PR", [NT, 1536], BF16)
            self.DTR = kb.dram("DTR", [NT, 16], F32)
            self.AD = kb.dram("AD", [NT, 1024], BF16)
            self.XBC = kb.dram("XBC", [NT, 1024], BF16)
            self.DTA = kb.dram("DTA", [NT, 32], F32)
            self.YF = kb.dram("YF", [NT, 512], F32)
            self.MIX = kb.dram("MIX", [NT, 1024], BF16)
            self.GD = kb.dram("GD", [2, 64, 128, 512], BF16)

    def ph_inproj(self, li):
        kb = self.kb
        I = self.I
        j = li // 2
        self.even_scratch()
        with contextlib.ExitStack() as es:
            svec = [self.load_vec(es, s, 1, "s1") for s in range(2)]
            shvec = [self.load_vec(es, s, 0, "sh1") for s in range(2)]
            negh = kb.sb(es, [128, 1], F32, "negh")
            kb.op("dve", lambda e: e.memset(negh[:, :], -0.5), writes=[negh])
            stage = [kb.sb(es, [128, 4096], F32, "stage") for _ in range(2)]
            W = kb.sb(es, [128, 8, 2064], BF16, "win")
            self.load_w_bf16(es, W, I["w_in_e"][j].rearrange("(kc p) n -> p kc n", p=128), 2064, stage)
            CS = self.cload(es, "CS")
            NB = 6
            xt = [kb.sb(es, [128, D], F32, "xt") for _ in range(NB)]
            hf = [kb.sb(es, [128, D], F32, "hf") for _ in range(NB)]
            hb = [kb.sb(es, [128, D], BF16, "hb") for _ in range(NB)]
            junk = kb.sb(es, [128, D], BF16, "junk")
            hT = [kb.sb(es, [128, 8, 128], BF16, "hT") for _ in range(NB)]
            sm = [[kb.sb(es, [128, 1], F32, "sm") for _ in range(2)] for _ in range(NB)]
            prt = [kb.sb(es, [128, 1536], BF16, "prt") for _ in range(2)]
            dtr = [kb.sb(es, [128, 16], F32, "dtr") for _ in range(2)]
            uT = [kb.sb(es, [128, 4, 128], BF16, "uT") for _ in range(2)]
            At = [kb.sb(es, [128, 2, 4, 128], BF16, "At") for _ in range(2)]
            pT = [kb.ps(es, [128, 1024], BF16, "pT") for _ in range(2)]
            pq = [kb.ps(es, [128, 512], F32, "pq") for _ in range(4)]
            pa = [kb.ps(es, [128, 512], F32, "pa") for _ in range(2)]

            def s0(ti):
                b = ti % NB
                r0 = ti * 128
                kb.dma("sp", out=xt[b][:, :], in_=self.X[r0:r0 + 128, :], reads=[self.X], writes=[xt[b]])

            def s1a(ti):
                b = ti % NB
                self.norm_a((sm[b][0], sm[b][1]), xt[b], junk, negh)

            def s1(ti):
                b = ti % NB
                s = 0 if ti < 64 else 1
                self.norm_b((sm[b][0], sm[b][1]), xt[b], svec[s], shvec[s], hf[b], hb[b])

            def s2(ti):
                b = ti % NB
                self.transpose_tile(hb[b], hT[b], pT[ti % 2])

            def s3(ti):
                b = ti % NB
                b2 = ti % 2
                r0 = ti * 128
                for ci, (c0, cn) in enumerate([(0, 512), (512, 512), (1024, 512), (1536, 16)]):
                    p = pq[(5 * ti + ci) % 4]
                    for kc in range(8):
                        kb.op("pe", lambda e, p=p, kc=kc, c0=c0, cn=cn, b=b: e.matmul(p[:, 0:cn], lhsT=hT[b][:, kc, :], rhs=W[:, kc, c0:c0 + cn],
                                                                                   start=(kc == 0), stop=(kc == 7)), reads=[W, hT[b]], writes=[p])
                    if ci < 3:
                        if ci % 2 == 0:
                            kb.op("act", lambda e, p=p, c0=c0, b2=b2: e.copy(out=prt[b2][:, c0:c0 + 512], in_=p[:, :]), reads=[p], writes=[prt[b2]])
                        else:
                            kb.op("dve", lambda e, p=p, c0=c0, b2=b2: e.tensor_copy(out=prt[b2][:, c0:c0 + 512], in_=p[:, :]), reads=[p], writes=[prt[b2]])
                    else:
                        kb.op("dve", lambda e, p=p, b2=b2: e.tensor_copy(out=dtr[b2][:, :], in_=p[:, 0:16]), reads=[p], writes=[dtr[b2]])
                kb.dma("sp", out=self.PR[r0:r0 + 128, :], in_=prt[b2][:, :], reads=[prt[b2]], writes=[self.PR])
                kb.dma("sp", out=self.DTR[r0:r0 + 128, :], in_=dtr[b2][:, :], reads=[dtr[b2]], writes=[self.DTR])
                p = pq[(5 * ti + 4) % 4]
                for g in range(4):
                    for kc in range(8):
                        kb.op("pe", lambda e, p=p, kc=kc, g=g, b=b: e.matmul(p[:, g * 128:(g + 1) * 128], lhsT=W[:, kc, 1552 + g * 128:1552 + (g + 1) * 128],
                                                                          rhs=hT[b][:, kc, :], start=(kc == 0), stop=(kc == 7)), reads=[W, hT[b]], writes=[p])
                kb.op("act", lambda e, p=p, b2=b2: e.copy(out=uT[b2][:, :, :], in_=p[:, :].rearrange("p (g t) -> p g t", g=4)), reads=[p], writes=[uT[b2]])
                for gh in range(2):
                    for g2 in range(2):
                        g = gh * 2 + g2
                        kb.op("pe", lambda e, gh=gh, g2=g2, g=g, b2=b2: e.matmul(pa[gh][:, g2 * 256:(g2 + 1) * 256], lhsT=uT[b2][:, g, :], rhs=CS[:, :],
                                                                              start=True, stop=True), reads=[uT[b2], CS], writes=[pa[gh]])
                    kb.op("dve", lambda e, gh=gh, b2=b2: e.tensor_copy(out=At[b2][:, :, 2 * gh:2 * gh + 2, :],
                                                                     in_=pa[gh][:, :].rearrange("p (g cs q) -> p cs g q", g=2, cs=2)),
                          reads=[pa[gh]], writes=[At[b2]])
                kb.dma("sp", out=self.AD[r0:r0 + 128, :], in_=At[b2][:, :, :, :].rearrange("p cs g q -> p (cs g q)"), reads=[At[b2]], writes=[self.AD])

            pipeline(NTILE, [s0, s1a, s1, s2, s3])

    def ph_conv(self, li):
        kb = self.kb
        I = self.I
        j = li // 2
        with contextlib.ExitStack() as es:
            cw = [kb.sb(es, [128, D], F32, "cw") for _ in range(3)]
            for k in range(3):
                kb.dma("sp", out=cw[k][:, :], in_=I["conv_w"][j, k, :].partition_broadcast(128), writes=[cw[k]])
            cb = kb.sb(es, [128, D], F32, "cb")
            kb.dma("sp", out=cb[:, :], in_=I["conv_b"][j, :].partition_broadcast(128), writes=[cb])
            dtb = kb.sb(es, [128, 16], F32, "dtb")
            kb.dma("sp", out=dtb[:, :], in_=I["dt_bias"][j, :].partition_broadcast(128), writes=[dtb])
            abc = kb.sb(es, [128, 16], F32, "abc")
            kb.dma("sp", out=abc[:, :], in_=I["a_log"][j, :].partition_broadcast(128), writes=[abc])
            kb.op("act", lambda e: e.activation(out=abc[:, :], in_=abc[:, :], func=AF.Exp), reads=[abc], writes=[abc])
            kb.op("dve", lambda e: e.tensor_scalar(out=abc[:, :], in0=abc[:, :], scalar1=-1.0, scalar2=None, op0=ALU.mult), reads=[abc], writes=[abc])
            NB = 4
            m1 = [kb.sb(es, [128, D], BF16, "m1") for _ in range(NB)]
            c0 = [kb.sb(es, [128, D], BF16, "c0") for _ in range(NB)]
            p1 = [kb.sb(es, [128, D], BF16, "p1") for _ in range(NB)]
            acc = [kb.sb(es, [128, D], F32, "acc") for _ in range(NB)]
            t2 = [kb.sb(es, [128, D], F32, "t2") for _ in range(NB)]
            t3 = [kb.sb(es, [128, D], F32, "t3") for _ in range(NB)]
            xo = [kb.sb(es, [128, D], BF16, "xo") for _ in range(NB)]
            dtr = [kb.sb(es, [128, 16], F32, "dtr") for _ in range(NB)]
            dta = [kb.sb(es, [128, 32], F32, "dta") for _ in range(NB)]
            src = self.PR

            def s0(ti):
                b = ti % NB
                r0 = ti * 128
                first = ti in (0, 64)
                last = ti in (63, 65)
                if first:
                    kb.op("dve", lambda e, b=b: e.memset(m1[b][:, :], 0.0), writes=[m1[b]])
                    kb.dma("sp", out=m1[b][1:128, :], in_=src[r0:r0 + 127, 512:1536], reads=[src], writes=[m1[b]])
                else:
                    kb.dma("sp", out=m1[b][:, :], in_=src[r0 - 1:r0 + 127, 512:1536], reads=[src], writes=[m1[b]])
                kb.dma("sp", out=c0[b][:, :], in_=src[r0:r0 + 128, 512:1536], reads=[src], writes=[c0[b]])
                if last:
                    kb.op("dve", lambda e, b=b: e.memset(p1[b][:, :], 0.0), writes=[p1[b]])
                    kb.dma("sp", out=p1[b][0:127, :], in_=src[r0 + 1:r0 + 128, 512:1536], reads=[src], writes=[p1[b]])
                else:
                    kb.dma("sp", out=p1[b][:, :], in_=src[r0 + 1:r0 + 129, 512:1536], reads=[src], writes=[p1[b]])
                kb.dma("sp", out=dtr[b][:, :], in_=self.DTR[r0:r0 + 128, :], reads=[self.DTR], writes=[dtr[b]])

            def s1(ti):
                b = ti % NB
                kb.op("dve", lambda e, b=b: e.tensor_tensor(out=acc[b][:, :], in0=m1[b][:, :], in1=cw[0][:, :], op=ALU.mult), reads=[m1[b], cw[0]], writes=[acc[b]])
                kb.op("pool", lambda e, b=b: e.tensor_tensor(out=t2[b][:, :], in0=c0[b][:, :], in1=cw[1][:, :], op=ALU.mult), reads=[c0[b], cw[1]], writes=[t2[b]])
                kb.op("pool", lambda e, b=b: e.tensor_tensor(out=t3[b][:, :], in0=p1[b][:, :], in1=cw[2][:, :], op=ALU.mult), reads=[p1[b], cw[2]], writes=[t3[b]])
                kb.op("dve", lambda e, b=b: e.tensor_tensor(out=dtr[b][:, :], in0=dtr[b][:, :], in1=dtb[:, :], op=ALU.add), reads=[dtr[b], dtb], writes=[dtr[b]])

            def s2(ti):
                b = ti % NB
                kb.op("dve", lambda e, b=b: e.tensor_tensor(out=acc[b][:, :], in0=acc[b][:, :], in1=t2[b][:, :], op=ALU.add), reads=[acc[b], t2[b]], writes=[acc[b]])
                kb.op("pool", lambda e, b=b: e.tensor_tensor(out=t3[b][:, :], in0=t3[b][:, :], in1=cb[:, :], op=ALU.add), reads=[t3[b], cb], writes=[t3[b]])
                kb.op("act", lambda e, b=b: e.activation(out=dtr[b][:, :], in_=dtr[b][:, :], func=AF.Exp), reads=[dtr[b]], writes=[dtr[b]])

            def s3(ti):
                b = ti % NB
                kb.op("dve", lambda e, b=b: e.tensor_tensor(out=acc[b][:, :], in0=acc[b][:, :], in1=t3[b][:, :], op=ALU.add), reads=[acc[b], t3[b]], writes=[acc[b]])
                kb.op("act", lambda e, b=b: e.activation(out=dta[b][:, 0:16], in_=dtr[b][:, :], func=AF.Ln, bias=1.0), reads=[dtr[b]], writes=[dta[b]])

            def s4(ti):
                b = ti % NB
                r0 = ti * 128
                kb.op("act", lambda e, b=b: e.activation(out=xo[b][:, :], in_=acc[b][:, :], func=AF.Silu), reads=[acc[b]], writes=[xo[b]])
                kb.dma("sp", out=self.XBC[r0:r0 + 128, :], in_=xo[b][:, :], reads=[xo[b]], writes=[self.XBC])
                kb.op("dve", lambda e, b=b: e.tensor_tensor(out=dta[b][:, 16:32], in0=dta[b][:, 0:16], in1=abc[:, :], op=ALU.mult), reads=[dta[b], abc], writes=[dta[b]])
                kb.dma("sp", out=self.DTA[r0:r0 + 128, :], in_=dta[b][:, :], reads=[dta[b]], writes=[self.DTA])

            pipeline(NTILE, [s0, s1, s2, s3, s4])

    def ph_ssd(self, li, d):
        kb = self.kb
        I = self.I
        j = li // 2
        with contextlib.ExitStack() as es:
            tri = self.cload(es, "triU" if d == 0 else "triL")
            ones = self.cload(es, "onesf")
            neg = self.cload(es, "negU" if d == 0 else "negL")
            dsk = kb.sb(es, [128, 8], F32, "dsk")
            kb.dma("sp", out=dsk[:, :], in_=I["d_skip"][j, :].partition_broadcast(128), writes=[dsk])
            gss = kb.sb(es, [128, 512], F32, "gss")
            kb.dma("sp", out=gss[:, :], in_=I["g_ssd"][j, :].partition_broadcast(128), writes=[gss])
            negh = kb.sb(es, [128, 1], F32, "negh")
            kb.op("dve", lambda e: e.memset(negh[:, :], -0.5), writes=[negh])
            hs = kb.sb(es, [128, 8, 64], F32, "hs")
            hsb = kb.sb(es, [128, 8, 64], BF16, "hsb")
            kb.op("dve", lambda e: e.memset(hs[:, :, :], 0.0), writes=[hs])
            kb.op("dve", lambda e: e.memset(hsb[:, :, :], 0.0), writes=[hsb])
            NB = 4
            R = lambda shape, ty, nm, n=NB: [kb.sb(es, shape, ty, nm) for _ in range(n)]
            xbc = R([128, D], BF16, "xbc")
            dta = R([128, 32], F32, "dta")
            BCT = R([128, 4, 128], BF16, "BCT")
            cum = R([128, 16], F32, "cum")
            te = R([128, 8], F32, "te")
            cd = R([128, 8], F32, "cd")
            xdt = R([128, 8, 64], BF16, "xdt")
            xw = R([128, 8, 64], BF16, "xw")
            CBT = R([128, 2, 128], BF16, "CBT", 2)
            dU = R([128, 8, 128], F32, "dU", 2)
            Ebc = R([128, 8, 128], BF16, "Ebc", 2)
            CsT = R([128, 8, 128], BF16, "CsT")
            Sg = R([128, 8, 128], F32, "Sg", 2)
            Dm = R([128, 8, 128], BF16, "Dm", 2)
            MT = R([128, 8, 128], BF16, "MT")
            tmp = R([128, 8, 64], F32, "tmp", 2)
            yo = R([128, 512], F32, "yo")
            yf = R([128, 512], F32, "yf")
            zt = R([128, 512], BF16, "zt")
            sz = R([128, 512], F32, "sz", 2)
            junk = kb.sb(es, [128, 256], BF16, "junk")
            ssq = R([128, 2], F32, "ssq")
            rsq = R([128, 2], F32, "rsq")
            mo = R([128, 512], BF16, "mo", 2)
            pT = kb.ps(es, [128, 1024], BF16, "pT")
            pc = kb.ps(es, [128, 16], F32, "pc")
            pcb = kb.ps(es, [128, 256], F32, "pcb")
            pA = [kb.ps(es, [128, 512], F32, "pA") for _ in range(2)]
            py = [kb.ps(es, [128, 512], F32, "py") for _ in range(2)]
            pst = kb.ps(es, [128, 512], F32, "pst")
            order = ([64, 65] + list(range(64))) if d == 0 else ([65, 64] + list(range(63, -1, -1)))

            def views(n):
                b = n % NB
                X_, DT_ = xbc[b], dta[b]
                return (b, n % 2, order[n] * 128, X_, DT_, DT_[:, 8 * d:8 * d + 8], DT_[:, 16 + 8 * d:16 + 8 * d + 8],
                        X_[:, 0:512].rearrange("p (h c) -> p h c", h=8))

            def s0(n):
                b, b2, r0, X_, DT_, dt_d, da_d, x3 = views(n)
                kb.dma("sp", out=X_[:, :], in_=self.XBC[r0:r0 + 128, :], reads=[self.XBC], writes=[X_])
                kb.dma("sp", out=DT_[:, :], in_=self.DTA[r0:r0 + 128, :], reads=[self.DTA], writes=[DT_])
                if d == 1:
                    kb.dma("sp", out=yf[b][:, :], in_=self.YF[r0:r0 + 128, :], reads=[self.YF], writes=[yf[b]])
                    kb.dma("sp", out=zt[b][:, :], in_=self.PR[r0:r0 + 128, 0:512], reads=[self.PR], writes=[zt[b]])

            def s1(n):
                b, b2, r0, X_, DT_, dt_d, da_d, x3 = views(n)
                for q in range(4):
                    kb.op("pe", lambda e, q=q, X_=X_: e.transpose(out=pT[:, q * 128:(q + 1) * 128], in_=X_[:, 512 + q * 128:512 + (q + 1) * 128],
                                                               identity=self.ident_bf[:, :]), reads=[X_, self.ident_bf], writes=[pT])
                kb.op("act", lambda e, b=b: e.copy(out=BCT[b][:, :, :], in_=pT[:, 0:512].rearrange("p (q t) -> p q t", q=4)), reads=[pT], writes=[BCT[b]])
                kb.op("pe", lambda e, da_d=da_d: e.matmul(pc[:, 0:8], lhsT=tri[:, :], rhs=da_d, start=True, stop=True), reads=[tri, DT_], writes=[pc])
                kb.op("pe", lambda e, da_d=da_d: e.matmul(pc[:, 8:16], lhsT=ones[:, :], rhs=da_d, start=True, stop=True), reads=[ones, DT_], writes=[pc])
                kb.op("dve", lambda e, b=b, da_d=da_d: e.tensor_tensor(out=dU[b2][:, :, :], in0=tri[:, :].unsqueeze(1).to_broadcast([128, 8, 128]),
                                                                     in1=da_d.unsqueeze(2).to_broadcast([128, 8, 128]), op=ALU.mult),
                      reads=[tri, DT_], writes=[dU[b2]])
                kb.op("dve", lambda e, b=b: e.tensor_copy(out=cum[b][:, :], in_=pc[:, :]), reads=[pc], writes=[cum[b]])
                for hh in range(2):
                    kb.op("pe", lambda e, hh=hh, b2=b2: e.matmul(pA[hh][:, :], lhsT=ones[:, :], rhs=dU[b2][:, 4 * hh:4 * hh + 4, :].rearrange("p h i -> p (h i)"),
                                                               start=True, stop=True), reads=[ones, dU[b2]], writes=[pA[hh]])
                for g in range(2):
                    kb.op("pe", lambda e, g=g, b=b: e.matmul(pcb[:, g * 128:(g + 1) * 128], lhsT=BCT[b][:, g, :], rhs=BCT[b][:, 2 + g, :], start=True, stop=True),
                          reads=[BCT[b]], writes=[pcb])
                kb.op("dve", lambda e, b=b: e.tensor_tensor(out=te[b][:, :], in0=cum[b][:, 8:16], in1=cum[b][:, 0:8], op=ALU.subtract), reads=[cum[b]], writes=[te[b]])
                kb.op("pool", lambda e, b=b, x3=x3, dt_d=dt_d: e.tensor_tensor(out=xdt[b][:, :, :], in0=x3, in1=dt_d.unsqueeze(2).to_broadcast([128, 8, 64]), op=ALU.mult),
                      reads=[X_, DT_], writes=[xdt[b]])
                for hh in range(2):
                    kb.op("act", lambda e, hh=hh, b2=b2: e.activation(out=Ebc[b2][:, 4 * hh:4 * hh + 4, :].rearrange("p h i -> p (h i)"), in_=pA[hh][:, :], func=AF.Exp),
                          reads=[pA[hh]], writes=[Ebc[b2]])
                kb.op("act", lambda e, b=b: e.activation(out=te[b][:, :], in_=te[b][:, :], func=AF.Exp), reads=[te[b]], writes=[te[b]])
                kb.op("act", lambda e, b=b: e.activation(out=cd[b][:, :], in_=cum[b][:, 8:16], func=AF.Exp), reads=[cum[b]], writes=[cd[b]])
                for h in range(8):
                    kb.op("dve", lambda e, h=h, b=b, b2=b2: e.scalar_tensor_tensor(out=Sg[b2][:, h, :], in0=pA[h // 4][:, (h % 4) * 128:(h % 4 + 1) * 128],
                                                                                scalar=cum[b][:, h:h + 1], in1=neg[:, :], op0=ALU.subtract, op1=ALU.add),
                          reads=[pA[h // 4], cum[b], neg], writes=[Sg[b2]])
                kb.op("act", lambda e, b2=b2: e.copy(out=CBT[b2][:, :, :], in_=pcb[:, :].rearrange("p (g t) -> p g t", g=2)), reads=[pcb], writes=[CBT[b2]])
                kb.op("act", lambda e, b2=b2: e.activation(out=Dm[b2][:, :, :], in_=Sg[b2][:, :, :], func=AF.Exp), reads=[Sg[b2]], writes=[Dm[b2]])
                kb.op("pool", lambda e, b=b: e.tensor_tensor(out=xw[b][:, :, :], in0=xdt[b][:, :, :], in1=te[b][:, :].unsqueeze(2).to_broadcast([128, 8, 64]), op=ALU.mult),
                      reads=[xdt[b], te[b]], writes=[xw[b]])
                for g in range(2):
                    kb.op("pool", lambda e, g=g, b=b, b2=b2: e.tensor_tensor(out=CsT[b][:, 4 * g:4 * g + 4, :], in0=Ebc[b2][:, 4 * g:4 * g + 4, :],
                                                                           in1=BCT[b][:, 2 + g, :].unsqueeze(1).to_broadcast([128, 4, 128]), op=ALU.mult),
                          reads=[Ebc[b2], BCT[b]], writes=[CsT[b]])
                for g in range(2):
                    kb.op("dve", lambda e, g=g, b=b, b2=b2: e.tensor_tensor(out=MT[b][:, 4 * g:4 * g + 4, :], in0=Dm[b2][:, 4 * g:4 * g + 4, :],
                                                                          in1=CBT[b2][:, g, :].unsqueeze(1).to_broadcast([128, 4, 128]), op=ALU.mult),
                          reads=[Dm[b2], CBT[b2]], writes=[MT[b]])

            def s2(n):
                b, b2, r0, X_, DT_, dt_d, da_d, x3 = views(n)
                p_y = py[n % 2]
                for h in range(8):
                    kb.op("pe", lambda e, h=h, b=b: e.matmul(p_y[:, h * 64:(h + 1) * 64], lhsT=MT[b][:, h, :], rhs=xdt[b][:, h, :], start=(h == 0), stop=False,
                                                           skip_group_check=True), reads=[MT[b], xdt[b]], writes=[p_y])
                for h in range(8):
                    kb.op("pe", lambda e, h=h, b=b: e.matmul(p_y[:, h * 64:(h + 1) * 64], lhsT=CsT[b][:, h, :], rhs=hsb[:, h, :], start=False, stop=(h == 7),
                                                           skip_group_check=True), reads=[CsT[b], hsb], writes=[p_y])
                for g in range(2):
                    kb.op("pe", lambda e, g=g, b=b, X_=X_: e.matmul(pst[:, g * 256:(g + 1) * 256], lhsT=X_[:, 512 + g * 128:512 + (g + 1) * 128],
                                                                 rhs=xw[b][:, 4 * g:4 * g + 4, :].rearrange("p h c -> p (h c)"), start=True, stop=True),
                          reads=[X_, xw[b]], writes=[pst])
                kb.op("dve", lambda e, b=b, b2=b2: e.tensor_tensor(out=tmp[b2][:, :, :], in0=hs[:, :, :], in1=cd[b][:, :].unsqueeze(2).to_broadcast([128, 8, 64]), op=ALU.mult),
                      reads=[hs, cd[b]], writes=[tmp[b2]])
                kb.op("dve", lambda e, b2=b2: e.tensor_tensor(out=hs[:, :, :], in0=tmp[b2][:, :, :], in1=pst[:, :].rearrange("p (h c) -> p h c", h=8), op=ALU.add),
                      reads=[tmp[b2], pst], writes=[hs])
                kb.op("act", lambda e: e.copy(out=hsb[:, :, :], in_=hs[:, :, :]), reads=[hs], writes=[hsb])

            def s3(n):
                b, b2, r0, X_, DT_, dt_d, da_d, x3 = views(n)
                p_y = py[n % 2]
                if d == 0:
                    kb.op("pool", lambda e, b=b, x3=x3: e.tensor_tensor(out=yo[b][:, :].rearrange("p (h c) -> p h c", h=8), in0=x3,
                                                                      in1=dsk[:, :].unsqueeze(2).to_broadcast([128, 8, 64]), op=ALU.mult),
                          reads=[X_, dsk], writes=[yo[b]])
                    kb.op("dve", lambda e, b=b: e.tensor_tensor(out=yo[b][:, :], in0=p_y[:, :], in1=yo[b][:, :], op=ALU.add), reads=[p_y, yo[b]], writes=[yo[b]])
                    kb.dma("sp", out=self.YF[r0:r0 + 128, :], in_=yo[b][:, :], reads=[yo[b]], writes=[self.YF])
                else:
                    kb.op("dve", lambda e, b=b: e.tensor_tensor(out=yo[b][:, :], in0=p_y[:, :], in1=yf[b][:, :], op=ALU.add), reads=[p_y, yf[b]], writes=[yo[b]])
                    kb.op("act", lambda e, b=b, b2=b2: e.activation(out=sz[b2][:, :], in_=zt[b][:, :], func=AF.Silu), reads=[zt[b]], writes=[sz[b2]])
                    kb.op("pool", lambda e, b=b, b2=b2: e.tensor_tensor(out=yo[b][:, :], in0=yo[b][:, :], in1=sz[b2][:, :], op=ALU.mult), reads=[yo[b], sz[b2]], writes=[yo[b]])
                    for g in range(2):
                        kb.op("act", lambda e, b=b, g=g: e.activation(out=junk[:, :], in_=yo[b][:, g * 256:(g + 1) * 256], func=AF.Square, accum_out=ssq[b][:, g:g + 1]),
                              reads=[yo[b]], writes=[junk, ssq[b]])
                    kb.op("dve", lambda e, b=b: e.tensor_scalar(out=ssq[b][:, :], in0=ssq[b][:, :], scalar1=1.0 / 256, scalar2=EPS, op0=ALU.mult, op1=ALU.add),
                          reads=[ssq[b]], writes=[ssq[b]])
                    kb.op("pool", lambda e, b=b: e.tensor_tensor(out=rsq[b][:, :], in0=ssq[b][:, :], in1=negh[:, 0:1].to_broadcast([128, 2]), op=ALU.pow),
                          reads=[ssq[b], negh], writes=[rsq[b]])
                    for g in range(2):
                        kb.op("dve", lambda e, b=b, g=g, b2=b2: e.scalar_tensor_tensor(out=mo[b2][:, g * 256:(g + 1) * 256], in0=yo[b][:, g * 256:(g + 1) * 256],
                                                                                     scalar=rsq[b][:, g:g + 1], in1=gss[:, g * 256:(g + 1) * 256], op0=ALU.mult, op1=ALU.mult),
                              reads=[yo[b], rsq[b], gss], writes=[mo[b2]])
                    kb.dma("sp", out=self.MIX[r0:r0 + 128, 0:512], in_=mo[b2][:, :], reads=[mo[b2]], writes=[self.MIX])

            pipeline(len(order), [s0, s1, s2, s3])

    def ph_fourier(self, li):
        kb = self.kb
        I = self.I
        SC = 1.0 / np.sqrt(8192.0 * 128.0)
        SCC = 1.0 / np.sqrt(256.0 * 128.0)
        with contextlib.ExitStack() as es:
            F1 = self.cload(es, "F1")
            blk = [kb.sb(es, [128, 16, 1024], BF16, "blk") for _ in range(2)]
            gt = [kb.sb(es, [128, 2, 512], BF16, "gt") for _ in range(3)]
            pg = [kb.ps(es, [128, 512], F32, "pg") for _ in range(4)]
            ADv = self.AD[0:L, :].rearrange("(n1 n2) c -> n1 n2 c", n2=64)
            for rd in range(4):
                bk = blk[rd % 2]
                kb.dma("sp", out=bk[:, :, :], in_=ADv[:, rd * 16:(rd + 1) * 16, :], reads=[self.AD], writes=[bk])
                for nl in range(16):
                    n2 = rd * 16 + nl
                    g = gt[n2 % 3]
                    pr_, pi_ = pg[(2 * n2) % 4], pg[(2 * n2 + 1) % 4]
                    Ac = bk[:, nl, 0:512]
                    As = bk[:, nl, 512:1024]
                    kb.op("pe", lambda e, pr_=pr_, Ac=Ac: e.matmul(pr_[:, :], lhsT=F1[:, 0, :], rhs=Ac, start=True, stop=False), reads=[F1, bk], writes=[pr_])
                    kb.op("pe", lambda e, pr_=pr_, As=As: e.matmul(pr_[:, :], lhsT=F1[:, 1, :], rhs=As, start=False, stop=True), reads=[F1, bk], writes=[pr_])
                    kb.op("pe", lambda e, pi_=pi_, Ac=Ac: e.matmul(pi_[:, :], lhsT=F1[:, 1, :], rhs=Ac, start=True, stop=False), reads=[F1, bk], writes=[pi_])
                    kb.op("pe", lambda e, pi_=pi_, As=As: e.matmul(pi_[:, :], lhsT=F1[:, 2, :], rhs=As, start=False, stop=True), reads=[F1, bk], writes=[pi_])
                    kb.op("act", lambda e, g=g, pr_=pr_: e.copy(out=g[:, 0, :], in_=pr_[:, :]), reads=[pr_], writes=[g])
                    kb.op("dve", lambda e, g=g, pi_=pi_: e.tensor_copy(out=g[:, 1, :], in_=pi_[:, :]), reads=[pi_], writes=[g])
                    kb.dma("sp", out=self.GD[:, n2, :, :].rearrange("ri p c -> p ri c"), in_=g[:, :, :], reads=[g], writes=[self.GD])
        kb.barrier()
        with contextlib.ExitStack() as es:
            TW = self.cload(es, "TW3")
            blk = [kb.sb(es, [128, 32, 512], BF16, "blk3") for _ in range(2)]
            ot = [kb.sb(es, [64, 32, 512], BF16, "ot") for _ in range(2)]
            pg = [kb.ps(es, [64, 512], F32, "pg3") for _ in range(4)]
            GDv = self.GD[:, :, :, :].rearrange("ri n2 p c -> (ri n2) p c")
            MIXv = self.MIX[0:L, :].rearrange("(p2 p1) c -> p2 p1 c", p1=128)
            for rd in range(4):
                bk = blk[rd % 2]
                o = ot[rd % 2]
                kb.dma("sp", out=bk[:, :, :], in_=GDv[:, rd * 32:(rd + 1) * 32, :], reads=[self.GD], writes=[bk])
                for pl in range(32):
                    p1 = rd * 32 + pl
                    p = pg[p1 % 4]
                    kb.op("pe", lambda e, p=p, p1=p1, pl=pl, bk=bk: e.matmul(p[:, :], lhsT=TW[:, p1, :], rhs=bk[:, pl, :], start=True, stop=True), reads=[TW, bk], writes=[p])
                    if pl % 2 == 0:
                        kb.op("act", lambda e, p=p, pl=pl, o=o: e.activation(out=o[:, pl, :], in_=p[:, :], func=AF.Copy, scale=float(SC)), reads=[p], writes=[o])
                    else:
                        kb.op("dve", lambda e, p=p, pl=pl, o=o: e.tensor_scalar(out=o[:, pl, :], in0=p[:, :], scalar1=float(SC), scalar2=None, op0=ALU.mult), reads=[p], writes=[o])
                kb.dma("sp", out=MIXv[:, rd * 32:(rd + 1) * 32, 512:1024], in_=o[:, :, :], reads=[o], writes=[self.MIX])
            C2 = self.cload(es, "C256")
            S2 = self.cload(es, "nS256")
            ac = kb.sb(es, [128, 2, 1024], BF16, "actx")
            kb.dma("sp", out=ac[:, :, :], in_=self.AD[L:NT, :].rearrange("(t p) c -> p t c", p=128), reads=[self.AD], writes=[ac])
            oc = kb.sb(es, [128, 2, 512], BF16, "octx")
            pcx = [kb.ps(es, [128, 512], F32, "pcx") for _ in range(2)]
            for pt in range(2):
                p = pcx[pt]
                k = 0
                for nt in range(2):
                    for (M, off) in ((C2, 0), (S2, 512)):
                        kb.op("pe", lambda e, p=p, M=M, nt=nt, pt=pt, off=off, k=k: e.matmul(p[:, :], lhsT=M[:, nt, pt * 128:(pt + 1) * 128], rhs=ac[:, nt, off:off + 512],
                                                                                          start=(k == 0), stop=(k == 3)), reads=[M, ac], writes=[p])
                        k += 1
                kb.op("act", lambda e, p=p, pt=pt: e.activation(out=oc[:, pt, :], in_=p[:, :], func=AF.Copy, scale=float(SCC)), reads=[p], writes=[oc])
            kb.dma("sp", out=self.MIX[L:NT, 512:1024].rearrange("(t p) c -> p t c", p=128), in_=oc[:, :, :], reads=[oc], writes=[self.MIX])

    def ph_final(self):
        kb = self.kb
        I = self.I
        with contextlib.ExitStack() as es:
            gf = kb.sb(es, [128, D], F32, "gf")
            kb.dma("sp", out=gf[:, :], in_=I["g_final"].partition_broadcast(128), writes=[gf])
            zero = kb.sb(es, [128, D], F32, "zero")
            kb.op("dve", lambda e: e.memset(zero[:, :], 0.0), writes=[zero])
            negh = kb.sb(es, [128, 1], F32, "negh")
            kb.op("dve", lambda e: e.memset(negh[:, :], -0.5), writes=[negh])
            junk = kb.sb(es, [128, D], BF16, "junk")
            NB = 4
            xt = [kb.sb(es, [128, D], F32, "xt") for _ in range(NB)]
            of = [kb.sb(es, [128, D], F32, "of") for _ in range(NB)]
            sm = [[kb.sb(es, [128, 1], F32, "sm") for _ in range(2)] for _ in range(NB)]

            def s0(ti):
                b = ti % NB
                r0 = ti * 128
                kb.dma("sp", out=xt[b][:, :], in_=self.X[r0:r0 + 128, :], reads=[self.X], writes=[xt[b]])

            def s1(ti):
                b = ti % NB
                r0 = ti * 128
                self.norm_tile((sm[b][0], sm[b][1]), xt[b], gf, zero, of[b], None, junk, negh)
                kb.dma("sp", out=self.out[r0:r0 + 128, :], in_=of[b][:, :], reads=[of[b]], writes=[self.outk])

            pipeline(64, [s0, s1])

    def ph_dumpx(self):
        kb = self.kb
        for r0 in range(0, L, 2048):
            kb.dma("sp", out=self.out[r0:r0 + 2048, :], in_=self.X[r0:r0 + 2048, :], reads=[self.X], writes=[self.outk])


def default_phases():
    ph = [("init",)]
    for li in range(DEPTH):
        need_ctx = li < DEPTH - 1
        ph += [("mod", li)]
        if li % 2 == 0:
            ph += [("inproj", li), ("conv", li), ("ssd", li, 0), ("ssd", li, 1), ("fourier", li),
                   ("oproj", li, "w_out_e", "MIX", need_ctx)]
        else:
            ph += [("qkv", li), ("attn", li, need_ctx), ("oproj", li, "w_o", "OD", need_ctx)]
        ph += [("router", li), ("select", li), ("moe", li)]
    ph += [("final",)]
    return ph


ATT_VARIANT_RP = [2, 0, 1, 62, 63]


def att_base(rp):
    r0 = 2 * rp
    return min(min(max(r0 - 4, 0), 120), 118)


def att_variant(rp):
    return 0 if 2 <= rp <= 61 else {0: 1, 1: 2, 62: 3, 63: 4}[rp]


def build_bias_table(rpb):
    no = rpb.shape[0]
    out = np.empty((no, 5, 16, 128, 5, 128), np.float32)
    p = np.arange(128)
    q = np.arange(128)
    c = np.arange(5)
    for v, rp in enumerate(ATT_VARIANT_RP):
        r0 = 2 * rp
        base = att_base(rp)
        krow = base + 2 * c[:, None, None] + (p // 64)[None, :, None]
        kcol = (p % 64)[None, :, None]
        r = (r0 + q // 64)[None, None, :]
        col = (q % 64)[None, None, :]
        rs = np.clip(r - 4, 0, 120)
        cs = np.clip(col - 8, 0, 48)
        valid = (krow >= rs) & (krow < rs + 8) & (kcol >= cs) & (kcol < cs + 16)
        ro = np.clip(krow - r + 7, 0, 14) + 0 * kcol
        co = np.clip(kcol - col + 15, 0, 30) + 0 * krow
        g = rpb[:, :, ro, co]
        g = np.where(valid[None, None], g, np.float32(-30000.0))
        out[:, v] = np.transpose(g, (0, 1, 3, 2, 4))
    return out


def make_in_maps(inputs, n_cores, names):
    consts = host_consts()
    shared = {}
    for k in names:
        if k in consts:
            shared[k] = consts[k]
        elif k in ("x", "ctx", "c"):
            pass
        elif k == "biasT":
            shared[k] = build_bias_table(inputs["rpb"])
        elif k in ("a_log", "dt_bias"):
            shared[k] = np.ascontiguousarray(inputs[k].reshape(2, 16))
        else:
            shared[k] = np.ascontiguousarray(inputs[k])
    maps = []
    for b in range(n_cores):
        m = dict(shared)
        for k in ("x", "ctx", "c"):
            if k in names:
                m[k] = np.ascontiguousarray(inputs[k][b])
        maps.append(m)
    return maps


def kernel(**inputs):
    inputs = {k: np.asarray(v) for k, v in inputs.items()}
    prog = Prog(default_phases())
    in_maps = make_in_maps(inputs, 4, list(prog.I.keys()))
    res = run_bass_kernel_spmd(prog.nc, in_maps, core_ids=list(range(4)))
    out = np.stack([res.results[b]["out"] for b in range(4)], axis=0)
    return out.astype(np.float32)
```

```python
import contextlib
import numpy as np
import ml_dtypes
import concourse.bass as bass
import concourse.mybir as mybir
from concourse.bass_utils import run_bass_kernel_spmd

F32 = mybir.dt.float32
BF16 = mybir.dt.bfloat16
I32 = mybir.dt.int32
AF = mybir.ActivationFunctionType
ALU = mybir.AluOpType
AX = mybir.AxisListType
IOA = bass.IndirectOffsetOnAxis if hasattr(bass, "IndirectOffsetOnAxis") else None

D = 1024
L = 8192
LC = 256
NT = L + LC
NTILE = NT // 128
DEPTH = 4
NE = 16
FF = 2048
NBLK = 4
BLK = L // NBLK
PSEL = NE * NBLK
CAPB = 320
NSLOT = NBLK * CAPB
NCALL = NSLOT // 128
CAPC = 32
XR = NT + (NCALL + 1) * 128
EPS = 1e-6


class Trk:
    __slots__ = ("w", "r", "x")

    def __init__(self, x=False):
        self.w = None
        self.r = {}
        self.x = x


class Buf:
    def __init__(self, t, x=False):
        self.t = t
        self.k = Trk(x)

    def __getitem__(self, key):
        return self.t[key]


class KB:
    ND = 40

    def __init__(self, nc):
        self.nc = nc
        self.es = contextlib.ExitStack()
        self.eng = {"pe": nc.tensor, "act": nc.scalar, "dve": nc.vector, "pool": nc.gpsimd, "sp": nc.sync}
        self.csem = {}
        self.ccnt = {}
        for e in ("pe", "act", "dve", "pool"):
            self.csem[e] = self.es.enter_context(nc.semaphore("cs_" + e))
            self.ccnt[e] = 0
        self.dsem = [self.es.enter_context(nc.semaphore("ds%d" % i)) for i in range(self.ND)]
        self.dcnt = [0] * self.ND
        self.dnext = 0
        self.seen = {e: {} for e in self.eng}
        self.uid = 0

    def sb(self, es, shape, dtype, name=None):
        self.uid += 1
        return Buf(es.enter_context(self.nc.sbuf_tensor("%s_%d" % (name or "sb", self.uid), list(shape), dtype)))

    def ps(self, es, shape, dtype, name=None):
        self.uid += 1
        return Buf(es.enter_context(self.nc.psum_tensor("%s_%d" % (name or "ps", self.uid), list(shape), dtype)), x=True)

    def dram(self, name, shape, dtype):
        return Buf(self.nc.dram_tensor(name, list(shape), dtype, kind="Internal").ap())

    def _sem(self, ch):
        return self.csem[ch] if isinstance(ch, str) else self.dsem[ch]

    def _deps(self, eng, reads, writes, is_dma):
        deps = {}

        def add(ev, raw):
            if ev is None:
                return
            ch, v = ev
            if (not is_dma) and ch == eng:
                if eng == "pe" or not raw:
                    return
            if deps.get(ch, 0) < v:
                deps[ch] = v

        for t in reads:
            add(t.w, True)
            if t.x:
                for ch, v in t.r.items():
                    if ch != eng:
                        add((ch, v), False)
        for t in writes:
            add(t.w, False)
            for ch, v in t.r.items():
                add((ch, v), False)
        return deps

    def _wait(self, eng, deps):
        s = self.seen[eng]
        for ch, v in deps.items():
            if s.get(ch, 0) < v:
                self.eng[eng].wait_ge(self._sem(ch), v)
                s[ch] = v

    @staticmethod
    def _trks(lst):
        return [b.k if isinstance(b, Buf) else b for b in lst]

    def op(self, eng, fn, reads=(), writes=()):
        reads = self._trks(reads)
        writes = self._trks(writes)
        self._wait(eng, self._deps(eng, reads, writes, False))
        ins = fn(self.eng[eng])
        self.ccnt[eng] += 1
        v = self.ccnt[eng]
        ins.then_inc(self.csem[eng], 1)
        for t in reads:
            if t.r.get(eng, 0) < v:
                t.r[eng] = v
        for t in writes:
            t.w = (eng, v)
            t.r = {}
        return ins

    def dma(self, q, out=None, in_=None, reads=(), writes=(), fn=None, **kw):
        reads = self._trks(reads)
        writes = self._trks(writes)
        s = self.dnext
        self.dnext = (s + 1) % self.ND
        deps = self._deps(q, reads, writes, True)
        if self.dcnt[s] > 0 and deps.get(s, 0) < self.dcnt[s]:
            deps[s] = self.dcnt[s]
        self._wait(q, deps)
        if fn is None:
            ins = self.eng[q].dma_start(out=out, in_=in_, **kw)
        else:
            ins = fn(self.eng[q])
        self.dcnt[s] += 16
        v = self.dcnt[s]
        ins.then_inc(self.dsem[s], 16)
        for t in reads:
            if t.r.get(s, 0) < v:
                t.r[s] = v
        for t in writes:
            t.w = (s, v)
            t.r = {}
        return ins

    def barrier(self):
        for e in self.eng:
            deps = {}
            for c in self.csem:
                if self.ccnt[c] > 0 and c != e:
                    deps[c] = self.ccnt[c]
            for s in range(self.ND):
                if self.dcnt[s] > 0:
                    deps[s] = self.dcnt[s]
            self._wait(e, deps)
        for e in self.csem:
            if self.ccnt[e] > 0:
                s = self.seen[e]
                if s.get(e, 0) < self.ccnt[e]:
                    self.eng[e].wait_ge(self.csem[e], self.ccnt[e])
                    s[e] = self.ccnt[e]


def host_consts():
    c = {}
    c["ident_bf"] = np.eye(128, dtype=np.float32).astype(ml_dtypes.bfloat16)
    c["ident_f"] = np.eye(128, dtype=np.float32)
    p = np.arange(128)
    c["gsum"] = (p[:, None] // NBLK == p[None, :] // NBLK).astype(np.float32)
    c["keyl"] = np.broadcast_to((BLK - np.arange(BLK, dtype=np.float32))[None, :], (128, BLK)).copy()
    c["keyc"] = np.broadcast_to((256 - np.arange(256, dtype=np.float32))[None, :], (128, 256)).copy()
    c["basel"] = ((p % NBLK) * BLK + BLK).astype(np.float32)[:, None].copy()
    c["basec"] = np.full((128, 1), L + 256, np.float32)
    sig = (p[:, None] % NBLK) * CAPB + np.arange(CAPB)[None, :]
    c["dumpl"] = (NT + sig).astype(np.float32)
    c["dumpc"] = np.broadcast_to((NT + NSLOT + np.arange(CAPC, dtype=np.float32))[None, :], (128, CAPC)).copy()
    k = np.arange(128)
    c["triU"] = (k[:, None] <= k[None, :]).astype(np.float32)
    c["triL"] = (k[:, None] >= k[None, :]).astype(np.float32)
    c["onesf"] = np.ones((128, 128), np.float32)
    c["negU"] = np.where(k[None, :] >= k[:, None], 0.0, -30000.0).astype(np.float32)
    c["negL"] = np.where(k[None, :] <= k[:, None], 0.0, -30000.0).astype(np.float32)
    ang = 2 * np.pi * np.outer(k, k) / 128.0
    bf = ml_dtypes.bfloat16
    c["CS"] = np.concatenate([np.cos(ang), np.sin(ang)], axis=1).astype(np.float32).astype(bf)
    c["F1"] = np.stack([np.cos(ang), -np.sin(ang), -np.cos(ang)], axis=1).astype(np.float32).astype(bf)
    n2 = np.arange(64)[:, None, None]
    p1 = np.arange(128)[None, :, None]
    p2 = np.arange(64)[None, None, :]
    th = 2 * np.pi * (n2 * p2 / 64.0 + n2 * p1 / 8192.0)
    c["TW3"] = np.concatenate([np.cos(th), np.sin(th)], axis=0).astype(np.float32).astype(bf)
    n = np.arange(256)
    a2 = 2 * np.pi * np.outer(n, n) / 256.0
    c["C256"] = np.cos(a2).reshape(2, 128, 256).transpose(1, 0, 2).astype(np.float32).astype(bf)
    c["nS256"] = (-np.sin(a2)).reshape(2, 128, 256).transpose(1, 0, 2).astype(np.float32).astype(bf)
    return c


CONST_SPECS = {
    "triU": ([128, 128], F32), "triL": ([128, 128], F32), "onesf": ([128, 128], F32), "negU": ([128, 128], F32),
    "negL": ([128, 128], F32), "CS": ([128, 256], BF16), "F1": ([128, 3, 128], BF16), "TW3": ([128, 128, 64], BF16),
    "C256": ([128, 2, 256], BF16), "nS256": ([128, 2, 256], BF16),
    "ident_bf": ([128, 128], BF16), "ident_f": ([128, 128], F32), "gsum": ([128, 128], F32),
    "keyl": ([128, BLK], F32), "keyc": ([128, 256], F32), "basel": ([128, 1], F32), "basec": ([128, 1], F32),
    "dumpl": ([128, CAPB], F32), "dumpc": ([128, CAPC], F32),
}


IN_SPECS = {
    "x": ([L, D], F32), "ctx": ([LC, D], F32), "c": ([D], F32), "c_ctx": ([D], F32),
    "w_mod": ([DEPTH, D, 6 * D], F32), "b_mod": ([DEPTH, 6 * D], F32), "g_mix": ([DEPTH, D], F32), "g_ffn": ([DEPTH, D], F32),
    "w_router": ([DEPTH, D, NE], F32), "w_e1": ([DEPTH, NE, D, FF], F32), "w_e3": ([DEPTH, NE, D, FF], F32),
    "w_e2": ([DEPTH, NE, FF, D], F32), "g_final": ([D], F32),
    "w_in_e": ([2, D, 2064], F32), "conv_w": ([2, 3, D], F32), "conv_b": ([2, D], F32), "a_log": ([2, 16], F32),
    "dt_bias": ([2, 16], F32), "d_skip": ([2, 8], F32), "g_ssd": ([2, 512], F32), "w_out_e": ([2, D, D], F32),
    "w_qkv": ([2, D, 3 * D], F32), "w_o": ([2, D, D], F32), "biasT": ([2, 5, 16, 128, 5, 128], F32),
}


def pipeline(n, stages):
    ns = len(stages)
    for step in range(n + ns - 1):
        for si in range(ns - 1, -1, -1):
            k = step - si
            if 0 <= k < n:
                stages[si](k)


class Prog:
    def __init__(self, phases, debug_out=None):
        self.phases = phases
        self.debug_out = debug_out
        nc = bass.Bass("TRN2", target_bir_lowering=False)
        self.nc = nc
        self.kb = KB(nc)
        kb = self.kb
        dt = nc.dram_tensor

        class LazyIn(dict):
            def __missing__(d, k):
                shp, ty = IN_SPECS[k] if k in IN_SPECS else CONST_SPECS[k]
                d[k] = dt(k, list(shp), ty, kind="ExternalInput").ap()
                return d[k]
        self.I = LazyIn()
        self.out = dt("out", [L, D], F32, kind="ExternalOutput").ap()
        self.X = kb.dram("Xres", [XR, D], F32)
        self.H2 = kb.dram("H2", [XR, D], BF16)
        self.AFF = kb.dram("AFF", [XR, NE], F32)
        self.AFFT = kb.dram("AFFT", [NE, L], F32)
        self.AFFTC = kb.dram("AFFTC", [NE, LC], F32)
        self.IDXD = kb.dram("IDXD", [PSEL, CAPB], I32)
        self.IDXC = kb.dram("IDXC", [NE, CAPC], I32)
        self.MODD = kb.dram("MODD", [2, 6 * D], F32)
        self.outk = Trk()
        self.build()

    def build(self):
        kb = self.kb
        with contextlib.ExitStack() as ges:
            self.ges = ges
            self.ident_bf = kb.sb(ges, [128, 128], BF16, "identbf")
            self.ident_f = kb.sb(ges, [128, 128], F32, "identf")
            kb.dma("sp", out=self.ident_bf[:, :], in_=self.I["ident_bf"][:, :], writes=[self.ident_bf])
            kb.dma("sp", out=self.ident_f[:, :], in_=self.I["ident_f"][:, :], writes=[self.ident_f])
            for ph in self.phases:
                name = ph[0]
                getattr(self, "ph_" + name)(*ph[1:])
                kb.barrier()
            kb.barrier()
        kb.es.close()

    def ph_init(self):
        kb = self.kb
        with contextlib.ExitStack() as es:
            for r0 in range(0, L, 2048):
                kb.dma("sp", out=self.X[r0:r0 + 2048, :], in_=self.I["x"][r0:r0 + 2048, :], writes=[self.X])
            kb.dma("sp", out=self.X[L:NT, :], in_=self.I["ctx"][:, :], writes=[self.X])
            z = kb.sb(es, [128, D], F32, "z")
            zb = kb.sb(es, [128, D], BF16, "zb")
            kb.op("dve", lambda e: e.memset(z[:, :], 0.0), writes=[z])
            kb.op("dve", lambda e: e.memset(zb[:, :], 0.0), writes=[zb])
            for j in range(NCALL + 1):
                r0 = NT + j * 128
                kb.dma("sp", out=self.X[r0:r0 + 128, :], in_=z[:, :], reads=[z], writes=[self.X])
                kb.dma("sp", out=self.H2[r0:r0 + 128, :], in_=zb[:, :], reads=[zb], writes=[self.H2])
                kb.dma("sp", out=self.AFF[r0:r0 + 128, :], in_=z[:, 0:NE], reads=[z], writes=[self.AFF])

    def ph_mod(self, li):
        kb = self.kb
        I = self.I
        with contextlib.ExitStack() as es:
            cv = kb.sb(es, [128, 2, 8], F32, "cv")
            kb.dma("sp", out=cv[:, 0, :], in_=I["c"].rearrange("(kc p) -> p kc", p=128), writes=[cv],
                   allow_slow_non_contiguous=True)
            kb.dma("sp", out=cv[:, 1, :], in_=I["c_ctx"].rearrange("(kc p) -> p kc", p=128), writes=[cv],
                   allow_slow_non_contiguous=True)
            sv = kb.sb(es, [128, 2, 8], F32, "sv")
            kb.op("act", lambda e: e.activation(out=sv[:, :, :], in_=cv[:, :, :], func=AF.Silu), reads=[cv], writes=[sv])
            lb = kb.sb(es, [128, 2, 8, 128], BF16, "lb")
            for s in range(2):
                kb.op("dve", lambda e, s=s: e.tensor_copy(out=lb[:, s, :, :], in_=sv[:, s, :].unsqueeze(2).to_broadcast([128, 8, 128])),
                      reads=[sv], writes=[lb])
            gmix = kb.sb(es, [128, D], F32, "gmix")
            gffn = kb.sb(es, [128, D], F32, "gffn")
            kb.dma("sp", out=gmix[:, :], in_=I["g_mix"][li, :].partition_broadcast(128), writes=[gmix])
            kb.dma("sp", out=gffn[:, :], in_=I["g_ffn"][li, :].partition_broadcast(128), writes=[gffn])
            NB = 3
            wm = [kb.sb(es, [128, 8, 512], F32, "wm") for _ in range(NB)]
            wmb = [kb.sb(es, [128, 8, 512], BF16, "wmb") for _ in range(NB)]
            bm = [kb.sb(es, [128, 512], F32, "bm") for _ in range(NB)]
            pp = [kb.ps(es, [128, 512], F32, "pm") for _ in range(4)]
            res = [kb.sb(es, [128, 512], F32, "res") for _ in range(4)]
            wsrc = I["w_mod"][li].rearrange("(kc p) n -> p kc n", p=128)

            def s0(n):
                w = wm[n % NB]
                b = bm[n % NB]
                kb.dma("sp", out=w[:, :, :], in_=wsrc[:, :, n * 512:(n + 1) * 512], writes=[w])
                kb.dma("sp", out=b[:, :], in_=I["b_mod"][li, n * 512:(n + 1) * 512].partition_broadcast(128), writes=[b])

            def s1(n):
                w = wm[n % NB]
                wb_ = wmb[n % NB]
                if n % 2 == 0:
                    kb.op("pool", lambda e: e.tensor_copy(out=wb_[:, :, :], in_=w[:, :, :]), reads=[w], writes=[wb_])
                else:
                    kb.op("act", lambda e: e.copy(out=wb_[:, :, :], in_=w[:, :, :]), reads=[w], writes=[wb_])

            def s2(n):
                w = wmb[n % NB]
                b = bm[n % NB]
                for s in range(2):
                    p = pp[(2 * n + s) % 4]
                    r = res[(2 * n + s) % 4]
                    for kc in range(8):
                        kb.op("pe", lambda e, kc=kc, s=s, p=p, w=w: e.matmul(p[:, :], lhsT=lb[:, s, kc, :], rhs=w[:, kc, :],
                                                                          start=(kc == 0), stop=(kc == 7)),
                              reads=[lb, w], writes=[p])
                    which = n // 2
                    kb.op("dve", lambda e, p=p, r=r, b=b: e.tensor_tensor(out=r[:, :], in0=p[:, :], in1=b[:, :], op=ALU.add),
                          reads=[p, b], writes=[r])
                    if which in (1, 4):
                        g = gmix if which == 1 else gffn
                        c0 = (n % 2) * 512
                        kb.op("dve", lambda e, r=r, g=g, c0=c0: e.scalar_tensor_tensor(out=r[:, :], in0=r[:, :], scalar=1.0,
                                                                                     in1=g[:, c0:c0 + 512], op0=ALU.add, op1=ALU.mult),
                              reads=[r, g], writes=[r])
                    kb.dma("sp", out=self.MODD[s:s + 1, n * 512:(n + 1) * 512], in_=r[0:1, :], reads=[r], writes=[self.MODD])

            pipeline(12, [s0, s1, s2])

    def load_vec(self, es, s, which, name="vec"):
        kb = self.kb
        t = kb.sb(es, [128, D], F32, name)
        kb.dma("sp", out=t[:, :], in_=self.MODD[s, which * D:(which + 1) * D].partition_broadcast(128),
               reads=[self.MODD], writes=[t])
        return t

    def norm_tile(self, rstd_tmp, xt, s_bc, sh_bc, out_f32, out_bf, junk, negh):
        self.norm_a(rstd_tmp, xt, junk, negh)
        self.norm_b(rstd_tmp, xt, s_bc, sh_bc, out_f32, out_bf)

    def norm_a(self, rstd_tmp, xt, junk, negh):
        kb = self.kb
        ss, rs = rstd_tmp
        kb.op("act", lambda e: e.activation(out=junk[:, :], in_=xt[:, :], func=AF.Square, accum_out=ss[:, 0:1]),
              reads=[xt], writes=[junk, ss])
        kb.op("dve", lambda e: e.tensor_scalar(out=ss[:, 0:1], in0=ss[:, 0:1], scalar1=1.0 / D, scalar2=EPS, op0=ALU.mult, op1=ALU.add),
              reads=[ss], writes=[ss])
        kb.op("pool", lambda e: e.tensor_tensor(out=rs[:, 0:1], in0=ss[:, 0:1], in1=negh[:, 0:1], op=ALU.pow),
              reads=[ss, negh], writes=[rs])

    def norm_b(self, rstd_tmp, xt, s_bc, sh_bc, out_f32, out_bf):
        kb = self.kb
        ss, rs = rstd_tmp
        kb.op("dve", lambda e: e.scalar_tensor_tensor(out=out_f32[:, :], in0=xt[:, :], scalar=rs[:, 0:1], in1=s_bc[:, :],
                                                      op0=ALU.mult, op1=ALU.mult),
              reads=[xt, rs, s_bc], writes=[out_f32])
        kb.op("pool", lambda e: e.tensor_tensor(out=out_f32[:, :], in0=out_f32[:, :], in1=sh_bc[:, :], op=ALU.add),
              reads=[out_f32, sh_bc], writes=[out_f32])
        if out_bf is not None:
            kb.op("act", lambda e: e.copy(out=out_bf[:, :], in_=out_f32[:, :]), reads=[out_f32], writes=[out_bf])

    def ph_router(self, li):
        kb = self.kb
        I = self.I
        with contextlib.ExitStack() as es:
            svec = [self.load_vec(es, s, 4, "s2") for s in range(2)]
            shvec = [self.load_vec(es, s, 3, "sh2") for s in range(2)]
            negh = kb.sb(es, [128, 1], F32, "negh")
            kb.op("dve", lambda e: e.memset(negh[:, :], -0.5), writes=[negh])
            wr = kb.sb(es, [128, 8, NE], F32, "wr")
            kb.dma("sp", out=wr[:, :, :], in_=I["w_router"][li].rearrange("(kc p) n -> p kc n", p=128), writes=[wr])
            AT = kb.sb(es, [NE, L], F32, "AT")
            ATC = kb.sb(es, [NE, LC], F32, "ATC")
            NB = 6
            xt = [kb.sb(es, [128, D], F32, "xt") for _ in range(NB)]
            hf = [kb.sb(es, [128, D], F32, "hf") for _ in range(NB)]
            hb = [kb.sb(es, [128, D], BF16, "hb") for _ in range(NB)]
            junk = kb.sb(es, [128, D], BF16, "junk")
            hT = [kb.sb(es, [128, 8, 128], F32, "hT") for _ in range(NB)]
            sm = [[kb.sb(es, [128, 1], F32, "sm") for _ in range(6)] for _ in range(NB)]
            lg = [kb.sb(es, [128, NE], F32, "lg") for _ in range(NB)]
            af = [kb.sb(es, [128, NE], F32, "af") for _ in range(NB)]
            pT = [kb.ps(es, [128, 512], F32, "pT") for _ in range(4)]
            pl = [kb.ps(es, [128, NE], F32, "pl") for _ in range(2)]
            pa = [kb.ps(es, [NE, 128], F32, "pa") for _ in range(2)]

            def s0(ti):
                b = ti % NB
                r0 = ti * 128
                kb.dma("sp", out=xt[b][:, :], in_=self.X[r0:r0 + 128, :], reads=[self.X], writes=[xt[b]])

            def s1a(ti):
                b = ti % NB
                self.norm_a((sm[b][0], sm[b][1]), xt[b], junk, negh)

            def s1(ti):
                b = ti % NB
                s = 0 if ti < 64 else 1
                r0 = ti * 128
                self.norm_b((sm[b][0], sm[b][1]), xt[b], svec[s], shvec[s], hf[b], hb[b])
                kb.dma("sp", out=self.H2[r0:r0 + 128, :], in_=hb[b][:, :], reads=[hb[b]], writes=[self.H2])

            def s2(ti):
                b = ti % NB
                for half in range(2):
                    p = pT[(2 * ti + half) % 4]
                    for q in range(4):
                        kc = half * 4 + q
                        kb.op("pe", lambda e, p=p, q=q, kc=kc, b=b: e.transpose(out=p[:, q * 128:(q + 1) * 128],
                                                                              in_=hf[b][:, kc * 128:(kc + 1) * 128],
                                                                              identity=self.ident_f[:, :]),
                              reads=[hf[b], self.ident_f], writes=[p])
                    kb.op("act", lambda e, p=p, half=half, b=b: e.copy(out=hT[b][:, half * 4:half * 4 + 4, :],
                                                                     in_=p[:, :].rearrange("p (q t) -> p q t", q=4)),
                          reads=[p], writes=[hT[b]])

            def s3(ti):
                b = ti % NB
                s = 0 if ti < 64 else 1
                r0 = ti * 128
                pp = pl[ti % 2]
                for kc in range(8):
                    kb.op("pe", lambda e, kc=kc, pp=pp, b=b: e.matmul(pp[:, :], lhsT=hT[b][:, kc, :], rhs=wr[:, kc, :],
                                                                    start=(kc == 0), stop=(kc == 7)),
                          reads=[hT[b], wr], writes=[pp])
                mx, nmx, se, rse = sm[b][2], sm[b][3], sm[b][4], sm[b][5]
                kb.op("dve", lambda e, pp=pp, mx=mx: e.reduce_max(out=mx[:, 0:1], in_=pp[:, :], axis=AX.X), reads=[pp], writes=[mx])
                kb.op("dve", lambda e, mx=mx, nmx=nmx: e.tensor_scalar(out=nmx[:, 0:1], in0=mx[:, 0:1], scalar1=-1.0, scalar2=None, op0=ALU.mult),
                      reads=[mx], writes=[nmx])
                kb.op("act", lambda e, pp=pp, b=b, nmx=nmx, se=se: e.activation(out=lg[b][:, :], in_=pp[:, :], func=AF.Exp, bias=nmx[:, 0:1],
                                                                              accum_out=se[:, 0:1]),
                      reads=[pp, nmx], writes=[lg[b], se])
                kb.op("dve", lambda e, se=se, rse=rse: e.reciprocal(out=rse[:, 0:1], in_=se[:, 0:1]), reads=[se], writes=[rse])
                kb.op("dve", lambda e, b=b, rse=rse: e.tensor_scalar(out=af[b][:, :], in0=lg[b][:, :], scalar1=rse[:, 0:1], scalar2=None, op0=ALU.mult),
                      reads=[lg[b], rse], writes=[af[b]])
                kb.dma("sp", out=self.AFF[r0:r0 + 128, :], in_=af[b][:, :], reads=[af[b]], writes=[self.AFF])
                pq = pa[ti % 2]
                kb.op("pe", lambda e, pq=pq, b=b: e.matmul(pq[:, :], lhsT=af[b][:, :], rhs=self.ident_f[:, :], start=True, stop=True),
                      reads=[af[b], self.ident_f], writes=[pq])
                if s == 0:
                    kb.op("act", lambda e, pq=pq, r0=r0: e.copy(out=AT[:, r0:r0 + 128], in_=pq[:, :]), reads=[pq], writes=[AT])
                else:
                    kb.op("act", lambda e, pq=pq, r0=r0: e.copy(out=ATC[:, r0 - L:r0 - L + 128], in_=pq[:, :]), reads=[pq], writes=[ATC])

            pipeline(NTILE, [s0, s1a, s1, s2, s3])
            kb.dma("sp", out=self.AFFT[:, :], in_=AT[:, :], reads=[AT], writes=[self.AFFT])
            kb.dma("sp", out=self.AFFTC[:, :], in_=ATC[:, :], reads=[ATC], writes=[self.AFFTC])

    def select(self, es, A, P, F, K, cap, key_c, base_c, dump_c, gsum, idx_out_dram, tag):
        kb = self.kb
        lo = kb.sb(es, [P, 1], F32, "lo" + tag)
        hi = kb.sb(es, [P, 1], F32, "hi" + tag)
        mid = kb.sb(es, [P, 1], F32, "mid" + tag)
        cnt = kb.sb(es, [P, 1], F32, "cnt" + tag)
        ge = kb.sb(es, [P, 1], F32, "ge" + tag)
        d1 = kb.sb(es, [P, 1], F32, "d1" + tag)
        W = kb.sb(es, [P, F], F32, "W" + tag)
        pc = kb.ps(es, [P, 1], F32, "pc" + tag)
        kb.op("dve", lambda e: e.memset(lo[:, :], 0.0), writes=[lo])
        kb.op("dve", lambda e: e.memset(hi[:, :], 1.0), writes=[hi])
        for it in range(36):
            kb.op("dve", lambda e: e.tensor_tensor(out=mid[:, :], in0=lo[:, :], in1=hi[:, :], op=ALU.add), reads=[lo, hi], writes=[mid])
            kb.op("dve", lambda e: e.tensor_scalar(out=mid[:, :], in0=mid[:, :], scalar1=0.5, scalar2=None, op0=ALU.mult), reads=[mid], writes=[mid])
            kb.op("dve", lambda e: e.tensor_scalar(out=W[:, :], in0=A[:, :], scalar1=mid[:, 0:1], scalar2=0.0, op0=ALU.is_ge, op1=ALU.add,
                                                   accum_out=cnt[:, 0:1]), reads=[A, mid], writes=[W, cnt])
            if gsum is not None:
                kb.op("pe", lambda e: e.matmul(pc[:, :], lhsT=gsum[0:P, 0:P], rhs=cnt[:, :], start=True, stop=True), reads=[gsum, cnt], writes=[pc])
                src = pc
            else:
                src = cnt
            kb.op("dve", lambda e, src=src: e.tensor_scalar(out=ge[:, :], in0=src[:, :], scalar1=float(K) - 0.5, scalar2=None, op0=ALU.is_ge),
                  reads=[src], writes=[ge])
            kb.op("dve", lambda e: e.tensor_tensor(out=d1[:, :], in0=mid[:, :], in1=lo[:, :], op=ALU.subtract), reads=[mid, lo], writes=[d1])
            kb.op("dve", lambda e: e.scalar_tensor_tensor(out=lo[:, :], in0=d1[:, :], scalar=ge[:, 0:1], in1=lo[:, :], op0=ALU.mult, op1=ALU.add),
                  reads=[d1, ge, lo], writes=[lo])
            kb.op("dve", lambda e: e.tensor_tensor(out=d1[:, :], in0=hi[:, :], in1=mid[:, :], op=ALU.subtract), reads=[hi, mid], writes=[d1])
            kb.op("dve", lambda e: e.scalar_tensor_tensor(out=hi[:, :], in0=d1[:, :], scalar=ge[:, 0:1], in1=mid[:, :], op0=ALU.mult, op1=ALU.add),
                  reads=[d1, ge, mid], writes=[hi])
        kb.op("dve", lambda e: e.scalar_tensor_tensor(out=W[:, :], in0=A[:, :], scalar=lo[:, 0:1], in1=key_c[0:P, :], op0=ALU.is_ge, op1=ALU.mult),
              reads=[A, lo, key_c], writes=[W])
        Lv = kb.sb(es, [P, cap], F32, "Lv" + tag)
        for r in range(cap // 8):
            kb.op("dve", lambda e, r=r: e.max(out=Lv[:, 8 * r:8 * r + 8], in_=W[:, :]), reads=[W], writes=[Lv])
            kb.op("dve", lambda e, r=r: e.match_replace(out=W[:, :], in_to_replace=Lv[:, 8 * r:8 * r + 8], in_values=W[:, :], imm_value=0.0),
                  reads=[W, Lv], writes=[W])
        tok = kb.sb(es, [P, cap], F32, "tok" + tag)
        vm = kb.sb(es, [P, cap], F32, "vm" + tag)
        idx = kb.sb(es, [P, cap], I32, "idx" + tag)
        kb.op("dve", lambda e: e.tensor_scalar(out=tok[:, :], in0=Lv[:, :], scalar1=-1.0, scalar2=base_c[0:P, 0:1], op0=ALU.mult, op1=ALU.add),
              reads=[Lv, base_c], writes=[tok])
        kb.op("dve", lambda e: e.tensor_tensor(out=tok[:, :], in0=tok[:, :], in1=dump_c[0:P, :], op=ALU.subtract), reads=[tok, dump_c], writes=[tok])
        kb.op("dve", lambda e: e.tensor_scalar(out=vm[:, :], in0=Lv[:, :], scalar1=0.5, scalar2=None, op0=ALU.is_ge), reads=[Lv], writes=[vm])
        kb.op("dve", lambda e: e.tensor_tensor(out=tok[:, :], in0=tok[:, :], in1=vm[:, :], op=ALU.mult), reads=[tok, vm], writes=[tok])
        kb.op("dve", lambda e: e.tensor_tensor(out=tok[:, :], in0=tok[:, :], in1=dump_c[0:P, :], op=ALU.add), reads=[tok, dump_c], writes=[tok])
        kb.op("dve", lambda e: e.tensor_copy(out=idx[:, :], in_=tok[:, :]), reads=[tok], writes=[idx])
        kb.dma("sp", out=idx_out_dram[:, :], in_=idx[:, :], reads=[idx], writes=[idx_out_dram])

    def ph_select(self, li):
        kb = self.kb
        I = self.I
        with contextlib.ExitStack() as es:
            cs = {}
            for k in ("gsum", "keyl", "keyc", "basel", "basec", "dumpl", "dumpc"):
                shp, ty = CONST_SPECS[k]
                cs[k] = kb.sb(es, shp, ty, k)
                kb.dma("sp", out=cs[k][:, :], in_=I[k][:, :], writes=[cs[k]])
            A = kb.sb(es, [PSEL, BLK], F32, "Asel")
            kb.dma("sp", out=A[:, :], in_=self.AFFT[:, :].rearrange("e (b t) -> (e b) t", b=NBLK), reads=[self.AFFT], writes=[A])
            self.select(es, A, PSEL, BLK, 1024, CAPB, cs["keyl"], cs["basel"], cs["dumpl"], cs["gsum"], self.IDXD, "l")
            Ac = kb.sb(es, [NE, LC], F32, "Aselc")
            kb.dma("sp", out=Ac[:, :], in_=self.AFFTC[:, :], reads=[self.AFFTC], writes=[Ac])
            self.select(es, Ac, NE, LC, CAPC, CAPC, cs["keyc"], cs["basec"], cs["dumpc"], None, self.IDXC, "c")

    def ph_moe(self, li):
        kb = self.kb
        I = self.I
        NS = NSLOT + CAPC
        chunks = []
        c0 = 0
        while c0 < NS:
            cn = min(512, NS - c0)
            chunks.append((c0, cn))
            c0 += cn
        NCH = len(chunks)
        with contextlib.ExitStack() as es:
            ga2 = [self.load_vec(es, s, 5, "ga2") for s in range(2)]
            idxt = kb.sb(es, [128, NE, NCALL], I32, "idxt")
            kb.dma("sp", out=idxt[:, :, :], in_=self.IDXD[:, :].rearrange("(e b) r -> e (b r)", b=NBLK).rearrange("e (j p) -> p e j", p=128),
                   reads=[self.IDXD], writes=[idxt], allow_slow_non_contiguous=True)
            idxc = kb.sb(es, [CAPC, NE], I32, "idxc")
            kb.dma("sp", out=idxc[:, :], in_=self.IDXC[:, :].rearrange("e p -> p e"), reads=[self.IDXC], writes=[idxc],
                   allow_slow_non_contiguous=True)
            stage = [kb.sb(es, [128, 4096], F32, "stage") for _ in range(2)]
            w13 = [[kb.sb(es, [128, 8, 512], BF16, "w13") for _ in range(2)] for _ in range(2)]
            w2 = kb.sb(es, [128, 16, D], BF16, "w2")
            w2k = [Trk() for _ in range(4)]
            xgT = kb.sb(es, [128, 8, NS], BF16, "xgT")
            gT = kb.sb(es, [128, 16, NS], BF16, "gT")
            gTk = [Trk() for _ in range(16)]
            G = [kb.sb(es, [128, D], BF16, "G") for _ in range(NCALL + 1)]
            gate = [kb.sb(es, [128, NCALL + 1, NE], F32, "gate") for _ in range(2)]
            gatek = [[Trk() for _ in range(NCALL + 1)] for _ in range(2)]
            yb = [kb.sb(es, [128, D], F32, "yb") for _ in range(2)]
            sa = [kb.sb(es, [128, 512], BF16, "sa") for _ in range(2)]
            pA = [kb.ps(es, [128, 512], F32, "pA") for _ in range(2)]
            pB = [kb.ps(es, [128, 512], F32, "pB") for _ in range(2)]
            pT = [kb.ps(es, [128, 1024], BF16, "pTm") for _ in range(2)]
            pY = [kb.ps(es, [128, 512], F32, "pY") for _ in range(2)]
            stage_i = [0]

            def load_cast(dst_ap, dst_trk, src_ap, shape3):
                st = stage[stage_i[0] % 2]
                a_, b_ = shape3
                sv = st[:, 0:a_ * b_].rearrange("p (a b) -> p a b", a=a_)
                kb.dma("sp", out=sv, in_=src_ap, writes=[st])
                if stage_i[0] % 2 == 0:
                    kb.op("dve", lambda e: e.tensor_copy(out=dst_ap, in_=sv), reads=[st], writes=[dst_trk])
                else:
                    kb.op("act", lambda e: e.copy(out=dst_ap, in_=sv), reads=[st], writes=[dst_trk])
                stage_i[0] += 1

            def calls_of(ex):
                return [(j, 128, idxt[:, ex, j:j + 1], j * 128) for j in range(NCALL)] + [(NCALL, CAPC, idxc[:, ex:ex + 1], NSLOT)]

            def issue_gathers(ex):
                gb = ex % 2
                for (j, rows, iap, col0) in calls_of(ex):
                    g = G[j]
                    kb.dma("pool", reads=[self.H2, idxt, idxc], writes=[g],
                           fn=lambda e, g=g, rows=rows, iap=iap: e.indirect_dma_start(out=g[0:rows, :], out_offset=None, in_=self.H2[:, :],
                                                                                      in_offset=bass.IndirectOffsetOnAxis(ap=iap, axis=0)))
                    kb.dma("pool", reads=[self.AFF, idxt, idxc], writes=[gatek[gb][j]],
                           fn=lambda e, gb=gb, j=j, rows=rows, iap=iap: e.indirect_dma_start(out=gate[gb][0:rows, j, :], out_offset=None, in_=self.AFF[:, :],
                                                                                           in_offset=bass.IndirectOffsetOnAxis(ap=iap, axis=0)))

            issue_gathers(0)
            tcount = 0
            for ex in range(NE):
                gb = ex % 2
                w1src = I["w_e1"][li, ex].rearrange("(kc p) f -> p kc f", p=128)
                w3src = I["w_e3"][li, ex].rearrange("(kc p) f -> p kc f", p=128)
                w2src = I["w_e2"][li, ex].rearrange("(fc p) d -> p fc d", p=128)
                calls = calls_of(ex)
                for (j, rows, iap, col0) in calls:
                    g = G[j]
                    p = pT[tcount % 2]
                    tcount += 1
                    for kc in range(8):
                        kb.op("pe", lambda e, p=p, g=g, kc=kc, rows=rows: e.transpose(out=p[:, kc * 128:kc * 128 + rows],
                                                                                    in_=g[0:rows, kc * 128:(kc + 1) * 128],
                                                                                    identity=self.ident_bf[0:rows, 0:rows]),
                              reads=[g, self.ident_bf], writes=[p])
                    ev = "dve" if tcount % 2 == 0 else "act"
                    if ev == "dve":
                        kb.op("dve", lambda e, p=p, rows=rows, col0=col0: e.tensor_copy(
                            out=xgT[:, :, col0:col0 + rows], in_=p[:, :].rearrange("p (k t) -> p k t", k=8)[:, :, 0:rows]),
                              reads=[p], writes=[xgT])
                    else:
                        kb.op("act", lambda e, p=p, rows=rows, col0=col0: e.copy(
                            out=xgT[:, :, col0:col0 + rows], in_=p[:, :].rearrange("p (k t) -> p k t", k=8)[:, :, 0:rows]),
                              reads=[p], writes=[xgT])
                if ex + 1 < NE:
                    issue_gathers(ex + 1)
                for q in range(4):
                    wb = w13[q % 2]
                    load_cast(wb[0][:, :, :], wb[0].k, w1src[:, :, q * 512:(q + 1) * 512], (8, 512))
                    load_cast(wb[1][:, :, :], wb[1].k, w3src[:, :, q * 512:(q + 1) * 512], (8, 512))
                    for f4 in range(4):
                        fc = q * 4 + f4
                        for ci, (c0, cn) in enumerate(chunks):
                            k = (fc * NCH + ci) % 2
                            for kc in range(8):
                                kb.op("pe", lambda e, k=k, kc=kc, f4=f4, c0=c0, cn=cn, wb=wb: e.matmul(
                                    pA[k][:, 0:cn], lhsT=wb[0][:, kc, f4 * 128:(f4 + 1) * 128], rhs=xgT[:, kc, c0:c0 + cn],
                                    start=(kc == 0), stop=(kc == 7)), reads=[wb[0], xgT], writes=[pA[k]])
                            for kc in range(8):
                                kb.op("pe", lambda e, k=k, kc=kc, f4=f4, c0=c0, cn=cn, wb=wb: e.matmul(
                                    pB[k][:, 0:cn], lhsT=wb[1][:, kc, f4 * 128:(f4 + 1) * 128], rhs=xgT[:, kc, c0:c0 + cn],
                                    start=(kc == 0), stop=(kc == 7)), reads=[wb[1], xgT], writes=[pB[k]])
                            kb.op("act", lambda e, k=k, cn=cn: e.activation(out=sa[k][:, 0:cn], in_=pA[k][:, 0:cn], func=AF.Silu),
                                  reads=[pA[k]], writes=[sa[k]])
                            kb.op("dve", lambda e, k=k, cn=cn, c0=c0, fc=fc: e.tensor_tensor(out=gT[:, fc, c0:c0 + cn], in0=sa[k][:, 0:cn],
                                                                                          in1=pB[k][:, 0:cn], op=ALU.mult),
                                  reads=[sa[k], pB[k]], writes=[gTk[fc]])
                for g4 in range(4):
                    load_cast(w2[:, g4 * 4:(g4 + 1) * 4, :], w2k[g4], w2src[:, g4 * 4:(g4 + 1) * 4, :], (4, D))
                for (j, rows, iap, col0) in calls:
                    s = 0 if j < NCALL else 1
                    y = yb[j % 2]
                    for h in range(2):
                        p = pY[h]
                        for fc in range(16):
                            kb.op("pe", lambda e, p=p, fc=fc, col0=col0, rows=rows, h=h: e.matmul(
                                p[0:rows, :], lhsT=gT[:, fc, col0:col0 + rows], rhs=w2[:, fc, h * 512:(h + 1) * 512],
                                start=(fc == 0), stop=(fc == 15)), reads=[gTk[fc], w2k[fc // 4]], writes=[p])
                        kb.op("dve", lambda e, p=p, y=y, h=h, rows=rows, gb=gb, j=j, s=s, ex=ex: e.scalar_tensor_tensor(
                            out=y[0:rows, h * 512:(h + 1) * 512], in0=p[0:rows, :], scalar=gate[gb][0:rows, j, ex:ex + 1],
                            in1=ga2[s][0:rows, h * 512:(h + 1) * 512], op0=ALU.mult, op1=ALU.mult),
                              reads=[p, gatek[gb][j], ga2[s]], writes=[y])
                    kb.dma("pool", reads=[y, idxt, idxc], writes=[self.X],
                           fn=lambda e, y=y, rows=rows, iap=iap: e.indirect_dma_start(out=self.X[:, :],
                                                                                      out_offset=bass.IndirectOffsetOnAxis(ap=iap, axis=0),
                                                                                      in_=y[0:rows, :], in_offset=None, compute_op=ALU.add))

    def load_w_bf16(self, es, dst, src3, ncols, stage):
        kb = self.kb
        i = 0
        for c0 in range(0, ncols, 512):
            cn = min(512, ncols - c0)
            st = stage[i % 2]
            sv = st[:, 0:8 * cn].rearrange("p (a b) -> p a b", a=8)
            kb.dma("sp", out=sv, in_=src3[:, :, c0:c0 + cn], writes=[st])
            if i % 2 == 0:
                kb.op("pool", lambda e, sv=sv, c0=c0, cn=cn: e.tensor_copy(out=dst[:, :, c0:c0 + cn], in_=sv), reads=[st], writes=[dst])
            else:
                kb.op("act", lambda e, sv=sv, c0=c0, cn=cn: e.copy(out=dst[:, :, c0:c0 + cn], in_=sv), reads=[st], writes=[dst])
            i += 1

    def transpose_tile(self, hb, hT, pT):
        kb = self.kb
        for kc in range(8):
            kb.op("pe", lambda e, kc=kc: e.transpose(out=pT[:, kc * 128:(kc + 1) * 128], in_=hb[:, kc * 128:(kc + 1) * 128],
                                                     identity=self.ident_bf[:, :]), reads=[hb, self.ident_bf], writes=[pT])
        kb.op("act", lambda e: e.copy(out=hT[:, :, :], in_=pT[:, :].rearrange("p (k t) -> p k t", k=8)), reads=[pT], writes=[hT])

    def ntiles(self, need_ctx):
        return NTILE if need_ctx else 64

    def ph_qkv(self, li):
        kb = self.kb
        I = self.I
        j = li // 2
        if not hasattr(self, "QKT"):
            self.QKT = kb.dram("QKT", [16, 128, NT], BF16)
            self.VD = kb.dram("VD", [NT + 64, 16 * 65], BF16)
            self.OD = kb.dram("OD", [NT, D], BF16)
        with contextlib.ExitStack() as es:
            svec = [self.load_vec(es, s, 1, "s1") for s in range(2)]
            shvec = [self.load_vec(es, s, 0, "sh1") for s in range(2)]
            negh = kb.sb(es, [128, 1], F32, "negh")
            kb.op("dve", lambda e: e.memset(negh[:, :], -0.5), writes=[negh])
            stage = [kb.sb(es, [128, 4096], F32, "stage") for _ in range(2)]
            W = kb.sb(es, [128, 8, 3 * D], BF16, "wqkv")
            self.load_w_bf16(es, W, I["w_qkv"][j].rearrange("(kc p) n -> p kc n", p=128), 3 * D, stage)
            NB = 6
            xt = [kb.sb(es, [128, D], F32, "xt") for _ in range(NB)]
            hf = [kb.sb(es, [128, D], F32, "hf") for _ in range(NB)]
            hb = [kb.sb(es, [128, D], BF16, "hb") for _ in range(NB)]
            junk = kb.sb(es, [128, D], BF16, "junk")
            hT = [kb.sb(es, [128, 8, 128], BF16, "hT") for _ in range(NB)]
            sm = [[kb.sb(es, [128, 1], F32, "sm") for _ in range(2)] for _ in range(NB)]
            qk = [kb.sb(es, [128, 16, 128], BF16, "qk") for _ in range(2)]
            vt = [kb.sb(es, [128, 16, 65], BF16, "vt") for _ in range(2)]
            for b in range(2):
                kb.op("dve", lambda e, b=b: e.memset(vt[b][:, :, :], 1.0), writes=[vt[b]])
            pT = [kb.ps(es, [128, 1024], BF16, "pT") for _ in range(2)]
            pq = [kb.ps(es, [128, 512], F32, "pq") for _ in range(6)]

            def s0(ti):
                b = ti % NB
                r0 = ti * 128
                kb.dma("sp", out=xt[b][:, :], in_=self.X[r0:r0 + 128, :], reads=[self.X], writes=[xt[b]])

            def s1a(ti):
                b = ti % NB
                self.norm_a((sm[b][0], sm[b][1]), xt[b], junk, negh)

            def s1(ti):
                b = ti % NB
                s = 0 if ti < 64 else 1
                self.norm_b((sm[b][0], sm[b][1]), xt[b], svec[s], shvec[s], hf[b], hb[b])

            def s2(ti):
                b = ti % NB
                self.transpose_tile(hb[b], hT[b], pT[ti % 2])

            def s3(ti):
                b = ti % NB
                b2 = ti % 2
                r0 = ti * 128
                for c4 in range(4):
                    p = pq[(6 * ti + c4) % 6]
                    for cc in range(4):
                        c = c4 * 4 + cc
                        for kc in range(8):
                            kb.op("pe", lambda e, p=p, cc=cc, c=c, kc=kc, b=b: e.matmul(p[:, cc * 128:(cc + 1) * 128], lhsT=W[:, kc, c * 128:(c + 1) * 128],
                                                                                     rhs=hT[b][:, kc, :], start=(kc == 0), stop=(kc == 7)),
                                  reads=[W, hT[b]], writes=[p])
                    sc = 0.125 if c4 < 2 else 1.0
                    kb.op("act", lambda e, p=p, c4=c4, b2=b2, sc=sc: e.activation(out=qk[b2][:, c4 * 4:c4 * 4 + 4, :], in_=p[:, :].rearrange("p (c t) -> p c t", c=4),
                                                                               func=AF.Copy, scale=sc), reads=[p], writes=[qk[b2]])
                kb.dma("sp", out=self.QKT[:, :, r0:r0 + 128].rearrange("c p t -> p c t"), in_=qk[b2][:, :, :], reads=[qk[b2]], writes=[self.QKT])
                for h2 in range(2):
                    p = pq[(6 * ti + 4 + h2) % 6]
                    for kc in range(8):
                        kb.op("pe", lambda e, p=p, kc=kc, h2=h2, b=b: e.matmul(p[:, :], lhsT=hT[b][:, kc, :], rhs=W[:, kc, 2 * D + h2 * 512:2 * D + (h2 + 1) * 512],
                                                                            start=(kc == 0), stop=(kc == 7)), reads=[W, hT[b]], writes=[p])
                    kb.op("dve", lambda e, p=p, h2=h2, b2=b2: e.tensor_copy(out=vt[b2][:, h2 * 8:(h2 + 1) * 8, 0:64], in_=p[:, :].rearrange("p (h d) -> p h d", h=8)),
                          reads=[p], writes=[vt[b2]])
                kb.dma("sp", out=self.VD[r0:r0 + 128, :], in_=vt[b2][:, :, :].rearrange("p h d -> p (h d)"), reads=[vt[b2]], writes=[self.VD])

            pipeline(NTILE, [s0, s1a, s1, s2, s3])

    def ph_attn(self, li, need_ctx):
        kb = self.kb
        I = self.I
        j = li // 2
        with contextlib.ExitStack() as es:
            KT = kb.sb(es, [128, NT], BF16, "KT")
            QT = kb.sb(es, [128, NT], BF16, "QT")
            V0 = kb.sb(es, [128, NTILE, 130], BF16, "V0")
            bst = [kb.sb(es, [128, 2, 5, 128], F32, "bst") for _ in range(2)]
            EB = [kb.sb(es, [128, 2, 5, 128], BF16, "EB") for _ in range(5)]
            NP = 5
            PT = [kb.sb(es, [128, 7, 128], BF16, "PT") for _ in range(NP)]
            osb = [kb.sb(es, [128, 128], BF16, "osb") for _ in range(3)]
            rec = [kb.sb(es, [128, 1], F32, "rec") for _ in range(4)]
            pSa = [kb.ps(es, [128, 512], F32, "pSa") for _ in range(2)]
            pSb = [kb.ps(es, [128, 512], F32, "pSb") for _ in range(2)]
            pO = [kb.ps(es, [128, 65], F32, "pO") for _ in range(3)]
            VDv = self.VD[0:NT, :].rearrange("(t p) c -> p t c", p=128)
            for hp in range(8):
                kb.dma("sp", out=QT[:, :], in_=self.QKT[hp, :, :], reads=[self.QKT], writes=[QT])
                kb.dma("sp", out=KT[:, :], in_=self.QKT[8 + hp, :, :], reads=[self.QKT], writes=[KT])
                kb.dma("sp", out=V0[:, :, :], in_=VDv[:, :, hp * 130:(hp + 1) * 130], reads=[self.VD], writes=[V0])
                for v in range(5):
                    st = bst[v % 2]
                    kb.dma("sp", out=st[:, :, :, :], in_=I["biasT"][j, v, 2 * hp:2 * hp + 2].rearrange("h p c q -> p h c q"), writes=[st])
                    kb.op("act", lambda e, st=st, v=v: e.activation(out=EB[v][:, :, :, :], in_=st[:, :, :, :], func=AF.Exp), reads=[st], writes=[EB[v]])
                units = []
                for rp in range(64):
                    for hl in range(2):
                        units.append((rp, hl))
                if need_ctx:
                    for cq in range(2):
                        for hl in range(2):
                            units.append((64 + cq, hl))

                def info(u):
                    rp, hl = units[u]
                    if rp < 64:
                        t0 = att_base(rp) // 2
                        tiles = [t0 + c for c in range(5)] + [64, 65]
                        q0 = 128 * rp
                        eb = EB[att_variant(rp)]
                    else:
                        tiles = [64, 65]
                        q0 = L + 128 * (rp - 64)
                        eb = None
                    return rp, hl, tiles, q0, eb

                def sA(u):
                    rp, hl, tiles, q0, eb = info(u)
                    ho = hl * 64
                    pa_, pb_ = pSa[u % 2], pSb[u % 2]
                    for c, t in enumerate(tiles):
                        dst = pa_[:, c * 128:(c + 1) * 128] if c < 4 else pb_[:, (c - 4) * 128:(c - 3) * 128]
                        trk = pa_ if c < 4 else pb_
                        kb.op("pe", lambda e, dst=dst, ho=ho, t=t, q0=q0: e.matmul(dst, lhsT=KT[ho:ho + 64, t * 128:(t + 1) * 128],
                                                                                rhs=QT[ho:ho + 64, q0:q0 + 128], start=True, stop=True),
                              reads=[KT, QT], writes=[trk])

                def sB(u):
                    rp, hl, tiles, q0, eb = info(u)
                    pa_, pb_ = pSa[u % 2], pSb[u % 2]
                    pt = PT[u % NP]
                    n = len(tiles)
                    na = min(n, 4)
                    kb.op("act", lambda e, pt=pt, pa_=pa_, na=na: e.activation(out=pt[:, 0:na, :].rearrange("p c q -> p (c q)"), in_=pa_[:, 0:na * 128], func=AF.Exp),
                          reads=[pa_], writes=[pt])
                    if n > 4:
                        kb.op("act", lambda e, pt=pt, pb_=pb_, n=n: e.activation(out=pt[:, 4:n, :].rearrange("p c q -> p (c q)"), in_=pb_[:, 0:(n - 4) * 128], func=AF.Exp),
                              reads=[pb_], writes=[pt])

                def sB2(u):
                    rp, hl, tiles, q0, eb = info(u)
                    pt = PT[u % NP]
                    if eb is not None:
                        me = "pool" if (u % 5) < 3 else "dve"
                        kb.op(me, lambda e, pt=pt, eb=eb, hl=hl: e.tensor_tensor(out=pt[:, 0:5, :], in0=pt[:, 0:5, :], in1=eb[:, hl, :, :], op=ALU.mult),
                              reads=[pt, eb], writes=[pt])

                def sC(u):
                    rp, hl, tiles, q0, eb = info(u)
                    pt = PT[u % NP]
                    po = pO[u % 3]
                    ob = osb[(u // 2) % 3]
                    n = len(tiles)
                    for c, t in enumerate(tiles):
                        kb.op("pe", lambda e, po=po, c=c, t=t, pt=pt, hl=hl, n=n: e.matmul(po[:, :], lhsT=pt[:, c, :], rhs=V0[:, t, hl * 65:(hl + 1) * 65],
                                                                                        start=(c == 0), stop=(c == n - 1)), reads=[pt, V0], writes=[po])
                    rc = rec[u % 4]
                    kb.op("dve", lambda e, po=po, rc=rc: e.reciprocal(out=rc[:, 0:1], in_=po[:, 64:65]), reads=[po], writes=[rc])
                    kb.op("dve", lambda e, po=po, rc=rc, hl=hl, ob=ob: e.tensor_scalar(out=ob[:, hl * 64:(hl + 1) * 64], in0=po[:, 0:64],
                                                                                      scalar1=rc[:, 0:1], scalar2=None, op0=ALU.mult),
                          reads=[po, rc], writes=[ob])
                    if hl == 1:
                        kb.dma("sp", out=self.OD[q0:q0 + 128, hp * 128:(hp + 1) * 128], in_=ob[:, :], reads=[ob], writes=[self.OD])

                pipeline(len(units), [sA, sB, sB2, sC])

    def ph_oproj(self, li, wname, srcname, need_ctx):
        kb = self.kb
        I = self.I
        j = li // 2
        SRC = getattr(self, srcname)
        with contextlib.ExitStack() as es:
            ga = [self.load_vec(es, s, 2, "ga1") for s in range(2)]
            stage = [kb.sb(es, [128, 4096], F32, "stage") for _ in range(2)]
            W = kb.sb(es, [128, 8, D], BF16, "wo")
            self.load_w_bf16(es, W, I[wname][j].rearrange("(kc p) n -> p kc n", p=128), D, stage)
            NB = 6
            xt = [kb.sb(es, [128, D], F32, "xt") for _ in range(NB)]
            hb = [kb.sb(es, [128, D], BF16, "hb") for _ in range(NB)]
            tmp = [kb.sb(es, [128, D], F32, "tmp") for _ in range(3)]
            hT = [kb.sb(es, [128, 8, 128], BF16, "hT") for _ in range(NB)]
            pT = [kb.ps(es, [128, 1024], BF16, "pT") for _ in range(2)]
            pq = [kb.ps(es, [128, 512], F32, "pq") for _ in range(4)]

            def s0(ti):
                b = ti % NB
                r0 = ti * 128
                kb.dma("sp", out=xt[b][:, :], in_=self.X[r0:r0 + 128, :], reads=[self.X], writes=[xt[b]])
                kb.dma("sp", out=hb[b][:, :], in_=SRC[r0:r0 + 128, :], reads=[SRC], writes=[hb[b]])

            def s1(ti):
                b = ti % NB
                self.transpose_tile(hb[b], hT[b], pT[ti % 2])

            def s2(ti):
                b = ti % NB
                b2 = ti % 3
                s = 0 if ti < 64 else 1
                for h2 in range(2):
                    p = pq[(2 * ti + h2) % 4]
                    for kc in range(8):
                        kb.op("pe", lambda e, p=p, kc=kc, h2=h2, b=b: e.matmul(p[:, :], lhsT=hT[b][:, kc, :], rhs=W[:, kc, h2 * 512:(h2 + 1) * 512],
                                                                            start=(kc == 0), stop=(kc == 7)), reads=[W, hT[b]], writes=[p])
                    kb.op("dve", lambda e, p=p, h2=h2, b2=b2, s=s: e.tensor_tensor(out=tmp[b2][:, h2 * 512:(h2 + 1) * 512], in0=p[:, :],
                                                                                in1=ga[s][:, h2 * 512:(h2 + 1) * 512], op=ALU.mult),
                          reads=[p, ga[s]], writes=[tmp[b2]])

            def s3(ti):
                b = ti % NB
                b2 = ti % 3
                r0 = ti * 128
                kb.op("pool", lambda e, b=b, b2=b2: e.tensor_tensor(out=xt[b][:, :], in0=xt[b][:, :], in1=tmp[b2][:, :], op=ALU.add),
                      reads=[xt[b], tmp[b2]], writes=[xt[b]])
                kb.dma("sp", out=self.X[r0:r0 + 128, :], in_=xt[b][:, :], reads=[xt[b]], writes=[self.X])

            pipeline(self.ntiles(need_ctx), [s0, s1, s2, s3])

    def cload(self, es, name):
        shp, ty = CONST_SPECS[name]
        t = self.kb.sb(es, shp, ty, name)
        sl = tuple(slice(None) for _ in shp)
        self.kb.dma("sp", out=t[sl], in_=self.I[name][sl], writes=[t])
        return t

    def even_scratch(self):
        kb = self.kb
        if not hasattr(self, "PR"):
            self.PR = kb.dram("PR", [NT, 1536], BF16)
            self.DTR = kb.dram("DTR", [NT, 16], F32)
            self.AD = kb.dram("AD", [NT, 1024], BF16)
            self.XBC = kb.dram("XBC", [NT, 1024], BF16)
            self.DTA = kb.dram("DTA", [NT, 32], F32)
            self.YF = kb.dram("YF", [NT, 512], F32)
            self.MIX = kb.dram("MIX", [NT, 1024], BF16)
            self.GD = kb.dram("GD", [2, 64, 128, 512], BF16)

    def ph_inproj(self, li):
        kb = self.kb
        I = self.I
        j = li // 2
        self.even_scratch()
        with contextlib.ExitStack() as es:
            svec = [self.load_vec(es, s, 1, "s1") for s in range(2)]
            shvec = [self.load_vec(es, s, 0, "sh1") for s in range(2)]
            negh = kb.sb(es, [128, 1], F32, "negh")
            kb.op("dve", lambda e: e.memset(negh[:, :], -0.5), writes=[negh])
            stage = [kb.sb(es, [128, 4096], F32, "stage") for _ in range(2)]
            W = kb.sb(es, [128, 8, 2064], BF16, "win")
            self.load_w_bf16(es, W, I["w_in_e"][j].rearrange("(kc p) n -> p kc n", p=128), 2064, stage)
            CS = self.cload(es, "CS")
            NB = 6
            xt = [kb.sb(es, [128, D], F32, "xt") for _ in range(NB)]
            hf = [kb.sb(es, [128, D], F32, "hf") for _ in range(NB)]
            hb = [kb.sb(es, [128, D], BF16, "hb") for _ in range(NB)]
            junk = kb.sb(es, [128, D], BF16, "junk")
            hT = [kb.sb(es, [128, 8, 128], BF16, "hT") for _ in range(NB)]
            sm = [[kb.sb(es, [128, 1], F32, "sm") for _ in range(2)] for _ in range(NB)]
            prt = [kb.sb(es, [128, 1536], BF16, "prt") for _ in range(2)]
            dtr = [kb.sb(es, [128, 16], F32, "dtr") for _ in range(2)]
            uT = [kb.sb(es, [128, 4, 128], BF16, "uT") for _ in range(2)]
            At = [kb.sb(es, [128, 2, 4, 128], BF16, "At") for _ in range(2)]
            pT = [kb.ps(es, [128, 1024], BF16, "pT") for _ in range(2)]
            pq = [kb.ps(es, [128, 512], F32, "pq") for _ in range(4)]
            pa = [kb.ps(es, [128, 512], F32, "pa") for _ in range(2)]

            def s0(ti):
                b = ti % NB
                r0 = ti * 128
                kb.dma("sp", out=xt[b][:, :], in_=self.X[r0:r0 + 128, :], reads=[self.X], writes=[xt[b]])

            def s1a(ti):
                b = ti % NB
                self.norm_a((sm[b][0], sm[b][1]), xt[b], junk, negh)

            def s1(ti):
                b = ti % NB
                s = 0 if ti < 64 else 1
                self.norm_b((sm[b][0], sm[b][1]), xt[b], svec[s], shvec[s], hf[b], hb[b])

            def s2(ti):
                b = ti % NB
                self.transpose_tile(hb[b], hT[b], pT[ti % 2])

            def s3(ti):
                b = ti % NB
                b2 = ti % 2
                r0 = ti * 128
                for ci, (c0, cn) in enumerate([(0, 512), (512, 512), (1024, 512), (1536, 16)]):
                    p = pq[(5 * ti + ci) % 4]
                    for kc in range(8):
                        kb.op("pe", lambda e, p=p, kc=kc, c0=c0, cn=cn, b=b: e.matmul(p[:, 0:cn], lhsT=hT[b][:, kc, :], rhs=W[:, kc, c0:c0 + cn],
                                                                                   start=(kc == 0), stop=(kc == 7)), reads=[W, hT[b]], writes=[p])
                    if ci < 3:
                        if ci % 2 == 0:
                            kb.op("act", lambda e, p=p, c0=c0, b2=b2: e.copy(out=prt[b2][:, c0:c0 + 512], in_=p[:, :]), reads=[p], writes=[prt[b2]])
                        else:
                            kb.op("dve", lambda e, p=p, c0=c0, b2=b2: e.tensor_copy(out=prt[b2][:, c0:c0 + 512], in_=p[:, :]), reads=[p], writes=[prt[b2]])
                    else:
                        kb.op("dve", lambda e, p=p, b2=b2: e.tensor_copy(out=dtr[b2][:, :], in_=p[:, 0:16]), reads=[p], writes=[dtr[b2]])
                kb.dma("sp", out=self.PR[r0:r0 + 128, :], in_=prt[b2][:, :], reads=[prt[b2]], writes=[self.PR])
                kb.dma("sp", out=self.DTR[r0:r0 + 128, :], in_=dtr[b2][:, :], reads=[dtr[b2]], writes=[self.DTR])
                p = pq[(5 * ti + 4) % 4]
                for g in range(4):
                    for kc in range(8):
                        kb.op("pe", lambda e, p=p, kc=kc, g=g, b=b: e.matmul(p[:, g * 128:(g + 1) * 128], lhsT=W[:, kc, 1552 + g * 128:1552 + (g + 1) * 128],
                                                                          rhs=hT[b][:, kc, :], start=(kc == 0), stop=(kc == 7)), reads=[W, hT[b]], writes=[p])
                kb.op("act", lambda e, p=p, b2=b2: e.copy(out=uT[b2][:, :, :], in_=p[:, :].rearrange("p (g t) -> p g t", g=4)), reads=[p], writes=[uT[b2]])
                for gh in range(2):
                    for g2 in range(2):
                        g = gh * 2 + g2
                        kb.op("pe", lambda e, gh=gh, g2=g2, g=g, b2=b2: e.matmul(pa[gh][:, g2 * 256:(g2 + 1) * 256], lhsT=uT[b2][:, g, :], rhs=CS[:, :],
                                                                              start=True, stop=True), reads=[uT[b2], CS], writes=[pa[gh]])
                    kb.op("dve", lambda e, gh=gh, b2=b2: e.tensor_copy(out=At[b2][:, :, 2 * gh:2 * gh + 2, :],
                                                                     in_=pa[gh][:, :].rearrange("p (g cs q) -> p cs g q", g=2, cs=2)),
                          reads=[pa[gh]], writes=[At[b2]])
                kb.dma("sp", out=self.AD[r0:r0 + 128, :], in_=At[b2][:, :, :, :].rearrange("p cs g q -> p (cs g q)"), reads=[At[b2]], writes=[self.AD])

            pipeline(NTILE, [s0, s1a, s1, s2, s3])

    def ph_conv(self, li):
        kb = self.kb
        I = self.I
        j = li // 2
        with contextlib.ExitStack() as es:
            cw = [kb.sb(es, [128, D], F32, "cw") for _ in range(3)]
            for k in range(3):
                kb.dma("sp", out=cw[k][:, :], in_=I["conv_w"][j, k, :].partition_broadcast(128), writes=[cw[k]])
            cb = kb.sb(es, [128, D], F32, "cb")
            kb.dma("sp", out=cb[:, :], in_=I["conv_b"][j, :].partition_broadcast(128), writes=[cb])
            dtb = kb.sb(es, [128, 16], F32, "dtb")
            kb.dma("sp", out=dtb[:, :], in_=I["dt_bias"][j, :].partition_broadcast(128), writes=[dtb])
            abc = kb.sb(es, [128, 16], F32, "abc")
            kb.dma("sp", out=abc[:, :], in_=I["a_log"][j, :].partition_broadcast(128), writes=[abc])
            kb.op("act", lambda e: e.activation(out=abc[:, :], in_=abc[:, :], func=AF.Exp), reads=[abc], writes=[abc])
            kb.op("dve", lambda e: e.tensor_scalar(out=abc[:, :], in0=abc[:, :], scalar1=-1.0, scalar2=None, op0=ALU.mult), reads=[abc], writes=[abc])
            NB = 4
            m1 = [kb.sb(es, [128, D], BF16, "m1") for _ in range(NB)]
            c0 = [kb.sb(es, [128, D], BF16, "c0") for _ in range(NB)]
            p1 = [kb.sb(es, [128, D], BF16, "p1") for _ in range(NB)]
            acc = [kb.sb(es, [128, D], F32, "acc") for _ in range(NB)]
            t2 = [kb.sb(es, [128, D], F32, "t2") for _ in range(NB)]
            t3 = [kb.sb(es, [128, D], F32, "t3") for _ in range(NB)]
            xo = [kb.sb(es, [128, D], BF16, "xo") for _ in range(NB)]
            dtr = [kb.sb(es, [128, 16], F32, "dtr") for _ in range(NB)]
            dta = [kb.sb(es, [128, 32], F32, "dta") for _ in range(NB)]
            src = self.PR

            def s0(ti):
                b = ti % NB
                r0 = ti * 128
                first = ti in (0, 64)
                last = ti in (63, 65)
                if first:
                    kb.op("dve", lambda e, b=b: e.memset(m1[b][:, :], 0.0), writes=[m1[b]])
                    kb.dma("sp", out=m1[b][1:128, :], in_=src[r0:r0 + 127, 512:1536], reads=[src], writes=[m1[b]])
                else:
                    kb.dma("sp", out=m1[b][:, :], in_=src[r0 - 1:r0 + 127, 512:1536], reads=[src], writes=[m1[b]])
                kb.dma("sp", out=c0[b][:, :], in_=src[r0:r0 + 128, 512:1536], reads=[src], writes=[c0[b]])
                if last:
                    kb.op("dve", lambda e, b=b: e.memset(p1[b][:, :], 0.0), writes=[p1[b]])
                    kb.dma("sp", out=p1[b][0:127, :], in_=src[r0 + 1:r0 + 128, 512:1536], reads=[src], writes=[p1[b]])
                else:
                    kb.dma("sp", out=p1[b][:, :], in_=src[r0 + 1:r0 + 129, 512:1536], reads=[src], writes=[p1[b]])
                kb.dma("sp", out=dtr[b][:, :], in_=self.DTR[r0:r0 + 128, :], reads=[self.DTR], writes=[dtr[b]])

            def s1(ti):
                b = ti % NB
                kb.op("dve", lambda e, b=b: e.tensor_tensor(out=acc[b][:, :], in0=m1[b][:, :], in1=cw[0][:, :], op=ALU.mult), reads=[m1[b], cw[0]], writes=[acc[b]])
                kb.op("pool", lambda e, b=b: e.tensor_tensor(out=t2[b][:, :], in0=c0[b][:, :], in1=cw[1][:, :], op=ALU.mult), reads=[c0[b], cw[1]], writes=[t2[b]])
                kb.op("pool", lambda e, b=b: e.tensor_tensor(out=t3[b][:, :], in0=p1[b][:, :], in1=cw[2][:, :], op=ALU.mult), reads=[p1[b], cw[2]], writes=[t3[b]])
                kb.op("dve", lambda e, b=b: e.tensor_tensor(out=dtr[b][:, :], in0=dtr[b][:, :], in1=dtb[:, :], op=ALU.add), reads=[dtr[b], dtb], writes=[dtr[b]])

            def s2(ti):
                b = ti % NB
                kb.op("dve", lambda e, b=b: e.tensor_tensor(out=acc[b][:, :], in0=acc[b][:, :], in1=t2[b][:, :], op=ALU.add), reads=[acc[b], t2[b]], writes=[acc[b]])
                kb.op("pool", lambda e, b=b: e.tensor_tensor(out=t3[b][:, :], in0=t3[b][:, :], in1=cb[:, :], op=ALU.add), reads=[t3[b], cb], writes=[t3[b]])
                kb.op("act", lambda e, b=b: e.activation(out=dtr[b][:, :], in_=dtr[b][:, :], func=AF.Exp), reads=[dtr[b]], writes=[dtr[b]])

            def s3(ti):
                b = ti % NB
                kb.op("dve", lambda e, b=b: e.tensor_tensor(out=acc[b][:, :], in0=acc[b][:, :], in1=t3[b][:, :], op=ALU.add), reads=[acc[b], t3[b]], writes=[acc[b]])
                kb.op("act", lambda e, b=b: e.activation(out=dta[b][:, 0:16], in_=dtr[b][:, :], func=AF.Ln, bias=1.0), reads=[dtr[b]], writes=[dta[b]])

            def s4(ti):
                b = ti % NB
                r0 = ti * 128
                kb.op("act", lambda e, b=b: e.activation(out=xo[b][:, :], in_=acc[b][:, :], func=AF.Silu), reads=[acc[b]], writes=[xo[b]])
                kb.dma("sp", out=self.XBC[r0:r0 + 128, :], in_=xo[b][:, :], reads=[xo[b]], writes=[self.XBC])
                kb.op("dve", lambda e, b=b: e.tensor_tensor(out=dta[b][:, 16:32], in0=dta[b][:, 0:16], in1=abc[:, :], op=ALU.mult), reads=[dta[b], abc], writes=[dta[b]])
                kb.dma("sp", out=self.DTA[r0:r0 + 128, :], in_=dta[b][:, :], reads=[dta[b]], writes=[self.DTA])

            pipeline(NTILE, [s0, s1, s2, s3, s4])

    def ph_ssd(self, li, d):
        kb = self.kb
        I = self.I
        j = li // 2
        with contextlib.ExitStack() as es:
            tri = self.cload(es, "triU" if d == 0 else "triL")
            ones = self.cload(es, "onesf")
            neg = self.cload(es, "negU" if d == 0 else "negL")
            dsk = kb.sb(es, [128, 8], F32, "dsk")
            kb.dma("sp", out=dsk[:, :], in_=I["d_skip"][j, :].partition_broadcast(128), writes=[dsk])
            gss = kb.sb(es, [128, 512], F32, "gss")
            kb.dma("sp", out=gss[:, :], in_=I["g_ssd"][j, :].partition_broadcast(128), writes=[gss])
            negh = kb.sb(es, [128, 1], F32, "negh")
            kb.op("dve", lambda e: e.memset(negh[:, :], -0.5), writes=[negh])
            hs = kb.sb(es, [128, 8, 64], F32, "hs")
            hsb = kb.sb(es, [128, 8, 64], BF16, "hsb")
            kb.op("dve", lambda e: e.memset(hs[:, :, :], 0.0), writes=[hs])
            kb.op("dve", lambda e: e.memset(hsb[:, :, :], 0.0), writes=[hsb])
            NB = 4
            R = lambda shape, ty, nm, n=NB: [kb.sb(es, shape, ty, nm) for _ in range(n)]
            xbc = R([128, D], BF16, "xbc")
            dta = R([128, 32], F32, "dta")
            BCT = R([128, 4, 128], BF16, "BCT")
            cum = R([128, 16], F32, "cum")
            te = R([128, 8], F32, "te")
            cd = R([128, 8], F32, "cd")
            xdt = R([128, 8, 64], BF16, "xdt")
            xw = R([128, 8, 64], BF16, "xw")
            CBT = R([128, 2, 128], BF16, "CBT", 2)
            dU = R([128, 8, 128], F32, "dU", 2)
            Ebc = R([128, 8, 128], BF16, "Ebc", 2)
            CsT = R([128, 8, 128], BF16, "CsT")
            Sg = R([128, 8, 128], F32, "Sg", 2)
            Dm = R([128, 8, 128], BF16, "Dm", 2)
            MT = R([128, 8, 128], BF16, "MT")
            tmp = R([128, 8, 64], F32, "tmp", 2)
            yo = R([128, 512], F32, "yo")
            yf = R([128, 512], F32, "yf")
            zt = R([128, 512], BF16, "zt")
            sz = R([128, 512], F32, "sz", 2)
            junk = kb.sb(es, [128, 256], BF16, "junk")
            ssq = R([128, 2], F32, "ssq")
            rsq = R([128, 2], F32, "rsq")
            mo = R([128, 512], BF16, "mo", 2)
            pT = kb.ps(es, [128, 1024], BF16, "pT")
            pc = kb.ps(es, [128, 16], F32, "pc")
            pcb = kb.ps(es, [128, 256], F32, "pcb")
            pA = [kb.ps(es, [128, 512], F32, "pA") for _ in range(2)]
            py = [kb.ps(es, [128, 512], F32, "py") for _ in range(2)]
            pst = kb.ps(es, [128, 512], F32, "pst")
            order = ([64, 65] + list(range(64))) if d == 0 else ([65, 64] + list(range(63, -1, -1)))

            def views(n):
                b = n % NB
                X_, DT_ = xbc[b], dta[b]
                return (b, n % 2, order[n] * 128, X_, DT_, DT_[:, 8 * d:8 * d + 8], DT_[:, 16 + 8 * d:16 + 8 * d + 8],
                        X_[:, 0:512].rearrange("p (h c) -> p h c", h=8))

            def s0(n):
                b, b2, r0, X_, DT_, dt_d, da_d, x3 = views(n)
                kb.dma("sp", out=X_[:, :], in_=self.XBC[r0:r0 + 128, :], reads=[self.XBC], writes=[X_])
                kb.dma("sp", out=DT_[:, :], in_=self.DTA[r0:r0 + 128, :], reads=[self.DTA], writes=[DT_])
                if d == 1:
                    kb.dma("sp", out=yf[b][:, :], in_=self.YF[r0:r0 + 128, :], reads=[self.YF], writes=[yf[b]])
                    kb.dma("sp", out=zt[b][:, :], in_=self.PR[r0:r0 + 128, 0:512], reads=[self.PR], writes=[zt[b]])

            def s1(n):
                b, b2, r0, X_, DT_, dt_d, da_d, x3 = views(n)
                for q in range(4):
                    kb.op("pe", lambda e, q=q, X_=X_: e.transpose(out=pT[:, q * 128:(q + 1) * 128], in_=X_[:, 512 + q * 128:512 + (q + 1) * 128],
                                                               identity=self.ident_bf[:, :]), reads=[X_, self.ident_bf], writes=[pT])
                kb.op("act", lambda e, b=b: e.copy(out=BCT[b][:, :, :], in_=pT[:, 0:512].rearrange("p (q t) -> p q t", q=4)), reads=[pT], writes=[BCT[b]])
                kb.op("pe", lambda e, da_d=da_d: e.matmul(pc[:, 0:8], lhsT=tri[:, :], rhs=da_d, start=True, stop=True), reads=[tri, DT_], writes=[pc])
                kb.op("pe", lambda e, da_d=da_d: e.matmul(pc[:, 8:16], lhsT=ones[:, :], rhs=da_d, start=True, stop=True), reads=[ones, DT_], writes=[pc])
                kb.op("dve", lambda e, b=b, da_d=da_d: e.tensor_tensor(out=dU[b2][:, :, :], in0=tri[:, :].unsqueeze(1).to_broadcast([128, 8, 128]),
                                                                     in1=da_d.unsqueeze(2).to_broadcast([128, 8, 128]), op=ALU.mult),
                      reads=[tri, DT_], writes=[dU[b2]])
                kb.op("dve", lambda e, b=b: e.tensor_copy(out=cum[b][:, :], in_=pc[:, :]), reads=[pc], writes=[cum[b]])
                for hh in range(2):
                    kb.op("pe", lambda e, hh=hh, b2=b2: e.matmul(pA[hh][:, :], lhsT=ones[:, :], rhs=dU[b2][:, 4 * hh:4 * hh + 4, :].rearrange("p h i -> p (h i)"),
                                                               start=True, stop=True), reads=[ones, dU[b2]], writes=[pA[hh]])
                for g in range(2):
                    kb.op("pe", lambda e, g=g, b=b: e.matmul(pcb[:, g * 128:(g + 1) * 128], lhsT=BCT[b][:, g, :], rhs=BCT[b][:, 2 + g, :], start=True, stop=True),
                          reads=[BCT[b]], writes=[pcb])
                kb.op("dve", lambda e, b=b: e.tensor_tensor(out=te[b][:, :], in0=cum[b][:, 8:16], in1=cum[b][:, 0:8], op=ALU.subtract), reads=[cum[b]], writes=[te[b]])
                kb.op("pool", lambda e, b=b, x3=x3, dt_d=dt_d: e.tensor_tensor(out=xdt[b][:, :, :], in0=x3, in1=dt_d.unsqueeze(2).to_broadcast([128, 8, 64]), op=ALU.mult),
                      reads=[X_, DT_], writes=[xdt[b]])
                for hh in range(2):
                    kb.op("act", lambda e, hh=hh, b2=b2: e.activation(out=Ebc[b2][:, 4 * hh:4 * hh + 4, :].rearrange("p h i -> p (h i)"), in_=pA[hh][:, :], func=AF.Exp),
                          reads=[pA[hh]], writes=[Ebc[b2]])
                kb.op("act", lambda e, b=b: e.activation(out=te[b][:, :], in_=te[b][:, :], func=AF.Exp), reads=[te[b]], writes=[te[b]])
                kb.op("act", lambda e, b=b: e.activation(out=cd[b][:, :], in_=cum[b][:, 8:16], func=AF.Exp), reads=[cum[b]], writes=[cd[b]])
                for h in range(8):
                    kb.op("dve", lambda e, h=h, b=b, b2=b2: e.scalar_tensor_tensor(out=Sg[b2][:, h, :], in0=pA[h // 4][:, (h % 4) * 128:(h % 4 + 1) * 128],
                                                                                scalar=cum[b][:, h:h + 1], in1=neg[:, :], op0=ALU.subtract, op1=ALU.add),
                          reads=[pA[h // 4], cum[b], neg], writes=[Sg[b2]])
                kb.op("act", lambda e, b2=b2: e.copy(out=CBT[b2][:, :, :], in_=pcb[:, :].rearrange("p (g t) -> p g t", g=2)), reads=[pcb], writes=[CBT[b2]])
                kb.op("act", lambda e, b2=b2: e.activation(out=Dm[b2][:, :, :], in_=Sg[b2][:, :, :], func=AF.Exp), reads=[Sg[b2]], writes=[Dm[b2]])
                kb.op("pool", lambda e, b=b: e.tensor_tensor(out=xw[b][:, :, :], in0=xdt[b][:, :, :], in1=te[b][:, :].unsqueeze(2).to_broadcast([128, 8, 64]), op=ALU.mult),
                      reads=[xdt[b], te[b]], writes=[xw[b]])
                for g in range(2):
                    kb.op("pool", lambda e, g=g, b=b, b2=b2: e.tensor_tensor(out=CsT[b][:, 4 * g:4 * g + 4, :], in0=Ebc[b2][:, 4 * g:4 * g + 4, :],
                                                                           in1=BCT[b][:, 2 + g, :].unsqueeze(1).to_broadcast([128, 4, 128]), op=ALU.mult),
                          reads=[Ebc[b2], BCT[b]], writes=[CsT[b]])
                for g in range(2):
                    kb.op("dve", lambda e, g=g, b=b, b2=b2: e.tensor_tensor(out=MT[b][:, 4 * g:4 * g + 4, :], in0=Dm[b2][:, 4 * g:4 * g + 4, :],
                                                                          in1=CBT[b2][:, g, :].unsqueeze(1).to_broadcast([128, 4, 128]), op=ALU.mult),
                          reads=[Dm[b2], CBT[b2]], writes=[MT[b]])

            def s2(n):
                b, b2, r0, X_, DT_, dt_d, da_d, x3 = views(n)
                p_y = py[n % 2]
                for h in range(8):
                    kb.op("pe", lambda e, h=h, b=b: e.matmul(p_y[:, h * 64:(h + 1) * 64], lhsT=MT[b][:, h, :], rhs=xdt[b][:, h, :], start=(h == 0), stop=False,
                                                           skip_group_check=True), reads=[MT[b], xdt[b]], writes=[p_y])
                for h in range(8):
                    kb.op("pe", lambda e, h=h, b=b: e.matmul(p_y[:, h * 64:(h + 1) * 64], lhsT=CsT[b][:, h, :], rhs=hsb[:, h, :], start=False, stop=(h == 7),
                                                           skip_group_check=True), reads=[CsT[b], hsb], writes=[p_y])
                for g in range(2):
                    kb.op("pe", lambda e, g=g, b=b, X_=X_: e.matmul(pst[:, g * 256:(g + 1) * 256], lhsT=X_[:, 512 + g * 128:512 + (g + 1) * 128],
                                                                 rhs=xw[b][:, 4 * g:4 * g + 4, :].rearrange("p h c -> p (h c)"), start=True, stop=True),
                          reads=[X_, xw[b]], writes=[pst])
                kb.op("dve", lambda e, b=b, b2=b2: e.tensor_tensor(out=tmp[b2][:, :, :], in0=hs[:, :, :], in1=cd[b][:, :].unsqueeze(2).to_broadcast([128, 8, 64]), op=ALU.mult),
                      reads=[hs, cd[b]], writes=[tmp[b2]])
                kb.op("dve", lambda e, b2=b2: e.tensor_tensor(out=hs[:, :, :], in0=tmp[b2][:, :, :], in1=pst[:, :].rearrange("p (h c) -> p h c", h=8), op=ALU.add),
                      reads=[tmp[b2], pst], writes=[hs])
                kb.op("act", lambda e: e.copy(out=hsb[:, :, :], in_=hs[:, :, :]), reads=[hs], writes=[hsb])

            def s3(n):
                b, b2, r0, X_, DT_, dt_d, da_d, x3 = views(n)
                p_y = py[n % 2]
                if d == 0:
                    kb.op("pool", lambda e, b=b, x3=x3: e.tensor_tensor(out=yo[b][:, :].rearrange("p (h c) -> p h c", h=8), in0=x3,
                                                                      in1=dsk[:, :].unsqueeze(2).to_broadcast([128, 8, 64]), op=ALU.mult),
                          reads=[X_, dsk], writes=[yo[b]])
                    kb.op("dve", lambda e, b=b: e.tensor_tensor(out=yo[b][:, :], in0=p_y[:, :], in1=yo[b][:, :], op=ALU.add), reads=[p_y, yo[b]], writes=[yo[b]])
                    kb.dma("sp", out=self.YF[r0:r0 + 128, :], in_=yo[b][:, :], reads=[yo[b]], writes=[self.YF])
                else:
                    kb.op("dve", lambda e, b=b: e.tensor_tensor(out=yo[b][:, :], in0=p_y[:, :], in1=yf[b][:, :], op=ALU.add), reads=[p_y, yf[b]], writes=[yo[b]])
                    kb.op("act", lambda e, b=b, b2=b2: e.activation(out=sz[b2][:, :], in_=zt[b][:, :], func=AF.Silu), reads=[zt[b]], writes=[sz[b2]])
                    kb.op("pool", lambda e, b=b, b2=b2: e.tensor_tensor(out=yo[b][:, :], in0=yo[b][:, :], in1=sz[b2][:, :], op=ALU.mult), reads=[yo[b], sz[b2]], writes=[yo[b]])
                    for g in range(2):
                        kb.op("act", lambda e, b=b, g=g: e.activation(out=junk[:, :], in_=yo[b][:, g * 256:(g + 1) * 256], func=AF.Square, accum_out=ssq[b][:, g:g + 1]),
                              reads=[yo[b]], writes=[junk, ssq[b]])
                    kb.op("dve", lambda e, b=b: e.tensor_scalar(out=ssq[b][:, :], in0=ssq[b][:, :], scalar1=1.0 / 256, scalar2=EPS, op0=ALU.mult, op1=ALU.add),
                          reads=[ssq[b]], writes=[ssq[b]])
                    kb.op("pool", lambda e, b=b: e.tensor_tensor(out=rsq[b][:, :], in0=ssq[b][:, :], in1=negh[:, 0:1].to_broadcast([128, 2]), op=ALU.pow),
                          reads=[ssq[b], negh], writes=[rsq[b]])
                    for g in range(2):
                        kb.op("dve", lambda e, b=b, g=g, b2=b2: e.scalar_tensor_tensor(out=mo[b2][:, g * 256:(g + 1) * 256], in0=yo[b][:, g * 256:(g + 1) * 256],
                                                                                     scalar=rsq[b][:, g:g + 1], in1=gss[:, g * 256:(g + 1) * 256], op0=ALU.mult, op1=ALU.mult),
                              reads=[yo[b], rsq[b], gss], writes=[mo[b2]])
                    kb.dma("sp", out=self.MIX[r0:r0 + 128, 0:512], in_=mo[b2][:, :], reads=[mo[b2]], writes=[self.MIX])

            pipeline(len(order), [s0, s1, s2, s3])

    def ph_fourier(self, li):
        kb = self.kb
        I = self.I
        SC = 1.0 / np.sqrt(8192.0 * 128.0)
        SCC = 1.0 / np.sqrt(256.0 * 128.0)
        with contextlib.ExitStack() as es:
            F1 = self.cload(es, "F1")
            blk = [kb.sb(es, [128, 16, 1024], BF16, "blk") for _ in range(2)]
            gt = [kb.sb(es, [128, 2, 512], BF16, "gt") for _ in range(3)]
            pg = [kb.ps(es, [128, 512], F32, "pg") for _ in range(4)]
            ADv = self.AD[0:L, :].rearrange("(n1 n2) c -> n1 n2 c", n2=64)
            for rd in range(4):
                bk = blk[rd % 2]
                kb.dma("sp", out=bk[:, :, :], in_=ADv[:, rd * 16:(rd + 1) * 16, :], reads=[self.AD], writes=[bk])
                for nl in range(16):
                    n2 = rd * 16 + nl
                    g = gt[n2 % 3]
                    pr_, pi_ = pg[(2 * n2) % 4], pg[(2 * n2 + 1) % 4]
                    Ac = bk[:, nl, 0:512]
                    As = bk[:, nl, 512:1024]
                    kb.op("pe", lambda e, pr_=pr_, Ac=Ac: e.matmul(pr_[:, :], lhsT=F1[:, 0, :], rhs=Ac, start=True, stop=False), reads=[F1, bk], writes=[pr_])
                    kb.op("pe", lambda e, pr_=pr_, As=As: e.matmul(pr_[:, :], lhsT=F1[:, 1, :], rhs=As, start=False, stop=True), reads=[F1, bk], writes=[pr_])
                    kb.op("pe", lambda e, pi_=pi_, Ac=Ac: e.matmul(pi_[:, :], lhsT=F1[:, 1, :], rhs=Ac, start=True, stop=False), reads=[F1, bk], writes=[pi_])
                    kb.op("pe", lambda e, pi_=pi_, As=As: e.matmul(pi_[:, :], lhsT=F1[:, 2, :], rhs=As, start=False, stop=True), reads=[F1, bk], writes=[pi_])
                    kb.op("act", lambda e, g=g, pr_=pr_: e.copy(out=g[:, 0, :], in_=pr_[:, :]), reads=[pr_], writes=[g])
                    kb.op("dve", lambda e, g=g, pi_=pi_: e.tensor_copy(out=g[:, 1, :], in_=pi_[:, :]), reads=[pi_], writes=[g])
                    kb.dma("sp", out=self.GD[:, n2, :, :].rearrange("ri p c -> p ri c"), in_=g[:, :, :], reads=[g], writes=[self.GD])
        kb.barrier()
        with contextlib.ExitStack() as es:
            TW = self.cload(es, "TW3")
            blk = [kb.sb(es, [128, 32, 512], BF16, "blk3") for _ in range(2)]
            ot = [kb.sb(es, [64, 32, 512], BF16, "ot") for _ in range(2)]
            pg = [kb.ps(es, [64, 512], F32, "pg3") for _ in range(4)]
            GDv = self.GD[:, :, :, :].rearrange("ri n2 p c -> (ri n2) p c")
            MIXv = self.MIX[0:L, :].rearrange("(p2 p1) c -> p2 p1 c", p1=128)
            for rd in range(4):
                bk = blk[rd % 2]
                o = ot[rd % 2]
                kb.dma("sp", out=bk[:, :, :], in_=GDv[:, rd * 32:(rd + 1) * 32, :], reads=[self.GD], writes=[bk])
                for pl in range(32):
                    p1 = rd * 32 + pl
                    p = pg[p1 % 4]
                    kb.op("pe", lambda e, p=p, p1=p1, pl=pl, bk=bk: e.matmul(p[:, :], lhsT=TW[:, p1, :], rhs=bk[:, pl, :], start=True, stop=True), reads=[TW, bk], writes=[p])
                    if pl % 2 == 0:
                        kb.op("act", lambda e, p=p, pl=pl, o=o: e.activation(out=o[:, pl, :], in_=p[:, :], func=AF.Copy, scale=float(SC)), reads=[p], writes=[o])
                    else:
                        kb.op("dve", lambda e, p=p, pl=pl, o=o: e.tensor_scalar(out=o[:, pl, :], in0=p[:, :], scalar1=float(SC), scalar2=None, op0=ALU.mult), reads=[p], writes=[o])
                kb.dma("sp", out=MIXv[:, rd * 32:(rd + 1) * 32, 512:1024], in_=o[:, :, :], reads=[o], writes=[self.MIX])
            C2 = self.cload(es, "C256")
            S2 = self.cload(es, "nS256")
            ac = kb.sb(es, [128, 2, 1024], BF16, "actx")
            kb.dma("sp", out=ac[:, :, :], in_=self.AD[L:NT, :].rearrange("(t p) c -> p t c", p=128), reads=[self.AD], writes=[ac])
            oc = kb.sb(es, [128, 2, 512], BF16, "octx")
            pcx = [kb.ps(es, [128, 512], F32, "pcx") for _ in range(2)]
            for pt in range(2):
                p = pcx[pt]
                k = 0
                for nt in range(2):
                    for (M, off) in ((C2, 0), (S2, 512)):
                        kb.op("pe", lambda e, p=p, M=M, nt=nt, pt=pt, off=off, k=k: e.matmul(p[:, :], lhsT=M[:, nt, pt * 128:(pt + 1) * 128], rhs=ac[:, nt, off:off + 512],
                                                                                          start=(k == 0), stop=(k == 3)), reads=[M, ac], writes=[p])
                        k += 1
                kb.op("act", lambda e, p=p, pt=pt: e.activation(out=oc[:, pt, :], in_=p[:, :], func=AF.Copy, scale=float(SCC)), reads=[p], writes=[oc])
            kb.dma("sp", out=self.MIX[L:NT, 512:1024].rearrange("(t p) c -> p t c", p=128), in_=oc[:, :, :], reads=[oc], writes=[self.MIX])

    def ph_final(self):
        kb = self.kb
        I = self.I
        with contextlib.ExitStack() as es:
            gf = kb.sb(es, [128, D], F32, "gf")
            kb.dma("sp", out=gf[:, :], in_=I["g_final"].partition_broadcast(128), writes=[gf])
            zero = kb.sb(es, [128, D], F32, "zero")
            kb.op("dve", lambda e: e.memset(zero[:, :], 0.0), writes=[zero])
            negh = kb.sb(es, [128, 1], F32, "negh")
            kb.op("dve", lambda e: e.memset(negh[:, :], -0.5), writes=[negh])
            junk = kb.sb(es, [128, D], BF16, "junk")
            NB = 4
            xt = [kb.sb(es, [128, D], F32, "xt") for _ in range(NB)]
            of = [kb.sb(es, [128, D], F32, "of") for _ in range(NB)]
            sm = [[kb.sb(es, [128, 1], F32, "sm") for _ in range(2)] for _ in range(NB)]

            def s0(ti):
                b = ti % NB
                r0 = ti * 128
                kb.dma("sp", out=xt[b][:, :], in_=self.X[r0:r0 + 128, :], reads=[self.X], writes=[xt[b]])

            def s1(ti):
                b = ti % NB
                r0 = ti * 128
                self.norm_tile((sm[b][0], sm[b][1]), xt[b], gf, zero, of[b], None, junk, negh)
                kb.dma("sp", out=self.out[r0:r0 + 128, :], in_=of[b][:, :], reads=[of[b]], writes=[self.outk])

            pipeline(64, [s0, s1])

    def ph_dumpx(self):
        kb = self.kb
        for r0 in range(0, L, 2048):
            kb.dma("sp", out=self.out[r0:r0 + 2048, :], in_=self.X[r0:r0 + 2048, :], reads=[self.X], writes=[self.outk])


def default_phases():
    ph = [("init",)]
    for li in range(DEPTH):
        need_ctx = li < DEPTH - 1
        ph += [("mod", li)]
        if li % 2 == 0:
            ph += [("inproj", li), ("conv", li), ("ssd", li, 0), ("ssd", li, 1), ("fourier", li),
                   ("oproj", li, "w_out_e", "MIX", need_ctx)]
        else:
            ph += [("qkv", li), ("attn", li, need_ctx), ("oproj", li, "w_o", "OD", need_ctx)]
        ph += [("router", li), ("select", li), ("moe", li)]
    ph += [("final",)]
    return ph


ATT_VARIANT_RP = [2, 0, 1, 62, 63]


def att_base(rp):
    r0 = 2 * rp
    return min(min(max(r0 - 4, 0), 120), 118)


def att_variant(rp):
    return 0 if 2 <= rp <= 61 else {0: 1, 1: 2, 62: 3, 63: 4}[rp]


def build_bias_table(rpb):
    no = rpb.shape[0]
    out = np.empty((no, 5, 16, 128, 5, 128), np.float32)
    p = np.arange(128)
    q = np.arange(128)
    c = np.arange(5)
    for v, rp in enumerate(ATT_VARIANT_RP):
        r0 = 2 * rp
        base = att_base(rp)
        krow = base + 2 * c[:, None, None] + (p // 64)[None, :, None]
        kcol = (p % 64)[None, :, None]
        r = (r0 + q // 64)[None, None, :]
        col = (q % 64)[None, None, :]
        rs = np.clip(r - 4, 0, 120)
        cs = np.clip(col - 8, 0, 48)
        valid = (krow >= rs) & (krow < rs + 8) & (kcol >= cs) & (kcol < cs + 16)
        ro = np.clip(krow - r + 7, 0, 14) + 0 * kcol
        co = np.clip(kcol - col + 15, 0, 30) + 0 * krow
        g = rpb[:, :, ro, co]
        g = np.where(valid[None, None], g, np.float32(-30000.0))
        out[:, v] = np.transpose(g, (0, 1, 3, 2, 4))
    return out


def make_in_maps(inputs, n_cores, names):
    consts = host_consts()
    shared = {}
    for k in names:
        if k in consts:
            shared[k] = consts[k]
        elif k in ("x", "ctx", "c"):
            pass
        elif k == "biasT":
            shared[k] = build_bias_table(inputs["rpb"])
        elif k in ("a_log", "dt_bias"):
            shared[k] = np.ascontiguousarray(inputs[k].reshape(2, 16))
        else:
            shared[k] = np.ascontiguousarray(inputs[k])
    maps = []
    for b in range(n_cores):
        m = dict(shared)
        for k in ("x", "ctx", "c"):
            if k in names:
                m[k] = np.ascontiguousarray(inputs[k][b])
        maps.append(m)
    return maps


def kernel(**inputs):
    inputs = {k: np.asarray(v) for k, v in inputs.items()}
    prog = Prog(default_phases())
    in_maps = make_in_maps(inputs, 4, list(prog.I.keys()))
    res = run_bass_kernel_spmd(prog.nc, in_maps, core_ids=list(range(4)))
    out = np.stack([res.results[b]["out"] for b in range(4)], axis=0)
    return out.astype(np.float32)
```
